# Optimizing a Trainium2 kernel written in Bass

```python
import jax, jax.numpy as jnp
from jax import lax
import numpy as np

D_MODEL = 2048
BATCH = 8
SEQ = 2048
DEPTH = 1

CHUNK = 64
NORM_EPS = 1e-6
N_ADA = 6

A_HEADS = 8
A_HEAD_DIM = 128
A_WIDTH = A_HEADS * A_HEAD_DIM
IDX_HEADS = 16
IDX_DIM = 64
TOPK_MAX = 256
Q_BLOCK = 64

B_HEADS = 8
B_HEAD_DIM = 128
B_WIDTH = B_HEADS * B_HEAD_DIM

PEER_HEADS = 8
PEER_N_KEYS = 128
PEER_N_EXPERTS = PEER_N_KEYS * PEER_N_KEYS
PEER_KEY_DIM = 256
PEER_HALF = PEER_KEY_DIM // 2
PEER_TOPK = 16
PEER_TOKEN_BLOCK = 128

IN_WIDTHS = (A_WIDTH, A_WIDTH, A_WIDTH, IDX_HEADS * IDX_DIM, IDX_DIM, IDX_HEADS,
             B_WIDTH, B_WIDTH, B_WIDTH, B_WIDTH, D_MODEL, D_MODEL)
IN_WIDTH = 3 * A_WIDTH + IDX_HEADS * IDX_DIM + IDX_DIM + IDX_HEADS + 4 * B_WIDTH + 2 * D_MODEL

kernel_name = "hybrid_dsa_hgrn2_peer_adaln_block"


def rms_norm(x, gain):
    xf = x.astype(jnp.float32)
    y = xf * lax.rsqrt(jnp.mean(xf * xf, axis=-1, keepdims=True) + NORM_EPS)
    return (y * gain.astype(jnp.float32)).astype(x.dtype)


def modulate(h, shift, scale):
    return h * (1 + scale[:, None, :]) + shift[:, None, :]


def split_columns(a, widths):
    parts, start = [], 0
    for w in widths:
        parts.append(a[..., start:start + w])
        start += w
    return parts


def dsa_attention(q, k, v, q_idx, k_idx, w_idx):
    B, S = q.shape[0], q.shape[1]
    top_k = min(TOPK_MAX, S // 4)
    n_blk = S // Q_BLOCK
    key_chunk = jnp.arange(S) // CHUNK
    idx_scale = (IDX_DIM * IDX_HEADS) ** -0.5
    att_scale = A_HEAD_DIM ** -0.5
    gather = jax.vmap(lambda table, ids: table[ids])

    def to_blocks(a):
        return jnp.moveaxis(a.reshape((B, n_blk, Q_BLOCK) + a.shape[2:]), 1, 0)

    def block(args):
        qb, qib, wb, blk = args
        q_chunk = (blk * Q_BLOCK + jnp.arange(Q_BLOCK)) // CHUNK
        admissible = key_chunk[None, :] <= q_chunk[:, None]
        dots = jnp.einsum('bqhd,bsd->bqhs', qib, k_idx)
        score = jnp.einsum('bqh,bqhs->bqs', wb, jax.nn.relu(dots)).astype(jnp.float32) * idx_scale
        score = jnp.where(admissible[None], score, -jnp.inf)
        _, sel = lax.top_k(score, top_k)
        valid = jnp.take(key_chunk, sel) <= q_chunk[None, :, None]
        k_sel = gather(k, sel)
        v_sel = gather(v, sel)
        logits = jnp.einsum('bqhd,bqkhd->bqhk', qb, k_sel).astype(jnp.float32) * att_scale
        logits = jnp.where(valid[:, :, None, :], logits, -jnp.inf)
        p = jax.nn.softmax(logits, axis=-1).astype(v.dtype)
        return jnp.einsum('bqhk,bqkhd->bqhd', p, v_sel)

    out = lax.map(block, (to_blocks(q), to_blocks(q_idx), to_blocks(w_idx), jnp.arange(n_blk)))
    return jnp.moveaxis(out, 0, 1).reshape(B, S, A_HEADS, A_HEAD_DIM)


def hgrn2(q, f_logit, i, lower_bound):
    B, S, H, D = q.shape
    out_dtype = q.dtype
    n_c = S // CHUNK
    lb = lower_bound.reshape(H, D).astype(jnp.float32)
    f = lb + (1 - lb) * jax.nn.sigmoid(f_logit.astype(jnp.float32))
    log_f = jnp.log(f)
    key = 1 - f
    qf = jax.nn.silu(q.astype(jnp.float32))
    val = i.astype(jnp.float32)
    causal = jnp.tril(jnp.ones((CHUNK, CHUNK), dtype=bool))

    def chunks(a):
        return a.reshape(B, n_c, CHUNK, H, D).transpose(1, 0, 3, 2, 4)

    def step(state, xs):
        qc, kc, vc, lfc = xs
        b = jnp.cumsum(lfc, axis=2)
        b_last = b[:, :, -1:, :]
        o_inter = jnp.einsum('bhtd,bhde->bhte', qc * jnp.exp(b), state)
        diff = b[:, :, :, None, :] - b[:, :, None, :, :]
        decay = jnp.exp(jnp.where(causal[None, None, :, :, None], diff, -jnp.inf))
        attn = jnp.einsum('bhtd,bhsd,bhtsd->bhts', qc, kc, decay)
        o_intra = jnp.einsum('bhts,bhse->bhte', attn, vc)
        k_to_end = kc * jnp.exp(b_last - b)
        new_state = (jnp.exp(b_last)[:, :, 0, :, None] * state
                     + jnp.einsum('bhsd,bhse->bhde', k_to_end, vc))
        return new_state, o_inter + o_intra

    state0 = jnp.zeros((B, H, D, D), jnp.float32)
    _, o = lax.scan(step, state0, (chunks(qf), chunks(key), chunks(val), chunks(log_f)))
    return o.transpose(1, 0, 3, 2, 4).reshape(B, S, H, D).astype(out_dtype)


def peer(h, w_q, sub_keys, u, v):
    B, S, D = h.shape
    T = B * S
    hf = h.reshape(T, D)
    q = (hf @ w_q).reshape(T, PEER_HEADS, 2, PEER_HALF)
    s = jnp.einsum('tphd,phnd->tphn', q, sub_keys).astype(jnp.float32)
    top_s, top_i = lax.top_k(s, PEER_TOPK)
    cand_s = top_s[:, :, 0, :, None] + top_s[:, :, 1, None, :]
    cand_i = top_i[:, :, 0, :, None] * PEER_N_KEYS + top_i[:, :, 1, None, :]
    best_s, best_pos = lax.top_k(cand_s.reshape(T, PEER_HEADS, PEER_TOPK * PEER_TOPK), PEER_TOPK)
    expert = jnp.take_along_axis(cand_i.reshape(T, PEER_HEADS, PEER_TOPK * PEER_TOPK), best_pos, axis=-1)
    gate = jax.nn.softmax(best_s, axis=-1)
    n_blk = T // PEER_TOKEN_BLOCK
    n_sel = PEER_HEADS * PEER_TOPK

    def block(args):
        hb, eb, gb = args
        ub = u[eb]
        vb = v[eb]
        act = jax.nn.gelu(jnp.einsum('td,ted->te', hb, ub).astype(jnp.float32), approximate=False) * gb
        return jnp.einsum('te,ted->td', act.astype(vb.dtype), vb)

    out = lax.map(block, (hf.reshape(n_blk, PEER_TOKEN_BLOCK, D),
                          expert.reshape(n_blk, PEER_TOKEN_BLOCK, n_sel),
                          gate.reshape(n_blk, PEER_TOKEN_BLOCK, n_sel)))
    return out.reshape(B, S, D).astype(h.dtype)


def setup_inputs(seed: int = 0) -> dict:
    key = jax.random.key(seed)
    ks = jax.random.split(key, 20)
    f32 = jnp.float32
    nrm = lambda k, shape, s: jax.random.normal(k, shape, f32) * s
    return {
        "x": nrm(ks[0], (BATCH, SEQ, D_MODEL), 1.0),
        "c": nrm(ks[1], (BATCH, D_MODEL), 1.0),
        "w_ada": nrm(ks[2], (DEPTH, D_MODEL, N_ADA * D_MODEL), 0.5 * D_MODEL ** -0.5),
        "b_ada": nrm(ks[3], (DEPTH, N_ADA * D_MODEL), 0.01),
        "norm_mix": 1.0 + nrm(ks[4], (DEPTH, D_MODEL), 0.01),
        "norm_ffn": 1.0 + nrm(ks[5], (DEPTH, D_MODEL), 0.01),
        "w_in": nrm(ks[6], (DEPTH, D_MODEL, IN_WIDTH), D_MODEL ** -0.5),
        "lb_logits": nrm(ks[7], (DEPTH + 1, B_WIDTH), 0.5),
        "hgrn_gain": 1.0 + nrm(ks[8], (DEPTH, B_HEAD_DIM), 0.01),
        "w_up_a": nrm(ks[9], (DEPTH, A_WIDTH, D_MODEL), A_WIDTH ** -0.5),
        "w_up_b": nrm(ks[10], (DEPTH, B_WIDTH, D_MODEL), B_WIDTH ** -0.5),
        "w_out": nrm(ks[11], (DEPTH, D_MODEL, D_MODEL), D_MODEL ** -0.5),
        "peer_w_q": nrm(ks[12], (DEPTH, D_MODEL, PEER_HEADS * PEER_KEY_DIM), D_MODEL ** -0.5),
        "peer_keys": nrm(ks[13], (DEPTH, PEER_HEADS, 2, PEER_N_KEYS, PEER_HALF), PEER_HALF ** -0.5),
        "peer_u": nrm(ks[14], (DEPTH, PEER_N_EXPERTS, D_MODEL), D_MODEL ** -0.5),
        "peer_v": nrm(ks[15], (DEPTH, PEER_N_EXPERTS, D_MODEL), PEER_HEADS ** -0.5),
        "final_norm": 1.0 + nrm(ks[16], (D_MODEL,), 0.01),
    }


def reference(x, c, w_ada, b_ada, norm_mix, norm_ffn, w_in, lb_logits, hgrn_gain,
              w_up_a, w_up_b, w_out, peer_w_q, peer_keys, peer_u, peer_v, final_norm):
    B, S, _ = x.shape
    lower_bounds = jnp.cumsum(jax.nn.softmax(lb_logits.astype(jnp.float32), axis=0), axis=0)
    cond = jax.nn.silu(c)
    for l in range(DEPTH):
        mod = cond @ w_ada[l] + b_ada[l]
        shift1, scale1, gate1, shift2, scale2, gate2 = jnp.split(mod, N_ADA, axis=-1)

        h = modulate(rms_norm(x, norm_mix[l]), shift1, scale1)
        proj = h @ w_in[l]
        qa, ka, va, qi, ki, wi, qb, fb, ib, gb, g_a, g_b = split_columns(proj, IN_WIDTHS)
        attn = dsa_attention(qa.reshape(B, S, A_HEADS, A_HEAD_DIM),
                             ka.reshape(B, S, A_HEADS, A_HEAD_DIM),
                             va.reshape(B, S, A_HEADS, A_HEAD_DIM),
                             qi.reshape(B, S, IDX_HEADS, IDX_DIM), ki, wi)
        rec = hgrn2(qb.reshape(B, S, B_HEADS, B_HEAD_DIM),
                    fb.reshape(B, S, B_HEADS, B_HEAD_DIM),
                    ib.reshape(B, S, B_HEADS, B_HEAD_DIM), lower_bounds[l])
        rec = rms_norm(rec, hgrn_gain[l]) * jax.nn.silu(gb.reshape(B, S, B_HEADS, B_HEAD_DIM))
        y_a = attn.reshape(B, S, A_WIDTH) @ w_up_a[l]
        y_b = rec.reshape(B, S, B_WIDTH) @ w_up_b[l]
        mixed = jax.nn.sigmoid(g_a) * y_a + jax.nn.sigmoid(g_b) * y_b
        x = x + gate1[:, None, :] * (mixed @ w_out[l])

        h = modulate(rms_norm(x, norm_ffn[l]), shift2, scale2)
        x = x + gate2[:, None, :] * peer(h, peer_w_q[l], peer_keys[l], peer_u[l], peer_v[l])
    return rms_norm(x, final_norm)
```

```python
import numpy as np
import concourse.bass as bass
import concourse.mybir as mybir
from concourse.bass_utils import run_bass_kernel_spmd
from contextlib import ExitStack

F32 = mybir.dt.float32
BF16 = mybir.dt.bfloat16
AF = mybir.ActivationFunctionType
ALU = mybir.AluOpType
AX = mybir.AxisListType

COMPUTE = ("tensor", "vector", "scalar", "gpsimd")
QUEUES = ("sync",)
ALL_ENG = COMPUTE + QUEUES


class Sched:
    def __init__(self, nc):
        self.nc = nc
        self.sem = {e: nc.alloc_semaphore(name="pg_" + e) for e in COMPUTE}
        self.cnt = {e: 0 for e in COMPUTE}
        self.streams = {e: [] for e in ALL_ENG}
        self.waited = {e: {} for e in ALL_ENG}
        self.lastw = {}
        self.readers = {}
        self.dsem = {}
        self.semobj = {}

    def _deps(self, eng, reads, writes):
        waits = {}

        def need(sv):
            s, v = sv
            sid = id(s)
            self.semobj[sid] = s
            if v > waits.get(sid, 0):
                waits[sid] = v

        for k in reads:
            if k in self.lastw:
                need(self.lastw[k])
        for k in writes:
            if k in self.lastw:
                need(self.lastw[k])
            for sv in self.readers.get(k, ()):
                need(sv)
        out = []
        wd = self.waited[eng]
        for sid, v in waits.items():
            if wd.get(sid, 0) < v:
                wd[sid] = v
                out.append((self.semobj[sid], v))
        return out

    def _commit(self, my, reads, writes):
        for k in writes:
            self.lastw[k] = my
            self.readers[k] = []
        for k in reads:
            if k in writes:
                continue
            self.readers.setdefault(k, []).append(my)

    def begin_capture(self):
        self._cap = []

    def end_capture(self):
        c = self._cap
        self._cap = None
        return c

    def op(self, eng, fn, reads=(), writes=(), sig=True):
        if getattr(self, "_cap", None) is not None:
            self._cap.append(lambda: self._op(eng, fn, reads, writes, sig))
            return
        self._op(eng, fn, reads, writes, sig)

    def dma(self, q, out, in_, reads=(), writes=(), **kw):
        if getattr(self, "_cap", None) is not None:
            self._cap.append(lambda: self._dma(q, out, in_, reads, writes, **kw))
            return
        self._dma(q, out, in_, reads, writes, **kw)

    def _op(self, eng, fn, reads=(), writes=(), sig=True):
        waits = self._deps(eng, reads, writes)
        if eng == "tensor":
            waits = [(s_, v_) for (s_, v_) in waits if s_ is not self.sem["tensor"]]
        if sig:
            self.cnt[eng] += 1
            my = (self.sem[eng], self.cnt[eng])
            inc = (self.sem[eng], 1)
        else:
            assert eng == "tensor"
            my = (self.sem[eng], self.cnt[eng] + 1)
            inc = None
        self._commit(my, reads, writes)
        self.streams[eng].append((waits, fn, inc))

    def _dma(self, q, out, in_, reads=(), writes=(), **kw):
        waits = self._deps(q, reads, writes)
        key = writes[0]
        if key not in self.dsem:
            self.dsem[key] = [self.nc.alloc_semaphore(name="d%d" % len(self.dsem)), 0]
        ent = self.dsem[key]
        ent[1] += 16
        my = (ent[0], ent[1])
        self._commit(my, reads, writes)

        def fn(e):
            return e.dma_start(out=out, in_=in_, **kw)

        self.streams[q].append((waits, fn, (ent[0], 16)))

    def drain_dmas(self, q="sync"):
        waits = []
        wd = self.waited[q]
        for key, (s, v) in self.dsem.items():
            if v > 0 and wd.get(id(s), 0) < v:
                wd[id(s)] = v
                waits.append((s, v))
        if waits:
            self.streams[q].append((waits, None, None))

    def flush(self, block):
        nc = self.nc
        streams = self.streams
        self.streams = {e: [] for e in ALL_ENG}

        def mk(name):
            lst = streams[name]

            def body(e):
                for waits, fn, inc in lst:
                    for s, v in waits:
                        e.wait_ge(s, v)
                    if fn is not None:
                        inst = fn(e)
                        if inc is not None:
                            inst.then_inc(inc[0], inc[1])

            return body

        for name in ALL_ENG:
            if streams[name]:
                getattr(block, name)(mk(name))

D = 2048
S = 2048
NDC = 16
NTT = 16
IN_W = 12368
EPS = 1e-6
NCORES = 8


def make_consts():
    cst = {}
    cst["ident"] = np.eye(128, dtype=np.float32)
    cst["ones"] = np.ones((128, 128), dtype=np.float32)
    cst["tri"] = np.triu(np.ones((64, 64), dtype=np.float32))
    return cst


class K:
    pass


def merged_units(lists):
    lists = [l for l in lists if l]
    pos = [0] * len(lists)
    total = sum(len(l) for l in lists)
    for _ in range(total):
        best, bi = None, None
        for i, l in enumerate(lists):
            if pos[i] < len(l):
                frac = (pos[i] + 0.5) / len(l)
                if best is None or frac < best:
                    best, bi = frac, i
        lists[bi][pos[bi]]()
        pos[bi] += 1


def build_nc(stage=99, dbg=None):
    nc = bass.Bass("TRN2", target_bir_lowering=False)
    k = K()
    k.nc = nc
    k.stage = stage

    def din(name, shape, dtype=F32):
        return nc.dram_tensor(name, list(shape), dtype, kind="ExternalInput").ap()

    k.x = din("x", [S, D])
    k.c = din("c", [1, D])
    k.w_ada = din("w_ada", [D, 6 * D])
    k.b_ada = din("b_ada", [1, 6 * D])
    k.norm_mix = din("norm_mix", [1, D])
    k.norm_ffn = din("norm_ffn", [1, D])
    k.w_in = din("w_in", [D, IN_W])
    k.ident_d = din("ident", [128, 128])
    k.tri_d = din("tri", [64, 64])
    k.lb_logits = din("lb_logits", [2, 1024])
    k.hgrn_gain = din("hgrn_gain", [1, 128])
    k.w_up_a = din("w_up_a", [1024, D])
    k.w_up_b = din("w_up_b", [1024, D])
    k.w_out = din("w_out", [D, D])
    k.peer_w_q = din("peer_w_q", [D, D])
    k.peer_keys = din("peer_keys", [8, 2, 128, 128])
    k.peer_u = din("peer_u", [16384, D])
    k.peer_v = din("peer_v", [16384, D])
    k.final_norm = din("final_norm", [D])
    k.ones_d = din("ones", [128, 128])
    k.out = nc.dram_tensor("out", [S, D], F32, kind="ExternalOutput").ap()
    if dbg is not None:
        k.dbg = nc.dram_tensor("dbg", list(dbg), F32, kind="ExternalOutput").ap()

    sc = Sched(nc)
    k.sc = sc
    with ExitStack() as top:
        def sb(name, shape, dtype=F32, ctx=top):
            return ctx.enter_context(nc.sbuf_tensor(name, list(shape), dtype))

        def ps(name, shape, dtype=F32, ctx=top):
            return ctx.enter_context(nc.psum_tensor(name, list(shape), dtype))
        k.sb = sb
        k.ps = ps
        k.modT = sb("modT", [128, 96])
        k.G1 = sb("G1", [128, D])
        k.G2 = sb("G2", [128, D])
        k.identf = sb("identf", [128, 128])
        k.onesf = sb("onesf", [128, 128])
        k.identb = sb("identb", [128, 128], BF16)
        k.onesb = sb("onesb", [128, 128], BF16)

        def run_phase(fn):
            with ExitStack() as ph:
                fn(k, ph)
                sc.drain_dmas()
                with nc.Block() as block:
                    sc.flush(block)

        make_scratch(k)
        run_phase(phase_a)
        with ExitStack() as s1:
            k.hT = sb("hT", [128, NDC, S], BF16, ctx=s1)
            if stage >= 2:
                run_phase(phase_b)
            if stage >= 3:
                run_phase(phase_c)
            if dbg is not None and stage in (2, 3):
                run_phase(phase_dbg)
        if stage >= 4:
            run_phase(phase_d)
        if stage >= 5:
            run_phase(phase_e)
        if stage >= 6:
            with ExitStack() as s2:
                k.hT = sb("hT2", [128, NDC, S], BF16, ctx=s2)
                run_phase(phase_f)
                run_phase(phase_f2)
        if stage >= 7:
            with ExitStack() as s3:
                k.hT = sb("hT3", [128, NDC, S], BF16, ctx=s3)
                run_phase(phase_g)
                run_phase(phase_h1)
            run_phase(phase_h2)
        if stage >= 8:
            with ExitStack() as s4:
                k.acc = sb("acc", [128, 8, D], ctx=s4)
                for hp in range(2):
                    run_phase(lambda k_, ph_, hp=hp: phase_i(k_, ph_, hp))
                    run_phase(lambda k_, ph_, hp=hp: phase_j(k_, ph_, hp))
        if dbg is not None and stage in (2, 3):
            return nc
        if dbg is not None:
            run_phase(phase_dbg)
    return nc


def phase_a(k, ph):
    nc, sc, sb, ps = k.nc, k.sc, k.sb, k.ps
    sc.dma("sync", k.identf[:], k.ident_d, writes=["identf"])
    sc.dma("sync", k.onesf[:], k.ones_d, writes=["onesf"])
    sc.op("vector", lambda e: e.tensor_copy(out=k.identb[:], in_=k.identf[:]), reads=["identf"], writes=["identb"])
    sc.op("vector", lambda e: e.tensor_copy(out=k.onesb[:], in_=k.onesf[:]), reads=["onesf"], writes=["onesb"])
    cs = sb("cs", [128, 16], ctx=ph)
    sc.dma("sync", cs[:], k.c.rearrange("o (p j) -> (o p) j", p=128), writes=["cs"])
    sc.op("scalar", lambda e: e.activation(out=cs[:], in_=cs[:], func=AF.Silu), reads=["cs"], writes=["cs"])
    wv = k.w_ada.rearrange("(p j) n -> p j n", p=128)
    NB = 24
    wts = [sb("wada%d" % i, [128, 16, 512], ctx=ph) for i in range(2)]
    brs = [sb("brow%d" % i, [1, 512], ctx=ph) for i in range(2)]
    mrs = [sb("mrow%d" % i, [1, 512], ctx=ph) for i in range(2)]
    pss = [ps("pa%d" % i, [128, 512], ctx=ph) for i in range(2)]
    pbs = [ps("pbc%d" % i, [128, 512], ctx=ph) for i in range(2)]
    pc = ps("pcol", [128, 96], ctx=ph)
    for nb in range(NB):
        i2 = nb % 2
        wt, br, mr, pt, pb = wts[i2], brs[i2], mrs[i2], pss[i2], pbs[i2]
        wk, bk, mk, pk, pbk = "wada%d" % i2, "brow%d" % i2, "mrow%d" % i2, "pa%d" % i2, "pbc%d" % i2
        q = "sync" if nb % 2 == 0 else "gpsimd"
        sc.dma(q, wt[:], wv[:, :, nb * 512:(nb + 1) * 512], writes=[wk])
        sc.dma("sync", br[:], k.b_ada[:, nb * 512:(nb + 1) * 512], writes=[bk])
        for j in range(16):
            sc.op("tensor", lambda e, j=j, wt=wt, pt=pt: e.matmul(pt[0:1, :], lhsT=cs[:, j:j + 1], rhs=wt[:, j, :],
                                                             start=(j == 0), stop=(j == 15)),
                  reads=["cs", wk], writes=[pk], sig=(j == 15))
        sc.op("vector", lambda e, pt=pt, mr=mr, br=br: e.tensor_tensor(out=mr[0:1, :], in0=pt[0:1, :], in1=br[0:1, :], op=ALU.add),
              reads=[pk, bk], writes=[mk])
        for c4 in range(4):
            ch = nb * 4 + c4
            sc.op("tensor", lambda e, ch=ch, c4=c4, mr=mr: e.matmul(pc[:, ch:ch + 1], lhsT=mr[0:1, c4 * 128:(c4 + 1) * 128],
                                                                   rhs=k.onesf[0:1, 0:1], start=True, stop=True),
                  reads=[mk, "onesf"], writes=["pcol"])
        for gi, (G, off) in enumerate(((k.G1, 2 * D), (k.G2, 5 * D))):
            if off <= nb * 512 < off + D:
                o = nb * 512 - off
                sc.op("tensor", lambda e, pb=pb, mr=mr: e.matmul(pb[:, :], lhsT=k.onesf[0:1, :], rhs=mr[0:1, :],
                                                                 start=True, stop=True),
                      reads=[mk, "onesf"], writes=[pbk])
                sc.op("vector", lambda e, pb=pb, G=G, o=o: e.tensor_copy(out=G[:, o:o + 512], in_=pb[:, :]),
                      reads=[pbk], writes=["G%d" % gi])
    sc.op("vector", lambda e: e.tensor_copy(out=k.modT[:], in_=pc[:]), reads=["pcol"], writes=["modT"])


def phase_b(k, ph):
    norm_modulate(k, ph, k.x, k.norm_mix, 0, 16, [])


def norm_modulate(k, ph, src, gain_d, sh_col, sc_col, src_reads, tag="g1"):
    nc, sc, sb, ps = k.nc, k.sc, k.sb, k.ps
    gT = sb(tag + "gT", [128, NDC], ctx=ph)
    with nc.allow_non_contiguous_dma(reason="tiny gain vector"):
        pass
    sc.dma("sync", gT[:], gain_d.rearrange("o (j p) -> (o p) j", p=128), writes=["gT"], allow_slow_non_contiguous=True)
    A1 = sb(tag + "A1", [128, NDC], ctx=ph)
    sc.op("vector", lambda e: e.scalar_tensor_tensor(out=A1[:], in0=k.modT[:, sc_col:sc_col + 16], scalar=1.0, in1=gT[:],
                                                     op0=ALU.add, op1=ALU.mult),
          reads=["modT", "gT"], writes=["A1"])
    xts = [sb(tag + "xt%d" % i, [128, D], ctx=ph) for i in range(8)]
    sq = sb(tag + "sqjunk", [128, D], ctx=ph)
    ss = sb(tag + "ss", [128, 16], ctx=ph)
    pts = [ps(tag + "pb%d" % i, [128, 512], ctx=ph) for i in range(4)]
    xv = src.rearrange("(n p) d -> n p d", p=128)
    for tg in range(4):
        for tt in range(4):
            ti = tg * 4 + tt
            bi = ti % 8
            xt = xts[bi]
            xk = "xt%d" % bi
            sc.dma("sync" if ti % 2 == 0 else "gpsimd", xt[:], xv[ti], reads=list(src_reads), writes=[xk])
            sc.op("scalar", lambda e, xt=xt, ti=ti: e.activation(out=sq[:], in_=xt[:], func=AF.Square,
                                                                 accum_out=ss[:, ti:ti + 1]),
                  reads=[xk], writes=["sq", ("ss", ti)])
            sc.op("vector", lambda e, ti=ti: e.tensor_scalar(out=ss[:, ti:ti + 1], in0=ss[:, ti:ti + 1], scalar1=1.0 / D,
                                                             scalar2=EPS, op0=ALU.mult, op1=ALU.add),
                  reads=[("ss", ti)], writes=[("ss", ti)])
            sc.op("scalar", lambda e, ti=ti: e.activation(out=ss[:, ti:ti + 1], in_=ss[:, ti:ti + 1], func=AF.Sqrt),
                  reads=[("ss", ti)], writes=[("ss", ti)])
            sc.op("vector", lambda e, ti=ti: e.reciprocal(out=ss[:, ti:ti + 1], in_=ss[:, ti:ti + 1]),
                  reads=[("ss", ti)], writes=[("ss", ti)])
            sc.op("vector", lambda e, xt=xt, ti=ti: e.tensor_scalar(out=xt[:], in0=xt[:], scalar1=ss[:, ti:ti + 1],
                                                                    scalar2=None, op0=ALU.mult),
                  reads=[xk, ("ss", ti)], writes=[xk])
        for dc in range(NDC):
            pt = pts[dc % 4]
            pk = "pb%d" % (dc % 4)
            for tt in range(4):
                ti = tg * 4 + tt
                bi = ti % 8
                sc.op("tensor", lambda e, pt=pt, tt=tt, bi=bi, dc=dc: e.transpose(out=pt[:, tt * 128:(tt + 1) * 128],
                                                                                 in_=xts[bi][:, dc * 128:(dc + 1) * 128],
                                                                                 identity=k.identf[:]),
                      reads=["xt%d" % bi, "identf"], writes=[pk], sig=(tt == 3))
            sc.op("scalar", lambda e, pt=pt, dc=dc, tg=tg: e.activation(out=k.hT[:, dc, tg * 512:(tg + 1) * 512], in_=pt[:, :],
                                                                        func=AF.Identity, scale=A1[:, dc:dc + 1],
                                                                        bias=k.modT[:, sh_col + dc:sh_col + dc + 1]),
                  reads=[pk, "A1", "modT"], writes=[("hT", dc, tg)])


def phase_dbg(k, ph):
    nc, sc, sb, ps = k.nc, k.sc, k.sb, k.ps
    if k.stage == 1:
        sc.dma("sync", k.dbg[:, 0:96], k.modT[:], reads=["modT"], writes=["dbg0"])
        sc.dma("sync", k.dbg[:, 128:128 + D], k.G1[:], reads=["G0"], writes=["dbg1"])
        sc.dma("sync", k.dbg[:, 128 + D:128 + 2 * D], k.G2[:], reads=["G1"], writes=["dbg2"])
    if k.stage == 7:
        tmp = sb("dbgtmp", [128, S], ctx=ph)
        tmh = sb("dbgtmh", [128, S], BF16, ctx=ph)
        for i in range(16):
            sc.dma("sync", tmh[:], k.scr["GT"][i * 128:(i + 1) * 128, :], reads=["d_GT"], writes=["dbgtmh"])
            sc.op("vector", lambda e: e.tensor_copy(out=tmp[:], in_=tmh[:]), reads=["dbgtmh"], writes=["dbgtmp"])
            sc.dma("sync", k.dbg[i * 128:(i + 1) * 128, :], tmp[:], reads=["dbgtmp"], writes=["dbg2"])
    if k.stage == 6:
        sc.dma("sync", k.dbg[0:2048, :], k.scr["x1"], reads=["d_x1"], writes=["dbg0"])
        tmp = sb("dbgtmp", [128, S], ctx=ph)
        tmh = sb("dbgtmh", [128, S], BF16, ctx=ph)
        for nm, off in (("attnT", 2048), ("recT", 3072)):
            for i in range(8):
                sc.dma("sync", tmh[:], k.scr[nm][i * 128:(i + 1) * 128, :], reads=["d_" + nm], writes=["dbgtmh"])
                sc.op("vector", lambda e: e.tensor_copy(out=tmp[:], in_=tmh[:]), reads=["dbgtmh"], writes=["dbgtmp"])
                sc.dma("sync", k.dbg[off + i * 128:off + (i + 1) * 128, :], tmp[:], reads=["dbgtmp"], writes=["dbg2"])
    if k.stage == 5:
        tmp = sb("dbgtmp", [128, S], ctx=ph)
        tmh = sb("dbgtmh", [128, S], BF16, ctx=ph)
        for i in range(8):
            sc.dma("sync", tmh[:], k.scr["recT"][i * 128:(i + 1) * 128, :], reads=["d_recT"], writes=["dbgtmh"])
            sc.op("vector", lambda e: e.tensor_copy(out=tmp[:], in_=tmh[:]), reads=["dbgtmh"], writes=["dbgtmp"])
            sc.dma("sync", k.dbg[i * 128:(i + 1) * 128, :], tmp[:], reads=["dbgtmp"], writes=["dbg2"])
    if k.stage == 4:
        tmp = sb("dbgtmp", [128, S], ctx=ph)
        tmh = sb("dbgtmh", [128, S], BF16, ctx=ph)
        for i in range(8):
            sc.dma("sync", tmh[:], k.scr["attnT"][i * 128:(i + 1) * 128, :], reads=["d_attnT"], writes=["dbgtmh"])
            sc.op("vector", lambda e: e.tensor_copy(out=tmp[:], in_=tmh[:]), reads=["dbgtmh"], writes=["dbgtmp"])
            sc.dma("sync", k.dbg[i * 128:(i + 1) * 128, :], tmp[:], reads=["dbgtmp"], writes=["dbg2"])
    if k.stage == 3:
        sc.dma("sync", k.dbg[0:1024, :], k.scr["qBT"], reads=["d_qBT"], writes=["dbg0"])
        sc.dma("sync", k.dbg[1024:1024 + 2048, 0:16], k.scr["wi"], reads=["d_wi"], writes=["dbg1"])
        tmp = sb("dbgtmp", [128, S], ctx=ph)
        tmh = sb("dbgtmh", [128, S], BF16, ctx=ph)
        for i in range(8):
            sc.dma("sync", tmh[:], k.scr["kAT"][i * 128:(i + 1) * 128, :], reads=["d_kAT"], writes=["dbgtmh"])
            sc.op("vector", lambda e: e.tensor_copy(out=tmp[:], in_=tmh[:]), reads=["dbgtmh"], writes=["dbgtmp"])
            sc.dma("sync", k.dbg[3072 + i * 128:3072 + (i + 1) * 128, :], tmp[:], reads=["dbgtmp"], writes=["dbg2"])
        for i in range(16):
            sc.dma("sync", tmh[:, 0:1024], k.scr["vA"][i * 128:(i + 1) * 128, :], reads=["d_vA"], writes=["dbgtmh"])
            sc.op("vector", lambda e: e.tensor_copy(out=tmp[:, 0:1024], in_=tmh[:, 0:1024]), reads=["dbgtmh"], writes=["dbgtmp"])
            sc.dma("sync", k.dbg[4096 + i * 128:4096 + (i + 1) * 128, 0:1024], tmp[:, 0:1024], reads=["dbgtmp"], writes=["dbg3"])
    if k.stage == 2:
        tmp = sb("dbgtmp", [128, S], ctx=ph)
        for dc in range(NDC):
            sc.op("vector", lambda e, dc=dc: e.tensor_copy(out=tmp[:], in_=k.hT[:, dc, :]),
                  reads=[("hT", dc, tg) for tg in range(4)], writes=["dbgtmp"])
            sc.dma("sync", k.dbg[dc * 128:(dc + 1) * 128, :], tmp[:], reads=["dbgtmp"], writes=["dbg0"])


class ProjRes:
    pass


def proj_setup(k, ph, KC, tag="pj"):
    r = ProjRes()
    sb, ps = k.sb, k.ps
    r.KC = KC
    r.wb = [sb("%s_wb%d" % (tag, i), [128, KC, 512], BF16, ctx=ph) for i in range(2)]
    r.wkey = ["%s_wb%d" % (tag, i) for i in range(2)]
    r.pt = [ps("%s_ps%d" % (tag, i), [128, 512], ctx=ph) for i in range(4)]
    r.pkey = ["%s_ps%d" % (tag, i) for i in range(4)]
    r.sf = [sb("%s_sf%d" % (tag, i), [128, 512], F32, ctx=ph) for i in range(3)]
    r.sh = [sb("%s_sh%d" % (tag, i), [128, 512], BF16, ctx=ph) for i in range(3)]
    r.nblk = 0
    r.npt = 0
    r.nst = 0
    r.tag = tag
    return r


def proj_load_w(k, r, w_ap, cb0, cb1):
    sc = k.sc
    KC = r.KC
    wv = w_ap.rearrange("(kc p) n -> p kc n", p=128)
    i2 = r.nblk % 2
    r.nblk += 1
    w = cb1 - cb0
    sc.dma("gpsimd", r.wb[i2][:, :, 0:w], wv[:, :, cb0:cb1], writes=[r.wkey[i2]])
    return r.wb[i2], r.wkey[i2]


def proj_stage(r, dtype):
    i = r.nst % 3
    r.nst += 1
    if dtype == F32:
        return r.sf[i], "%s_sf%d" % (r.tag, i)
    return r.sh[i], "%s_sh%d" % (r.tag, i)


def proj_psum(r):
    i = r.npt % 4
    r.npt += 1
    return r.pt[i], r.pkey[i]


def proj(k, r, actT, act_keys, w_ap, c0, c1, mode, func, dst, dst_key, dtype):
    sc = k.sc
    KC = r.KC
    cb0 = c0
    while cb0 < c1:
        cb1 = min(c1, cb0 + 512)
        w = cb1 - cb0
        wb, wk = proj_load_w(k, r, w_ap, cb0, cb1)
        if mode == "fm":
            for sub in range((w + 127) // 128):
                cw = min(128, w - sub * 128)
                for tb in range(4):
                    pt, pk = proj_psum(r)
                    for kc in range(KC):
                        sc.op("tensor", lambda e, pt=pt, wb=wb, kc=kc, sub=sub, cw=cw, tb=tb: e.matmul(
                            pt[0:cw, :], lhsT=wb[:, kc, sub * 128: sub * 128 + cw], rhs=actT[:, kc, tb * 512:(tb + 1) * 512],
                            start=(kc == 0), stop=(kc == KC - 1)),
                            reads=[wk] + act_keys(kc, tb), writes=[pk], sig=(kc == KC - 1))
                    st, sk = proj_stage(r, dtype)
                    sc.op("scalar", lambda e, pt=pt, st=st, cw=cw: e.activation(out=st[0:cw, :], in_=pt[0:cw, :], func=func),
                          reads=[pk], writes=[sk])
                    r0 = cb0 - c0 + sub * 128
                    sc.dma("sync", dst[r0:r0 + cw, tb * 512:(tb + 1) * 512], st[0:cw, :], reads=[sk], writes=[dst_key])
        else:
            for ti in range(NTT):
                pt, pk = proj_psum(r)
                for kc in range(KC):
                    sc.op("tensor", lambda e, pt=pt, wb=wb, kc=kc, ti=ti, w=w: e.matmul(
                        pt[:, 0:w], lhsT=actT[:, kc, ti * 128:(ti + 1) * 128], rhs=wb[:, kc, 0:w],
                        start=(kc == 0), stop=(kc == KC - 1)),
                        reads=[wk] + act_keys(kc, ti // 4), writes=[pk], sig=(kc == KC - 1))
                st, sk = proj_stage(r, dtype)
                sc.op("scalar", lambda e, pt=pt, st=st, w=w: e.activation(out=st[:, 0:w], in_=pt[:, 0:w], func=func),
                      reads=[pk], writes=[sk])
                sc.dma("sync", dst[ti * 128:(ti + 1) * 128, cb0 - c0:cb1 - c0], st[:, 0:w], reads=[sk], writes=[dst_key])
        cb0 = cb1


def hT_keys(kc, tb):
    return [("hT", kc, tb)]


PROJ_SPECS = [
    ("qAT", 0, 1024, "fm", "Identity", "bf16"),
    ("kAT", 1024, 2048, "fm", "Identity", "bf16"),
    ("vA", 2048, 3072, "tm", "Identity", "bf16"),
    ("qiT", 3072, 4096, "fm", "Identity", "bf16"),
    ("kiT", 4096, 4160, "fm", "Identity", "bf16"),
    ("wi", 4160, 4176, "tm", "Identity", "f32"),
    ("qBT", 4176, 5200, "fm", "Silu", "f32"),
    ("fBT", 5200, 6224, "fm", "Sigmoid", "f32"),
    ("iB", 6224, 7248, "tm", "Identity", "bf16"),
    ("gBT", 7248, 8272, "fm", "Silu", "bf16"),
    ("sgAT", 8272, 10320, "fm", "Sigmoid", "bf16"),
    ("sgBT", 10320, 12368, "fm", "Sigmoid", "bf16"),
]


def make_scratch(k):
    nc = k.nc
    k.scr = {}
    for name, c0, c1, mode, fn, dtn in PROJ_SPECS:
        dt_ = BF16 if dtn == "bf16" else F32
        shape = [c1 - c0, S] if mode == "fm" else [S, c1 - c0]
        k.scr[name] = nc.dram_tensor("scr_" + name, shape, dt_, kind="Internal").ap()
    k.scr["attnT"] = nc.dram_tensor("scr_attnT", [1024, S], BF16, kind="Internal").ap()
    k.scr["recT"] = nc.dram_tensor("scr_recT", [1024, S], BF16, kind="Internal").ap()
    k.scr["x1"] = nc.dram_tensor("scr_x1", [S, D], F32, kind="Internal").ap()
    k.scr["h2T"] = nc.dram_tensor("scr_h2T", [D, S], BF16, kind="Internal").ap()
    k.scr["pqT"] = nc.dram_tensor("scr_pqT", [D, S], BF16, kind="Internal").ap()
    k.scr["GT"] = nc.dram_tensor("scr_GT", [16384, S], BF16, kind="Internal").ap()


def phase_c(k, ph):
    r = proj_setup(k, ph, NDC, "pc")
    for name, c0, c1, mode, fn, dtn in PROJ_SPECS:
        dt_ = BF16 if dtn == "bf16" else F32
        proj(k, r, k.hT, hT_keys, k.w_in, c0, c1, mode, getattr(AF, fn), k.scr[name], "d_" + name, dt_)


def phase_d(k, ph):
    nc, sc, sb, ps = k.nc, k.sc, k.sb, k.ps
    A = k.scr
    kT = sb("kT", [128, 8, S], BF16, ctx=ph)
    sc.dma("sync", kT[:], A["kAT"].rearrange("(h p) t -> p h t", p=128), reads=["d_kAT"], writes=["kT"])
    vS = sb("vS", [128, 16, 1024], BF16, ctx=ph)
    sc.dma("sync", vS[:], A["vA"].rearrange("(j p) c -> p j c", p=128), reads=["d_vA"], writes=["vS"])
    kiT2 = sb("kiT2", [128, S], BF16, ctx=ph)
    sc.dma("sync", kiT2[0:64, :], A["kiT"], reads=["d_kiT"], writes=["kiT2a"])
    sc.dma("sync", kiT2[64:128, :], A["kiT"], reads=["d_kiT"], writes=["kiT2b"])
    cm29 = sb("cm29", [128, 1], ctx=ph)
    sc.op("vector", lambda e: e.memset(cm29[:], -1e29), writes=["cm29"])
    qTs = [sb("qT%d" % i, [128, 8, 512], BF16, ctx=ph) for i in range(2)]
    qiT = sb("qiT", [128, 8, 512], BF16, ctx=ph)
    wi = sb("wi", [128, 4, 16], ctx=ph)
    wabs = sb("wabs", [128, 4, 16], ctx=ph)
    wsgn = sb("wsgn", [128, 4, 16], ctx=ph)
    scrs = [sb("scr%d" % i, [128, S], ctx=ph) for i in range(2)]
    work = sb("work", [128, S], ctx=ph)
    mks = [sb("mk%d" % i, [128, S], BF16, ctx=ph) for i in range(2)]
    tmps = [sb("itmp%d" % i, [128, 512], ctx=ph) for i in range(3)]
    m8 = sb("m8", [128, 8], ctx=ph)
    NBIS = 20
    pw2 = sb("pw2", [128, NBIS], ctx=ph)
    for kk_ in range(NBIS):
        sc.op("vector", lambda e, kk_=kk_: e.memset(pw2[:, kk_:kk_ + 1], 2.0 ** -(kk_ + 1)), writes=["pw2"])
    bwk = sb("bwk", [128, NBIS], ctx=ph)
    blo = sb("blo", [128, 1], ctx=ph)
    brg = sb("brg", [128, 1], ctx=ph)
    bmid = sb("bmid", [128, 1], ctx=ph)
    bcnt = sb("bcnt", [128, 1], ctx=ph)
    bg = sb("bg", [128, 1], ctx=ph)
    maskTs = [sb("maskT%d" % i, [128, 16, 512], BF16, ctx=ph) for i in range(2)]
    pes = [sb("pe%d" % i, [128, 512], BF16, ctx=ph) for i in range(3)]
    pms = [sb("pm%d" % i, [128, 512], BF16, ctx=ph) for i in range(3)]
    rds = [sb("rd%d" % i, [128, 512], ctx=ph) for i in range(2)]
    aos = [sb("ao%d" % i, [128, 512], BF16, ctx=ph) for i in range(2)]
    pis = [ps("pi%d" % i, [128, 512], ctx=ph) for i in range(2)]
    ptr = ps("ptr", [128, 512], BF16, ctx=ph)
    pls = [ps("pl%d" % i, [128, 512], ctx=ph) for i in range(2)]
    po = ps("po", [128, 512], ctx=ph)
    pd = ps("pd", [128, 512], ctx=ph)
    att_scale = 128.0 ** -0.5
    NEG = -1e30
    n_i = 0
    n_e = 0
    n_h = 0
    streams = {}
    for tb in range(4):
        sc.begin_capture()
        maskT = maskTs[tb % 2]
        mkey = "maskT%d" % (tb % 2)
        qT = qTs[tb % 2]
        qTk = "qT%d" % (tb % 2)
        sc.dma("sync", qT[:], A["qAT"].rearrange("(h p) t -> p h t", p=128)[:, :, tb * 512:(tb + 1) * 512],
               reads=["d_qAT"], writes=[qTk])
        sc.dma("sync", qiT[:], A["qiT"].rearrange("(h p) t -> p h t", p=128)[:, :, tb * 512:(tb + 1) * 512],
               reads=["d_qiT"], writes=["qiT"])
        sc.dma("sync", wi[:], A["wi"].rearrange("(n p) h -> p n h", p=128)[:, tb * 4:(tb + 1) * 4, :],
               reads=["d_wi"], writes=["wi"])
        sc.op("scalar", lambda e: e.activation(out=wabs[:], in_=wi[:], func=AF.Abs), reads=["wi"], writes=["wabs"])
        sc.op("scalar", lambda e: e.activation(out=wsgn[:], in_=wi[:], func=AF.Sign), reads=["wi"], writes=["wsgn"])
        sc.op("vector", lambda e, maskT=maskT: e.memset(maskT[:], -30000.0), writes=[mkey])
        for tt in range(4):
            i = tb * 4 + tt
            L = 128 * (i + 1)
            scr = scrs[i % 2]
            skey = "scr%d" % (i % 2)
            mk = mks[i % 2]
            mkk = "mk%d" % (i % 2)
            for h in range(16):
                cch = h // 2
                p0 = 64 * (h % 2)
                kik = "kiT2a" if p0 == 0 else "kiT2b"
                for s0 in range(0, L, 512):
                    sw = min(512, L - s0)
                    pi = pis[n_i % 2]
                    pik = "pi%d" % (n_i % 2)
                    tmp = tmps[n_i % 3]
                    tk = "itmp%d" % (n_i % 3)
                    n_i += 1
                    sc.op("tensor", lambda e, pi=pi, p0=p0, cch=cch, tt=tt, s0=s0, sw=sw: e.matmul(
                        pi[:, 0:sw], lhsT=qiT[p0:p0 + 64, cch, tt * 128:(tt + 1) * 128], rhs=kiT2[p0:p0 + 64, s0:s0 + sw],
                        start=True, stop=True), reads=["qiT", kik], writes=[pik])
                    sc.op("scalar", lambda e, pi=pi, tmp=tmp, sw=sw, tt=tt, h=h: e.activation(
                        out=tmp[:, 0:sw], in_=pi[:, 0:sw], func=AF.Relu, scale=wabs[:, tt, h:h + 1]),
                        reads=[pik, "wabs"], writes=[tk])
                    if h == 0:
                        sc.op("vector", lambda e, tmp=tmp, scr=scr, s0=s0, sw=sw, tt=tt, h=h: e.tensor_scalar(
                            out=scr[:, s0:s0 + sw], in0=tmp[:, 0:sw], scalar1=wsgn[:, tt, h:h + 1], scalar2=None, op0=ALU.mult),
                            reads=[tk, "wsgn"], writes=[skey])
                    else:
                        sc.op("vector", lambda e, tmp=tmp, scr=scr, s0=s0, sw=sw, tt=tt, h=h: e.scalar_tensor_tensor(
                            out=scr[:, s0:s0 + sw], in0=tmp[:, 0:sw], scalar=wsgn[:, tt, h:h + 1], in1=scr[:, s0:s0 + sw],
                            op0=ALU.mult, op1=ALU.add), reads=[tk, "wsgn", skey], writes=[skey])
            if i >= 2:
                sc.op("vector", lambda e, scr=scr, L=L: e.tensor_reduce(out=blo[:], in_=scr[:, 0:L], axis=AX.X, op=ALU.min),
                      reads=[skey], writes=["blo"])
            sc.op("vector", lambda e, scr=scr, L=L: e.memset(scr[0:64, L - 64:L], NEG), writes=[skey])
            if i >= 2:
                sc.op("vector", lambda e, scr=scr, L=L: e.max(out=m8[:], in_=scr[:, 0:L]), reads=[skey], writes=["m8"])
                sc.op("vector", lambda e: e.tensor_tensor(out=brg[:], in0=m8[:, 0:1], in1=blo[:], op=ALU.subtract),
                      reads=["m8", "blo"], writes=["brg"])
                sc.op("vector", lambda e: e.tensor_scalar(out=bwk[:], in0=pw2[:], scalar1=brg[:, 0:1], scalar2=None, op0=ALU.mult),
                      reads=["pw2", "brg"], writes=["bwk"])
                sc.op("vector", lambda e: e.tensor_tensor(out=bmid[:], in0=blo[:], in1=bwk[:, 0:1], op=ALU.add),
                      reads=["blo", "bwk"], writes=["bmid"])
                for kk_ in range(NBIS):
                    sc.op("vector", lambda e, scr=scr, L=L: e.tensor_scalar(out=work[:, 0:L], in0=scr[:, 0:L], scalar1=bmid[:, 0:1], scalar2=0.0,
                                                                            op0=ALU.is_ge, op1=ALU.add, accum_out=bcnt[:, 0:1]),
                          reads=[skey, "bmid"], writes=["work", "bcnt"])
                    sc.op("vector", lambda e, kk_=kk_: e.tensor_scalar(out=bg[:], in0=bcnt[:], scalar1=255.5, scalar2=bwk[:, kk_:kk_ + 1],
                                                                       op0=ALU.is_ge, op1=ALU.mult), reads=["bcnt", "bwk"], writes=["bg"])
                    sc.op("vector", lambda e: e.tensor_tensor(out=blo[:], in0=blo[:], in1=bg[:], op=ALU.add),
                          reads=["blo", "bg"], writes=["blo"])
                    if kk_ + 1 < NBIS:
                        sc.op("vector", lambda e, kk_=kk_: e.tensor_tensor(out=bmid[:], in0=blo[:], in1=bwk[:, kk_ + 1:kk_ + 2], op=ALU.add),
                              reads=["blo", "bwk"], writes=["bmid"])
                tau = blo[:, 0:1]
                tauk = "blo"
            else:
                tau = cm29[:, 0:1]
                tauk = "cm29"
            sc.op("vector", lambda e, scr=scr, mk=mk, L=L, tau=tau: e.tensor_scalar(
                out=mk[:, 0:L], in0=scr[:, 0:L], scalar1=tau, scalar2=None, op0=ALU.is_ge),
                reads=[skey, tauk], writes=[mkk])
            for j0 in range(0, i + 1, 4):
                n = min(4, i + 1 - j0)
                for jj in range(n):
                    j = j0 + jj
                    sc.op("tensor", lambda e, jj=jj, j=j, mk=mk: e.transpose(out=ptr[:, jj * 128:(jj + 1) * 128],
                                                                             in_=mk[:, j * 128:(j + 1) * 128], identity=k.identb[:]),
                          reads=[mkk, "identb"], writes=["ptr"], sig=(jj == n - 1))
                sc.op("scalar", lambda e, j0=j0, n=n, tt=tt, maskT=maskT: e.activation(
                    out=maskT[:, j0:j0 + n, tt * 128:(tt + 1) * 128],
                    in_=ptr[:, 0:n * 128].rearrange("p (n t) -> p n t", t=128), func=AF.Identity, scale=30000.0, bias=-30000.0),
                    reads=["ptr"], writes=[mkey])
        streams[("i", tb)] = sc.end_capture()
        sc.begin_capture()
        nj = 4 * (tb + 1)
        units = [(h, j) for h in range(8) for j in range(nj)]
        ubuf = {}

        def emit_lg(u):
            h, j = units[u]
            pl = pls[u % 2]
            plk = "pl%d" % (u % 2)
            sc.op("tensor", lambda e, pl=pl, h=h, j=j, qT=qT: e.matmul(pl[:, :], lhsT=kT[:, h, j * 128:(j + 1) * 128], rhs=qT[:, h, :],
                                                               start=True, stop=False), reads=["kT", qTk], writes=[plk], sig=False)
            sc.op("tensor", lambda e, pl=pl, j=j, maskT=maskT: e.matmul(pl[:, :], lhsT=k.identb[:], rhs=maskT[:, j, :],
                                                                       start=False, stop=True), reads=["identb", mkey], writes=[plk])
            pe = pes[u % 3]
            pek = "pe%d" % (u % 3)
            sc.op("scalar", lambda e, pl=pl, pe=pe: e.activation(out=pe[:], in_=pl[:, :], func=AF.Exp, scale=att_scale),
                  reads=[plk], writes=[pek])

        emit_lg(0)
        for u, (h, j) in enumerate(units):
            if u + 1 < len(units):
                emit_lg(u + 1)
            pm = pes[u % 3]
            pmk = "pe%d" % (u % 3)
            sc.op("tensor", lambda e, pm=pm, h=h, j=j: e.matmul(po[:, :], lhsT=vS[:, j, h * 128:(h + 1) * 128], rhs=pm[:],
                                                               start=(j == 0), stop=(j == nj - 1)), reads=["vS", pmk], writes=["po"], sig=False)
            sc.op("tensor", lambda e, pm=pm, j=j: e.matmul(pd[:, :], lhsT=k.onesb[:], rhs=pm[:],
                                                          start=(j == 0), stop=(j == nj - 1)), reads=["onesb", pmk], writes=["pd"])
            if j == nj - 1:
                rd = rds[n_h % 2]
                rdk = "rd%d" % (n_h % 2)
                ao = aos[n_h % 2]
                aok = "ao%d" % (n_h % 2)
                n_h += 1
                sc.op("vector", lambda e, rd=rd: e.reciprocal(out=rd[:], in_=pd[:, :]), reads=["pd"], writes=[rdk])
                sc.op("vector", lambda e, rd=rd, ao=ao: e.tensor_tensor(out=ao[:], in0=po[:, :], in1=rd[:], op=ALU.mult),
                      reads=["po", rdk], writes=[aok])
                sc.dma("sync", A["attnT"][h * 128:(h + 1) * 128, tb * 512:(tb + 1) * 512], ao[:], reads=[aok], writes=["d_attnT"])
        streams[("a", tb)] = sc.end_capture()
    for u in streams[("i", 0)]:
        u()
    for tb in range(4):
        merged_units([streams[("a", tb)], streams.get(("i", tb + 1), [])])


def phase_e(k, ph):
    nc, sc, sb, ps = k.nc, k.sc, k.sb, k.ps
    A = k.scr
    NCH = 32
    lbl = sb("lbl", [128, 2, 8], ctx=ph)
    sc.dma("sync", lbl[:], k.lb_logits.rearrange("r (h d) -> d r h", d=128), writes=["lbl"], allow_slow_non_contiguous=True)
    lb = sb("lb", [128, 8], ctx=ph)
    oml = sb("oml", [128, 8], ctx=ph)
    sc.op("vector", lambda e: e.tensor_tensor(out=lb[:], in0=lbl[:, 0, :], in1=lbl[:, 1, :], op=ALU.subtract), reads=["lbl"], writes=["lb"])
    sc.op("scalar", lambda e: e.activation(out=lb[:], in_=lb[:], func=AF.Sigmoid), reads=["lb"], writes=["lb"])
    sc.op("vector", lambda e: e.tensor_scalar(out=oml[:], in0=lb[:], scalar1=-1.0, scalar2=1.0, op0=ALU.mult, op1=ALU.add),
          reads=["lb"], writes=["oml"])
    gain = sb("hgain", [128, 1], ctx=ph)
    sc.dma("sync", gain[:], k.hgrn_gain.rearrange("o d -> d o"), writes=["hgain"], allow_slow_non_contiguous=True)
    tri = sb("tri_sb", [64, 64], ctx=ph)
    sc.dma("sync", tri[:], k.tri_d, writes=["tri"])
    rst = sb("rst", [128, S], ctx=ph)
    sc.op("vector", lambda e: e.memset(rst[:], 1.0), writes=["rst"])
    sc.op("vector", lambda e: e.memset(rst[:].rearrange("p (c s) -> p c s", s=64)[:, :, 0:1], 0.0), writes=["rst"])
    qf = sb("qf", [128, S], ctx=ph)
    ff = sb("ff", [128, S], ctx=ph)
    kk = sb("kk", [128, S], ctx=ph)
    lf = sb("lf", [128, S], ctx=ph)
    bb = sb("bb", [128, S], ctx=ph)
    bp = sb("bp", [128, S], ctx=ph)
    tA = sb("tA", [128, S], ctx=ph)
    qhat = sb("qhat", [128, S], BF16, ctx=ph)
    qtil = sb("qtil", [128, S], BF16, ctx=ph)
    ktil = sb("ktil", [128, S], BF16, ctx=ph)
    kendT = sb("kendT", [128, S], BF16, ctx=ph)
    kend = sb("kend", [64, NCH, 128], BF16, ctx=ph)
    vS = sb("hvS", [64, NCH, 128], BF16, ctx=ph)
    atT = sb("atT", [64, NCH, 64], BF16, ctx=ph)
    recT = sb("recT", [128, S], ctx=ph)
    sgb = sb("sgb", [128, S], BF16, ctx=ph)
    ebl = sb("ebl", [128, NCH], ctx=ph)
    Sf = sb("Sf", [128, 128], ctx=ph)
    Sb = sb("Sb", [128, 128], BF16, ctx=ph)
    rs = sb("hrs", [128, 512], ctx=ph)
    t1 = sb("ht1", [128, 512], ctx=ph)
    obs = [sb("hob%d" % i, [128, 512], BF16, ctx=ph) for i in range(2)]
    ptk = ps("ptk", [64, 512], BF16, ctx=ph)
    pat = ps("pat", [64, 512], ctx=ph)
    pos = [ps("hpo%d" % i, [128, 512], ctx=ph) for i in range(2)]
    pSs = [ps("hpS%d" % i, [128, 128], ctx=ph) for i in range(2)]
    pn = ps("hpn", [128, 512], ctx=ph)
    v3 = lambda t: t[:].rearrange("p (c s) -> p c s", s=64)
    n_ob = 0
    for h in range(8):
        rows = slice(h * 128, (h + 1) * 128)
        sc.dma("sync", qf[:], A["qBT"][rows, :], reads=["d_qBT"], writes=["qf"])
        sc.dma("sync", ff[:], A["fBT"][rows, :], reads=["d_fBT"], writes=["ff"])
        sc.dma("sync", sgb[:], A["gBT"][rows, :], reads=["d_gBT"], writes=["sgb"])
        sc.dma("sync", vS[:], A["iB"].rearrange("(c s) n -> s c n", s=64)[:, :, rows], reads=["d_iB"], writes=["hvS"])
        sc.op("vector", lambda e, h=h: e.tensor_scalar(out=ff[:], in0=ff[:], scalar1=oml[:, h:h + 1], scalar2=lb[:, h:h + 1],
                                                       op0=ALU.mult, op1=ALU.add), reads=["ff", "oml", "lb"], writes=["ff"])
        sc.op("vector", lambda e: e.tensor_scalar(out=kk[:], in0=ff[:], scalar1=-1.0, scalar2=1.0, op0=ALU.mult, op1=ALU.add),
              reads=["ff"], writes=["kk"])
        sc.op("scalar", lambda e: e.activation(out=lf[:], in_=ff[:], func=AF.Ln), reads=["ff"], writes=["lf"])
        sc.op("vector", lambda e: e.tensor_tensor_scan(out=bb[:], data0=rst[:], data1=lf[:], initial=0.0, op0=ALU.mult, op1=ALU.add),
              reads=["rst", "lf"], writes=["bb"])
        sc.op("scalar", lambda e: e.activation(out=tA[:], in_=bb[:], func=AF.Exp), reads=["bb"], writes=["tA"])
        sc.op("vector", lambda e: e.tensor_tensor(out=qhat[:], in0=qf[:], in1=tA[:], op=ALU.mult), reads=["qf", "tA"], writes=["qhat"])
        sc.op("scalar", lambda e: e.activation(out=ebl[:], in_=v3(bb)[:, :, 63], func=AF.Exp), reads=["bb"], writes=["ebl"])
        sc.op("vector", lambda e: e.tensor_tensor(out=v3(bp), in0=v3(bb), in1=v3(bb)[:, :, 31:32].to_broadcast([128, NCH, 64]),
                                                  op=ALU.subtract), reads=["bb"], writes=["bp"])
        sc.op("scalar", lambda e: e.activation(out=tA[:], in_=bp[:], func=AF.Exp), reads=["bp"], writes=["tA"])
        sc.op("vector", lambda e: e.tensor_tensor(out=qtil[:], in0=qf[:], in1=tA[:], op=ALU.mult), reads=["qf", "tA"], writes=["qtil"])
        sc.op("scalar", lambda e: e.activation(out=tA[:], in_=bp[:], func=AF.Exp, scale=-1.0), reads=["bp"], writes=["tA"])
        sc.op("vector", lambda e: e.tensor_tensor(out=ktil[:], in0=kk[:], in1=tA[:], op=ALU.mult), reads=["kk", "tA"], writes=["ktil"])
        sc.op("vector", lambda e: e.tensor_tensor(out=v3(bp), in0=v3(bb)[:, :, 63:64].to_broadcast([128, NCH, 64]), in1=v3(bb),
                                                  op=ALU.subtract), reads=["bb"], writes=["bp"])
        sc.op("scalar", lambda e: e.activation(out=tA[:], in_=bp[:], func=AF.Exp), reads=["bp"], writes=["tA"])
        sc.op("vector", lambda e: e.tensor_tensor(out=kendT[:], in0=kk[:], in1=tA[:], op=ALU.mult), reads=["kk", "tA"], writes=["kendT"])
        for c0 in range(0, NCH, 4):
            for jj in range(4):
                c = c0 + jj
                sc.op("tensor", lambda e, jj=jj, c=c: e.transpose(out=ptk[:, jj * 128:(jj + 1) * 128], in_=kendT[:, c * 64:(c + 1) * 64],
                                                                 identity=k.identb[:]), reads=["kendT", "identb"], writes=["ptk"], sig=(jj == 3))
            sc.op("scalar", lambda e, c0=c0: e.activation(out=kend[:, c0:c0 + 4, :], in_=ptk[:, :].rearrange("p (n d) -> p n d", d=128),
                                                          func=AF.Copy), reads=["ptk"], writes=["kend"])
        for c0 in range(0, NCH, 8):
            for jj in range(8):
                c = c0 + jj
                sc.op("tensor", lambda e, jj=jj, c=c: e.matmul(pat[:, jj * 64:(jj + 1) * 64], lhsT=ktil[:, c * 64:(c + 1) * 64],
                                                              rhs=qtil[:, c * 64:(c + 1) * 64], start=True, stop=True),
                      reads=["ktil", "qtil"], writes=["pat"], sig=(jj == 7))
            sc.op("vector", lambda e, c0=c0: e.tensor_tensor(out=atT[:, c0:c0 + 8, :], in0=pat[:, :].rearrange("p (n t) -> p n t", t=64),
                                                             in1=tri[:, :].unsqueeze(1).to_broadcast([64, 8, 64]), op=ALU.mult),
                  reads=["pat", "tri"], writes=["atT"])
        for c in range(NCH):
            po = pos[(c // 8) % 2]
            pok = "hpo%d" % ((c // 8) % 2)
            col = (c % 8) * 64
            if c > 0:
                sc.op("tensor", lambda e, po=po, col=col, c=c: e.matmul(po[:, col:col + 64], lhsT=Sb[:], rhs=qhat[:, c * 64:(c + 1) * 64],
                                                                       start=True, stop=False), reads=["Sb", "qhat"], writes=[pok], sig=False)
            sc.op("tensor", lambda e, po=po, col=col, c=c: e.matmul(po[:, col:col + 64], lhsT=vS[:, c, :], rhs=atT[:, c, :],
                                                                   start=(c == 0), stop=True), reads=["hvS", "atT"], writes=[pok])
            if c < NCH - 1:
                pS = pSs[c % 2]
                pSk = "hpS%d" % (c % 2)
                sc.op("tensor", lambda e, pS=pS, c=c: e.matmul(pS[:, :], lhsT=kend[:, c, :], rhs=vS[:, c, :], start=True, stop=True),
                      reads=["kend", "hvS"], writes=[pSk])
                if c == 0:
                    sc.op("vector", lambda e, pS=pS: e.tensor_copy(out=Sb[:], in_=pS[:, :]), reads=[pSk], writes=["Sb"])
                    sc.op("vector", lambda e, pS=pS: e.tensor_copy(out=Sf[:], in_=pS[:, :]), reads=[pSk], writes=["Sf"])
                else:
                    sc.op("vector", lambda e, pS=pS, c=c: e.scalar_tensor_tensor(out=Sb[:], in0=Sf[:], scalar=ebl[:, c:c + 1], in1=pS[:, :],
                                                                                op0=ALU.mult, op1=ALU.add),
                          reads=["Sf", "ebl", pSk], writes=["Sb"])
                    sc.op("vector", lambda e, pS=pS, c=c: e.scalar_tensor_tensor(out=Sf[:], in0=Sf[:], scalar=ebl[:, c:c + 1], in1=pS[:, :],
                                                                                op0=ALU.mult, op1=ALU.add),
                          reads=["Sf", "ebl", pSk], writes=["Sf"])
            if c % 8 == 7:
                sc.op("scalar", lambda e, po=po, c=c: e.activation(out=recT[:, (c - 7) * 64:(c + 1) * 64], in_=po[:, :], func=AF.Copy),
                      reads=[pok], writes=["recT"])
        sc.op("scalar", lambda e: e.activation(out=lf[:], in_=recT[:], func=AF.Square), reads=["recT"], writes=["lf"])
        for tb in range(4):
            cs_ = slice(tb * 512, (tb + 1) * 512)
            sc.op("tensor", lambda e, cs_=cs_: e.matmul(pn[:, :], lhsT=k.onesf[:], rhs=lf[:, cs_], start=True, stop=True),
                  reads=["onesf", "lf"], writes=["hpn"])
            sc.op("vector", lambda e: e.tensor_scalar(out=rs[:], in0=pn[:, :], scalar1=1.0 / 128, scalar2=EPS, op0=ALU.mult, op1=ALU.add),
                  reads=["hpn"], writes=["hrs"])
            sc.op("scalar", lambda e: e.activation(out=rs[:], in_=rs[:], func=AF.Sqrt), reads=["hrs"], writes=["hrs"])
            sc.op("vector", lambda e: e.reciprocal(out=rs[:], in_=rs[:]), reads=["hrs"], writes=["hrs"])
            sc.op("vector", lambda e, cs_=cs_: e.scalar_tensor_tensor(out=t1[:], in0=recT[:, cs_], scalar=gain[:, 0:1], in1=rs[:],
                                                                      op0=ALU.mult, op1=ALU.mult), reads=["recT", "hgain", "hrs"], writes=["ht1"])
            ob = obs[n_ob % 2]
            obk = "hob%d" % (n_ob % 2)
            n_ob += 1
            sc.op("vector", lambda e, ob=ob, cs_=cs_: e.tensor_tensor(out=ob[:], in0=t1[:], in1=sgb[:, cs_], op=ALU.mult),
                  reads=["ht1", "sgb"], writes=[obk])
            sc.dma("sync", A["recT"][rows, cs_], ob[:], reads=[obk], writes=["d_recT"])


def phase_f(k, ph):
    nc, sc, sb, ps = k.nc, k.sc, k.sb, k.ps
    A = k.scr
    aT = sb("f_aT", [128, 8, S], BF16, ctx=ph)
    rT = sb("f_rT", [128, 8, S], BF16, ctx=ph)
    sc.dma("sync", aT[:], A["attnT"].rearrange("(h p) t -> p h t", p=128), reads=["d_attnT"], writes=["f_aT"])
    sc.dma("sync", rT[:], A["recT"].rearrange("(h p) t -> p h t", p=128), reads=["d_recT"], writes=["f_rT"])
    mixT = k.hT
    was = [sb("f_wa%d" % i, [128, 8, 512], BF16, ctx=ph) for i in range(2)]
    wbs = [sb("f_wb%d" % i, [128, 8, 512], BF16, ctx=ph) for i in range(2)]
    sgas = [sb("f_sga%d" % i, [128, 512], BF16, ctx=ph) for i in range(2)]
    sgbs = [sb("f_sgb%d" % i, [128, 512], BF16, ctx=ph) for i in range(2)]
    yas = [sb("f_ya%d" % i, [128, 512], ctx=ph) for i in range(2)]
    ybs = [sb("f_yb%d" % i, [128, 512], ctx=ph) for i in range(2)]
    pas = [ps("f_pa%d" % i, [128, 512], ctx=ph) for i in range(2)]
    pbs = [ps("f_pb%d" % i, [128, 512], ctx=ph) for i in range(2)]
    wva = k.w_up_a.rearrange("(kc p) n -> p kc n", p=128)
    wvb = k.w_up_b.rearrange("(kc p) n -> p kc n", p=128)
    n = 0
    for nb in range(4):
        wa, wb = was[nb % 2], wbs[nb % 2]
        wak, wbk = "f_wa%d" % (nb % 2), "f_wb%d" % (nb % 2)
        sc.dma("gpsimd", wa[:], wva[:, :, nb * 512:(nb + 1) * 512], writes=[wak])
        sc.dma("gpsimd", wb[:], wvb[:, :, nb * 512:(nb + 1) * 512], writes=[wbk])
        for sub in range(4):
            nch = nb * 4 + sub
            for tb in range(4):
                i2 = n % 2
                n += 1
                pa, pb = pas[i2], pbs[i2]
                pak, pbk = "f_pa%d" % i2, "f_pb%d" % i2
                sga, sgb = sgas[i2], sgbs[i2]
                ya, yb = yas[i2], ybs[i2]
                cs_ = slice(tb * 512, (tb + 1) * 512)
                sc.dma("sync", sga[:], A["sgAT"][nch * 128:(nch + 1) * 128, cs_], reads=["d_sgAT"], writes=["f_sga%d" % i2])
                sc.dma("sync", sgb[:], A["sgBT"][nch * 128:(nch + 1) * 128, cs_], reads=["d_sgBT"], writes=["f_sgb%d" % i2])
                for kc in range(8):
                    sc.op("tensor", lambda e, pa=pa, wa=wa, kc=kc, sub=sub, cs_=cs_: e.matmul(
                        pa[:, :], lhsT=wa[:, kc, sub * 128:(sub + 1) * 128], rhs=aT[:, kc, cs_], start=(kc == 0), stop=(kc == 7)),
                        reads=[wak, "f_aT"], writes=[pak], sig=(kc == 7))
                for kc in range(8):
                    sc.op("tensor", lambda e, pb=pb, wb=wb, kc=kc, sub=sub, cs_=cs_: e.matmul(
                        pb[:, :], lhsT=wb[:, kc, sub * 128:(sub + 1) * 128], rhs=rT[:, kc, cs_], start=(kc == 0), stop=(kc == 7)),
                        reads=[wbk, "f_rT"], writes=[pbk], sig=(kc == 7))
                sc.op("vector", lambda e, pa=pa, ya=ya, sga=sga: e.tensor_tensor(out=ya[:], in0=pa[:, :], in1=sga[:], op=ALU.mult),
                      reads=[pak, "f_sga%d" % i2], writes=["f_ya%d" % i2])
                sc.op("vector", lambda e, pb=pb, yb=yb, sgb=sgb: e.tensor_tensor(out=yb[:], in0=pb[:, :], in1=sgb[:], op=ALU.mult),
                      reads=[pbk, "f_sgb%d" % i2], writes=["f_yb%d" % i2])
                sc.op("vector", lambda e, ya=ya, yb=yb, nch=nch, cs_=cs_: e.tensor_tensor(out=mixT[:, nch, cs_], in0=ya[:], in1=yb[:], op=ALU.add),
                      reads=["f_ya%d" % i2, "f_yb%d" % i2], writes=[("hT", nch, tb)])


def phase_f2(k, ph):
    nc, sc, sb, ps = k.nc, k.sc, k.sb, k.ps
    A = k.scr
    mixT = k.hT
    r = proj_setup(k, ph, NDC, "fo")
    xss = [sb("f_xs%d" % i, [128, 512], ctx=ph) for i in range(3)]
    tts = [sb("f_tt%d" % i, [128, 512], ctx=ph) for i in range(3)]
    xv = k.x.rearrange("(n p) d -> n p d", p=128)
    x1v = A["x1"].rearrange("(n p) d -> n p d", p=128)
    m = 0
    for nb in range(4):
        cs_ = slice(nb * 512, (nb + 1) * 512)
        wb, wk = proj_load_w(k, r, k.w_out, nb * 512, (nb + 1) * 512)
        for ti in range(NTT):
            pt, pk = proj_psum(r)
            i3 = m % 3
            m += 1
            xs, tt_ = xss[i3], tts[i3]
            sc.dma("sync", xs[:], xv[ti][:, cs_], writes=["f_xs%d" % i3])
            for kc in range(NDC):
                sc.op("tensor", lambda e, pt=pt, wb=wb, kc=kc, ti=ti: e.matmul(
                    pt[:, :], lhsT=mixT[:, kc, ti * 128:(ti + 1) * 128], rhs=wb[:, kc, :], start=(kc == 0), stop=(kc == NDC - 1)),
                    reads=[wk, ("hT", kc, ti // 4)], writes=[pk], sig=(kc == NDC - 1))
            sc.op("vector", lambda e, pt=pt, tt_=tt_, cs_=cs_: e.tensor_tensor(out=tt_[:], in0=pt[:, :], in1=k.G1[:, cs_], op=ALU.mult),
                  reads=[pk, "G0"], writes=["f_tt%d" % i3])
            sc.op("vector", lambda e, tt_=tt_, xs=xs: e.tensor_tensor(out=tt_[:], in0=tt_[:], in1=xs[:], op=ALU.add),
                  reads=["f_tt%d" % i3, "f_xs%d" % i3], writes=["f_tt%d" % i3])
            sc.dma("sync", x1v[ti][:, cs_], tt_[:], reads=["f_tt%d" % i3], writes=["d_x1"])


def phase_g(k, ph):
    sc = k.sc
    norm_modulate(k, ph, k.scr["x1"], k.norm_ffn, 48, 64, ["d_x1"], tag="g2")
    for dc in range(NDC):
        sc.dma("sync", k.scr["h2T"][dc * 128:(dc + 1) * 128, :], k.hT[:, dc, :], reads=[("hT", dc, tg) for tg in range(4)],
               writes=["d_h2T"])


def phase_h1(k, ph):
    r = proj_setup(k, ph, NDC, "ph")
    proj(k, r, k.hT, hT_keys, k.peer_w_q, 0, 2048, "fm", AF.Identity, k.scr["pqT"], "d_pqT", BF16)


def phase_h2(k, ph):
    nc, sc, sb, ps = k.nc, k.sc, k.sb, k.ps
    A = k.scr
    NEG = -1e30
    kl = sb("h_kl", [128, 16, 128], ctx=ph)
    sc.dma("sync", kl[:], k.peer_keys.rearrange("p h n d -> n (p h) d"), writes=["h_kl"])
    keysT = sb("h_keysT", [128, 16, 128], BF16, ctx=ph)
    pss = [ps("h_ps%d" % i, [128, 512], ctx=ph) for i in range(4)]
    for ch in range(16):
        b = ch // 4
        sc.op("tensor", lambda e, ch=ch, b=b: e.transpose(out=pss[b][:, (ch % 4) * 128:(ch % 4 + 1) * 128], in_=kl[:, ch, :],
                                                         identity=k.identf[:]), reads=["h_kl", "identf"], writes=["h_ps%d" % b], sig=(ch % 4 == 3))
    for b in range(4):
        sc.op("vector", lambda e, b=b: e.tensor_copy(out=keysT[:, b * 4:(b + 1) * 4, :],
                                                     in_=pss[b][:, :].rearrange("p (c n) -> p c n", n=128)),
              reads=["h_ps%d" % b], writes=["h_keysT"])
    qts = [sb("h_qt%d" % i, [128, 16, 128], BF16, ctx=ph) for i in range(2)]
    s_sb = sb("h_s", [128, 16, 128], ctx=ph)
    wk = sb("h_wk", [128, 16, 128], ctx=ph)
    tops = sb("h_tops", [128, 16, 16], ctx=ph)
    cand = sb("h_cand", [128, 8, 256], ctx=ph)
    cwk = sb("h_cwk", [128, 8, 256], ctx=ph)
    best = sb("h_best", [128, 8, 16], ctx=ph)
    ez = sb("h_ez", [128, 8, 16], ctx=ph)
    Z = sb("h_Z", [128, 8], ctx=ph)
    bias = sb("h_bias", [128, 8], ctx=ph)
    th = sb("h_th", [128, 8, 16], ctx=ph)
    e1 = sb("h_e1", [128, 8, 16], ctx=ph)
    e1T = sb("h_e1T", [128, 128], ctx=ph)
    e2 = sb("h_e2", [128, 8, 128], ctx=ph)
    Rt = sb("h_R", [128, 128, 128], BF16, ctx=ph)
    Oh = sb("h_O", [128, 64, 128], BF16, ctx=ph)
    RT = sb("h_RT", [128, 128, 128], BF16, ctx=ph)
    OT = sb("h_OT", [128, 64, 128], BF16, ctx=ph)
    gsts = [sb("h_gst%d" % i, [128, 64, 128], BF16, ctx=ph) for i in range(2)]
    ptrs = [ps("h_ptr%d" % i, [128, 512], BF16, ctx=ph) for i in range(2)]
    pgs = [ps("h_pg%d" % i, [128, 512], ctx=ph) for i in range(2)]
    pqv = A["pqT"].rearrange("(c p) t -> p c t", p=128)
    GTv = A["GT"].rearrange("(i j) t -> j i t", j=128)
    s4 = s_sb[:].rearrange("p (a h) n -> p a h n", h=2)
    t4 = tops[:].rearrange("p (a h) n -> p a h n", h=2)
    n_s = 0
    n_g = 0
    n_pg = 0
    for ti in range(NTT):
        qt = qts[ti % 2]
        qk = "h_qt%d" % (ti % 2)
        sc.dma("sync", qt[:], pqv[:, :, ti * 128:(ti + 1) * 128], reads=["d_pqT"], writes=[qk])
        for ch in range(16):
            b = ch // 4
            sc.op("tensor", lambda e, ch=ch, b=b, qt=qt: e.matmul(pss[b][:, (ch % 4) * 128:(ch % 4 + 1) * 128], lhsT=qt[:, ch, :],
                                                                 rhs=keysT[:, ch, :], start=True, stop=True),
                  reads=[qk, "h_keysT"], writes=["h_ps%d" % b], sig=(ch % 4 == 3))
        for b in range(4):
            sc.op("scalar", lambda e, b=b: e.activation(out=s_sb[:, b * 4:(b + 1) * 4, :],
                                                        in_=pss[b][:, :].rearrange("p (c n) -> p c n", n=128), func=AF.Copy),
                  reads=["h_ps%d" % b], writes=["h_s"])
        for ch in range(16):
            sc.op("vector", lambda e, ch=ch: e.max(out=tops[:, ch, 0:8], in_=s_sb[:, ch, :]), reads=["h_s"], writes=["h_tops"])
            sc.op("vector", lambda e, ch=ch: e.match_replace(out=wk[:, ch, :], in_to_replace=tops[:, ch, 0:8], in_values=s_sb[:, ch, :],
                                                             imm_value=NEG), reads=["h_s", "h_tops"], writes=["h_wk"])
            sc.op("vector", lambda e, ch=ch: e.max(out=tops[:, ch, 8:16], in_=wk[:, ch, :]), reads=["h_wk"], writes=["h_tops"])
        sc.op("vector", lambda e: e.tensor_tensor(out=cand[:].rearrange("p a (r c) -> p a r c", c=16),
                                                  in0=t4[:, :, 0, :].unsqueeze(3).to_broadcast([128, 8, 16, 16]),
                                                  in1=t4[:, :, 1, :].unsqueeze(2).to_broadcast([128, 8, 16, 16]), op=ALU.add),
              reads=["h_tops"], writes=["h_cand"])
        for p in range(8):
            sc.op("vector", lambda e, p=p: e.max(out=best[:, p, 0:8], in_=cand[:, p, :]), reads=["h_cand"], writes=["h_best"])
            sc.op("vector", lambda e, p=p: e.match_replace(out=cwk[:, p, :], in_to_replace=best[:, p, 0:8], in_values=cand[:, p, :],
                                                           imm_value=NEG), reads=["h_cand", "h_best"], writes=["h_cwk"])
            sc.op("vector", lambda e, p=p: e.max(out=best[:, p, 8:16], in_=cwk[:, p, :]), reads=["h_cwk"], writes=["h_best"])
        sc.op("vector", lambda e: e.tensor_tensor(out=ez[:], in0=best[:], in1=best[:, :, 0:1].to_broadcast([128, 8, 16]), op=ALU.subtract),
              reads=["h_best"], writes=["h_ez"])
        sc.op("scalar", lambda e: e.activation(out=ez[:], in_=ez[:], func=AF.Exp), reads=["h_ez"], writes=["h_ez"])
        sc.op("vector", lambda e: e.tensor_reduce(out=Z[:], in_=ez[:], axis=AX.X, op=ALU.add), reads=["h_ez"], writes=["h_Z"])
        sc.op("scalar", lambda e: e.activation(out=Z[:], in_=Z[:], func=AF.Ln), reads=["h_Z"], writes=["h_Z"])
        sc.op("vector", lambda e: e.tensor_tensor(out=bias[:], in0=best[:, :, 15], in1=best[:, :, 0], op=ALU.subtract),
              reads=["h_best"], writes=["h_bias"])
        sc.op("vector", lambda e: e.tensor_tensor(out=bias[:], in0=bias[:], in1=Z[:], op=ALU.subtract),
              reads=["h_bias", "h_Z"], writes=["h_bias"])
        sc.op("vector", lambda e: e.tensor_tensor(out=th[:], in0=best[:, :, 15:16].to_broadcast([128, 8, 16]), in1=t4[:, :, 0, :],
                                                  op=ALU.subtract), reads=["h_best", "h_tops"], writes=["h_th"])
        sc.op("scalar", lambda e: e.activation(out=e1[:], in_=th[:], func=AF.Exp, scale=-1.0), reads=["h_th"], writes=["h_e1"])
        sc.op("tensor", lambda e: e.transpose(out=pss[0][:, 0:128], in_=e1[:].rearrange("t p r -> t (p r)"), identity=k.identf[:]),
              reads=["h_e1", "identf"], writes=["h_ps0"])
        sc.op("scalar", lambda e: e.activation(out=e1T[:], in_=pss[0][:, 0:128], func=AF.Copy), reads=["h_ps0"], writes=["h_e1T"])
        for p in range(8):
            sc.op("scalar", lambda e, p=p: e.activation(out=e2[:, p, :], in_=s4[:, p, 1, :], func=AF.Exp, bias=bias[:, p:p + 1]),
                  reads=["h_s", "h_bias"], writes=["h_e2"])
        R4 = Rt[:].rearrange("t j (p r) -> t j p r", r=16)
        sc.op("vector", lambda e: e.tensor_tensor(
            out=R4, in0=s4[:, :, 1, :].rearrange("t p j -> t j p").unsqueeze(3).to_broadcast([128, 128, 8, 16]),
            in1=th[:].unsqueeze(1).to_broadcast([128, 128, 8, 16]), op=ALU.is_ge),
            reads=["h_s", "h_th"], writes=["h_R"])
        sc.op("vector", lambda e: e.tensor_tensor(
            out=R4, in0=R4, in1=e2[:].rearrange("t p j -> t j p").unsqueeze(3).to_broadcast([128, 128, 8, 16]), op=ALU.mult),
            reads=["h_R", "h_e2"], writes=["h_R"])

        def emit_OH(ih):
            O4 = Oh[:].rearrange("t i (p r) -> t i p r", r=16)
            sc.op("vector", lambda e, ih=ih: e.tensor_tensor(
                out=O4, in0=s4[:, :, 0, ih * 64:(ih + 1) * 64].rearrange("t p i -> t i p").unsqueeze(3).to_broadcast([128, 64, 8, 16]),
                in1=t4[:, :, 0, :].unsqueeze(1).to_broadcast([128, 64, 8, 16]), op=ALU.is_equal),
                reads=["h_s", "h_tops"], writes=["h_O"])

        emit_OH(0)
        for j0 in range(0, 128, 4):
            ptr = ptrs[n_pg % 2]
            ptk = "h_ptr%d" % (n_pg % 2)
            n_pg += 1
            for jj in range(4):
                sc.op("tensor", lambda e, ptr=ptr, jj=jj, j0=j0: e.transpose(out=ptr[:, jj * 128:(jj + 1) * 128], in_=Rt[:, j0 + jj, :],
                                                                            identity=k.identb[:]),
                      reads=["h_R", "identb"], writes=[ptk], sig=(jj == 3))
            sc.op("vector", lambda e, ptr=ptr, j0=j0: e.tensor_tensor(
                out=RT[:, j0:j0 + 4, :], in0=ptr[:, :].rearrange("k (j t) -> k j t", t=128),
                in1=e1T[:, :].unsqueeze(1).to_broadcast([128, 4, 128]), op=ALU.mult),
                reads=[ptk, "h_e1T"], writes=["h_RT"])
        for ih in range(2):
            if ih == 1:
                emit_OH(1)
            for i0_ in range(0, 64, 4):
                ptr = ptrs[n_pg % 2]
                ptk = "h_ptr%d" % (n_pg % 2)
                n_pg += 1
                for ii in range(4):
                    sc.op("tensor", lambda e, ptr=ptr, ii=ii, i0_=i0_: e.transpose(out=ptr[:, ii * 128:(ii + 1) * 128], in_=Oh[:, i0_ + ii, :],
                                                                                  identity=k.identb[:]),
                          reads=["h_O", "identb"], writes=[ptk], sig=(ii == 3))
                sc.op("scalar", lambda e, ptr=ptr, i0_=i0_: e.activation(
                    out=OT[:, i0_:i0_ + 4, :], in_=ptr[:, :].rearrange("k (i t) -> k i t", t=128), func=AF.Copy),
                    reads=[ptk], writes=["h_OT"])
            gst = gsts[n_g % 2]
            gk = "h_gst%d" % (n_g % 2)
            n_g += 1
            for t0 in range(0, 128, 8):
                pg = pgs[n_s % 2]
                pgk = "h_pg%d" % (n_s % 2)
                n_s += 1
                for tt_ in range(8):
                    t_ = t0 + tt_
                    sc.op("tensor", lambda e, pg=pg, tt_=tt_, t_=t_: e.matmul(pg[:, :].rearrange("j (i t) -> j i t", t=8)[:, :, tt_],
                                                                             lhsT=RT[:, :, t_], rhs=OT[:, :, t_], start=True, stop=True),
                          reads=["h_RT", "h_OT"], writes=[pgk], sig=(tt_ == 7))
                sc.op("scalar", lambda e, pg=pg, gst=gst, t0=t0: e.activation(
                    out=gst[:, :, t0:t0 + 8], in_=pg[:, :].rearrange("j (i t) -> j i t", t=8), func=AF.Copy),
                    reads=[pgk], writes=[gk])
            sc.dma("sync", GTv[:, ih * 64:(ih + 1) * 64, ti * 128:(ti + 1) * 128], gst[:], reads=[gk], writes=["d_GT"])


def phase_i(k, ph, hp):
    nc, sc = k.nc, k.sc
    sb = lambda name, *a, **kw: k.sb("p%d_" % hp + name, *a, **kw)
    ps = lambda name, *a, **kw: k.ps("p%d_" % hp + name, *a, **kw)
    A = k.scr
    GE = 2
    T0 = hp * 1024
    hh = sb("i_hh", [128, NDC, 1024], BF16, ctx=ph)
    sc.dma("sync", hh[:], A["h2T"].rearrange("(c p) t -> p c t", p=128)[:, :, T0:T0 + 1024], reads=["d_h2T"], writes=["i_hh"])
    acc = k.acc
    for tt in range(8):
        sc.op("vector", lambda e, tt=tt: e.memset(acc[:, tt, :], 0.0), writes=[("acc", tt)])
    ubs = [sb("i_ub%d" % i, [128, D], BF16, ctx=ph) for i in range(4)]
    uTs = [sb("i_uT%d" % i, [128, NDC, GE * 128], BF16, ctx=ph) for i in range(2)]
    vbs = [sb("i_vb%d" % i, [128, GE, D], BF16, ctx=ph) for i in range(3)]
    gTs = [sb("i_gT%d" % i, [128, GE, 1024], BF16, ctx=ph) for i in range(2)]
    WTs = [sb("i_WT%d" % i, [128, GE, 1024], BF16, ctx=ph) for i in range(2)]
    ges = [sb("i_ge%d" % i, [128, 512], BF16, ctx=ph) for i in range(2)]
    ptus = [ps("i_ptu%d" % i, [128, 512], BF16, ctx=ph) for i in range(2)]
    pAs = [ps("i_pA%d" % i, [128, 512], ctx=ph) for i in range(2)]
    pOs = [ps("i_pO%d" % i, [128, 512], ctx=ph) for i in range(4)]
    GTv = A["GT"].rearrange("(c p) t -> p c t", p=128)
    cnt = {"u": 0, "t": 0, "a": 0, "o": 0}
    NG = 128 // GE

    def dma_ub(eg):
        for ec in range(GE):
            ch = eg * GE + ec
            sc.dma("gpsimd", ubs[ch % 4][:], k.peer_u[ch * 128:(ch + 1) * 128, :], writes=["i_ub%d" % (ch % 4)])

    def units_T(eg):
        g2 = eg % 2
        g3 = eg % 3
        uT, vb, gT = uTs[g2], vbs[g3], gTs[g2]
        uTk, gTk = "i_uT%d" % g2, "i_gT%d" % g2
        units = []

        def u0():
            sc.dma("sync", gT[:], GTv[:, eg * GE:(eg + 1) * GE, T0:T0 + 1024], reads=["d_GT"], writes=[gTk])
            if eg + 1 < NG:
                dma_ub(eg + 1)
            for ec in range(GE):
                e0 = (eg * GE + ec) * 128
                sc.dma("gpsimd", vb[:, ec, :], k.peer_v[e0:e0 + 128, :], writes=[("i_vb", g3, ec)])
        units.append(u0)
        for ec in range(GE):
            e0 = (eg * GE + ec) * 128
            ubi = (eg * GE + ec) % 4
            ub = ubs[ubi]
            ubk = "i_ub%d" % ubi
            for d0 in range(0, NDC, 4):
                def ub_(ec=ec, e0=e0, ub=ub, ubk=ubk, d0=d0):
                    ptu = ptus[cnt["t"] % 2]
                    ptk = "i_ptu%d" % (cnt["t"] % 2)
                    cnt["t"] += 1
                    for dd in range(4):
                        dc = d0 + dd
                        sc.op("tensor", lambda e, ptu=ptu, dd=dd, dc=dc, ub=ub: e.transpose(out=ptu[:, dd * 128:(dd + 1) * 128],
                                                                                           in_=ub[:, dc * 128:(dc + 1) * 128], identity=k.identb[:]),
                              reads=[ubk, "identb"], writes=[ptk], sig=(dd == 3))
                    if (d0 // 4) % 2 == 0:
                        sc.op("vector", lambda e, ptu=ptu, uT=uT, d0=d0, ec=ec: e.tensor_copy(
                            out=uT[:, d0:d0 + 4, ec * 128:(ec + 1) * 128], in_=ptu[:, :].rearrange("p (c n) -> p c n", n=128)),
                            reads=[ptk], writes=[(uTk, ec)])
                    else:
                        sc.op("scalar", lambda e, ptu=ptu, uT=uT, d0=d0, ec=ec: e.activation(
                            out=uT[:, d0:d0 + 4, ec * 128:(ec + 1) * 128], in_=ptu[:, :].rearrange("p (c n) -> p c n", n=128), func=AF.Copy),
                            reads=[ptk], writes=[(uTk, ec)])
                units.append(ub_)
        return units

    def units_A(eg):
        g2 = eg % 2
        uT, gT, WT = uTs[g2], gTs[g2], WTs[g2]
        uTk, gTk = "i_uT%d" % g2, "i_gT%d" % g2
        units = []
        for ec in range(GE):
            for tb in range(2):
                st = {}
                for q in range(8):
                    def ua(ec=ec, tb=tb, q=q, st=st):
                        if q == 0:
                            st["i"] = cnt["a"] % 2
                            cnt["a"] += 1
                        ai = st["i"]
                        pA = pAs[ai]
                        pAk = "i_pA%d" % ai
                        ge = ges[ai]
                        gek = "i_ge%d" % ai
                        cs_ = slice(tb * 512, (tb + 1) * 512)
                        for dc in (2 * q, 2 * q + 1):
                            sc.op("tensor", lambda e, pA=pA, dc=dc, cs_=cs_: e.matmul(
                                pA[:, :], lhsT=uT[:, dc, ec * 128:(ec + 1) * 128], rhs=hh[:, dc, cs_], start=(dc == 0), stop=(dc == NDC - 1)),
                                reads=[(uTk, ec), "i_hh"], writes=[pAk], sig=(dc == NDC - 1))
                        if q == 7:
                            sc.op("scalar", lambda e, pA=pA, ge=ge: e.activation(out=ge[:], in_=pA[:, :], func=AF.Gelu), reads=[pAk], writes=[gek])
                            sc.op("vector", lambda e, ge=ge, cs_=cs_: e.tensor_tensor(out=WT[:, ec, cs_], in0=ge[:], in1=gT[:, ec, cs_], op=ALU.mult),
                                  reads=[gek, gTk], writes=[("i_WT", g2, ec, tb)])
                    units.append(ua)
        return units

    def units_O(eg):
        g2 = eg % 2
        g3 = eg % 3
        vb, WT = vbs[g3], WTs[g2]
        units = []
        for tt in range(8):
            for nb in range(4):
                def uo(tt=tt, nb=nb):
                    pO = pOs[cnt["o"] % 4]
                    pOk = "i_pO%d" % (cnt["o"] % 4)
                    cnt["o"] += 1
                    ns_ = slice(nb * 512, (nb + 1) * 512)
                    for ec in range(GE):
                        sc.op("tensor", lambda e, pO=pO, ec=ec, ns_=ns_: e.matmul(
                            pO[:, :], lhsT=WT[:, ec, tt * 128:(tt + 1) * 128], rhs=vb[:, ec, ns_], start=(ec == 0), stop=(ec == GE - 1)),
                            reads=[("i_WT", g2, ec, tt // 4), ("i_vb", g3, ec)], writes=[pOk], sig=(ec == GE - 1))
                    sc.op("vector", lambda e, pO=pO, ns_=ns_: e.tensor_tensor(out=acc[:, tt, ns_], in0=pO[:, :], in1=acc[:, tt, ns_], op=ALU.add),
                          reads=[pOk, ("acc", tt)], writes=[("acc", tt)])
                units.append(uo)
        return units

    def merged(lists):
        lists = [l for l in lists if l]
        pos = [0] * len(lists)
        total = sum(len(l) for l in lists)
        for _ in range(total):
            best, bi = None, None
            for i, l in enumerate(lists):
                if pos[i] < len(l):
                    frac = (pos[i] + 0.5) / len(l)
                    if best is None or frac < best:
                        best, bi = frac, i
            lists[bi][pos[bi]]()
            pos[bi] += 1

    dma_ub(0)
    for u in units_T(0):
        u()
    for eg in range(NG):
        merged([units_T(eg + 1) if eg + 1 < NG else [], units_A(eg), units_O(eg - 1) if eg >= 1 else []])
    for u in units_O(NG - 1):
        u()


def phase_j(k, ph, hp):
    nc, sc, sb, ps = k.nc, k.sc, k.sb, k.ps
    A = k.scr
    acc = k.acc
    tag = "j%d_" % hp
    fnb = sb(tag + "fnb", [128, D], ctx=ph)
    sc.dma("sync", fnb[:], k.final_norm.partition_broadcast(128), writes=[tag + "fnb"])
    x1s = [sb(tag + "x1%d" % i, [128, D], ctx=ph) for i in range(2)]
    junk = sb(tag + "junk", [128, D], ctx=ph)
    ss = sb(tag + "ss", [128, 8], ctx=ph)
    x1v = A["x1"].rearrange("(n p) d -> n p d", p=128)
    ov = k.out.rearrange("(n p) d -> n p d", p=128)
    for tt in range(8):
        ti = hp * 8 + tt
        x1 = x1s[tt % 2]
        xk = tag + "x1%d" % (tt % 2)
        sc.dma("sync", x1[:], x1v[ti], reads=["d_x1"], writes=[xk])
        sc.op("vector", lambda e, tt=tt: e.tensor_tensor(out=acc[:, tt, :], in0=acc[:, tt, :], in1=k.G2[:], op=ALU.mult),
              reads=[("acc", tt), "G1"], writes=[("acc", tt)])
        sc.op("vector", lambda e, tt=tt, x1=x1: e.tensor_tensor(out=x1[:], in0=x1[:], in1=acc[:, tt, :], op=ALU.add),
              reads=[("acc", tt), xk], writes=[xk])
        sc.op("scalar", lambda e, tt=tt, x1=x1: e.activation(out=junk[:], in_=x1[:], func=AF.Square, accum_out=ss[:, tt:tt + 1]),
              reads=[xk], writes=[tag + "junk", (tag + "ss", tt)])
        sc.op("vector", lambda e, tt=tt: e.tensor_scalar(out=ss[:, tt:tt + 1], in0=ss[:, tt:tt + 1], scalar1=1.0 / D, scalar2=EPS,
                                                         op0=ALU.mult, op1=ALU.add), reads=[(tag + "ss", tt)], writes=[(tag + "ss", tt)])
        sc.op("scalar", lambda e, tt=tt: e.activation(out=ss[:, tt:tt + 1], in_=ss[:, tt:tt + 1], func=AF.Sqrt),
              reads=[(tag + "ss", tt)], writes=[(tag + "ss", tt)])
        sc.op("vector", lambda e, tt=tt: e.reciprocal(out=ss[:, tt:tt + 1], in_=ss[:, tt:tt + 1]),
              reads=[(tag + "ss", tt)], writes=[(tag + "ss", tt)])
        sc.op("vector", lambda e, tt=tt, x1=x1: e.scalar_tensor_tensor(out=x1[:], in0=x1[:], scalar=ss[:, tt:tt + 1], in1=fnb[:],
                                                                       op0=ALU.mult, op1=ALU.mult),
              reads=[xk, (tag + "ss", tt), tag + "fnb"], writes=[xk])
        sc.dma("sync", ov[ti], x1[:], reads=[xk], writes=["d_out"])


def make_in_maps(inputs, cores):
    cst = make_consts()
    f = lambda a: np.ascontiguousarray(np.asarray(a), dtype=np.float32)
    shared = {
        "w_ada": f(inputs["w_ada"][0]), "b_ada": f(inputs["b_ada"]), "norm_mix": f(inputs["norm_mix"]),
        "norm_ffn": f(inputs["norm_ffn"]), "w_in": f(inputs["w_in"][0]), "lb_logits": f(inputs["lb_logits"]),
        "hgrn_gain": f(inputs["hgrn_gain"]), "w_up_a": f(inputs["w_up_a"][0]), "w_up_b": f(inputs["w_up_b"][0]),
        "w_out": f(inputs["w_out"][0]), "peer_w_q": f(inputs["peer_w_q"][0]), "peer_keys": f(inputs["peer_keys"][0]),
        "peer_u": f(inputs["peer_u"][0]), "peer_v": f(inputs["peer_v"][0]), "final_norm": f(inputs["final_norm"]),
    }
    shared.update(cst)
    maps = []
    for b in cores:
        m = dict(shared)
        m["x"] = f(inputs["x"][b])
        m["c"] = f(inputs["c"][b:b + 1])
        maps.append(m)
    return maps


def kernel(**inputs):
    nc = build_nc(stage=99)
    cores = list(range(NCORES))
    in_maps = make_in_maps(inputs, cores)
    res = run_bass_kernel_spmd(nc, in_maps, core_ids=cores)
    return np.stack([np.asarray(r["out"], dtype=np.float32) for r in res.results], axis=0)
```

```python
import numpy as np
import concourse.bass as bass
import concourse.mybir as mybir
from concourse.bass_utils import run_bass_kernel_spmd
from contextlib import ExitStack

F32 = mybir.dt.float32
BF16 = mybir.dt.bfloat16
AF = mybir.ActivationFunctionType
ALU = mybir.AluOpType
AX = mybir.AxisListType

COMPUTE = ("tensor", "vector", "scalar", "gpsimd")
QUEUES = ("sync",)
ALL_ENG = COMPUTE + QUEUES


class Sched:
    def __init__(self, nc):
        self.nc = nc
        self.sem = {e: nc.alloc_semaphore(name="pg_" + e) for e in COMPUTE}
        self.cnt = {e: 0 for e in COMPUTE}
        self.streams = {e: [] for e in ALL_ENG}
        self.waited = {e: {} for e in ALL_ENG}
        self.lastw = {}
        self.readers = {}
        self.dsem = {}
        self.semobj = {}

    def _deps(self, eng, reads, writes):
        waits = {}

        def need(sv):
            s, v = sv
            sid = id(s)
            self.semobj[sid] = s
            if v > waits.get(sid, 0):
                waits[sid] = v

        for k in reads:
            if k in self.lastw:
                need(self.lastw[k])
        for k in writes:
            if k in self.lastw:
                need(self.lastw[k])
            for sv in self.readers.get(k, ()):
                need(sv)
        out = []
        wd = self.waited[eng]
        for sid, v in waits.items():
            if wd.get(sid, 0) < v:
                wd[sid] = v
                out.append((self.semobj[sid], v))
        return out

    def _commit(self, my, reads, writes):
        for k in writes:
            self.lastw[k] = my
            self.readers[k] = []
        for k in reads:
            if k in writes:
                continue
            self.readers.setdefault(k, []).append(my)

    def begin_capture(self):
        self._cap = []

    def end_capture(self):
        c = self._cap
        self._cap = None
        return c

    def op(self, eng, fn, reads=(), writes=(), sig=True):
        if getattr(self, "_cap", None) is not None:
            self._cap.append(lambda: self._op(eng, fn, reads, writes, sig))
            return
        self._op(eng, fn, reads, writes, sig)

    def dma(self, q, out, in_, reads=(), writes=(), **kw):
        if getattr(self, "_cap", None) is not None:
            self._cap.append(lambda: self._dma(q, out, in_, reads, writes, **kw))
            return
        self._dma(q, out, in_, reads, writes, **kw)

    def _op(self, eng, fn, reads=(), writes=(), sig=True):
        waits = self._deps(eng, reads, writes)
        if eng == "tensor":
            waits = [(s_, v_) for (s_, v_) in waits if s_ is not self.sem["tensor"]]
        if sig:
            self.cnt[eng] += 1
            my = (self.sem[eng], self.cnt[eng])
            inc = (self.sem[eng], 1)
        else:
            assert eng == "tensor"
            my = (self.sem[eng], self.cnt[eng] + 1)
            inc = None
        self._commit(my, reads, writes)
        self.streams[eng].append((waits, fn, inc))

    def _dma(self, q, out, in_, reads=(), writes=(), **kw):
        waits = self._deps(q, reads, writes)
        key = writes[0]
        if key not in self.dsem:
            self.dsem[key] = [self.nc.alloc_semaphore(name="d%d" % len(self.dsem)), 0]
        ent = self.dsem[key]
        ent[1] += 16
        my = (ent[0], ent[1])
        self._commit(my, reads, writes)

        def fn(e):
            return e.dma_start(out=out, in_=in_, **kw)

        self.streams[q].append((waits, fn, (ent[0], 16)))

    def drain_dmas(self, q="sync"):
        waits = []
        wd = self.waited[q]
        for key, (s, v) in self.dsem.items():
            if v > 0 and wd.get(id(s), 0) < v:
                wd[id(s)] = v
                waits.append((s, v))
        if waits:
            self.streams[q].append((waits, None, None))

    def flush(self, block):
        nc = self.nc
        streams = self.streams
        self.streams = {e: [] for e in ALL_ENG}

        def mk(name):
            lst = streams[name]

            def body(e):
                for waits, fn, inc in lst:
                    for s, v in waits:
                        e.wait_ge(s, v)
                    if fn is not None:
                        inst = fn(e)
                        if inc is not None:
                            inst.then_inc(inc[0], inc[1])

            return body

        for name in ALL_ENG:
            if streams[name]:
                getattr(block, name)(mk(name))

D = 2048
S = 2048
NDC = 16
NTT = 16
IN_W = 12368
EPS = 1e-6
NCORES = 8


def make_consts():
    cst = {}
    cst["ident"] = np.eye(128, dtype=np.float32)
    cst["ones"] = np.ones((128, 128), dtype=np.float32)
    cst["tri"] = np.triu(np.ones((64, 64), dtype=np.float32))
    return cst


class K:
    pass


def merged_units(lists):
    lists = [l for l in lists if l]
    pos = [0] * len(lists)
    total = sum(len(l) for l in lists)
    for _ in range(total):
        best, bi = None, None
        for i, l in enumerate(lists):
            if pos[i] < len(l):
                frac = (pos[i] + 0.5) / len(l)
                if best is None or frac < best:
                    best, bi = frac, i
        lists[bi][pos[bi]]()
        pos[bi] += 1


def build_nc(stage=99, dbg=None):
    nc = bass.Bass("TRN2", target_bir_lowering=False)
    k = K()
    k.nc = nc
    k.stage = stage

    def din(name, shape, dtype=F32):
        return nc.dram_tensor(name, list(shape), dtype, kind="ExternalInput").ap()

    k.x = din("x", [S, D])
    k.c = din("c", [1, D])
    k.w_ada = din("w_ada", [D, 6 * D])
    k.b_ada = din("b_ada", [1, 6 * D])
    k.norm_mix = din("norm_mix", [1, D])
    k.norm_ffn = din("norm_ffn", [1, D])
    k.w_in = din("w_in", [D, IN_W])
    k.ident_d = din("ident", [128, 128])
    k.tri_d = din("tri", [64, 64])
    k.lb_logits = din("lb_logits", [2, 1024])
    k.hgrn_gain = din("hgrn_gain", [1, 128])
    k.w_up_a = din("w_up_a", [1024, D])
    k.w_up_b = din("w_up_b", [1024, D])
    k.w_out = din("w_out", [D, D])
    k.peer_w_q = din("peer_w_q", [D, D])
    k.peer_keys = din("peer_keys", [8, 2, 128, 128])
    k.peer_u = din("peer_u", [16384, D])
    k.peer_v = din("peer_v", [16384, D])
    k.final_norm = din("final_norm", [D])
    k.ones_d = din("ones", [128, 128])
    k.out = nc.dram_tensor("out", [S, D], F32, kind="ExternalOutput").ap()
    if dbg is not None:
        k.dbg = nc.dram_tensor("dbg", list(dbg), F32, kind="ExternalOutput").ap()

    sc = Sched(nc)
    k.sc = sc
    with ExitStack() as top:
        def sb(name, shape, dtype=F32, ctx=top):
            return ctx.enter_context(nc.sbuf_tensor(name, list(shape), dtype))

        def ps(name, shape, dtype=F32, ctx=top):
            return ctx.enter_context(nc.psum_tensor(name, list(shape), dtype))
        k.sb = sb
        k.ps = ps
        k.modT = sb("modT", [128, 96])
        k.G1 = sb("G1", [128, D])
        k.G2 = sb("G2", [128, D])
        k.identf = sb("identf", [128, 128])
        k.onesf = sb("onesf", [128, 128])
        k.identb = sb("identb", [128, 128], BF16)
        k.onesb = sb("onesb", [128, 128], BF16)

        def run_phase(fn):
            with ExitStack() as ph:
                fn(k, ph)
                sc.drain_dmas()
                with nc.Block() as block:
                    sc.flush(block)

        make_scratch(k)
        run_phase(phase_a)
        with ExitStack() as s1:
            k.hT = sb("hT", [128, NDC, S], BF16, ctx=s1)
            if stage >= 2:
                run_phase(phase_b)
            if stage >= 3:
                run_phase(phase_c)
            if dbg is not None and stage in (2, 3):
                run_phase(phase_dbg)
        if stage >= 4:
            run_phase(phase_d)
        if stage >= 5:
            run_phase(phase_e)
        if stage >= 6:
            with ExitStack() as s2:
                k.hT = sb("hT2", [128, NDC, S], BF16, ctx=s2)
                run_phase(phase_f)
                run_phase(phase_f2)
        if stage >= 7:
            with ExitStack() as s3:
                k.hT = sb("hT3", [128, NDC, S], BF16, ctx=s3)
                run_phase(phase_g)
                run_phase(phase_h1)
            run_phase(phase_h2)
        if stage >= 8:
            with ExitStack() as s4:
                k.acc = sb("acc", [128, 8, D], ctx=s4)
                for hp in range(2):
                    run_phase(lambda k_, ph_, hp=hp: phase_i(k_, ph_, hp))
                    run_phase(lambda k_, ph_, hp=hp: phase_j(k_, ph_, hp))
        if dbg is not None and stage in (2, 3):
            return nc
        if dbg is not None:
            run_phase(phase_dbg)
    return nc


def phase_a(k, ph):
    nc, sc, sb, ps = k.nc, k.sc, k.sb, k.ps
    sc.dma("sync", k.identf[:], k.ident_d, writes=["identf"])
    sc.dma("sync", k.onesf[:], k.ones_d, writes=["onesf"])
    sc.op("vector", lambda e: e.tensor_copy(out=k.identb[:], in_=k.identf[:]), reads=["identf"], writes=["identb"])
    sc.op("vector", lambda e: e.tensor_copy(out=k.onesb[:], in_=k.onesf[:]), reads=["onesf"], writes=["onesb"])
    cs = sb("cs", [128, 16], ctx=ph)
    sc.dma("sync", cs[:], k.c.rearrange("o (p j) -> (o p) j", p=128), writes=["cs"])
    sc.op("scalar", lambda e: e.activation(out=cs[:], in_=cs[:], func=AF.Silu), reads=["cs"], writes=["cs"])
    wv = k.w_ada.rearrange("(p j) n -> p j n", p=128)
    NB = 24
    wts = [sb("wada%d" % i, [128, 16, 512], ctx=ph) for i in range(2)]
    brs = [sb("brow%d" % i, [1, 512], ctx=ph) for i in range(2)]
    mrs = [sb("mrow%d" % i, [1, 512], ctx=ph) for i in range(2)]
    pss = [ps("pa%d" % i, [128, 512], ctx=ph) for i in range(2)]
    pbs = [ps("pbc%d" % i, [128, 512], ctx=ph) for i in range(2)]
    pc = ps("pcol", [128, 96], ctx=ph)
    for nb in range(NB):
        i2 = nb % 2
        wt, br, mr, pt, pb = wts[i2], brs[i2], mrs[i2], pss[i2], pbs[i2]
        wk, bk, mk, pk, pbk = "wada%d" % i2, "brow%d" % i2, "mrow%d" % i2, "pa%d" % i2, "pbc%d" % i2
        q = "sync" if nb % 2 == 0 else "gpsimd"
        sc.dma(q, wt[:], wv[:, :, nb * 512:(nb + 1) * 512], writes=[wk])
        sc.dma("sync", br[:], k.b_ada[:, nb * 512:(nb + 1) * 512], writes=[bk])
        for j in range(16):
            sc.op("tensor", lambda e, j=j, wt=wt, pt=pt: e.matmul(pt[0:1, :], lhsT=cs[:, j:j + 1], rhs=wt[:, j, :],
                                                             start=(j == 0), stop=(j == 15)),
                  reads=["cs", wk], writes=[pk], sig=(j == 15))
        sc.op("vector", lambda e, pt=pt, mr=mr, br=br: e.tensor_tensor(out=mr[0:1, :], in0=pt[0:1, :], in1=br[0:1, :], op=ALU.add),
              reads=[pk, bk], writes=[mk])
        for c4 in range(4):
            ch = nb * 4 + c4
            sc.op("tensor", lambda e, ch=ch, c4=c4, mr=mr: e.matmul(pc[:, ch:ch + 1], lhsT=mr[0:1, c4 * 128:(c4 + 1) * 128],
                                                                   rhs=k.onesf[0:1, 0:1], start=True, stop=True),
                  reads=[mk, "onesf"], writes=["pcol"])
        for gi, (G, off) in enumerate(((k.G1, 2 * D), (k.G2, 5 * D))):
            if off <= nb * 512 < off + D:
                o = nb * 512 - off
                sc.op("tensor", lambda e, pb=pb, mr=mr: e.matmul(pb[:, :], lhsT=k.onesf[0:1, :], rhs=mr[0:1, :],
                                                                 start=True, stop=True),
                      reads=[mk, "onesf"], writes=[pbk])
                sc.op("vector", lambda e, pb=pb, G=G, o=o: e.tensor_copy(out=G[:, o:o + 512], in_=pb[:, :]),
                      reads=[pbk], writes=["G%d" % gi])
    sc.op("vector", lambda e: e.tensor_copy(out=k.modT[:], in_=pc[:]), reads=["pcol"], writes=["modT"])


def phase_b(k, ph):
    norm_modulate(k, ph, k.x, k.norm_mix, 0, 16, [])


def norm_modulate(k, ph, src, gain_d, sh_col, sc_col, src_reads, tag="g1"):
    nc, sc, sb, ps = k.nc, k.sc, k.sb, k.ps
    gT = sb(tag + "gT", [128, NDC], ctx=ph)
    with nc.allow_non_contiguous_dma(reason="tiny gain vector"):
        pass
    sc.dma("sync", gT[:], gain_d.rearrange("o (j p) -> (o p) j", p=128), writes=["gT"], allow_slow_non_contiguous=True)
    A1 = sb(tag + "A1", [128, NDC], ctx=ph)
    sc.op("vector", lambda e: e.scalar_tensor_tensor(out=A1[:], in0=k.modT[:, sc_col:sc_col + 16], scalar=1.0, in1=gT[:],
                                                     op0=ALU.add, op1=ALU.mult),
          reads=["modT", "gT"], writes=["A1"])
    xts = [sb(tag + "xt%d" % i, [128, D], ctx=ph) for i in range(8)]
    sq = sb(tag + "sqjunk", [128, D], ctx=ph)
    ss = sb(tag + "ss", [128, 16], ctx=ph)
    pts = [ps(tag + "pb%d" % i, [128, 512], ctx=ph) for i in range(4)]
    xv = src.rearrange("(n p) d -> n p d", p=128)
    for tg in range(4):
        for tt in range(4):
            ti = tg * 4 + tt
            bi = ti % 8
            xt = xts[bi]
            xk = "xt%d" % bi
            sc.dma("sync" if ti % 2 == 0 else "gpsimd", xt[:], xv[ti], reads=list(src_reads), writes=[xk])
            sc.op("scalar", lambda e, xt=xt, ti=ti: e.activation(out=sq[:], in_=xt[:], func=AF.Square,
                                                                 accum_out=ss[:, ti:ti + 1]),
                  reads=[xk], writes=["sq", ("ss", ti)])
            sc.op("vector", lambda e, ti=ti: e.tensor_scalar(out=ss[:, ti:ti + 1], in0=ss[:, ti:ti + 1], scalar1=1.0 / D,
                                                             scalar2=EPS, op0=ALU.mult, op1=ALU.add),
                  reads=[("ss", ti)], writes=[("ss", ti)])
            sc.op("scalar", lambda e, ti=ti: e.activation(out=ss[:, ti:ti + 1], in_=ss[:, ti:ti + 1], func=AF.Sqrt),
                  reads=[("ss", ti)], writes=[("ss", ti)])
            sc.op("vector", lambda e, ti=ti: e.reciprocal(out=ss[:, ti:ti + 1], in_=ss[:, ti:ti + 1]),
                  reads=[("ss", ti)], writes=[("ss", ti)])
            sc.op("vector", lambda e, xt=xt, ti=ti: e.tensor_scalar(out=xt[:], in0=xt[:], scalar1=ss[:, ti:ti + 1],
                                                                    scalar2=None, op0=ALU.mult),
                  reads=[xk, ("ss", ti)], writes=[xk])
        for dc in range(NDC):
            pt = pts[dc % 4]
            pk = "pb%d" % (dc % 4)
            for tt in range(4):
                ti = tg * 4 + tt
                bi = ti % 8
                sc.op("tensor", lambda e, pt=pt, tt=tt, bi=bi, dc=dc: e.transpose(out=pt[:, tt * 128:(tt + 1) * 128],
                                                                                 in_=xts[bi][:, dc * 128:(dc + 1) * 128],
                                                                                 identity=k.identf[:]),
                      reads=["xt%d" % bi, "identf"], writes=[pk], sig=(tt == 3))
            sc.op("scalar", lambda e, pt=pt, dc=dc, tg=tg: e.activation(out=k.hT[:, dc, tg * 512:(tg + 1) * 512], in_=pt[:, :],
                                                                        func=AF.Identity, scale=A1[:, dc:dc + 1],
                                                                        bias=k.modT[:, sh_col + dc:sh_col + dc + 1]),
                  reads=[pk, "A1", "modT"], writes=[("hT", dc, tg)])


def phase_dbg(k, ph):
    nc, sc, sb, ps = k.nc, k.sc, k.sb, k.ps
    if k.stage == 1:
        sc.dma("sync", k.dbg[:, 0:96], k.modT[:], reads=["modT"], writes=["dbg0"])
        sc.dma("sync", k.dbg[:, 128:128 + D], k.G1[:], reads=["G0"], writes=["dbg1"])
        sc.dma("sync", k.dbg[:, 128 + D:128 + 2 * D], k.G2[:], reads=["G1"], writes=["dbg2"])
    if k.stage == 7:
        tmp = sb("dbgtmp", [128, S], ctx=ph)
        tmh = sb("dbgtmh", [128, S], BF16, ctx=ph)
        for i in range(16):
            sc.dma("sync", tmh[:], k.scr["GT"][i * 128:(i + 1) * 128, :], reads=["d_GT"], writes=["dbgtmh"])
            sc.op("vector", lambda e: e.tensor_copy(out=tmp[:], in_=tmh[:]), reads=["dbgtmh"], writes=["dbgtmp"])
            sc.dma("sync", k.dbg[i * 128:(i + 1) * 128, :], tmp[:], reads=["dbgtmp"], writes=["dbg2"])
    if k.stage == 6:
        sc.dma("sync", k.dbg[0:2048, :], k.scr["x1"], reads=["d_x1"], writes=["dbg0"])
        tmp = sb("dbgtmp", [128, S], ctx=ph)
        tmh = sb("dbgtmh", [128, S], BF16, ctx=ph)
        for nm, off in (("attnT", 2048), ("recT", 3072)):
            for i in range(8):
                sc.dma("sync", tmh[:], k.scr[nm][i * 128:(i + 1) * 128, :], reads=["d_" + nm], writes=["dbgtmh"])
                sc.op("vector", lambda e: e.tensor_copy(out=tmp[:], in_=tmh[:]), reads=["dbgtmh"], writes=["dbgtmp"])
                sc.dma("sync", k.dbg[off + i * 128:off + (i + 1) * 128, :], tmp[:], reads=["dbgtmp"], writes=["dbg2"])
    if k.stage == 5:
        tmp = sb("dbgtmp", [128, S], ctx=ph)
        tmh = sb("dbgtmh", [128, S], BF16, ctx=ph)
        for i in range(8):
            sc.dma("sync", tmh[:], k.scr["recT"][i * 128:(i + 1) * 128, :], reads=["d_recT"], writes=["dbgtmh"])
            sc.op("vector", lambda e: e.tensor_copy(out=tmp[:], in_=tmh[:]), reads=["dbgtmh"], writes=["dbgtmp"])
            sc.dma("sync", k.dbg[i * 128:(i + 1) * 128, :], tmp[:], reads=["dbgtmp"], writes=["dbg2"])
    if k.stage == 4:
        tmp = sb("dbgtmp", [128, S], ctx=ph)
        tmh = sb("dbgtmh", [128, S], BF16, ctx=ph)
        for i in range(8):
            sc.dma("sync", tmh[:], k.scr["attnT"][i * 128:(i + 1) * 128, :], reads=["d_attnT"], writes=["dbgtmh"])
            sc.op("vector", lambda e: e.tensor_copy(out=tmp[:], in_=tmh[:]), reads=["dbgtmh"], writes=["dbgtmp"])
            sc.dma("sync", k.dbg[i * 128:(i + 1) * 128, :], tmp[:], reads=["dbgtmp"], writes=["dbg2"])
    if k.stage == 3:
        sc.dma("sync", k.dbg[0:1024, :], k.scr["qBT"], reads=["d_qBT"], writes=["dbg0"])
        sc.dma("sync", k.dbg[1024:1024 + 2048, 0:16], k.scr["wi"], reads=["d_wi"], writes=["dbg1"])
        tmp = sb("dbgtmp", [128, S], ctx=ph)
        tmh = sb("dbgtmh", [128, S], BF16, ctx=ph)
        for i in range(8):
            sc.dma("sync", tmh[:], k.scr["kAT"][i * 128:(i + 1) * 128, :], reads=["d_kAT"], writes=["dbgtmh"])
            sc.op("vector", lambda e: e.tensor_copy(out=tmp[:], in_=tmh[:]), reads=["dbgtmh"], writes=["dbgtmp"])
            sc.dma("sync", k.dbg[3072 + i * 128:3072 + (i + 1) * 128, :], tmp[:], reads=["dbgtmp"], writes=["dbg2"])
        for i in range(16):
            sc.dma("sync", tmh[:, 0:1024], k.scr["vA"][i * 128:(i + 1) * 128, :], reads=["d_vA"], writes=["dbgtmh"])
            sc.op("vector", lambda e: e.tensor_copy(out=tmp[:, 0:1024], in_=tmh[:, 0:1024]), reads=["dbgtmh"], writes=["dbgtmp"])
            sc.dma("sync", k.dbg[4096 + i * 128:4096 + (i + 1) * 128, 0:1024], tmp[:, 0:1024], reads=["dbgtmp"], writes=["dbg3"])
    if k.stage == 2:
        tmp = sb("dbgtmp", [128, S], ctx=ph)
        for dc in range(NDC):
            sc.op("vector", lambda e, dc=dc: e.tensor_copy(out=tmp[:], in_=k.hT[:, dc, :]),
                  reads=[("hT", dc, tg) for tg in range(4)], writes=["dbgtmp"])
            sc.dma("sync", k.dbg[dc * 128:(dc + 1) * 128, :], tmp[:], reads=["dbgtmp"], writes=["dbg0"])


class ProjRes:
    pass


def proj_setup(k, ph, KC, tag="pj"):
    r = ProjRes()
    sb, ps = k.sb, k.ps
    r.KC = KC
    r.wb = [sb("%s_wb%d" % (tag, i), [128, KC, 512], BF16, ctx=ph) for i in range(2)]
    r.wkey = ["%s_wb%d" % (tag, i) for i in range(2)]
    r.pt = [ps("%s_ps%d" % (tag, i), [128, 512], ctx=ph) for i in range(4)]
    r.pkey = ["%s_ps%d" % (tag, i) for i in range(4)]
    r.sf = [sb("%s_sf%d" % (tag, i), [128, 512], F32, ctx=ph) for i in range(3)]
    r.sh = [sb("%s_sh%d" % (tag, i), [128, 512], BF16, ctx=ph) for i in range(3)]
    r.nblk = 0
    r.npt = 0
    r.nst = 0
    r.tag = tag
    return r


def proj_load_w(k, r, w_ap, cb0, cb1):
    sc = k.sc
    KC = r.KC
    wv = w_ap.rearrange("(kc p) n -> p kc n", p=128)
    i2 = r.nblk % 2
    r.nblk += 1
    w = cb1 - cb0
    sc.dma("gpsimd", r.wb[i2][:, :, 0:w], wv[:, :, cb0:cb1], writes=[r.wkey[i2]])
    return r.wb[i2], r.wkey[i2]


def proj_stage(r, dtype):
    i = r.nst % 3
    r.nst += 1
    if dtype == F32:
        return r.sf[i], "%s_sf%d" % (r.tag, i)
    return r.sh[i], "%s_sh%d" % (r.tag, i)


def proj_psum(r):
    i = r.npt % 4
    r.npt += 1
    return r.pt[i], r.pkey[i]


def proj(k, r, actT, act_keys, w_ap, c0, c1, mode, func, dst, dst_key, dtype):
    sc = k.sc
    KC = r.KC
    cb0 = c0
    while cb0 < c1:
        cb1 = min(c1, cb0 + 512)
        w = cb1 - cb0
        wb, wk = proj_load_w(k, r, w_ap, cb0, cb1)
        if mode == "fm":
            for sub in range((w + 127) // 128):
                cw = min(128, w - sub * 128)
                for tb in range(4):
                    pt, pk = proj_psum(r)
                    for kc in range(KC):
                        sc.op("tensor", lambda e, pt=pt, wb=wb, kc=kc, sub=sub, cw=cw, tb=tb: e.matmul(
                            pt[0:cw, :], lhsT=wb[:, kc, sub * 128: sub * 128 + cw], rhs=actT[:, kc, tb * 512:(tb + 1) * 512],
                            start=(kc == 0), stop=(kc == KC - 1)),
                            reads=[wk] + act_keys(kc, tb), writes=[pk], sig=(kc == KC - 1))
                    st, sk = proj_stage(r, dtype)
                    sc.op("scalar", lambda e, pt=pt, st=st, cw=cw: e.activation(out=st[0:cw, :], in_=pt[0:cw, :], func=func),
                          reads=[pk], writes=[sk])
                    r0 = cb0 - c0 + sub * 128
                    sc.dma("sync", dst[r0:r0 + cw, tb * 512:(tb + 1) * 512], st[0:cw, :], reads=[sk], writes=[dst_key])
        else:
            for ti in range(NTT):
                pt, pk = proj_psum(r)
                for kc in range(KC):
                    sc.op("tensor", lambda e, pt=pt, wb=wb, kc=kc, ti=ti, w=w: e.matmul(
                        pt[:, 0:w], lhsT=actT[:, kc, ti * 128:(ti + 1) * 128], rhs=wb[:, kc, 0:w],
                        start=(kc == 0), stop=(kc == KC - 1)),
                        reads=[wk] + act_keys(kc, ti // 4), writes=[pk], sig=(kc == KC - 1))
                st, sk = proj_stage(r, dtype)
                sc.op("scalar", lambda e, pt=pt, st=st, w=w: e.activation(out=st[:, 0:w], in_=pt[:, 0:w], func=func),
                      reads=[pk], writes=[sk])
                sc.dma("sync", dst[ti * 128:(ti + 1) * 128, cb0 - c0:cb1 - c0], st[:, 0:w], reads=[sk], writes=[dst_key])
        cb0 = cb1


def hT_keys(kc, tb):
    return [("hT", kc, tb)]


PROJ_SPECS = [
    ("qAT", 0, 1024, "fm", "Identity", "bf16"),
    ("kAT", 1024, 2048, "fm", "Identity", "bf16"),
    ("vA", 2048, 3072, "tm", "Identity", "bf16"),
    ("qiT", 3072, 4096, "fm", "Identity", "bf16"),
    ("kiT", 4096, 4160, "fm", "Identity", "bf16"),
    ("wi", 4160, 4176, "tm", "Identity", "f32"),
    ("qBT", 4176, 5200, "fm", "Silu", "f32"),
    ("fBT", 5200, 6224, "fm", "Sigmoid", "f32"),
    ("iB", 6224, 7248, "tm", "Identity", "bf16"),
    ("gBT", 7248, 8272, "fm", "Silu", "bf16"),
    ("sgAT", 8272, 10320, "fm", "Sigmoid", "bf16"),
    ("sgBT", 10320, 12368, "fm", "Sigmoid", "bf16"),
]


def make_scratch(k):
    nc = k.nc
    k.scr = {}
    for name, c0, c1, mode, fn, dtn in PROJ_SPECS:
        dt_ = BF16 if dtn == "bf16" else F32
        shape = [c1 - c0, S] if mode == "fm" else [S, c1 - c0]
        k.scr[name] = nc.dram_tensor("scr_" + name, shape, dt_, kind="Internal").ap()
    k.scr["attnT"] = nc.dram_tensor("scr_attnT", [1024, S], BF16, kind="Internal").ap()
    k.scr["recT"] = nc.dram_tensor("scr_recT", [1024, S], BF16, kind="Internal").ap()
    k.scr["x1"] = nc.dram_tensor("scr_x1", [S, D], F32, kind="Internal").ap()
    k.scr["h2T"] = nc.dram_tensor("scr_h2T", [D, S], BF16, kind="Internal").ap()
    k.scr["pqT"] = nc.dram_tensor("scr_pqT", [D, S], BF16, kind="Internal").ap()
    k.scr["GT"] = nc.dram_tensor("scr_GT", [16384, S], BF16, kind="Internal").ap()


def phase_c(k, ph):
    r = proj_setup(k, ph, NDC, "pc")
    for name, c0, c1, mode, fn, dtn in PROJ_SPECS:
        dt_ = BF16 if dtn == "bf16" else F32
        proj(k, r, k.hT, hT_keys, k.w_in, c0, c1, mode, getattr(AF, fn), k.scr[name], "d_" + name, dt_)


def phase_d(k, ph):
    nc, sc, sb, ps = k.nc, k.sc, k.sb, k.ps
    A = k.scr
    kT = sb("kT", [128, 8, S], BF16, ctx=ph)
    sc.dma("sync", kT[:], A["kAT"].rearrange("(h p) t -> p h t", p=128), reads=["d_kAT"], writes=["kT"])
    vS = sb("vS", [128, 16, 1024], BF16, ctx=ph)
    sc.dma("sync", vS[:], A["vA"].rearrange("(j p) c -> p j c", p=128), reads=["d_vA"], writes=["vS"])
    kiT2 = sb("kiT2", [128, S], BF16, ctx=ph)
    sc.dma("sync", kiT2[0:64, :], A["kiT"], reads=["d_kiT"], writes=["kiT2a"])
    sc.dma("sync", kiT2[64:128, :], A["kiT"], reads=["d_kiT"], writes=["kiT2b"])
    cm29 = sb("cm29", [128, 1], ctx=ph)
    sc.op("vector", lambda e: e.memset(cm29[:], -1e29), writes=["cm29"])
    qTs = [sb("qT%d" % i, [128, 8, 512], BF16, ctx=ph) for i in range(2)]
    qiT = sb("qiT", [128, 8, 512], BF16, ctx=ph)
    wi = sb("wi", [128, 4, 16], ctx=ph)
    wabs = sb("wabs", [128, 4, 16], ctx=ph)
    wsgn = sb("wsgn", [128, 4, 16], ctx=ph)
    scrs = [sb("scr%d" % i, [128, S], ctx=ph) for i in range(2)]
    work = sb("work", [128, S], ctx=ph)
    mks = [sb("mk%d" % i, [128, S], BF16, ctx=ph) for i in range(2)]
    tmps = [sb("itmp%d" % i, [128, 512], ctx=ph) for i in range(3)]
    m8 = sb("m8", [128, 8], ctx=ph)
    NBIS = 20
    pw2 = sb("pw2", [128, NBIS], ctx=ph)
    for kk_ in range(NBIS):
        sc.op("vector", lambda e, kk_=kk_: e.memset(pw2[:, kk_:kk_ + 1], 2.0 ** -(kk_ + 1)), writes=["pw2"])
    bwk = sb("bwk", [128, NBIS], ctx=ph)
    blo = sb("blo", [128, 1], ctx=ph)
    brg = sb("brg", [128, 1], ctx=ph)
    bmid = sb("bmid", [128, 1], ctx=ph)
    bcnt = sb("bcnt", [128, 1], ctx=ph)
    bg = sb("bg", [128, 1], ctx=ph)
    maskTs = [sb("maskT%d" % i, [128, 16, 512], BF16, ctx=ph) for i in range(2)]
    pes = [sb("pe%d" % i, [128, 512], BF16, ctx=ph) for i in range(3)]
    pms = [sb("pm%d" % i, [128, 512], BF16, ctx=ph) for i in range(3)]
    rds = [sb("rd%d" % i, [128, 512], ctx=ph) for i in range(2)]
    aos = [sb("ao%d" % i, [128, 512], BF16, ctx=ph) for i in range(2)]
    pis = [ps("pi%d" % i, [128, 512], ctx=ph) for i in range(2)]
    ptr = ps("ptr", [128, 512], BF16, ctx=ph)
    pls = [ps("pl%d" % i, [128, 512], ctx=ph) for i in range(2)]
    po = ps("po", [128, 512], ctx=ph)
    pd = ps("pd", [128, 512], ctx=ph)
    att_scale = 128.0 ** -0.5
    NEG = -1e30
    n_i = 0
    n_e = 0
    n_h = 0
    streams = {}
    for tb in range(4):
        sc.begin_capture()
        maskT = maskTs[tb % 2]
        mkey = "maskT%d" % (tb % 2)
        qT = qTs[tb % 2]
        qTk = "qT%d" % (tb % 2)
        sc.dma("sync", qT[:], A["qAT"].rearrange("(h p) t -> p h t", p=128)[:, :, tb * 512:(tb + 1) * 512],
               reads=["d_qAT"], writes=[qTk])
        sc.dma("sync", qiT[:], A["qiT"].rearrange("(h p) t -> p h t", p=128)[:, :, tb * 512:(tb + 1) * 512],
               reads=["d_qiT"], writes=["qiT"])
        sc.dma("sync", wi[:], A["wi"].rearrange("(n p) h -> p n h", p=128)[:, tb * 4:(tb + 1) * 4, :],
               reads=["d_wi"], writes=["wi"])
        sc.op("scalar", lambda e: e.activation(out=wabs[:], in_=wi[:], func=AF.Abs), reads=["wi"], writes=["wabs"])
        sc.op("scalar", lambda e: e.activation(out=wsgn[:], in_=wi[:], func=AF.Sign), reads=["wi"], writes=["wsgn"])
        sc.op("vector", lambda e, maskT=maskT: e.memset(maskT[:], -30000.0), writes=[mkey])
        for tt in range(4):
            i = tb * 4 + tt
            L = 128 * (i + 1)
            scr = scrs[i % 2]
            skey = "scr%d" % (i % 2)
            mk = mks[i % 2]
            mkk = "mk%d" % (i % 2)
            for h in range(16):
                cch = h // 2
                p0 = 64 * (h % 2)
                kik = "kiT2a" if p0 == 0 else "kiT2b"
                for s0 in range(0, L, 512):
                    sw = min(512, L - s0)
                    pi = pis[n_i % 2]
                    pik = "pi%d" % (n_i % 2)
                    tmp = tmps[n_i % 3]
                    tk = "itmp%d" % (n_i % 3)
                    n_i += 1
                    sc.op("tensor", lambda e, pi=pi, p0=p0, cch=cch, tt=tt, s0=s0, sw=sw: e.matmul(
                        pi[:, 0:sw], lhsT=qiT[p0:p0 + 64, cch, tt * 128:(tt + 1) * 128], rhs=kiT2[p0:p0 + 64, s0:s0 + sw],
                        start=True, stop=True), reads=["qiT", kik], writes=[pik])
                    sc.op("scalar", lambda e, pi=pi, tmp=tmp, sw=sw, tt=tt, h=h: e.activation(
                        out=tmp[:, 0:sw], in_=pi[:, 0:sw], func=AF.Relu, scale=wabs[:, tt, h:h + 1]),
                        reads=[pik, "wabs"], writes=[tk])
                    if h == 0:
                        sc.op("vector", lambda e, tmp=tmp, scr=scr, s0=s0, sw=sw, tt=tt, h=h: e.tensor_scalar(
                            out=scr[:, s0:s0 + sw], in0=tmp[:, 0:sw], scalar1=wsgn[:, tt, h:h + 1], scalar2=None, op0=ALU.mult),
                            reads=[tk, "wsgn"], writes=[skey])
                    else:
                        sc.op("vector", lambda e, tmp=tmp, scr=scr, s0=s0, sw=sw, tt=tt, h=h: e.scalar_tensor_tensor(
                            out=scr[:, s0:s0 + sw], in0=tmp[:, 0:sw], scalar=wsgn[:, tt, h:h + 1], in1=scr[:, s0:s0 + sw],
                            op0=ALU.mult, op1=ALU.add), reads=[tk, "wsgn", skey], writes=[skey])
            if i >= 2:
                sc.op("vector", lambda e, scr=scr, L=L: e.tensor_reduce(out=blo[:], in_=scr[:, 0:L], axis=AX.X, op=ALU.min),
                      reads=[skey], writes=["blo"])
            sc.op("vector", lambda e, scr=scr, L=L: e.memset(scr[0:64, L - 64:L], NEG), writes=[skey])
            if i >= 2:
                sc.op("vector", lambda e, scr=scr, L=L: e.max(out=m8[:], in_=scr[:, 0:L]), reads=[skey], writes=["m8"])
                sc.op("vector", lambda e: e.tensor_tensor(out=brg[:], in0=m8[:, 0:1], in1=blo[:], op=ALU.subtract),
                      reads=["m8", "blo"], writes=["brg"])
                sc.op("vector", lambda e: e.tensor_scalar(out=bwk[:], in0=pw2[:], scalar1=brg[:, 0:1], scalar2=None, op0=ALU.mult),
                      reads=["pw2", "brg"], writes=["bwk"])
                sc.op("vector", lambda e: e.tensor_tensor(out=bmid[:], in0=blo[:], in1=bwk[:, 0:1], op=ALU.add),
                      reads=["blo", "bwk"], writes=["bmid"])
                for kk_ in range(NBIS):
                    sc.op("vector", lambda e, scr=scr, L=L: e.tensor_scalar(out=work[:, 0:L], in0=scr[:, 0:L], scalar1=bmid[:, 0:1], scalar2=0.0,
                                                                            op0=ALU.is_ge, op1=ALU.add, accum_out=bcnt[:, 0:1]),
                          reads=[skey, "bmid"], writes=["work", "bcnt"])
                    sc.op("vector", lambda e, kk_=kk_: e.tensor_scalar(out=bg[:], in0=bcnt[:], scalar1=255.5, scalar2=bwk[:, kk_:kk_ + 1],
                                                                       op0=ALU.is_ge, op1=ALU.mult), reads=["bcnt", "bwk"], writes=["bg"])
                    sc.op("vector", lambda e: e.tensor_tensor(out=blo[:], in0=blo[:], in1=bg[:], op=ALU.add),
                          reads=["blo", "bg"], writes=["blo"])
                    if kk_ + 1 < NBIS:
                        sc.op("vector", lambda e, kk_=kk_: e.tensor_tensor(out=bmid[:], in0=blo[:], in1=bwk[:, kk_ + 1:kk_ + 2], op=ALU.add),
                              reads=["blo", "bwk"], writes=["bmid"])
                tau = blo[:, 0:1]
                tauk = "blo"
            else:
                tau = cm29[:, 0:1]
                tauk = "cm29"
            sc.op("vector", lambda e, scr=scr, mk=mk, L=L, tau=tau: e.tensor_scalar(
                out=mk[:, 0:L], in0=scr[:, 0:L], scalar1=tau, scalar2=None, op0=ALU.is_ge),
                reads=[skey, tauk], writes=[mkk])
            for j0 in range(0, i + 1, 4):
                n = min(4, i + 1 - j0)
                for jj in range(n):
                    j = j0 + jj
                    sc.op("tensor", lambda e, jj=jj, j=j, mk=mk: e.transpose(out=ptr[:, jj * 128:(jj + 1) * 128],
                                                                             in_=mk[:, j * 128:(j + 1) * 128], identity=k.identb[:]),
                          reads=[mkk, "identb"], writes=["ptr"], sig=(jj == n - 1))
                sc.op("scalar", lambda e, j0=j0, n=n, tt=tt, maskT=maskT: e.activation(
                    out=maskT[:, j0:j0 + n, tt * 128:(tt + 1) * 128],
                    in_=ptr[:, 0:n * 128].rearrange("p (n t) -> p n t", t=128), func=AF.Identity, scale=30000.0, bias=-30000.0),
                    reads=["ptr"], writes=[mkey])
        streams[("i", tb)] = sc.end_capture()
        sc.begin_capture()
        nj = 4 * (tb + 1)
        units = [(h, j) for h in range(8) for j in range(nj)]
        ubuf = {}

        def emit_lg(u):
            h, j = units[u]
            pl = pls[u % 2]
            plk = "pl%d" % (u % 2)
            sc.op("tensor", lambda e, pl=pl, h=h, j=j, qT=qT: e.matmul(pl[:, :], lhsT=kT[:, h, j * 128:(j + 1) * 128], rhs=qT[:, h, :],
                                                               start=True, stop=False), reads=["kT", qTk], writes=[plk], sig=False)
            sc.op("tensor", lambda e, pl=pl, j=j, maskT=maskT: e.matmul(pl[:, :], lhsT=k.identb[:], rhs=maskT[:, j, :],
                                                                       start=False, stop=True), reads=["identb", mkey], writes=[plk])
            pe = pes[u % 3]
            pek = "pe%d" % (u % 3)
            sc.op("scalar", lambda e, pl=pl, pe=pe: e.activation(out=pe[:], in_=pl[:, :], func=AF.Exp, scale=att_scale),
                  reads=[plk], writes=[pek])

        emit_lg(0)
        for u, (h, j) in enumerate(units):
            if u + 1 < len(units):
                emit_lg(u + 1)
            pm = pes[u % 3]
            pmk = "pe%d" % (u % 3)
            sc.op("tensor", lambda e, pm=pm, h=h, j=j: e.matmul(po[:, :], lhsT=vS[:, j, h * 128:(h + 1) * 128], rhs=pm[:],
                                                               start=(j == 0), stop=(j == nj - 1)), reads=["vS", pmk], writes=["po"], sig=False)
            sc.op("tensor", lambda e, pm=pm, j=j: e.matmul(pd[:, :], lhsT=k.onesb[:], rhs=pm[:],
                                                          start=(j == 0), stop=(j == nj - 1)), reads=["onesb", pmk], writes=["pd"])
            if j == nj - 1:
                rd = rds[n_h % 2]
                rdk = "rd%d" % (n_h % 2)
                ao = aos[n_h % 2]
                aok = "ao%d" % (n_h % 2)
                n_h += 1
                sc.op("vector", lambda e, rd=rd: e.reciprocal(out=rd[:], in_=pd[:, :]), reads=["pd"], writes=[rdk])
                sc.op("vector", lambda e, rd=rd, ao=ao: e.tensor_tensor(out=ao[:], in0=po[:, :], in1=rd[:], op=ALU.mult),
                      reads=["po", rdk], writes=[aok])
                sc.dma("sync", A["attnT"][h * 128:(h + 1) * 128, tb * 512:(tb + 1) * 512], ao[:], reads=[aok], writes=["d_attnT"])
        streams[("a", tb)] = sc.end_capture()
    for u in streams[("i", 0)]:
        u()
    for tb in range(4):
        merged_units([streams[("a", tb)], streams.get(("i", tb + 1), [])])


def phase_e(k, ph):
    nc, sc, sb, ps = k.nc, k.sc, k.sb, k.ps
    A = k.scr
    NCH = 32
    lbl = sb("lbl", [128, 2, 8], ctx=ph)
    sc.dma("sync", lbl[:], k.lb_logits.rearrange("r (h d) -> d r h", d=128), writes=["lbl"], allow_slow_non_contiguous=True)
    lb = sb("lb", [128, 8], ctx=ph)
    oml = sb("oml", [128, 8], ctx=ph)
    sc.op("vector", lambda e: e.tensor_tensor(out=lb[:], in0=lbl[:, 0, :], in1=lbl[:, 1, :], op=ALU.subtract), reads=["lbl"], writes=["lb"])
    sc.op("scalar", lambda e: e.activation(out=lb[:], in_=lb[:], func=AF.Sigmoid), reads=["lb"], writes=["lb"])
    sc.op("vector", lambda e: e.tensor_scalar(out=oml[:], in0=lb[:], scalar1=-1.0, scalar2=1.0, op0=ALU.mult, op1=ALU.add),
          reads=["lb"], writes=["oml"])
    gain = sb("hgain", [128, 1], ctx=ph)
    sc.dma("sync", gain[:], k.hgrn_gain.rearrange("o d -> d o"), writes=["hgain"], allow_slow_non_contiguous=True)
    tri = sb("tri_sb", [64, 64], ctx=ph)
    sc.dma("sync", tri[:], k.tri_d, writes=["tri"])
    rst = sb("rst", [128, S], ctx=ph)
    sc.op("vector", lambda e: e.memset(rst[:], 1.0), writes=["rst"])
    sc.op("vector", lambda e: e.memset(rst[:].rearrange("p (c s) -> p c s", s=64)[:, :, 0:1], 0.0), writes=["rst"])
    qf = sb("qf", [128, S], ctx=ph)
    ff = sb("ff", [128, S], ctx=ph)
    kk = sb("kk", [128, S], ctx=ph)
    lf = sb("lf", [128, S], ctx=ph)
    bb = sb("bb", [128, S], ctx=ph)
    bp = sb("bp", [128, S], ctx=ph)
    tA = sb("tA", [128, S], ctx=ph)
    qhats = [sb("qhat%d" % i, [128, S], BF16, ctx=ph) for i in range(2)]
    qtils = [sb("qtil%d" % i, [128, S], BF16, ctx=ph) for i in range(2)]
    ktils = [sb("ktil%d" % i, [128, S], BF16, ctx=ph) for i in range(2)]
    kendTs = [sb("kendT%d" % i, [128, S], BF16, ctx=ph) for i in range(2)]
    sq = sb("hsq", [128, S], ctx=ph)
    kend = sb("kend", [64, NCH, 128], BF16, ctx=ph)
    vSs = [sb("hvS%d" % i, [64, NCH, 128], BF16, ctx=ph) for i in range(2)]
    atT = sb("atT", [64, NCH, 64], BF16, ctx=ph)
    recT = sb("recT", [128, S], ctx=ph)
    sgbs = [sb("sgb%d" % i, [128, S], BF16, ctx=ph) for i in range(2)]
    ebls = [sb("ebl%d" % i, [128, NCH], ctx=ph) for i in range(2)]
    Sf = sb("Sf", [128, 128], ctx=ph)
    Sb = sb("Sb", [128, 128], BF16, ctx=ph)
    rs = sb("hrs", [128, 512], ctx=ph)
    t1 = sb("ht1", [128, 512], ctx=ph)
    obs = [sb("hob%d" % i, [128, 512], BF16, ctx=ph) for i in range(2)]
    ptk = ps("ptk", [64, 512], BF16, ctx=ph)
    pat = ps("pat", [64, 512], ctx=ph)
    pos = [ps("hpo%d" % i, [128, 512], ctx=ph) for i in range(2)]
    pSs = [ps("hpS%d" % i, [128, 128], ctx=ph) for i in range(2)]
    pn = ps("hpn", [128, 512], ctx=ph)
    v3 = lambda t: t[:].rearrange("p (c s) -> p c s", s=64)
    n_ob = [0]
    Pst, Lst = {}, {}

    def do_head(h):
        hp_ = str(h % 2)
        qhat, qtil, ktil, kendT, vS, sgb, ebl = qhats[h % 2], qtils[h % 2], ktils[h % 2], kendTs[h % 2], vSs[h % 2], sgbs[h % 2], ebls[h % 2]
        rows = slice(h * 128, (h + 1) * 128)
        sc.begin_capture()
        sc.dma("sync", qf[:], A["qBT"][rows, :], reads=["d_qBT"], writes=["qf"])
        sc.dma("sync", ff[:], A["fBT"][rows, :], reads=["d_fBT"], writes=["ff"])
        sc.dma("sync", sgb[:], A["gBT"][rows, :], reads=["d_gBT"], writes=["sgb" + hp_])
        sc.dma("sync", vS[:], A["iB"].rearrange("(c s) n -> s c n", s=64)[:, :, rows], reads=["d_iB"], writes=["hvS" + hp_])
        sc.op("vector", lambda e, h=h: e.tensor_scalar(out=ff[:], in0=ff[:], scalar1=oml[:, h:h + 1], scalar2=lb[:, h:h + 1],
                                                       op0=ALU.mult, op1=ALU.add), reads=["ff", "oml", "lb"], writes=["ff"])
        sc.op("vector", lambda e: e.tensor_scalar(out=kk[:], in0=ff[:], scalar1=-1.0, scalar2=1.0, op0=ALU.mult, op1=ALU.add),
              reads=["ff"], writes=["kk"])
        sc.op("scalar", lambda e: e.activation(out=lf[:], in_=ff[:], func=AF.Ln), reads=["ff"], writes=["lf"])
        sc.op("vector", lambda e: e.tensor_tensor_scan(out=bb[:], data0=rst[:], data1=lf[:], initial=0.0, op0=ALU.mult, op1=ALU.add),
              reads=["rst", "lf"], writes=["bb"])
        sc.op("scalar", lambda e: e.activation(out=tA[:], in_=bb[:], func=AF.Exp), reads=["bb"], writes=["tA"])
        sc.op("vector", lambda e: e.tensor_tensor(out=qhat[:], in0=qf[:], in1=tA[:], op=ALU.mult), reads=["qf", "tA"], writes=["qhat" + hp_])
        sc.op("scalar", lambda e: e.activation(out=ebl[:], in_=v3(bb)[:, :, 63], func=AF.Exp), reads=["bb"], writes=["ebl" + hp_])
        sc.op("vector", lambda e: e.tensor_tensor(out=v3(bp), in0=v3(bb), in1=v3(bb)[:, :, 31:32].to_broadcast([128, NCH, 64]),
                                                  op=ALU.subtract), reads=["bb"], writes=["bp"])
        sc.op("scalar", lambda e: e.activation(out=tA[:], in_=bp[:], func=AF.Exp), reads=["bp"], writes=["tA"])
        sc.op("vector", lambda e: e.tensor_tensor(out=qtil[:], in0=qf[:], in1=tA[:], op=ALU.mult), reads=["qf", "tA"], writes=["qtil" + hp_])
        sc.op("scalar", lambda e: e.activation(out=tA[:], in_=bp[:], func=AF.Exp, scale=-1.0), reads=["bp"], writes=["tA"])
        sc.op("vector", lambda e: e.tensor_tensor(out=ktil[:], in0=kk[:], in1=tA[:], op=ALU.mult), reads=["kk", "tA"], writes=["ktil" + hp_])
        sc.op("vector", lambda e: e.tensor_tensor(out=v3(bp), in0=v3(bb)[:, :, 63:64].to_broadcast([128, NCH, 64]), in1=v3(bb),
                                                  op=ALU.subtract), reads=["bb"], writes=["bp"])
        sc.op("scalar", lambda e: e.activation(out=tA[:], in_=bp[:], func=AF.Exp), reads=["bp"], writes=["tA"])
        sc.op("vector", lambda e: e.tensor_tensor(out=kendT[:], in0=kk[:], in1=tA[:], op=ALU.mult), reads=["kk", "tA"], writes=["kendT" + hp_])
        Pst[h] = sc.end_capture()
        sc.begin_capture()
        for c0 in range(0, NCH, 4):
            for jj in range(4):
                c = c0 + jj
                sc.op("tensor", lambda e, jj=jj, c=c: e.transpose(out=ptk[:, jj * 128:(jj + 1) * 128], in_=kendT[:, c * 64:(c + 1) * 64],
                                                                 identity=k.identb[:]), reads=["kendT" + hp_, "identb"], writes=["ptk"], sig=(jj == 3))
            sc.op("scalar", lambda e, c0=c0: e.activation(out=kend[:, c0:c0 + 4, :], in_=ptk[:, :].rearrange("p (n d) -> p n d", d=128),
                                                          func=AF.Copy), reads=["ptk"], writes=["kend"])
        for c0 in range(0, NCH, 8):
            for jj in range(8):
                c = c0 + jj
                sc.op("tensor", lambda e, jj=jj, c=c: e.matmul(pat[:, jj * 64:(jj + 1) * 64], lhsT=ktil[:, c * 64:(c + 1) * 64],
                                                              rhs=qtil[:, c * 64:(c + 1) * 64], start=True, stop=True),
                      reads=["ktil" + hp_, "qtil" + hp_], writes=["pat"], sig=(jj == 7))
            sc.op("vector", lambda e, c0=c0: e.tensor_tensor(out=atT[:, c0:c0 + 8, :], in0=pat[:, :].rearrange("p (n t) -> p n t", t=64),
                                                             in1=tri[:, :].unsqueeze(1).to_broadcast([64, 8, 64]), op=ALU.mult),
                  reads=["pat", "tri"], writes=["atT"])
        for c in range(NCH):
            po = pos[(c // 8) % 2]
            pok = "hpo%d" % ((c // 8) % 2)
            col = (c % 8) * 64
            if c > 0:
                sc.op("tensor", lambda e, po=po, col=col, c=c: e.matmul(po[:, col:col + 64], lhsT=Sb[:], rhs=qhat[:, c * 64:(c + 1) * 64],
                                                                       start=True, stop=False), reads=["Sb", "qhat" + hp_], writes=[pok], sig=False)
            sc.op("tensor", lambda e, po=po, col=col, c=c: e.matmul(po[:, col:col + 64], lhsT=vS[:, c, :], rhs=atT[:, c, :],
                                                                   start=(c == 0), stop=True), reads=["hvS" + hp_, "atT"], writes=[pok])
            if c < NCH - 1:
                pS = pSs[c % 2]
                pSk = "hpS%d" % (c % 2)
                sc.op("tensor", lambda e, pS=pS, c=c: e.matmul(pS[:, :], lhsT=kend[:, c, :], rhs=vS[:, c, :], start=True, stop=True),
                      reads=["kend", "hvS" + hp_], writes=[pSk])
                if c == 0:
                    sc.op("vector", lambda e, pS=pS: e.tensor_copy(out=Sb[:], in_=pS[:, :]), reads=[pSk], writes=["Sb"])
                    sc.op("vector", lambda e, pS=pS: e.tensor_copy(out=Sf[:], in_=pS[:, :]), reads=[pSk], writes=["Sf"])
                else:
                    sc.op("vector", lambda e, pS=pS, c=c: e.scalar_tensor_tensor(out=Sb[:], in0=Sf[:], scalar=ebl[:, c:c + 1], in1=pS[:, :],
                                                                                op0=ALU.mult, op1=ALU.add),
                          reads=["Sf", "ebl" + hp_, pSk], writes=["Sb"])
                    sc.op("vector", lambda e, pS=pS, c=c: e.scalar_tensor_tensor(out=Sf[:], in0=Sf[:], scalar=ebl[:, c:c + 1], in1=pS[:, :],
                                                                                op0=ALU.mult, op1=ALU.add),
                          reads=["Sf", "ebl" + hp_, pSk], writes=["Sf"])
            if c % 8 == 7:
                sc.op("scalar", lambda e, po=po, c=c: e.activation(out=recT[:, (c - 7) * 64:(c + 1) * 64], in_=po[:, :], func=AF.Copy),
                      reads=[pok], writes=["recT"])
        sc.op("scalar", lambda e: e.activation(out=sq[:], in_=recT[:], func=AF.Square), reads=["recT"], writes=["hsq"])
        for tb in range(4):
            cs_ = slice(tb * 512, (tb + 1) * 512)
            sc.op("tensor", lambda e, cs_=cs_: e.matmul(pn[:, :], lhsT=k.onesf[:], rhs=sq[:, cs_], start=True, stop=True),
                  reads=["onesf", "hsq"], writes=["hpn"])
            sc.op("vector", lambda e: e.tensor_scalar(out=rs[:], in0=pn[:, :], scalar1=1.0 / 128, scalar2=EPS, op0=ALU.mult, op1=ALU.add),
                  reads=["hpn"], writes=["hrs"])
            sc.op("scalar", lambda e: e.activation(out=rs[:], in_=rs[:], func=AF.Sqrt), reads=["hrs"], writes=["hrs"])
            sc.op("vector", lambda e: e.reciprocal(out=rs[:], in_=rs[:]), reads=["hrs"], writes=["hrs"])
            sc.op("vector", lambda e, cs_=cs_: e.scalar_tensor_tensor(out=t1[:], in0=recT[:, cs_], scalar=gain[:, 0:1], in1=rs[:],
                                                                      op0=ALU.mult, op1=ALU.mult), reads=["recT", "hgain", "hrs"], writes=["ht1"])
            ob = obs[n_ob[0] % 2]
            obk = "hob%d" % (n_ob[0] % 2)
            n_ob[0] += 1
            sc.op("vector", lambda e, ob=ob, cs_=cs_: e.tensor_tensor(out=ob[:], in0=t1[:], in1=sgb[:, cs_], op=ALU.mult),
                  reads=["ht1", "sgb" + hp_], writes=[obk])
            sc.dma("sync", A["recT"][rows, cs_], ob[:], reads=[obk], writes=["d_recT"])
        Lst[h] = sc.end_capture()

    for h in range(8):
        do_head(h)
    for u in Pst[0]:
        u()
    for h in range(8):
        merged_units([Lst[h], Pst.get(h + 1, [])])


def phase_f(k, ph):
    nc, sc, sb, ps = k.nc, k.sc, k.sb, k.ps
    A = k.scr
    aT = sb("f_aT", [128, 8, S], BF16, ctx=ph)
    rT = sb("f_rT", [128, 8, S], BF16, ctx=ph)
    sc.dma("sync", aT[:], A["attnT"].rearrange("(h p) t -> p h t", p=128), reads=["d_attnT"], writes=["f_aT"])
    sc.dma("sync", rT[:], A["recT"].rearrange("(h p) t -> p h t", p=128), reads=["d_recT"], writes=["f_rT"])
    mixT = k.hT
    was = [sb("f_wa%d" % i, [128, 8, 512], BF16, ctx=ph) for i in range(2)]
    wbs = [sb("f_wb%d" % i, [128, 8, 512], BF16, ctx=ph) for i in range(2)]
    sgas = [sb("f_sga%d" % i, [128, 512], BF16, ctx=ph) for i in range(2)]
    sgbs = [sb("f_sgb%d" % i, [128, 512], BF16, ctx=ph) for i in range(2)]
    yas = [sb("f_ya%d" % i, [128, 512], ctx=ph) for i in range(2)]
    ybs = [sb("f_yb%d" % i, [128, 512], ctx=ph) for i in range(2)]
    pas = [ps("f_pa%d" % i, [128, 512], ctx=ph) for i in range(2)]
    pbs = [ps("f_pb%d" % i, [128, 512], ctx=ph) for i in range(2)]
    wva = k.w_up_a.rearrange("(kc p) n -> p kc n", p=128)
    wvb = k.w_up_b.rearrange("(kc p) n -> p kc n", p=128)
    n = 0
    for nb in range(4):
        wa, wb = was[nb % 2], wbs[nb % 2]
        wak, wbk = "f_wa%d" % (nb % 2), "f_wb%d" % (nb % 2)
        sc.dma("gpsimd", wa[:], wva[:, :, nb * 512:(nb + 1) * 512], writes=[wak])
        sc.dma("gpsimd", wb[:], wvb[:, :, nb * 512:(nb + 1) * 512], writes=[wbk])
        for sub in range(4):
            nch = nb * 4 + sub
            for tb in range(4):
                i2 = n % 2
                n += 1
                pa, pb = pas[i2], pbs[i2]
                pak, pbk = "f_pa%d" % i2, "f_pb%d" % i2
                sga, sgb = sgas[i2], sgbs[i2]
                ya, yb = yas[i2], ybs[i2]
                cs_ = slice(tb * 512, (tb + 1) * 512)
                sc.dma("sync", sga[:], A["sgAT"][nch * 128:(nch + 1) * 128, cs_], reads=["d_sgAT"], writes=["f_sga%d" % i2])
                sc.dma("sync", sgb[:], A["sgBT"][nch * 128:(nch + 1) * 128, cs_], reads=["d_sgBT"], writes=["f_sgb%d" % i2])
                for kc in range(8):
                    sc.op("tensor", lambda e, pa=pa, wa=wa, kc=kc, sub=sub, cs_=cs_: e.matmul(
                        pa[:, :], lhsT=wa[:, kc, sub * 128:(sub + 1) * 128], rhs=aT[:, kc, cs_], start=(kc == 0), stop=(kc == 7)),
                        reads=[wak, "f_aT"], writes=[pak], sig=(kc == 7))
                for kc in range(8):
                    sc.op("tensor", lambda e, pb=pb, wb=wb, kc=kc, sub=sub, cs_=cs_: e.matmul(
                        pb[:, :], lhsT=wb[:, kc, sub * 128:(sub + 1) * 128], rhs=rT[:, kc, cs_], start=(kc == 0), stop=(kc == 7)),
                        reads=[wbk, "f_rT"], writes=[pbk], sig=(kc == 7))
                sc.op("vector", lambda e, pa=pa, ya=ya, sga=sga: e.tensor_tensor(out=ya[:], in0=pa[:, :], in1=sga[:], op=ALU.mult),
                      reads=[pak, "f_sga%d" % i2], writes=["f_ya%d" % i2])
                sc.op("vector", lambda e, pb=pb, yb=yb, sgb=sgb: e.tensor_tensor(out=yb[:], in0=pb[:, :], in1=sgb[:], op=ALU.mult),
                      reads=[pbk, "f_sgb%d" % i2], writes=["f_yb%d" % i2])
                sc.op("vector", lambda e, ya=ya, yb=yb, nch=nch, cs_=cs_: e.tensor_tensor(out=mixT[:, nch, cs_], in0=ya[:], in1=yb[:], op=ALU.add),
                      reads=["f_ya%d" % i2, "f_yb%d" % i2], writes=[("hT", nch, tb)])


def phase_f2(k, ph):
    nc, sc, sb, ps = k.nc, k.sc, k.sb, k.ps
    A = k.scr
    mixT = k.hT
    r = proj_setup(k, ph, NDC, "fo")
    xss = [sb("f_xs%d" % i, [128, 512], ctx=ph) for i in range(3)]
    tts = [sb("f_tt%d" % i, [128, 512], ctx=ph) for i in range(3)]
    xv = k.x.rearrange("(n p) d -> n p d", p=128)
    x1v = A["x1"].rearrange("(n p) d -> n p d", p=128)
    m = 0
    for nb in range(4):
        cs_ = slice(nb * 512, (nb + 1) * 512)
        wb, wk = proj_load_w(k, r, k.w_out, nb * 512, (nb + 1) * 512)
        for ti in range(NTT):
            pt, pk = proj_psum(r)
            i3 = m % 3
            m += 1
            xs, tt_ = xss[i3], tts[i3]
            sc.dma("sync", xs[:], xv[ti][:, cs_], writes=["f_xs%d" % i3])
            for kc in range(NDC):
                sc.op("tensor", lambda e, pt=pt, wb=wb, kc=kc, ti=ti: e.matmul(
                    pt[:, :], lhsT=mixT[:, kc, ti * 128:(ti + 1) * 128], rhs=wb[:, kc, :], start=(kc == 0), stop=(kc == NDC - 1)),
                    reads=[wk, ("hT", kc, ti // 4)], writes=[pk], sig=(kc == NDC - 1))
            sc.op("vector", lambda e, pt=pt, tt_=tt_, cs_=cs_: e.tensor_tensor(out=tt_[:], in0=pt[:, :], in1=k.G1[:, cs_], op=ALU.mult),
                  reads=[pk, "G0"], writes=["f_tt%d" % i3])
            sc.op("vector", lambda e, tt_=tt_, xs=xs: e.tensor_tensor(out=tt_[:], in0=tt_[:], in1=xs[:], op=ALU.add),
                  reads=["f_tt%d" % i3, "f_xs%d" % i3], writes=["f_tt%d" % i3])
            sc.dma("sync", x1v[ti][:, cs_], tt_[:], reads=["f_tt%d" % i3], writes=["d_x1"])


def phase_g(k, ph):
    sc = k.sc
    norm_modulate(k, ph, k.scr["x1"], k.norm_ffn, 48, 64, ["d_x1"], tag="g2")
    for dc in range(NDC):
        sc.dma("sync", k.scr["h2T"][dc * 128:(dc + 1) * 128, :], k.hT[:, dc, :], reads=[("hT", dc, tg) for tg in range(4)],
               writes=["d_h2T"])


def phase_h1(k, ph):
    r = proj_setup(k, ph, NDC, "ph")
    proj(k, r, k.hT, hT_keys, k.peer_w_q, 0, 2048, "fm", AF.Identity, k.scr["pqT"], "d_pqT", BF16)


def phase_h2(k, ph):
    nc, sc, sb, ps = k.nc, k.sc, k.sb, k.ps
    A = k.scr
    NEG = -1e30
    kl = sb("h_kl", [128, 16, 128], ctx=ph)
    sc.dma("sync", kl[:], k.peer_keys.rearrange("p h n d -> n (p h) d"), writes=["h_kl"])
    keysT = sb("h_keysT", [128, 16, 128], BF16, ctx=ph)
    pss = [ps("h_ps%d" % i, [128, 512], ctx=ph) for i in range(4)]
    for ch in range(16):
        b = ch // 4
        sc.op("tensor", lambda e, ch=ch, b=b: e.transpose(out=pss[b][:, (ch % 4) * 128:(ch % 4 + 1) * 128], in_=kl[:, ch, :],
                                                         identity=k.identf[:]), reads=["h_kl", "identf"], writes=["h_ps%d" % b], sig=(ch % 4 == 3))
    for b in range(4):
        sc.op("vector", lambda e, b=b: e.tensor_copy(out=keysT[:, b * 4:(b + 1) * 4, :],
                                                     in_=pss[b][:, :].rearrange("p (c n) -> p c n", n=128)),
              reads=["h_ps%d" % b], writes=["h_keysT"])
    qts = [sb("h_qt%d" % i, [128, 16, 128], BF16, ctx=ph) for i in range(2)]
    s_sb = sb("h_s", [128, 16, 128], ctx=ph)
    wk = sb("h_wk", [128, 16, 128], ctx=ph)
    tops = sb("h_tops", [128, 16, 16], ctx=ph)
    cand = sb("h_cand", [128, 8, 256], ctx=ph)
    cwk = sb("h_cwk", [128, 8, 256], ctx=ph)
    best = sb("h_best", [128, 8, 16], ctx=ph)
    ez = sb("h_ez", [128, 8, 16], ctx=ph)
    Z = sb("h_Z", [128, 8], ctx=ph)
    bias = sb("h_bias", [128, 8], ctx=ph)
    th = sb("h_th", [128, 8, 16], ctx=ph)
    e1 = sb("h_e1", [128, 8, 16], ctx=ph)
    e1T = sb("h_e1T", [128, 128], ctx=ph)
    e2 = sb("h_e2", [128, 8, 128], ctx=ph)
    Rt = sb("h_R", [128, 128, 128], BF16, ctx=ph)
    Oh = sb("h_O", [128, 64, 128], BF16, ctx=ph)
    RT = sb("h_RT", [128, 128, 128], BF16, ctx=ph)
    OT = sb("h_OT", [128, 64, 128], BF16, ctx=ph)
    gsts = [sb("h_gst%d" % i, [128, 64, 128], BF16, ctx=ph) for i in range(2)]
    ptrs = [ps("h_ptr%d" % i, [128, 512], BF16, ctx=ph) for i in range(2)]
    pgs = [ps("h_pg%d" % i, [128, 512], ctx=ph) for i in range(2)]
    pqv = A["pqT"].rearrange("(c p) t -> p c t", p=128)
    GTv = A["GT"].rearrange("(i j) t -> j i t", j=128)
    s4 = s_sb[:].rearrange("p (a h) n -> p a h n", h=2)
    t4 = tops[:].rearrange("p (a h) n -> p a h n", h=2)
    n_s = 0
    n_g = 0
    n_pg = 0
    for ti in range(NTT):
        qt = qts[ti % 2]
        qk = "h_qt%d" % (ti % 2)
        sc.dma("sync", qt[:], pqv[:, :, ti * 128:(ti + 1) * 128], reads=["d_pqT"], writes=[qk])
        for ch in range(16):
            b = ch // 4
            sc.op("tensor", lambda e, ch=ch, b=b, qt=qt: e.matmul(pss[b][:, (ch % 4) * 128:(ch % 4 + 1) * 128], lhsT=qt[:, ch, :],
                                                                 rhs=keysT[:, ch, :], start=True, stop=True),
                  reads=[qk, "h_keysT"], writes=["h_ps%d" % b], sig=(ch % 4 == 3))
        for b in range(4):
            sc.op("scalar", lambda e, b=b: e.activation(out=s_sb[:, b * 4:(b + 1) * 4, :],
                                                        in_=pss[b][:, :].rearrange("p (c n) -> p c n", n=128), func=AF.Copy),
                  reads=["h_ps%d" % b], writes=["h_s"])
        for ch in range(16):
            sc.op("vector", lambda e, ch=ch: e.max(out=tops[:, ch, 0:8], in_=s_sb[:, ch, :]), reads=["h_s"], writes=["h_tops"])
            sc.op("vector", lambda e, ch=ch: e.match_replace(out=wk[:, ch, :], in_to_replace=tops[:, ch, 0:8], in_values=s_sb[:, ch, :],
                                                             imm_value=NEG), reads=["h_s", "h_tops"], writes=["h_wk"])
            sc.op("vector", lambda e, ch=ch: e.max(out=tops[:, ch, 8:16], in_=wk[:, ch, :]), reads=["h_wk"], writes=["h_tops"])
        sc.op("vector", lambda e: e.tensor_tensor(out=cand[:].rearrange("p a (r c) -> p a r c", c=16),
                                                  in0=t4[:, :, 0, :].unsqueeze(3).to_broadcast([128, 8, 16, 16]),
                                                  in1=t4[:, :, 1, :].unsqueeze(2).to_broadcast([128, 8, 16, 16]), op=ALU.add),
              reads=["h_tops"], writes=["h_cand"])
        for p in range(8):
            sc.op("vector", lambda e, p=p: e.max(out=best[:, p, 0:8], in_=cand[:, p, :]), reads=["h_cand"], writes=["h_best"])
            sc.op("vector", lambda e, p=p: e.match_replace(out=cwk[:, p, :], in_to_replace=best[:, p, 0:8], in_values=cand[:, p, :],
                                                           imm_value=NEG), reads=["h_cand", "h_best"], writes=["h_cwk"])
            sc.op("vector", lambda e, p=p: e.max(out=best[:, p, 8:16], in_=cwk[:, p, :]), reads=["h_cwk"], writes=["h_best"])
        sc.op("vector", lambda e: e.tensor_tensor(out=ez[:], in0=best[:], in1=best[:, :, 0:1].to_broadcast([128, 8, 16]), op=ALU.subtract),
              reads=["h_best"], writes=["h_ez"])
        sc.op("scalar", lambda e: e.activation(out=ez[:], in_=ez[:], func=AF.Exp), reads=["h_ez"], writes=["h_ez"])
        sc.op("vector", lambda e: e.tensor_reduce(out=Z[:], in_=ez[:], axis=AX.X, op=ALU.add), reads=["h_ez"], writes=["h_Z"])
        sc.op("scalar", lambda e: e.activation(out=Z[:], in_=Z[:], func=AF.Ln), reads=["h_Z"], writes=["h_Z"])
        sc.op("vector", lambda e: e.tensor_tensor(out=bias[:], in0=best[:, :, 15], in1=best[:, :, 0], op=ALU.subtract),
              reads=["h_best"], writes=["h_bias"])
        sc.op("vector", lambda e: e.tensor_tensor(out=bias[:], in0=bias[:], in1=Z[:], op=ALU.subtract),
              reads=["h_bias", "h_Z"], writes=["h_bias"])
        sc.op("vector", lambda e: e.tensor_tensor(out=th[:], in0=best[:, :, 15:16].to_broadcast([128, 8, 16]), in1=t4[:, :, 0, :],
                                                  op=ALU.subtract), reads=["h_best", "h_tops"], writes=["h_th"])
        sc.op("scalar", lambda e: e.activation(out=e1[:], in_=th[:], func=AF.Exp, scale=-1.0), reads=["h_th"], writes=["h_e1"])
        sc.op("tensor", lambda e: e.transpose(out=pss[0][:, 0:128], in_=e1[:].rearrange("t p r -> t (p r)"), identity=k.identf[:]),
              reads=["h_e1", "identf"], writes=["h_ps0"])
        sc.op("scalar", lambda e: e.activation(out=e1T[:], in_=pss[0][:, 0:128], func=AF.Copy), reads=["h_ps0"], writes=["h_e1T"])
        for p in range(8):
            sc.op("scalar", lambda e, p=p: e.activation(out=e2[:, p, :], in_=s4[:, p, 1, :], func=AF.Exp, bias=bias[:, p:p + 1]),
                  reads=["h_s", "h_bias"], writes=["h_e2"])
        R4 = Rt[:].rearrange("t j (p r) -> t j p r", r=16)
        sc.op("vector", lambda e: e.tensor_tensor(
            out=R4, in0=s4[:, :, 1, :].rearrange("t p j -> t j p").unsqueeze(3).to_broadcast([128, 128, 8, 16]),
            in1=th[:].unsqueeze(1).to_broadcast([128, 128, 8, 16]), op=ALU.is_ge),
            reads=["h_s", "h_th"], writes=["h_R"])
        sc.op("vector", lambda e: e.tensor_tensor(
            out=R4, in0=R4, in1=e2[:].rearrange("t p j -> t j p").unsqueeze(3).to_broadcast([128, 128, 8, 16]), op=ALU.mult),
            reads=["h_R", "h_e2"], writes=["h_R"])

        def emit_OH(ih):
            O4 = Oh[:].rearrange("t i (p r) -> t i p r", r=16)
            sc.op("vector", lambda e, ih=ih: e.tensor_tensor(
                out=O4, in0=s4[:, :, 0, ih * 64:(ih + 1) * 64].rearrange("t p i -> t i p").unsqueeze(3).to_broadcast([128, 64, 8, 16]),
                in1=t4[:, :, 0, :].unsqueeze(1).to_broadcast([128, 64, 8, 16]), op=ALU.is_equal),
                reads=["h_s", "h_tops"], writes=["h_O"])

        emit_OH(0)
        for j0 in range(0, 128, 4):
            ptr = ptrs[n_pg % 2]
            ptk = "h_ptr%d" % (n_pg % 2)
            n_pg += 1
            for jj in range(4):
                sc.op("tensor", lambda e, ptr=ptr, jj=jj, j0=j0: e.transpose(out=ptr[:, jj * 128:(jj + 1) * 128], in_=Rt[:, j0 + jj, :],
                                                                            identity=k.identb[:]),
                      reads=["h_R", "identb"], writes=[ptk], sig=(jj == 3))
            sc.op("vector", lambda e, ptr=ptr, j0=j0: e.tensor_tensor(
                out=RT[:, j0:j0 + 4, :], in0=ptr[:, :].rearrange("k (j t) -> k j t", t=128),
                in1=e1T[:, :].unsqueeze(1).to_broadcast([128, 4, 128]), op=ALU.mult),
                reads=[ptk, "h_e1T"], writes=["h_RT"])
        for ih in range(2):
            if ih == 1:
                emit_OH(1)
            for i0_ in range(0, 64, 4):
                ptr = ptrs[n_pg % 2]
                ptk = "h_ptr%d" % (n_pg % 2)
                n_pg += 1
                for ii in range(4):
                    sc.op("tensor", lambda e, ptr=ptr, ii=ii, i0_=i0_: e.transpose(out=ptr[:, ii * 128:(ii + 1) * 128], in_=Oh[:, i0_ + ii, :],
                                                                                  identity=k.identb[:]),
                          reads=["h_O", "identb"], writes=[ptk], sig=(ii == 3))
                sc.op("scalar", lambda e, ptr=ptr, i0_=i0_: e.activation(
                    out=OT[:, i0_:i0_ + 4, :], in_=ptr[:, :].rearrange("k (i t) -> k i t", t=128), func=AF.Copy),
                    reads=[ptk], writes=["h_OT"])
            gst = gsts[n_g % 2]
            gk = "h_gst%d" % (n_g % 2)
            n_g += 1
            for t0 in range(0, 128, 8):
                pg = pgs[n_s % 2]
                pgk = "h_pg%d" % (n_s % 2)
                n_s += 1
                for tt_ in range(8):
                    t_ = t0 + tt_
                    sc.op("tensor", lambda e, pg=pg, tt_=tt_, t_=t_: e.matmul(pg[:, :].rearrange("j (i t) -> j i t", t=8)[:, :, tt_],
                                                                             lhsT=RT[:, :, t_], rhs=OT[:, :, t_], start=True, stop=True),
                          reads=["h_RT", "h_OT"], writes=[pgk], sig=(tt_ == 7))
                sc.op("scalar", lambda e, pg=pg, gst=gst, t0=t0: e.activation(
                    out=gst[:, :, t0:t0 + 8], in_=pg[:, :].rearrange("j (i t) -> j i t", t=8), func=AF.Copy),
                    reads=[pgk], writes=[gk])
            sc.dma("sync", GTv[:, ih * 64:(ih + 1) * 64, ti * 128:(ti + 1) * 128], gst[:], reads=[gk], writes=["d_GT"])


def phase_i(k, ph, hp):
    nc, sc = k.nc, k.sc
    sb = lambda name, *a, **kw: k.sb("p%d_" % hp + name, *a, **kw)
    ps = lambda name, *a, **kw: k.ps("p%d_" % hp + name, *a, **kw)
    A = k.scr
    GE = 2
    T0 = hp * 1024
    hh = sb("i_hh", [128, NDC, 1024], BF16, ctx=ph)
    sc.dma("sync", hh[:], A["h2T"].rearrange("(c p) t -> p c t", p=128)[:, :, T0:T0 + 1024], reads=["d_h2T"], writes=["i_hh"])
    acc = k.acc
    for tt in range(8):
        sc.op("vector", lambda e, tt=tt: e.memset(acc[:, tt, :], 0.0), writes=[("acc", tt)])
    ubs = [sb("i_ub%d" % i, [128, D], BF16, ctx=ph) for i in range(4)]
    uTs = [sb("i_uT%d" % i, [128, NDC, GE * 128], BF16, ctx=ph) for i in range(2)]
    vbs = [sb("i_vb%d" % i, [128, GE, D], BF16, ctx=ph) for i in range(3)]
    gTs = [sb("i_gT%d" % i, [128, GE, 1024], BF16, ctx=ph) for i in range(2)]
    WTs = [sb("i_WT%d" % i, [128, GE, 1024], BF16, ctx=ph) for i in range(2)]
    ges = [sb("i_ge%d" % i, [128, 512], BF16, ctx=ph) for i in range(2)]
    ptus = [ps("i_ptu%d" % i, [128, 512], BF16, ctx=ph) for i in range(2)]
    pAs = [ps("i_pA%d" % i, [128, 512], ctx=ph) for i in range(2)]
    pOs = [ps("i_pO%d" % i, [128, 512], ctx=ph) for i in range(4)]
    GTv = A["GT"].rearrange("(c p) t -> p c t", p=128)
    cnt = {"u": 0, "t": 0, "a": 0, "o": 0}
    NG = 128 // GE

    def dma_ub(eg):
        for ec in range(GE):
            ch = eg * GE + ec
            sc.dma("gpsimd", ubs[ch % 4][:], k.peer_u[ch * 128:(ch + 1) * 128, :], writes=["i_ub%d" % (ch % 4)])

    def units_T(eg):
        g2 = eg % 2
        g3 = eg % 3
        uT, vb, gT = uTs[g2], vbs[g3], gTs[g2]
        uTk, gTk = "i_uT%d" % g2, "i_gT%d" % g2
        units = []

        def u0():
            sc.dma("sync", gT[:], GTv[:, eg * GE:(eg + 1) * GE, T0:T0 + 1024], reads=["d_GT"], writes=[gTk])
            if eg + 1 < NG:
                dma_ub(eg + 1)
            for ec in range(GE):
                e0 = (eg * GE + ec) * 128
                sc.dma("gpsimd", vb[:, ec, :], k.peer_v[e0:e0 + 128, :], writes=[("i_vb", g3, ec)])
        units.append(u0)
        for ec in range(GE):
            e0 = (eg * GE + ec) * 128
            ubi = (eg * GE + ec) % 4
            ub = ubs[ubi]
            ubk = "i_ub%d" % ubi
            for d0 in range(0, NDC, 4):
                def ub_(ec=ec, e0=e0, ub=ub, ubk=ubk, d0=d0):
                    ptu = ptus[cnt["t"] % 2]
                    ptk = "i_ptu%d" % (cnt["t"] % 2)
                    cnt["t"] += 1
                    for dd in range(4):
                        dc = d0 + dd
                        sc.op("tensor", lambda e, ptu=ptu, dd=dd, dc=dc, ub=ub: e.transpose(out=ptu[:, dd * 128:(dd + 1) * 128],
                                                                                           in_=ub[:, dc * 128:(dc + 1) * 128], identity=k.identb[:]),
                              reads=[ubk, "identb"], writes=[ptk], sig=(dd == 3))
                    if (d0 // 4) % 2 == 0:
                        sc.op("vector", lambda e, ptu=ptu, uT=uT, d0=d0, ec=ec: e.tensor_copy(
                            out=uT[:, d0:d0 + 4, ec * 128:(ec + 1) * 128], in_=ptu[:, :].rearrange("p (c n) -> p c n", n=128)),
                            reads=[ptk], writes=[(uTk, ec)])
                    else:
                        sc.op("scalar", lambda e, ptu=ptu, uT=uT, d0=d0, ec=ec: e.activation(
                            out=uT[:, d0:d0 + 4, ec * 128:(ec + 1) * 128], in_=ptu[:, :].rearrange("p (c n) -> p c n", n=128), func=AF.Copy),
                            reads=[ptk], writes=[(uTk, ec)])
                units.append(ub_)
        return units

    def units_A(eg):
        g2 = eg % 2
        uT, gT, WT = uTs[g2], gTs[g2], WTs[g2]
        uTk, gTk = "i_uT%d" % g2, "i_gT%d" % g2
        units = []
        for ec in range(GE):
            for tb in range(2):
                st = {}
                for q in range(8):
                    def ua(ec=ec, tb=tb, q=q, st=st):
                        if q == 0:
                            st["i"] = cnt["a"] % 2
                            cnt["a"] += 1
                        ai = st["i"]
                        pA = pAs[ai]
                        pAk = "i_pA%d" % ai
                        ge = ges[ai]
                        gek = "i_ge%d" % ai
                        cs_ = slice(tb * 512, (tb + 1) * 512)
                        for dc in (2 * q, 2 * q + 1):
                            sc.op("tensor", lambda e, pA=pA, dc=dc, cs_=cs_: e.matmul(
                                pA[:, :], lhsT=uT[:, dc, ec * 128:(ec + 1) * 128], rhs=hh[:, dc, cs_], start=(dc == 0), stop=(dc == NDC - 1)),
                                reads=[(uTk, ec), "i_hh"], writes=[pAk], sig=(dc == NDC - 1))
                        if q == 7:
                            sc.op("scalar", lambda e, pA=pA, ge=ge: e.activation(out=ge[:], in_=pA[:, :], func=AF.Gelu), reads=[pAk], writes=[gek])
                            sc.op("vector", lambda e, ge=ge, cs_=cs_: e.tensor_tensor(out=WT[:, ec, cs_], in0=ge[:], in1=gT[:, ec, cs_], op=ALU.mult),
                                  reads=[gek, gTk], writes=[("i_WT", g2, ec, tb)])
                    units.append(ua)
        return units

    def units_O(eg):
        g2 = eg % 2
        g3 = eg % 3
        vb, WT = vbs[g3], WTs[g2]
        units = []
        for tt in range(8):
            for nb in range(4):
                def uo(tt=tt, nb=nb):
                    pO = pOs[cnt["o"] % 4]
                    pOk = "i_pO%d" % (cnt["o"] % 4)
                    cnt["o"] += 1
                    ns_ = slice(nb * 512, (nb + 1) * 512)
                    for ec in range(GE):
                        sc.op("tensor", lambda e, pO=pO, ec=ec, ns_=ns_: e.matmul(
                            pO[:, :], lhsT=WT[:, ec, tt * 128:(tt + 1) * 128], rhs=vb[:, ec, ns_], start=(ec == 0), stop=(ec == GE - 1)),
                            reads=[("i_WT", g2, ec, tt // 4), ("i_vb", g3, ec)], writes=[pOk], sig=(ec == GE - 1))
                    sc.op("vector", lambda e, pO=pO, ns_=ns_: e.tensor_tensor(out=acc[:, tt, ns_], in0=pO[:, :], in1=acc[:, tt, ns_], op=ALU.add),
                          reads=[pOk, ("acc", tt)], writes=[("acc", tt)])
                units.append(uo)
        return units

    def merged(lists):
        lists = [l for l in lists if l]
        pos = [0] * len(lists)
        total = sum(len(l) for l in lists)
        for _ in range(total):
            best, bi = None, None
            for i, l in enumerate(lists):
                if pos[i] < len(l):
                    frac = (pos[i] + 0.5) / len(l)
                    if best is None or frac < best:
                        best, bi = frac, i
            lists[bi][pos[bi]]()
            pos[bi] += 1

    dma_ub(0)
    for u in units_T(0):
        u()
    for eg in range(NG):
        merged([units_T(eg + 1) if eg + 1 < NG else [], units_A(eg), units_O(eg - 1) if eg >= 1 else []])
    for u in units_O(NG - 1):
        u()


def phase_j(k, ph, hp):
    nc, sc, sb, ps = k.nc, k.sc, k.sb, k.ps
    A = k.scr
    acc = k.acc
    tag = "j%d_" % hp
    fnb = sb(tag + "fnb", [128, D], ctx=ph)
    sc.dma("sync", fnb[:], k.final_norm.partition_broadcast(128), writes=[tag + "fnb"])
    x1s = [sb(tag + "x1%d" % i, [128, D], ctx=ph) for i in range(2)]
    junk = sb(tag + "junk", [128, D], ctx=ph)
    ss = sb(tag + "ss", [128, 8], ctx=ph)
    x1v = A["x1"].rearrange("(n p) d -> n p d", p=128)
    ov = k.out.rearrange("(n p) d -> n p d", p=128)
    for tt in range(8):
        ti = hp * 8 + tt
        x1 = x1s[tt % 2]
        xk = tag + "x1%d" % (tt % 2)
        sc.dma("sync", x1[:], x1v[ti], reads=["d_x1"], writes=[xk])
        sc.op("vector", lambda e, tt=tt: e.tensor_tensor(out=acc[:, tt, :], in0=acc[:, tt, :], in1=k.G2[:], op=ALU.mult),
              reads=[("acc", tt), "G1"], writes=[("acc", tt)])
        sc.op("vector", lambda e, tt=tt, x1=x1: e.tensor_tensor(out=x1[:], in0=x1[:], in1=acc[:, tt, :], op=ALU.add),
              reads=[("acc", tt), xk], writes=[xk])
        sc.op("scalar", lambda e, tt=tt, x1=x1: e.activation(out=junk[:], in_=x1[:], func=AF.Square, accum_out=ss[:, tt:tt + 1]),
              reads=[xk], writes=[tag + "junk", (tag + "ss", tt)])
        sc.op("vector", lambda e, tt=tt: e.tensor_scalar(out=ss[:, tt:tt + 1], in0=ss[:, tt:tt + 1], scalar1=1.0 / D, scalar2=EPS,
                                                         op0=ALU.mult, op1=ALU.add), reads=[(tag + "ss", tt)], writes=[(tag + "ss", tt)])
        sc.op("scalar", lambda e, tt=tt: e.activation(out=ss[:, tt:tt + 1], in_=ss[:, tt:tt + 1], func=AF.Sqrt),
              reads=[(tag + "ss", tt)], writes=[(tag + "ss", tt)])
        sc.op("vector", lambda e, tt=tt: e.reciprocal(out=ss[:, tt:tt + 1], in_=ss[:, tt:tt + 1]),
              reads=[(tag + "ss", tt)], writes=[(tag + "ss", tt)])
        sc.op("vector", lambda e, tt=tt, x1=x1: e.scalar_tensor_tensor(out=x1[:], in0=x1[:], scalar=ss[:, tt:tt + 1], in1=fnb[:],
                                                                       op0=ALU.mult, op1=ALU.mult),
              reads=[xk, (tag + "ss", tt), tag + "fnb"], writes=[xk])
        sc.dma("sync", ov[ti], x1[:], reads=[xk], writes=["d_out"])


def make_in_maps(inputs, cores):
    cst = make_consts()
    f = lambda a: np.ascontiguousarray(np.asarray(a), dtype=np.float32)
    shared = {
        "w_ada": f(inputs["w_ada"][0]), "b_ada": f(inputs["b_ada"]), "norm_mix": f(inputs["norm_mix"]),
        "norm_ffn": f(inputs["norm_ffn"]), "w_in": f(inputs["w_in"][0]), "lb_logits": f(inputs["lb_logits"]),
        "hgrn_gain": f(inputs["hgrn_gain"]), "w_up_a": f(inputs["w_up_a"][0]), "w_up_b": f(inputs["w_up_b"][0]),
        "w_out": f(inputs["w_out"][0]), "peer_w_q": f(inputs["peer_w_q"][0]), "peer_keys": f(inputs["peer_keys"][0]),
        "peer_u": f(inputs["peer_u"][0]), "peer_v": f(inputs["peer_v"][0]), "final_norm": f(inputs["final_norm"]),
    }
    shared.update(cst)
    maps = []
    for b in cores:
        m = dict(shared)
        m["x"] = f(inputs["x"][b])
        m["c"] = f(inputs["c"][b:b + 1])
        maps.append(m)
    return maps


def kernel(**inputs):
    nc = build_nc(stage=99)
    cores = list(range(NCORES))
    in_maps = make_in_maps(inputs, cores)
    res = run_bass_kernel_spmd(nc, in_maps, core_ids=cores)
    return np.stack([np.asarray(r["out"], dtype=np.float32) for r in res.results], axis=0)
```

```python
import numpy as np
import concourse.bass as bass
import concourse.mybir as mybir
from concourse.bass_utils import run_bass_kernel_spmd
from contextlib import ExitStack

F32 = mybir.dt.float32
BF16 = mybir.dt.bfloat16
AF = mybir.ActivationFunctionType
ALU = mybir.AluOpType
AX = mybir.AxisListType

COMPUTE = ("tensor", "vector", "scalar", "gpsimd")
QUEUES = ("sync",)
ALL_ENG = COMPUTE + QUEUES


class Sched:
    def __init__(self, nc):
        self.nc = nc
        self.sem = {e: nc.alloc_semaphore(name="pg_" + e) for e in COMPUTE}
        self.cnt = {e: 0 for e in COMPUTE}
        self.streams = {e: [] for e in ALL_ENG}
        self.waited = {e: {} for e in ALL_ENG}
        self.lastw = {}
        self.readers = {}
        self.dsem = {}
        self.semobj = {}

    def _deps(self, eng, reads, writes):
        waits = {}

        def need(sv):
            s, v = sv
            sid = id(s)
            self.semobj[sid] = s
            if v > waits.get(sid, 0):
                waits[sid] = v

        for k in reads:
            if k in self.lastw:
                need(self.lastw[k])
        for k in writes:
            if k in self.lastw:
                need(self.lastw[k])
            for sv in self.readers.get(k, ()):
                need(sv)
        out = []
        wd = self.waited[eng]
        for sid, v in waits.items():
            if wd.get(sid, 0) < v:
                wd[sid] = v
                out.append((self.semobj[sid], v))
        return out

    def _commit(self, my, reads, writes):
        for k in writes:
            self.lastw[k] = my
            self.readers[k] = []
        for k in reads:
            if k in writes:
                continue
            self.readers.setdefault(k, []).append(my)

    def begin_capture(self):
        self._cap = []

    def end_capture(self):
        c = self._cap
        self._cap = None
        return c

    def op(self, eng, fn, reads=(), writes=(), sig=True):
        if getattr(self, "_cap", None) is not None:
            self._cap.append(lambda: self._op(eng, fn, reads, writes, sig))
            return
        self._op(eng, fn, reads, writes, sig)

    def dma(self, q, out, in_, reads=(), writes=(), **kw):
        if getattr(self, "_cap", None) is not None:
            self._cap.append(lambda: self._dma(q, out, in_, reads, writes, **kw))
            return
        self._dma(q, out, in_, reads, writes, **kw)

    def _op(self, eng, fn, reads=(), writes=(), sig=True):
        waits = self._deps(eng, reads, writes)
        if eng == "tensor":
            waits = [(s_, v_) for (s_, v_) in waits if s_ is not self.sem["tensor"]]
        if sig:
            self.cnt[eng] += 1
            my = (self.sem[eng], self.cnt[eng])
            inc = (self.sem[eng], 1)
        else:
            assert eng == "tensor"
            my = (self.sem[eng], self.cnt[eng] + 1)
            inc = None
        self._commit(my, reads, writes)
        self.streams[eng].append((waits, fn, inc))

    def _dma(self, q, out, in_, reads=(), writes=(), **kw):
        waits = self._deps(q, reads, writes)
        key = writes[0]
        if key not in self.dsem:
            self.dsem[key] = [self.nc.alloc_semaphore(name="d%d" % len(self.dsem)), 0]
        ent = self.dsem[key]
        ent[1] += 16
        my = (ent[0], ent[1])
        self._commit(my, reads, writes)

        def fn(e):
            return e.dma_start(out=out, in_=in_, **kw)

        self.streams[q].append((waits, fn, (ent[0], 16)))

    def drain_dmas(self, q="sync"):
        waits = []
        wd = self.waited[q]
        for key, (s, v) in self.dsem.items():
            if v > 0 and wd.get(id(s), 0) < v:
                wd[id(s)] = v
                waits.append((s, v))
        if waits:
            self.streams[q].append((waits, None, None))

    def flush(self, block):
        nc = self.nc
        streams = self.streams
        self.streams = {e: [] for e in ALL_ENG}

        def mk(name):
            lst = streams[name]

            def body(e):
                for waits, fn, inc in lst:
                    for s, v in waits:
                        e.wait_ge(s, v)
                    if fn is not None:
                        inst = fn(e)
                        if inc is not None:
                            inst.then_inc(inc[0], inc[1])

            return body

        for name in ALL_ENG:
            if streams[name]:
                getattr(block, name)(mk(name))

D = 2048
S = 2048
NDC = 16
NTT = 16
IN_W = 12368
EPS = 1e-6
NCORES = 8


def make_consts():
    cst = {}
    cst["ident"] = np.eye(128, dtype=np.float32)
    cst["ones"] = np.ones((128, 128), dtype=np.float32)
    cst["tri"] = np.triu(np.ones((64, 64), dtype=np.float32))
    return cst


class K:
    pass


def merged_units(lists):
    lists = [l for l in lists if l]
    pos = [0] * len(lists)
    total = sum(len(l) for l in lists)
    for _ in range(total):
        best, bi = None, None
        for i, l in enumerate(lists):
            if pos[i] < len(l):
                frac = (pos[i] + 0.5) / len(l)
                if best is None or frac < best:
                    best, bi = frac, i
        lists[bi][pos[bi]]()
        pos[bi] += 1


def build_nc(stage=99, dbg=None):
    nc = bass.Bass("TRN2", target_bir_lowering=False)
    k = K()
    k.nc = nc
    k.stage = stage

    def din(name, shape, dtype=F32):
        return nc.dram_tensor(name, list(shape), dtype, kind="ExternalInput").ap()

    k.x = din("x", [S, D])
    k.c = din("c", [1, D])
    k.w_ada = din("w_ada", [D, 6 * D])
    k.b_ada = din("b_ada", [1, 6 * D])
    k.norm_mix = din("norm_mix", [1, D])
    k.norm_ffn = din("norm_ffn", [1, D])
    k.w_in = din("w_in", [D, IN_W])
    k.ident_d = din("ident", [128, 128])
    k.tri_d = din("tri", [64, 64])
    k.lb_logits = din("lb_logits", [2, 1024])
    k.hgrn_gain = din("hgrn_gain", [1, 128])
    k.w_up_a = din("w_up_a", [1024, D])
    k.w_up_b = din("w_up_b", [1024, D])
    k.w_out = din("w_out", [D, D])
    k.peer_w_q = din("peer_w_q", [D, D])
    k.peer_keys = din("peer_keys", [8, 2, 128, 128])
    k.peer_u = din("peer_u", [16384, D])
    k.peer_v = din("peer_v", [16384, D])
    k.final_norm = din("final_norm", [D])
    k.ones_d = din("ones", [128, 128])
    k.out = nc.dram_tensor("out", [S, D], F32, kind="ExternalOutput").ap()
    if dbg is not None:
        k.dbg = nc.dram_tensor("dbg", list(dbg), F32, kind="ExternalOutput").ap()

    sc = Sched(nc)
    k.sc = sc
    with ExitStack() as top:
        def sb(name, shape, dtype=F32, ctx=top):
            return ctx.enter_context(nc.sbuf_tensor(name, list(shape), dtype))

        def ps(name, shape, dtype=F32, ctx=top):
            return ctx.enter_context(nc.psum_tensor(name, list(shape), dtype))
        k.sb = sb
        k.ps = ps
        k.modT = sb("modT", [128, 96])
        k.G1 = sb("G1", [128, D])
        k.G2 = sb("G2", [128, D])
        k.identf = sb("identf", [128, 128])
        k.onesf = sb("onesf", [128, 128])
        k.identb = sb("identb", [128, 128], BF16)
        k.onesb = sb("onesb", [128, 128], BF16)

        def run_phase(fn):
            with ExitStack() as ph:
                fn(k, ph)
                sc.drain_dmas()
                with nc.Block() as block:
                    sc.flush(block)

        make_scratch(k)
        run_phase(phase_a)
        with ExitStack() as s1:
            k.hT = sb("hT", [128, NDC, S], BF16, ctx=s1)
            if stage >= 2:
                run_phase(phase_b)
            if stage >= 3:
                run_phase(phase_c)
            if dbg is not None and stage in (2, 3):
                run_phase(phase_dbg)
        if stage >= 4:
            run_phase(phase_d)
        if stage >= 5:
            run_phase(phase_e)
        if stage >= 6:
            with ExitStack() as s2:
                k.hT = sb("hT2", [128, NDC, S], BF16, ctx=s2)
                run_phase(phase_f)
                run_phase(phase_f2)
        if stage >= 7:
            with ExitStack() as s3:
                k.hT = sb("hT3", [128, NDC, S], BF16, ctx=s3)
                run_phase(phase_g)
                run_phase(phase_h1)
            run_phase(phase_h2)
        if stage >= 8:
            with ExitStack() as s4:
                k.acc = sb("acc", [128, 8, D], ctx=s4)
                for hp in range(2):
                    run_phase(lambda k_, ph_, hp=hp: phase_i(k_, ph_, hp))
                    run_phase(lambda k_, ph_, hp=hp: phase_j(k_, ph_, hp))
        if dbg is not None and stage in (2, 3):
            return nc
        if dbg is not None:
            run_phase(phase_dbg)
    return nc


def phase_a(k, ph):
    nc, sc, sb, ps = k.nc, k.sc, k.sb, k.ps
    sc.dma("sync", k.identf[:], k.ident_d, writes=["identf"])
    sc.dma("sync", k.onesf[:], k.ones_d, writes=["onesf"])
    sc.op("vector", lambda e: e.tensor_copy(out=k.identb[:], in_=k.identf[:]), reads=["identf"], writes=["identb"])
    sc.op("vector", lambda e: e.tensor_copy(out=k.onesb[:], in_=k.onesf[:]), reads=["onesf"], writes=["onesb"])
    cs = sb("cs", [128, 16], ctx=ph)
    sc.dma("sync", cs[:], k.c.rearrange("o (p j) -> (o p) j", p=128), writes=["cs"])
    sc.op("scalar", lambda e: e.activation(out=cs[:], in_=cs[:], func=AF.Silu), reads=["cs"], writes=["cs"])
    wv = k.w_ada.rearrange("(p j) n -> p j n", p=128)
    NB = 24
    wts = [sb("wada%d" % i, [128, 16, 512], ctx=ph) for i in range(2)]
    brs = [sb("brow%d" % i, [1, 512], ctx=ph) for i in range(2)]
    mrs = [sb("mrow%d" % i, [1, 512], ctx=ph) for i in range(2)]
    pss = [ps("pa%d" % i, [128, 512], ctx=ph) for i in range(2)]
    pbs = [ps("pbc%d" % i, [128, 512], ctx=ph) for i in range(2)]
    pc = ps("pcol", [128, 96], ctx=ph)
    for nb in range(NB):
        i2 = nb % 2
        wt, br, mr, pt, pb = wts[i2], brs[i2], mrs[i2], pss[i2], pbs[i2]
        wk, bk, mk, pk, pbk = "wada%d" % i2, "brow%d" % i2, "mrow%d" % i2, "pa%d" % i2, "pbc%d" % i2
        q = "sync" if nb % 2 == 0 else "gpsimd"
        sc.dma(q, wt[:], wv[:, :, nb * 512:(nb + 1) * 512], writes=[wk])
        sc.dma("sync", br[:], k.b_ada[:, nb * 512:(nb + 1) * 512], writes=[bk])
        for j in range(16):
            sc.op("tensor", lambda e, j=j, wt=wt, pt=pt: e.matmul(pt[0:1, :], lhsT=cs[:, j:j + 1], rhs=wt[:, j, :],
                                                             start=(j == 0), stop=(j == 15)),
                  reads=["cs", wk], writes=[pk], sig=(j == 15))
        sc.op("vector", lambda e, pt=pt, mr=mr, br=br: e.tensor_tensor(out=mr[0:1, :], in0=pt[0:1, :], in1=br[0:1, :], op=ALU.add),
              reads=[pk, bk], writes=[mk])
        for c4 in range(4):
            ch = nb * 4 + c4
            sc.op("tensor", lambda e, ch=ch, c4=c4, mr=mr: e.matmul(pc[:, ch:ch + 1], lhsT=mr[0:1, c4 * 128:(c4 + 1) * 128],
                                                                   rhs=k.onesf[0:1, 0:1], start=True, stop=True),
                  reads=[mk, "onesf"], writes=["pcol"])
        for gi, (G, off) in enumerate(((k.G1, 2 * D), (k.G2, 5 * D))):
            if off <= nb * 512 < off + D:
                o = nb * 512 - off
                sc.op("tensor", lambda e, pb=pb, mr=mr: e.matmul(pb[:, :], lhsT=k.onesf[0:1, :], rhs=mr[0:1, :],
                                                                 start=True, stop=True),
                      reads=[mk, "onesf"], writes=[pbk])
                sc.op("vector", lambda e, pb=pb, G=G, o=o: e.tensor_copy(out=G[:, o:o + 512], in_=pb[:, :]),
                      reads=[pbk], writes=["G%d" % gi])
    sc.op("vector", lambda e: e.tensor_copy(out=k.modT[:], in_=pc[:]), reads=["pcol"], writes=["modT"])


def phase_b(k, ph):
    norm_modulate(k, ph, k.x, k.norm_mix, 0, 16, [])


def norm_modulate(k, ph, src, gain_d, sh_col, sc_col, src_reads, tag="g1"):
    nc, sc, sb, ps = k.nc, k.sc, k.sb, k.ps
    gT = sb(tag + "gT", [128, NDC], ctx=ph)
    with nc.allow_non_contiguous_dma(reason="tiny gain vector"):
        pass
    sc.dma("sync", gT[:], gain_d.rearrange("o (j p) -> (o p) j", p=128), writes=["gT"], allow_slow_non_contiguous=True)
    A1 = sb(tag + "A1", [128, NDC], ctx=ph)
    sc.op("vector", lambda e: e.scalar_tensor_tensor(out=A1[:], in0=k.modT[:, sc_col:sc_col + 16], scalar=1.0, in1=gT[:],
                                                     op0=ALU.add, op1=ALU.mult),
          reads=["modT", "gT"], writes=["A1"])
    xts = [sb(tag + "xt%d" % i, [128, D], ctx=ph) for i in range(8)]
    sq = sb(tag + "sqjunk", [128, D], ctx=ph)
    ss = sb(tag + "ss", [128, 16], ctx=ph)
    pts = [ps(tag + "pb%d" % i, [128, 512], ctx=ph) for i in range(4)]
    xv = src.rearrange("(n p) d -> n p d", p=128)
    for tg in range(4):
        for tt in range(4):
            ti = tg * 4 + tt
            bi = ti % 8
            xt = xts[bi]
            xk = "xt%d" % bi
            sc.dma("sync" if ti % 2 == 0 else "gpsimd", xt[:], xv[ti], reads=list(src_reads), writes=[xk])
            sc.op("scalar", lambda e, xt=xt, ti=ti: e.activation(out=sq[:], in_=xt[:], func=AF.Square,
                                                                 accum_out=ss[:, ti:ti + 1]),
                  reads=[xk], writes=["sq", ("ss", ti)])
            sc.op("vector", lambda e, ti=ti: e.tensor_scalar(out=ss[:, ti:ti + 1], in0=ss[:, ti:ti + 1], scalar1=1.0 / D,
                                                             scalar2=EPS, op0=ALU.mult, op1=ALU.add),
                  reads=[("ss", ti)], writes=[("ss", ti)])
            sc.op("scalar", lambda e, ti=ti: e.activation(out=ss[:, ti:ti + 1], in_=ss[:, ti:ti + 1], func=AF.Sqrt),
                  reads=[("ss", ti)], writes=[("ss", ti)])
            sc.op("vector", lambda e, ti=ti: e.reciprocal(out=ss[:, ti:ti + 1], in_=ss[:, ti:ti + 1]),
                  reads=[("ss", ti)], writes=[("ss", ti)])
            sc.op("vector", lambda e, xt=xt, ti=ti: e.tensor_scalar(out=xt[:], in0=xt[:], scalar1=ss[:, ti:ti + 1],
                                                                    scalar2=None, op0=ALU.mult),
                  reads=[xk, ("ss", ti)], writes=[xk])
        for dc in range(NDC):
            pt = pts[dc % 4]
            pk = "pb%d" % (dc % 4)
            for tt in range(4):
                ti = tg * 4 + tt
                bi = ti % 8
                sc.op("tensor", lambda e, pt=pt, tt=tt, bi=bi, dc=dc: e.transpose(out=pt[:, tt * 128:(tt + 1) * 128],
                                                                                 in_=xts[bi][:, dc * 128:(dc + 1) * 128],
                                                                                 identity=k.identf[:]),
                      reads=["xt%d" % bi, "identf"], writes=[pk], sig=(tt == 3))
            sc.op("scalar", lambda e, pt=pt, dc=dc, tg=tg: e.activation(out=k.hT[:, dc, tg * 512:(tg + 1) * 512], in_=pt[:, :],
                                                                        func=AF.Identity, scale=A1[:, dc:dc + 1],
                                                                        bias=k.modT[:, sh_col + dc:sh_col + dc + 1]),
                  reads=[pk, "A1", "modT"], writes=[("hT", dc, tg)])


def phase_dbg(k, ph):
    nc, sc, sb, ps = k.nc, k.sc, k.sb, k.ps
    if k.stage == 1:
        sc.dma("sync", k.dbg[:, 0:96], k.modT[:], reads=["modT"], writes=["dbg0"])
        sc.dma("sync", k.dbg[:, 128:128 + D], k.G1[:], reads=["G0"], writes=["dbg1"])
        sc.dma("sync", k.dbg[:, 128 + D:128 + 2 * D], k.G2[:], reads=["G1"], writes=["dbg2"])
    if k.stage == 7:
        tmp = sb("dbgtmp", [128, S], ctx=ph)
        tmh = sb("dbgtmh", [128, S], BF16, ctx=ph)
        for i in range(16):
            sc.dma("sync", tmh[:], k.scr["GT"][i * 128:(i + 1) * 128, :], reads=["d_GT"], writes=["dbgtmh"])
            sc.op("vector", lambda e: e.tensor_copy(out=tmp[:], in_=tmh[:]), reads=["dbgtmh"], writes=["dbgtmp"])
            sc.dma("sync", k.dbg[i * 128:(i + 1) * 128, :], tmp[:], reads=["dbgtmp"], writes=["dbg2"])
    if k.stage == 6:
        sc.dma("sync", k.dbg[0:2048, :], k.scr["x1"], reads=["d_x1"], writes=["dbg0"])
        tmp = sb("dbgtmp", [128, S], ctx=ph)
        tmh = sb("dbgtmh", [128, S], BF16, ctx=ph)
        for nm, off in (("attnT", 2048), ("recT", 3072)):
            for i in range(8):
                sc.dma("sync", tmh[:], k.scr[nm][i * 128:(i + 1) * 128, :], reads=["d_" + nm], writes=["dbgtmh"])
                sc.op("vector", lambda e: e.tensor_copy(out=tmp[:], in_=tmh[:]), reads=["dbgtmh"], writes=["dbgtmp"])
                sc.dma("sync", k.dbg[off + i * 128:off + (i + 1) * 128, :], tmp[:], reads=["dbgtmp"], writes=["dbg2"])
    if k.stage == 5:
        tmp = sb("dbgtmp", [128, S], ctx=ph)
        tmh = sb("dbgtmh", [128, S], BF16, ctx=ph)
        for i in range(8):
            sc.dma("sync", tmh[:], k.scr["recT"][i * 128:(i + 1) * 128, :], reads=["d_recT"], writes=["dbgtmh"])
            sc.op("vector", lambda e: e.tensor_copy(out=tmp[:], in_=tmh[:]), reads=["dbgtmh"], writes=["dbgtmp"])
            sc.dma("sync", k.dbg[i * 128:(i + 1) * 128, :], tmp[:], reads=["dbgtmp"], writes=["dbg2"])
    if k.stage == 4:
        tmp = sb("dbgtmp", [128, S], ctx=ph)
        tmh = sb("dbgtmh", [128, S], BF16, ctx=ph)
        for i in range(8):
            sc.dma("sync", tmh[:], k.scr["attnT"][i * 128:(i + 1) * 128, :], reads=["d_attnT"], writes=["dbgtmh"])
            sc.op("vector", lambda e: e.tensor_copy(out=tmp[:], in_=tmh[:]), reads=["dbgtmh"], writes=["dbgtmp"])
            sc.dma("sync", k.dbg[i * 128:(i + 1) * 128, :], tmp[:], reads=["dbgtmp"], writes=["dbg2"])
    if k.stage == 3:
        sc.dma("sync", k.dbg[0:1024, :], k.scr["qBT"], reads=["d_qBT"], writes=["dbg0"])
        sc.dma("sync", k.dbg[1024:1024 + 2048, 0:16], k.scr["wi"], reads=["d_wi"], writes=["dbg1"])
        tmp = sb("dbgtmp", [128, S], ctx=ph)
        tmh = sb("dbgtmh", [128, S], BF16, ctx=ph)
        for i in range(8):
            sc.dma("sync", tmh[:], k.scr["kAT"][i * 128:(i + 1) * 128, :], reads=["d_kAT"], writes=["dbgtmh"])
            sc.op("vector", lambda e: e.tensor_copy(out=tmp[:], in_=tmh[:]), reads=["dbgtmh"], writes=["dbgtmp"])
            sc.dma("sync", k.dbg[3072 + i * 128:3072 + (i + 1) * 128, :], tmp[:], reads=["dbgtmp"], writes=["dbg2"])
        for i in range(16):
            sc.dma("sync", tmh[:, 0:1024], k.scr["vA"][i * 128:(i + 1) * 128, :], reads=["d_vA"], writes=["dbgtmh"])
            sc.op("vector", lambda e: e.tensor_copy(out=tmp[:, 0:1024], in_=tmh[:, 0:1024]), reads=["dbgtmh"], writes=["dbgtmp"])
            sc.dma("sync", k.dbg[4096 + i * 128:4096 + (i + 1) * 128, 0:1024], tmp[:, 0:1024], reads=["dbgtmp"], writes=["dbg3"])
    if k.stage == 2:
        tmp = sb("dbgtmp", [128, S], ctx=ph)
        for dc in range(NDC):
            sc.op("vector", lambda e, dc=dc: e.tensor_copy(out=tmp[:], in_=k.hT[:, dc, :]),
                  reads=[("hT", dc, tg) for tg in range(4)], writes=["dbgtmp"])
            sc.dma("sync", k.dbg[dc * 128:(dc + 1) * 128, :], tmp[:], reads=["dbgtmp"], writes=["dbg0"])


class ProjRes:
    pass


def proj_setup(k, ph, KC, tag="pj"):
    r = ProjRes()
    sb, ps = k.sb, k.ps
    r.KC = KC
    r.wb = [sb("%s_wb%d" % (tag, i), [128, KC, 512], BF16, ctx=ph) for i in range(2)]
    r.wkey = ["%s_wb%d" % (tag, i) for i in range(2)]
    r.pt = [ps("%s_ps%d" % (tag, i), [128, 512], ctx=ph) for i in range(4)]
    r.pkey = ["%s_ps%d" % (tag, i) for i in range(4)]
    r.sf = [sb("%s_sf%d" % (tag, i), [128, 512], F32, ctx=ph) for i in range(3)]
    r.sh = [sb("%s_sh%d" % (tag, i), [128, 512], BF16, ctx=ph) for i in range(3)]
    r.nblk = 0
    r.npt = 0
    r.nst = 0
    r.tag = tag
    return r


def proj_load_w(k, r, w_ap, cb0, cb1):
    sc = k.sc
    KC = r.KC
    wv = w_ap.rearrange("(kc p) n -> p kc n", p=128)
    i2 = r.nblk % 2
    r.nblk += 1
    w = cb1 - cb0
    sc.dma("gpsimd", r.wb[i2][:, :, 0:w], wv[:, :, cb0:cb1], writes=[r.wkey[i2]])
    return r.wb[i2], r.wkey[i2]


def proj_stage(r, dtype):
    i = r.nst % 3
    r.nst += 1
    if dtype == F32:
        return r.sf[i], "%s_sf%d" % (r.tag, i)
    return r.sh[i], "%s_sh%d" % (r.tag, i)


def proj_psum(r):
    i = r.npt % 4
    r.npt += 1
    return r.pt[i], r.pkey[i]


def proj(k, r, actT, act_keys, w_ap, c0, c1, mode, func, dst, dst_key, dtype):
    sc = k.sc
    KC = r.KC
    cb0 = c0
    while cb0 < c1:
        cb1 = min(c1, cb0 + 512)
        w = cb1 - cb0
        wb, wk = proj_load_w(k, r, w_ap, cb0, cb1)
        if mode == "fm":
            for sub in range((w + 127) // 128):
                cw = min(128, w - sub * 128)
                for tb in range(4):
                    pt, pk = proj_psum(r)
                    for kc in range(KC):
                        sc.op("tensor", lambda e, pt=pt, wb=wb, kc=kc, sub=sub, cw=cw, tb=tb: e.matmul(
                            pt[0:cw, :], lhsT=wb[:, kc, sub * 128: sub * 128 + cw], rhs=actT[:, kc, tb * 512:(tb + 1) * 512],
                            start=(kc == 0), stop=(kc == KC - 1)),
                            reads=[wk] + act_keys(kc, tb), writes=[pk], sig=(kc == KC - 1))
                    st, sk = proj_stage(r, dtype)
                    sc.op("scalar", lambda e, pt=pt, st=st, cw=cw: e.activation(out=st[0:cw, :], in_=pt[0:cw, :], func=func),
                          reads=[pk], writes=[sk])
                    r0 = cb0 - c0 + sub * 128
                    sc.dma("sync", dst[r0:r0 + cw, tb * 512:(tb + 1) * 512], st[0:cw, :], reads=[sk], writes=[dst_key])
        else:
            for ti in range(NTT):
                pt, pk = proj_psum(r)
                for kc in range(KC):
                    sc.op("tensor", lambda e, pt=pt, wb=wb, kc=kc, ti=ti, w=w: e.matmul(
                        pt[:, 0:w], lhsT=actT[:, kc, ti * 128:(ti + 1) * 128], rhs=wb[:, kc, 0:w],
                        start=(kc == 0), stop=(kc == KC - 1)),
                        reads=[wk] + act_keys(kc, ti // 4), writes=[pk], sig=(kc == KC - 1))
                st, sk = proj_stage(r, dtype)
                sc.op("scalar", lambda e, pt=pt, st=st, w=w: e.activation(out=st[:, 0:w], in_=pt[:, 0:w], func=func),
                      reads=[pk], writes=[sk])
                sc.dma("sync", dst[ti * 128:(ti + 1) * 128, cb0 - c0:cb1 - c0], st[:, 0:w], reads=[sk], writes=[dst_key])
        cb0 = cb1


def hT_keys(kc, tb):
    return [("hT", kc, tb)]


PROJ_SPECS = [
    ("qAT", 0, 1024, "fm", "Identity", "bf16"),
    ("kAT", 1024, 2048, "fm", "Identity", "bf16"),
    ("vA", 2048, 3072, "tm", "Identity", "bf16"),
    ("qiT", 3072, 4096, "fm", "Identity", "bf16"),
    ("kiT", 4096, 4160, "fm", "Identity", "bf16"),
    ("wi", 4160, 4176, "tm", "Identity", "f32"),
    ("qBT", 4176, 5200, "fm", "Silu", "f32"),
    ("fBT", 5200, 6224, "fm", "Sigmoid", "f32"),
    ("iB", 6224, 7248, "tm", "Identity", "bf16"),
    ("gBT", 7248, 8272, "fm", "Silu", "bf16"),
    ("sgAT", 8272, 10320, "fm", "Sigmoid", "bf16"),
    ("sgBT", 10320, 12368, "fm", "Sigmoid", "bf16"),
]


def make_scratch(k):
    nc = k.nc
    k.scr = {}
    for name, c0, c1, mode, fn, dtn in PROJ_SPECS:
        dt_ = BF16 if dtn == "bf16" else F32
        shape = [c1 - c0, S] if mode == "fm" else [S, c1 - c0]
        k.scr[name] = nc.dram_tensor("scr_" + name, shape, dt_, kind="Internal").ap()
    k.scr["attnT"] = nc.dram_tensor("scr_attnT", [1024, S], BF16, kind="Internal").ap()
    k.scr["recT"] = nc.dram_tensor("scr_recT", [1024, S], BF16, kind="Internal").ap()
    k.scr["x1"] = nc.dram_tensor("scr_x1", [S, D], F32, kind="Internal").ap()
    k.scr["h2T"] = nc.dram_tensor("scr_h2T", [D, S], BF16, kind="Internal").ap()
    k.scr["pqT"] = nc.dram_tensor("scr_pqT", [D, S], BF16, kind="Internal").ap()
    k.scr["GT"] = nc.dram_tensor("scr_GT", [16384, S], BF16, kind="Internal").ap()
    k.scr["uT"] = nc.dram_tensor("scr_uT", [D, 16384], BF16, kind="Internal").ap()


def phase_c(k, ph):
    r = proj_setup(k, ph, NDC, "pc")
    for name, c0, c1, mode, fn, dtn in PROJ_SPECS:
        dt_ = BF16 if dtn == "bf16" else F32
        proj(k, r, k.hT, hT_keys, k.w_in, c0, c1, mode, getattr(AF, fn), k.scr[name], "d_" + name, dt_)


def phase_d(k, ph):
    nc, sc, sb, ps = k.nc, k.sc, k.sb, k.ps
    A = k.scr
    kT = sb("kT", [128, 8, S], BF16, ctx=ph)
    sc.dma("sync", kT[:], A["kAT"].rearrange("(h p) t -> p h t", p=128), reads=["d_kAT"], writes=["kT"])
    vS = sb("vS", [128, 16, 1024], BF16, ctx=ph)
    sc.dma("sync", vS[:], A["vA"].rearrange("(j p) c -> p j c", p=128), reads=["d_vA"], writes=["vS"])
    kiT2 = sb("kiT2", [128, S], BF16, ctx=ph)
    sc.dma("sync", kiT2[0:64, :], A["kiT"], reads=["d_kiT"], writes=["kiT2a"])
    sc.dma("sync", kiT2[64:128, :], A["kiT"], reads=["d_kiT"], writes=["kiT2b"])
    cm29 = sb("cm29", [128, 1], ctx=ph)
    sc.op("vector", lambda e: e.memset(cm29[:], -1e29), writes=["cm29"])
    qTs = [sb("qT%d" % i, [128, 8, 512], BF16, ctx=ph) for i in range(2)]
    qiT = sb("qiT", [128, 8, 512], BF16, ctx=ph)
    wi = sb("wi", [128, 4, 16], ctx=ph)
    wabs = sb("wabs", [128, 4, 16], ctx=ph)
    wsgn = sb("wsgn", [128, 4, 16], ctx=ph)
    scrs = [sb("scr%d" % i, [128, S], ctx=ph) for i in range(2)]
    work = sb("work", [128, S], ctx=ph)
    mks = [sb("mk%d" % i, [128, S], BF16, ctx=ph) for i in range(2)]
    tmps = [sb("itmp%d" % i, [128, 512], ctx=ph) for i in range(3)]
    m8 = sb("m8", [128, 8], ctx=ph)
    NBIS = 20
    pw2 = sb("pw2", [128, NBIS], ctx=ph)
    for kk_ in range(NBIS):
        sc.op("vector", lambda e, kk_=kk_: e.memset(pw2[:, kk_:kk_ + 1], 2.0 ** -(kk_ + 1)), writes=["pw2"])
    bwk = sb("bwk", [128, NBIS], ctx=ph)
    blo = sb("blo", [128, 1], ctx=ph)
    brg = sb("brg", [128, 1], ctx=ph)
    bmid = sb("bmid", [128, 1], ctx=ph)
    bcnt = sb("bcnt", [128, 1], ctx=ph)
    bg = sb("bg", [128, 1], ctx=ph)
    maskTs = [sb("maskT%d" % i, [128, 16, 512], BF16, ctx=ph) for i in range(2)]
    pes = [sb("pe%d" % i, [128, 512], BF16, ctx=ph) for i in range(3)]
    pms = [sb("pm%d" % i, [128, 512], BF16, ctx=ph) for i in range(3)]
    rds = [sb("rd%d" % i, [128, 512], ctx=ph) for i in range(2)]
    aos = [sb("ao%d" % i, [128, 512], BF16, ctx=ph) for i in range(2)]
    pis = [ps("pi%d" % i, [128, 512], ctx=ph) for i in range(2)]
    ptr = ps("ptr", [128, 512], BF16, ctx=ph)
    pls = [ps("pl%d" % i, [128, 512], ctx=ph) for i in range(2)]
    po = ps("po", [128, 512], ctx=ph)
    pd = ps("pd", [128, 512], ctx=ph)
    att_scale = 128.0 ** -0.5
    NEG = -1e30
    n_i = 0
    n_e = 0
    n_h = 0
    streams = {}
    for tb in range(4):
        sc.begin_capture()
        maskT = maskTs[tb % 2]
        mkey = "maskT%d" % (tb % 2)
        qT = qTs[tb % 2]
        qTk = "qT%d" % (tb % 2)
        sc.dma("sync", qT[:], A["qAT"].rearrange("(h p) t -> p h t", p=128)[:, :, tb * 512:(tb + 1) * 512],
               reads=["d_qAT"], writes=[qTk])
        sc.dma("sync", qiT[:], A["qiT"].rearrange("(h p) t -> p h t", p=128)[:, :, tb * 512:(tb + 1) * 512],
               reads=["d_qiT"], writes=["qiT"])
        sc.dma("sync", wi[:], A["wi"].rearrange("(n p) h -> p n h", p=128)[:, tb * 4:(tb + 1) * 4, :],
               reads=["d_wi"], writes=["wi"])
        sc.op("scalar", lambda e: e.activation(out=wabs[:], in_=wi[:], func=AF.Abs), reads=["wi"], writes=["wabs"])
        sc.op("scalar", lambda e: e.activation(out=wsgn[:], in_=wi[:], func=AF.Sign), reads=["wi"], writes=["wsgn"])
        sc.op("vector", lambda e, maskT=maskT: e.memset(maskT[:], -30000.0), writes=[mkey])
        for tt in range(4):
            i = tb * 4 + tt
            L = 128 * (i + 1)
            scr = scrs[i % 2]
            skey = "scr%d" % (i % 2)
            mk = mks[i % 2]
            mkk = "mk%d" % (i % 2)
            for h in range(16):
                cch = h // 2
                p0 = 64 * (h % 2)
                kik = "kiT2a" if p0 == 0 else "kiT2b"
                for s0 in range(0, L, 512):
                    sw = min(512, L - s0)
                    pi = pis[n_i % 2]
                    pik = "pi%d" % (n_i % 2)
                    tmp = tmps[n_i % 3]
                    tk = "itmp%d" % (n_i % 3)
                    n_i += 1
                    sc.op("tensor", lambda e, pi=pi, p0=p0, cch=cch, tt=tt, s0=s0, sw=sw: e.matmul(
                        pi[:, 0:sw], lhsT=qiT[p0:p0 + 64, cch, tt * 128:(tt + 1) * 128], rhs=kiT2[p0:p0 + 64, s0:s0 + sw],
                        start=True, stop=True), reads=["qiT", kik], writes=[pik])
                    sc.op("scalar", lambda e, pi=pi, tmp=tmp, sw=sw, tt=tt, h=h: e.activation(
                        out=tmp[:, 0:sw], in_=pi[:, 0:sw], func=AF.Relu, scale=wabs[:, tt, h:h + 1]),
                        reads=[pik, "wabs"], writes=[tk])
                    if h == 0:
                        sc.op("vector", lambda e, tmp=tmp, scr=scr, s0=s0, sw=sw, tt=tt, h=h: e.tensor_scalar(
                            out=scr[:, s0:s0 + sw], in0=tmp[:, 0:sw], scalar1=wsgn[:, tt, h:h + 1], scalar2=None, op0=ALU.mult),
                            reads=[tk, "wsgn"], writes=[skey])
                    else:
                        sc.op("vector", lambda e, tmp=tmp, scr=scr, s0=s0, sw=sw, tt=tt, h=h: e.scalar_tensor_tensor(
                            out=scr[:, s0:s0 + sw], in0=tmp[:, 0:sw], scalar=wsgn[:, tt, h:h + 1], in1=scr[:, s0:s0 + sw],
                            op0=ALU.mult, op1=ALU.add), reads=[tk, "wsgn", skey], writes=[skey])
            if i >= 2:
                sc.op("vector", lambda e, scr=scr, L=L: e.tensor_reduce(out=blo[:], in_=scr[:, 0:L], axis=AX.X, op=ALU.min),
                      reads=[skey], writes=["blo"])
            sc.op("vector", lambda e, scr=scr, L=L: e.memset(scr[0:64, L - 64:L], NEG), writes=[skey])
            if i >= 2:
                sc.op("vector", lambda e, scr=scr, L=L: e.max(out=m8[:], in_=scr[:, 0:L]), reads=[skey], writes=["m8"])
                sc.op("vector", lambda e: e.tensor_tensor(out=brg[:], in0=m8[:, 0:1], in1=blo[:], op=ALU.subtract),
                      reads=["m8", "blo"], writes=["brg"])
                sc.op("vector", lambda e: e.tensor_scalar(out=bwk[:], in0=pw2[:], scalar1=brg[:, 0:1], scalar2=None, op0=ALU.mult),
                      reads=["pw2", "brg"], writes=["bwk"])
                sc.op("vector", lambda e: e.tensor_tensor(out=bmid[:], in0=blo[:], in1=bwk[:, 0:1], op=ALU.add),
                      reads=["blo", "bwk"], writes=["bmid"])
                for kk_ in range(NBIS):
                    sc.op("vector", lambda e, scr=scr, L=L: e.tensor_scalar(out=work[:, 0:L], in0=scr[:, 0:L], scalar1=bmid[:, 0:1], scalar2=0.0,
                                                                            op0=ALU.is_ge, op1=ALU.add, accum_out=bcnt[:, 0:1]),
                          reads=[skey, "bmid"], writes=["work", "bcnt"])
                    sc.op("vector", lambda e, kk_=kk_: e.tensor_scalar(out=bg[:], in0=bcnt[:], scalar1=255.5, scalar2=bwk[:, kk_:kk_ + 1],
                                                                       op0=ALU.is_ge, op1=ALU.mult), reads=["bcnt", "bwk"], writes=["bg"])
                    sc.op("vector", lambda e: e.tensor_tensor(out=blo[:], in0=blo[:], in1=bg[:], op=ALU.add),
                          reads=["blo", "bg"], writes=["blo"])
                    if kk_ + 1 < NBIS:
                        sc.op("vector", lambda e, kk_=kk_: e.tensor_tensor(out=bmid[:], in0=blo[:], in1=bwk[:, kk_ + 1:kk_ + 2], op=ALU.add),
                              reads=["blo", "bwk"], writes=["bmid"])
                tau = blo[:, 0:1]
                tauk = "blo"
            else:
                tau = cm29[:, 0:1]
                tauk = "cm29"
            sc.op("vector", lambda e, scr=scr, mk=mk, L=L, tau=tau: e.tensor_scalar(
                out=mk[:, 0:L], in0=scr[:, 0:L], scalar1=tau, scalar2=None, op0=ALU.is_ge),
                reads=[skey, tauk], writes=[mkk])
            for j0 in range(0, i + 1, 4):
                n = min(4, i + 1 - j0)
                for jj in range(n):
                    j = j0 + jj
                    sc.op("tensor", lambda e, jj=jj, j=j, mk=mk: e.transpose(out=ptr[:, jj * 128:(jj + 1) * 128],
                                                                             in_=mk[:, j * 128:(j + 1) * 128], identity=k.identb[:]),
                          reads=[mkk, "identb"], writes=["ptr"], sig=(jj == n - 1))
                sc.op("scalar", lambda e, j0=j0, n=n, tt=tt, maskT=maskT: e.activation(
                    out=maskT[:, j0:j0 + n, tt * 128:(tt + 1) * 128],
                    in_=ptr[:, 0:n * 128].rearrange("p (n t) -> p n t", t=128), func=AF.Identity, scale=30000.0, bias=-30000.0),
                    reads=["ptr"], writes=[mkey])
        streams[("i", tb)] = sc.end_capture()
        sc.begin_capture()
        nj = 4 * (tb + 1)
        units = [(h, j) for h in range(8) for j in range(nj)]
        ubuf = {}

        def emit_lg(u):
            h, j = units[u]
            pl = pls[u % 2]
            plk = "pl%d" % (u % 2)
            sc.op("tensor", lambda e, pl=pl, h=h, j=j, qT=qT: e.matmul(pl[:, :], lhsT=kT[:, h, j * 128:(j + 1) * 128], rhs=qT[:, h, :],
                                                               start=True, stop=False), reads=["kT", qTk], writes=[plk], sig=False)
            sc.op("tensor", lambda e, pl=pl, j=j, maskT=maskT: e.matmul(pl[:, :], lhsT=k.identb[:], rhs=maskT[:, j, :],
                                                                       start=False, stop=True), reads=["identb", mkey], writes=[plk])
            pe = pes[u % 3]
            pek = "pe%d" % (u % 3)
            sc.op("scalar", lambda e, pl=pl, pe=pe: e.activation(out=pe[:], in_=pl[:, :], func=AF.Exp, scale=att_scale),
                  reads=[plk], writes=[pek])

        emit_lg(0)
        for u, (h, j) in enumerate(units):
            if u + 1 < len(units):
                emit_lg(u + 1)
            pm = pes[u % 3]
            pmk = "pe%d" % (u % 3)
            sc.op("tensor", lambda e, pm=pm, h=h, j=j: e.matmul(po[:, :], lhsT=vS[:, j, h * 128:(h + 1) * 128], rhs=pm[:],
                                                               start=(j == 0), stop=(j == nj - 1)), reads=["vS", pmk], writes=["po"], sig=False)
            sc.op("tensor", lambda e, pm=pm, j=j: e.matmul(pd[:, :], lhsT=k.onesb[:], rhs=pm[:],
                                                          start=(j == 0), stop=(j == nj - 1)), reads=["onesb", pmk], writes=["pd"])
            if j == nj - 1:
                rd = rds[n_h % 2]
                rdk = "rd%d" % (n_h % 2)
                ao = aos[n_h % 2]
                aok = "ao%d" % (n_h % 2)
                n_h += 1
                sc.op("vector", lambda e, rd=rd: e.reciprocal(out=rd[:], in_=pd[:, :]), reads=["pd"], writes=[rdk])
                sc.op("vector", lambda e, rd=rd, ao=ao: e.tensor_tensor(out=ao[:], in0=po[:, :], in1=rd[:], op=ALU.mult),
                      reads=["po", rdk], writes=[aok])
                sc.dma("sync", A["attnT"][h * 128:(h + 1) * 128, tb * 512:(tb + 1) * 512], ao[:], reads=[aok], writes=["d_attnT"])
        streams[("a", tb)] = sc.end_capture()
    for u in streams[("i", 0)]:
        u()
    for tb in range(4):
        merged_units([streams[("a", tb)], streams.get(("i", tb + 1), [])])


def phase_e(k, ph):
    nc, sc, sb, ps = k.nc, k.sc, k.sb, k.ps
    A = k.scr
    NCH = 32
    lbl = sb("lbl", [128, 2, 8], ctx=ph)
    sc.dma("sync", lbl[:], k.lb_logits.rearrange("r (h d) -> d r h", d=128), writes=["lbl"], allow_slow_non_contiguous=True)
    lb = sb("lb", [128, 8], ctx=ph)
    oml = sb("oml", [128, 8], ctx=ph)
    sc.op("vector", lambda e: e.tensor_tensor(out=lb[:], in0=lbl[:, 0, :], in1=lbl[:, 1, :], op=ALU.subtract), reads=["lbl"], writes=["lb"])
    sc.op("scalar", lambda e: e.activation(out=lb[:], in_=lb[:], func=AF.Sigmoid), reads=["lb"], writes=["lb"])
    sc.op("vector", lambda e: e.tensor_scalar(out=oml[:], in0=lb[:], scalar1=-1.0, scalar2=1.0, op0=ALU.mult, op1=ALU.add),
          reads=["lb"], writes=["oml"])
    gain = sb("hgain", [128, 1], ctx=ph)
    sc.dma("sync", gain[:], k.hgrn_gain.rearrange("o d -> d o"), writes=["hgain"], allow_slow_non_contiguous=True)
    tri = sb("tri_sb", [64, 64], ctx=ph)
    sc.dma("sync", tri[:], k.tri_d, writes=["tri"])
    rst = sb("rst", [128, S], ctx=ph)
    sc.op("vector", lambda e: e.memset(rst[:], 1.0), writes=["rst"])
    sc.op("vector", lambda e: e.memset(rst[:].rearrange("p (c s) -> p c s", s=64)[:, :, 0:1], 0.0), writes=["rst"])
    qf = sb("qf", [128, S], ctx=ph)
    ff = sb("ff", [128, S], ctx=ph)
    kk = sb("kk", [128, S], ctx=ph)
    lf = sb("lf", [128, S], ctx=ph)
    bb = sb("bb", [128, S], ctx=ph)
    bp = sb("bp", [128, S], ctx=ph)
    tA = sb("tA", [128, S], ctx=ph)
    qhats = [sb("qhat%d" % i, [128, S], BF16, ctx=ph) for i in range(2)]
    qtils = [sb("qtil%d" % i, [128, S], BF16, ctx=ph) for i in range(2)]
    ktils = [sb("ktil%d" % i, [128, S], BF16, ctx=ph) for i in range(2)]
    kendTs = [sb("kendT%d" % i, [128, S], BF16, ctx=ph) for i in range(2)]
    sq = sb("hsq", [128, S], ctx=ph)
    kend = sb("kend", [64, NCH, 128], BF16, ctx=ph)
    vSs = [sb("hvS%d" % i, [64, NCH, 128], BF16, ctx=ph) for i in range(2)]
    atT = sb("atT", [64, NCH, 64], BF16, ctx=ph)
    recT = sb("recT", [128, S], ctx=ph)
    sgbs = [sb("sgb%d" % i, [128, S], BF16, ctx=ph) for i in range(2)]
    ebls = [sb("ebl%d" % i, [128, NCH], ctx=ph) for i in range(2)]
    Sf = sb("Sf", [128, 128], ctx=ph)
    Sb = sb("Sb", [128, 128], BF16, ctx=ph)
    rs = sb("hrs", [128, 512], ctx=ph)
    t1 = sb("ht1", [128, 512], ctx=ph)
    obs = [sb("hob%d" % i, [128, 512], BF16, ctx=ph) for i in range(2)]
    ptk = ps("ptk", [64, 512], BF16, ctx=ph)
    pat = ps("pat", [64, 512], ctx=ph)
    pos = [ps("hpo%d" % i, [128, 512], ctx=ph) for i in range(2)]
    pSs = [ps("hpS%d" % i, [128, 128], ctx=ph) for i in range(2)]
    pn = ps("hpn", [128, 512], ctx=ph)
    v3 = lambda t: t[:].rearrange("p (c s) -> p c s", s=64)
    n_ob = [0]
    Pst, Lst = {}, {}

    def do_head(h):
        hp_ = str(h % 2)
        qhat, qtil, ktil, kendT, vS, sgb, ebl = qhats[h % 2], qtils[h % 2], ktils[h % 2], kendTs[h % 2], vSs[h % 2], sgbs[h % 2], ebls[h % 2]
        rows = slice(h * 128, (h + 1) * 128)
        sc.begin_capture()
        sc.dma("sync", qf[:], A["qBT"][rows, :], reads=["d_qBT"], writes=["qf"])
        sc.dma("sync", ff[:], A["fBT"][rows, :], reads=["d_fBT"], writes=["ff"])
        sc.dma("sync", sgb[:], A["gBT"][rows, :], reads=["d_gBT"], writes=["sgb" + hp_])
        sc.dma("sync", vS[:], A["iB"].rearrange("(c s) n -> s c n", s=64)[:, :, rows], reads=["d_iB"], writes=["hvS" + hp_])
        sc.op("vector", lambda e, h=h: e.tensor_scalar(out=ff[:], in0=ff[:], scalar1=oml[:, h:h + 1], scalar2=lb[:, h:h + 1],
                                                       op0=ALU.mult, op1=ALU.add), reads=["ff", "oml", "lb"], writes=["ff"])
        sc.op("vector", lambda e: e.tensor_scalar(out=kk[:], in0=ff[:], scalar1=-1.0, scalar2=1.0, op0=ALU.mult, op1=ALU.add),
              reads=["ff"], writes=["kk"])
        sc.op("scalar", lambda e: e.activation(out=lf[:], in_=ff[:], func=AF.Ln), reads=["ff"], writes=["lf"])
        sc.op("vector", lambda e: e.tensor_tensor_scan(out=bb[:], data0=rst[:], data1=lf[:], initial=0.0, op0=ALU.mult, op1=ALU.add),
              reads=["rst", "lf"], writes=["bb"])
        sc.op("scalar", lambda e: e.activation(out=tA[:], in_=bb[:], func=AF.Exp), reads=["bb"], writes=["tA"])
        sc.op("vector", lambda e: e.tensor_tensor(out=qhat[:], in0=qf[:], in1=tA[:], op=ALU.mult), reads=["qf", "tA"], writes=["qhat" + hp_])
        sc.op("scalar", lambda e: e.activation(out=ebl[:], in_=v3(bb)[:, :, 63], func=AF.Exp), reads=["bb"], writes=["ebl" + hp_])
        sc.op("vector", lambda e: e.tensor_tensor(out=v3(bp), in0=v3(bb), in1=v3(bb)[:, :, 31:32].to_broadcast([128, NCH, 64]),
                                                  op=ALU.subtract), reads=["bb"], writes=["bp"])
        sc.op("scalar", lambda e: e.activation(out=tA[:], in_=bp[:], func=AF.Exp), reads=["bp"], writes=["tA"])
        sc.op("vector", lambda e: e.tensor_tensor(out=qtil[:], in0=qf[:], in1=tA[:], op=ALU.mult), reads=["qf", "tA"], writes=["qtil" + hp_])
        sc.op("scalar", lambda e: e.activation(out=tA[:], in_=bp[:], func=AF.Exp, scale=-1.0), reads=["bp"], writes=["tA"])
        sc.op("vector", lambda e: e.tensor_tensor(out=ktil[:], in0=kk[:], in1=tA[:], op=ALU.mult), reads=["kk", "tA"], writes=["ktil" + hp_])
        sc.op("vector", lambda e: e.tensor_tensor(out=v3(bp), in0=v3(bb)[:, :, 63:64].to_broadcast([128, NCH, 64]), in1=v3(bb),
                                                  op=ALU.subtract), reads=["bb"], writes=["bp"])
        sc.op("scalar", lambda e: e.activation(out=tA[:], in_=bp[:], func=AF.Exp), reads=["bp"], writes=["tA"])
        sc.op("vector", lambda e: e.tensor_tensor(out=kendT[:], in0=kk[:], in1=tA[:], op=ALU.mult), reads=["kk", "tA"], writes=["kendT" + hp_])
        Pst[h] = sc.end_capture()
        sc.begin_capture()
        for c0 in range(0, NCH, 4):
            for jj in range(4):
                c = c0 + jj
                sc.op("tensor", lambda e, jj=jj, c=c: e.transpose(out=ptk[:, jj * 128:(jj + 1) * 128], in_=kendT[:, c * 64:(c + 1) * 64],
                                                                 identity=k.identb[:]), reads=["kendT" + hp_, "identb"], writes=["ptk"], sig=(jj == 3))
            sc.op("scalar", lambda e, c0=c0: e.activation(out=kend[:, c0:c0 + 4, :], in_=ptk[:, :].rearrange("p (n d) -> p n d", d=128),
                                                          func=AF.Copy), reads=["ptk"], writes=["kend"])
        for c0 in range(0, NCH, 8):
            for jj in range(8):
                c = c0 + jj
                sc.op("tensor", lambda e, jj=jj, c=c: e.matmul(pat[:, jj * 64:(jj + 1) * 64], lhsT=ktil[:, c * 64:(c + 1) * 64],
                                                              rhs=qtil[:, c * 64:(c + 1) * 64], start=True, stop=True),
                      reads=["ktil" + hp_, "qtil" + hp_], writes=["pat"], sig=(jj == 7))
            sc.op("vector", lambda e, c0=c0: e.tensor_tensor(out=atT[:, c0:c0 + 8, :], in0=pat[:, :].rearrange("p (n t) -> p n t", t=64),
                                                             in1=tri[:, :].unsqueeze(1).to_broadcast([64, 8, 64]), op=ALU.mult),
                  reads=["pat", "tri"], writes=["atT"])
        for c in range(NCH):
            po = pos[(c // 8) % 2]
            pok = "hpo%d" % ((c // 8) % 2)
            col = (c % 8) * 64
            if c > 0:
                sc.op("tensor", lambda e, po=po, col=col, c=c: e.matmul(po[:, col:col + 64], lhsT=Sb[:], rhs=qhat[:, c * 64:(c + 1) * 64],
                                                                       start=True, stop=False), reads=["Sb", "qhat" + hp_], writes=[pok], sig=False)
            sc.op("tensor", lambda e, po=po, col=col, c=c: e.matmul(po[:, col:col + 64], lhsT=vS[:, c, :], rhs=atT[:, c, :],
                                                                   start=(c == 0), stop=True), reads=["hvS" + hp_, "atT"], writes=[pok])
            if c < NCH - 1:
                pS = pSs[c % 2]
                pSk = "hpS%d" % (c % 2)
                sc.op("tensor", lambda e, pS=pS, c=c: e.matmul(pS[:, :], lhsT=kend[:, c, :], rhs=vS[:, c, :], start=True, stop=True),
                      reads=["kend", "hvS" + hp_], writes=[pSk])
                if c == 0:
                    sc.op("vector", lambda e, pS=pS: e.tensor_copy(out=Sb[:], in_=pS[:, :]), reads=[pSk], writes=["Sb"])
                    sc.op("vector", lambda e, pS=pS: e.tensor_copy(out=Sf[:], in_=pS[:, :]), reads=[pSk], writes=["Sf"])
                else:
                    sc.op("vector", lambda e, pS=pS, c=c: e.scalar_tensor_tensor(out=Sb[:], in0=Sf[:], scalar=ebl[:, c:c + 1], in1=pS[:, :],
                                                                                op0=ALU.mult, op1=ALU.add),
                          reads=["Sf", "ebl" + hp_, pSk], writes=["Sb"])
                    sc.op("vector", lambda e, pS=pS, c=c: e.scalar_tensor_tensor(out=Sf[:], in0=Sf[:], scalar=ebl[:, c:c + 1], in1=pS[:, :],
                                                                                op0=ALU.mult, op1=ALU.add),
                          reads=["Sf", "ebl" + hp_, pSk], writes=["Sf"])
            if c % 8 == 7:
                sc.op("scalar", lambda e, po=po, c=c: e.activation(out=recT[:, (c - 7) * 64:(c + 1) * 64], in_=po[:, :], func=AF.Copy),
                      reads=[pok], writes=["recT"])
        sc.op("scalar", lambda e: e.activation(out=sq[:], in_=recT[:], func=AF.Square), reads=["recT"], writes=["hsq"])
        for tb in range(4):
            cs_ = slice(tb * 512, (tb + 1) * 512)
            sc.op("tensor", lambda e, cs_=cs_: e.matmul(pn[:, :], lhsT=k.onesf[:], rhs=sq[:, cs_], start=True, stop=True),
                  reads=["onesf", "hsq"], writes=["hpn"])
            sc.op("vector", lambda e: e.tensor_scalar(out=rs[:], in0=pn[:, :], scalar1=1.0 / 128, scalar2=EPS, op0=ALU.mult, op1=ALU.add),
                  reads=["hpn"], writes=["hrs"])
            sc.op("scalar", lambda e: e.activation(out=rs[:], in_=rs[:], func=AF.Sqrt), reads=["hrs"], writes=["hrs"])
            sc.op("vector", lambda e: e.reciprocal(out=rs[:], in_=rs[:]), reads=["hrs"], writes=["hrs"])
            sc.op("vector", lambda e, cs_=cs_: e.scalar_tensor_tensor(out=t1[:], in0=recT[:, cs_], scalar=gain[:, 0:1], in1=rs[:],
                                                                      op0=ALU.mult, op1=ALU.mult), reads=["recT", "hgain", "hrs"], writes=["ht1"])
            ob = obs[n_ob[0] % 2]
            obk = "hob%d" % (n_ob[0] % 2)
            n_ob[0] += 1
            sc.op("vector", lambda e, ob=ob, cs_=cs_: e.tensor_tensor(out=ob[:], in0=t1[:], in1=sgb[:, cs_], op=ALU.mult),
                  reads=["ht1", "sgb" + hp_], writes=[obk])
            sc.dma("sync", A["recT"][rows, cs_], ob[:], reads=[obk], writes=["d_recT"])
        Lst[h] = sc.end_capture()

    for h in range(8):
        do_head(h)
    for u in Pst[0]:
        u()
    for h in range(8):
        merged_units([Lst[h], Pst.get(h + 1, [])])


def phase_f(k, ph):
    nc, sc, sb, ps = k.nc, k.sc, k.sb, k.ps
    A = k.scr
    aT = sb("f_aT", [128, 8, S], BF16, ctx=ph)
    rT = sb("f_rT", [128, 8, S], BF16, ctx=ph)
    sc.dma("sync", aT[:], A["attnT"].rearrange("(h p) t -> p h t", p=128), reads=["d_attnT"], writes=["f_aT"])
    sc.dma("sync", rT[:], A["recT"].rearrange("(h p) t -> p h t", p=128), reads=["d_recT"], writes=["f_rT"])
    mixT = k.hT
    was = [sb("f_wa%d" % i, [128, 8, 512], BF16, ctx=ph) for i in range(2)]
    wbs = [sb("f_wb%d" % i, [128, 8, 512], BF16, ctx=ph) for i in range(2)]
    sgas = [sb("f_sga%d" % i, [128, 512], BF16, ctx=ph) for i in range(2)]
    sgbs = [sb("f_sgb%d" % i, [128, 512], BF16, ctx=ph) for i in range(2)]
    yas = [sb("f_ya%d" % i, [128, 512], ctx=ph) for i in range(2)]
    ybs = [sb("f_yb%d" % i, [128, 512], ctx=ph) for i in range(2)]
    pas = [ps("f_pa%d" % i, [128, 512], ctx=ph) for i in range(2)]
    pbs = [ps("f_pb%d" % i, [128, 512], ctx=ph) for i in range(2)]
    wva = k.w_up_a.rearrange("(kc p) n -> p kc n", p=128)
    wvb = k.w_up_b.rearrange("(kc p) n -> p kc n", p=128)
    n = 0
    for nb in range(4):
        wa, wb = was[nb % 2], wbs[nb % 2]
        wak, wbk = "f_wa%d" % (nb % 2), "f_wb%d" % (nb % 2)
        sc.dma("gpsimd", wa[:], wva[:, :, nb * 512:(nb + 1) * 512], writes=[wak])
        sc.dma("gpsimd", wb[:], wvb[:, :, nb * 512:(nb + 1) * 512], writes=[wbk])
        for sub in range(4):
            nch = nb * 4 + sub
            for tb in range(4):
                i2 = n % 2
                n += 1
                pa, pb = pas[i2], pbs[i2]
                pak, pbk = "f_pa%d" % i2, "f_pb%d" % i2
                sga, sgb = sgas[i2], sgbs[i2]
                ya, yb = yas[i2], ybs[i2]
                cs_ = slice(tb * 512, (tb + 1) * 512)
                sc.dma("sync", sga[:], A["sgAT"][nch * 128:(nch + 1) * 128, cs_], reads=["d_sgAT"], writes=["f_sga%d" % i2])
                sc.dma("sync", sgb[:], A["sgBT"][nch * 128:(nch + 1) * 128, cs_], reads=["d_sgBT"], writes=["f_sgb%d" % i2])
                for kc in range(8):
                    sc.op("tensor", lambda e, pa=pa, wa=wa, kc=kc, sub=sub, cs_=cs_: e.matmul(
                        pa[:, :], lhsT=wa[:, kc, sub * 128:(sub + 1) * 128], rhs=aT[:, kc, cs_], start=(kc == 0), stop=(kc == 7)),
                        reads=[wak, "f_aT"], writes=[pak], sig=(kc == 7))
                for kc in range(8):
                    sc.op("tensor", lambda e, pb=pb, wb=wb, kc=kc, sub=sub, cs_=cs_: e.matmul(
                        pb[:, :], lhsT=wb[:, kc, sub * 128:(sub + 1) * 128], rhs=rT[:, kc, cs_], start=(kc == 0), stop=(kc == 7)),
                        reads=[wbk, "f_rT"], writes=[pbk], sig=(kc == 7))
                sc.op("vector", lambda e, pa=pa, ya=ya, sga=sga: e.tensor_tensor(out=ya[:], in0=pa[:, :], in1=sga[:], op=ALU.mult),
                      reads=[pak, "f_sga%d" % i2], writes=["f_ya%d" % i2])
                sc.op("vector", lambda e, pb=pb, yb=yb, sgb=sgb: e.tensor_tensor(out=yb[:], in0=pb[:, :], in1=sgb[:], op=ALU.mult),
                      reads=[pbk, "f_sgb%d" % i2], writes=["f_yb%d" % i2])
                sc.op("vector", lambda e, ya=ya, yb=yb, nch=nch, cs_=cs_: e.tensor_tensor(out=mixT[:, nch, cs_], in0=ya[:], in1=yb[:], op=ALU.add),
                      reads=["f_ya%d" % i2, "f_yb%d" % i2], writes=[("hT", nch, tb)])


def phase_f2(k, ph):
    nc, sc, sb, ps = k.nc, k.sc, k.sb, k.ps
    A = k.scr
    mixT = k.hT
    r = proj_setup(k, ph, NDC, "fo")
    xss = [sb("f_xs%d" % i, [128, 512], ctx=ph) for i in range(3)]
    tts = [sb("f_tt%d" % i, [128, 512], ctx=ph) for i in range(3)]
    xv = k.x.rearrange("(n p) d -> n p d", p=128)
    x1v = A["x1"].rearrange("(n p) d -> n p d", p=128)
    m = 0
    for nb in range(4):
        cs_ = slice(nb * 512, (nb + 1) * 512)
        wb, wk = proj_load_w(k, r, k.w_out, nb * 512, (nb + 1) * 512)
        for ti in range(NTT):
            pt, pk = proj_psum(r)
            i3 = m % 3
            m += 1
            xs, tt_ = xss[i3], tts[i3]
            sc.dma("sync", xs[:], xv[ti][:, cs_], writes=["f_xs%d" % i3])
            for kc in range(NDC):
                sc.op("tensor", lambda e, pt=pt, wb=wb, kc=kc, ti=ti: e.matmul(
                    pt[:, :], lhsT=mixT[:, kc, ti * 128:(ti + 1) * 128], rhs=wb[:, kc, :], start=(kc == 0), stop=(kc == NDC - 1)),
                    reads=[wk, ("hT", kc, ti // 4)], writes=[pk], sig=(kc == NDC - 1))
            sc.op("vector", lambda e, pt=pt, tt_=tt_, cs_=cs_: e.tensor_tensor(out=tt_[:], in0=pt[:, :], in1=k.G1[:, cs_], op=ALU.mult),
                  reads=[pk, "G0"], writes=["f_tt%d" % i3])
            sc.op("vector", lambda e, tt_=tt_, xs=xs: e.tensor_tensor(out=tt_[:], in0=tt_[:], in1=xs[:], op=ALU.add),
                  reads=["f_tt%d" % i3, "f_xs%d" % i3], writes=["f_tt%d" % i3])
            sc.dma("sync", x1v[ti][:, cs_], tt_[:], reads=["f_tt%d" % i3], writes=["d_x1"])


def phase_g(k, ph):
    sc = k.sc
    norm_modulate(k, ph, k.scr["x1"], k.norm_ffn, 48, 64, ["d_x1"], tag="g2")
    for dc in range(NDC):
        sc.dma("sync", k.scr["h2T"][dc * 128:(dc + 1) * 128, :], k.hT[:, dc, :], reads=[("hT", dc, tg) for tg in range(4)],
               writes=["d_h2T"])


def phase_h1(k, ph):
    r = proj_setup(k, ph, NDC, "ph")
    proj(k, r, k.hT, hT_keys, k.peer_w_q, 0, 2048, "fm", AF.Identity, k.scr["pqT"], "d_pqT", BF16)


def phase_h2(k, ph):
    nc, sc, sb, ps = k.nc, k.sc, k.sb, k.ps
    A = k.scr
    NEG = -1e30
    kl = sb("h_kl", [128, 16, 128], ctx=ph)
    sc.dma("sync", kl[:], k.peer_keys.rearrange("p h n d -> n (p h) d"), writes=["h_kl"])
    keysT = sb("h_keysT", [128, 16, 128], BF16, ctx=ph)
    pss = [ps("h_ps%d" % i, [128, 512], ctx=ph) for i in range(4)]
    for ch in range(16):
        b = ch // 4
        sc.op("tensor", lambda e, ch=ch, b=b: e.transpose(out=pss[b][:, (ch % 4) * 128:(ch % 4 + 1) * 128], in_=kl[:, ch, :],
                                                         identity=k.identf[:]), reads=["h_kl", "identf"], writes=["h_ps%d" % b], sig=(ch % 4 == 3))
    for b in range(4):
        sc.op("vector", lambda e, b=b: e.tensor_copy(out=keysT[:, b * 4:(b + 1) * 4, :],
                                                     in_=pss[b][:, :].rearrange("p (c n) -> p c n", n=128)),
              reads=["h_ps%d" % b], writes=["h_keysT"])
    qts = [sb("h_qt%d" % i, [128, 16, 128], BF16, ctx=ph) for i in range(2)]
    s_sb = sb("h_s", [128, 16, 128], ctx=ph)
    wk = sb("h_wk", [128, 16, 128], ctx=ph)
    tops = sb("h_tops", [128, 16, 16], ctx=ph)
    cand = sb("h_cand", [128, 8, 256], ctx=ph)
    cwk = sb("h_cwk", [128, 8, 256], ctx=ph)
    best = sb("h_best", [128, 8, 16], ctx=ph)
    ez = sb("h_ez", [128, 8, 16], ctx=ph)
    Z = sb("h_Z", [128, 8], ctx=ph)
    bias = sb("h_bias", [128, 8], ctx=ph)
    th = sb("h_th", [128, 8, 16], ctx=ph)
    e1 = sb("h_e1", [128, 8, 16], ctx=ph)
    e1T = sb("h_e1T", [128, 128], ctx=ph)
    e2 = sb("h_e2", [128, 8, 128], ctx=ph)
    Rt = sb("h_R", [128, 128, 128], BF16, ctx=ph)
    Oh = sb("h_O", [128, 64, 128], BF16, ctx=ph)
    RT = sb("h_RT", [128, 128, 128], BF16, ctx=ph)
    OT = sb("h_OT", [128, 64, 128], BF16, ctx=ph)
    gsts = [sb("h_gst%d" % i, [128, 64, 128], BF16, ctx=ph) for i in range(2)]
    ptrs = [ps("h_ptr%d" % i, [128, 512], BF16, ctx=ph) for i in range(2)]
    pgs = [ps("h_pg%d" % i, [128, 512], ctx=ph) for i in range(2)]
    pqv = A["pqT"].rearrange("(c p) t -> p c t", p=128)
    GTv = A["GT"].rearrange("(i j) t -> j i t", j=128)
    s4 = s_sb[:].rearrange("p (a h) n -> p a h n", h=2)
    t4 = tops[:].rearrange("p (a h) n -> p a h n", h=2)
    n_s = 0
    n_g = 0
    n_pg = 0
    for ti in range(NTT):
        qt = qts[ti % 2]
        qk = "h_qt%d" % (ti % 2)
        sc.dma("sync", qt[:], pqv[:, :, ti * 128:(ti + 1) * 128], reads=["d_pqT"], writes=[qk])
        for ch in range(16):
            b = ch // 4
            sc.op("tensor", lambda e, ch=ch, b=b, qt=qt: e.matmul(pss[b][:, (ch % 4) * 128:(ch % 4 + 1) * 128], lhsT=qt[:, ch, :],
                                                                 rhs=keysT[:, ch, :], start=True, stop=True),
                  reads=[qk, "h_keysT"], writes=["h_ps%d" % b], sig=(ch % 4 == 3))
        for b in range(4):
            sc.op("scalar", lambda e, b=b: e.activation(out=s_sb[:, b * 4:(b + 1) * 4, :],
                                                        in_=pss[b][:, :].rearrange("p (c n) -> p c n", n=128), func=AF.Copy),
                  reads=["h_ps%d" % b], writes=["h_s"])
        for ch in range(16):
            sc.op("vector", lambda e, ch=ch: e.max(out=tops[:, ch, 0:8], in_=s_sb[:, ch, :]), reads=["h_s"], writes=["h_tops"])
            sc.op("vector", lambda e, ch=ch: e.match_replace(out=wk[:, ch, :], in_to_replace=tops[:, ch, 0:8], in_values=s_sb[:, ch, :],
                                                             imm_value=NEG), reads=["h_s", "h_tops"], writes=["h_wk"])
            sc.op("vector", lambda e, ch=ch: e.max(out=tops[:, ch, 8:16], in_=wk[:, ch, :]), reads=["h_wk"], writes=["h_tops"])
        sc.op("vector", lambda e: e.tensor_tensor(out=cand[:].rearrange("p a (r c) -> p a r c", c=16),
                                                  in0=t4[:, :, 0, :].unsqueeze(3).to_broadcast([128, 8, 16, 16]),
                                                  in1=t4[:, :, 1, :].unsqueeze(2).to_broadcast([128, 8, 16, 16]), op=ALU.add),
              reads=["h_tops"], writes=["h_cand"])
        for p in range(8):
            sc.op("vector", lambda e, p=p: e.max(out=best[:, p, 0:8], in_=cand[:, p, :]), reads=["h_cand"], writes=["h_best"])
            sc.op("vector", lambda e, p=p: e.match_replace(out=cwk[:, p, :], in_to_replace=best[:, p, 0:8], in_values=cand[:, p, :],
                                                           imm_value=NEG), reads=["h_cand", "h_best"], writes=["h_cwk"])
            sc.op("vector", lambda e, p=p: e.max(out=best[:, p, 8:16], in_=cwk[:, p, :]), reads=["h_cwk"], writes=["h_best"])
        sc.op("vector", lambda e: e.tensor_tensor(out=ez[:], in0=best[:], in1=best[:, :, 0:1].to_broadcast([128, 8, 16]), op=ALU.subtract),
              reads=["h_best"], writes=["h_ez"])
        sc.op("scalar", lambda e: e.activation(out=ez[:], in_=ez[:], func=AF.Exp), reads=["h_ez"], writes=["h_ez"])
        sc.op("vector", lambda e: e.tensor_reduce(out=Z[:], in_=ez[:], axis=AX.X, op=ALU.add), reads=["h_ez"], writes=["h_Z"])
        sc.op("scalar", lambda e: e.activation(out=Z[:], in_=Z[:], func=AF.Ln), reads=["h_Z"], writes=["h_Z"])
        sc.op("vector", lambda e: e.tensor_tensor(out=bias[:], in0=best[:, :, 15], in1=best[:, :, 0], op=ALU.subtract),
              reads=["h_best"], writes=["h_bias"])
        sc.op("vector", lambda e: e.tensor_tensor(out=bias[:], in0=bias[:], in1=Z[:], op=ALU.subtract),
              reads=["h_bias", "h_Z"], writes=["h_bias"])
        sc.op("vector", lambda e: e.tensor_tensor(out=th[:], in0=best[:, :, 15:16].to_broadcast([128, 8, 16]), in1=t4[:, :, 0, :],
                                                  op=ALU.subtract), reads=["h_best", "h_tops"], writes=["h_th"])
        sc.op("scalar", lambda e: e.activation(out=e1[:], in_=th[:], func=AF.Exp, scale=-1.0), reads=["h_th"], writes=["h_e1"])
        sc.op("tensor", lambda e: e.transpose(out=pss[0][:, 0:128], in_=e1[:].rearrange("t p r -> t (p r)"), identity=k.identf[:]),
              reads=["h_e1", "identf"], writes=["h_ps0"])
        sc.op("scalar", lambda e: e.activation(out=e1T[:], in_=pss[0][:, 0:128], func=AF.Copy), reads=["h_ps0"], writes=["h_e1T"])
        for p in range(8):
            sc.op("scalar", lambda e, p=p: e.activation(out=e2[:, p, :], in_=s4[:, p, 1, :], func=AF.Exp, bias=bias[:, p:p + 1]),
                  reads=["h_s", "h_bias"], writes=["h_e2"])
        R4 = Rt[:].rearrange("t j (p r) -> t j p r", r=16)
        sc.op("vector", lambda e: e.tensor_tensor(
            out=R4, in0=s4[:, :, 1, :].rearrange("t p j -> t j p").unsqueeze(3).to_broadcast([128, 128, 8, 16]),
            in1=th[:].unsqueeze(1).to_broadcast([128, 128, 8, 16]), op=ALU.is_ge),
            reads=["h_s", "h_th"], writes=["h_R"])
        sc.op("vector", lambda e: e.tensor_tensor(
            out=R4, in0=R4, in1=e2[:].rearrange("t p j -> t j p").unsqueeze(3).to_broadcast([128, 128, 8, 16]), op=ALU.mult),
            reads=["h_R", "h_e2"], writes=["h_R"])

        def emit_OH(ih):
            O4 = Oh[:].rearrange("t i (p r) -> t i p r", r=16)
            sc.op("vector", lambda e, ih=ih: e.tensor_tensor(
                out=O4, in0=s4[:, :, 0, ih * 64:(ih + 1) * 64].rearrange("t p i -> t i p").unsqueeze(3).to_broadcast([128, 64, 8, 16]),
                in1=t4[:, :, 0, :].unsqueeze(1).to_broadcast([128, 64, 8, 16]), op=ALU.is_equal),
                reads=["h_s", "h_tops"], writes=["h_O"])

        emit_OH(0)
        for j0 in range(0, 128, 4):
            ptr = ptrs[n_pg % 2]
            ptk = "h_ptr%d" % (n_pg % 2)
            n_pg += 1
            for jj in range(4):
                sc.op("tensor", lambda e, ptr=ptr, jj=jj, j0=j0: e.transpose(out=ptr[:, jj * 128:(jj + 1) * 128], in_=Rt[:, j0 + jj, :],
                                                                            identity=k.identb[:]),
                      reads=["h_R", "identb"], writes=[ptk], sig=(jj == 3))
            sc.op("vector", lambda e, ptr=ptr, j0=j0: e.tensor_tensor(
                out=RT[:, j0:j0 + 4, :], in0=ptr[:, :].rearrange("k (j t) -> k j t", t=128),
                in1=e1T[:, :].unsqueeze(1).to_broadcast([128, 4, 128]), op=ALU.mult),
                reads=[ptk, "h_e1T"], writes=["h_RT"])
        for ih in range(2):
            if ih == 1:
                emit_OH(1)
            for i0_ in range(0, 64, 4):
                ptr = ptrs[n_pg % 2]
                ptk = "h_ptr%d" % (n_pg % 2)
                n_pg += 1
                for ii in range(4):
                    sc.op("tensor", lambda e, ptr=ptr, ii=ii, i0_=i0_: e.transpose(out=ptr[:, ii * 128:(ii + 1) * 128], in_=Oh[:, i0_ + ii, :],
                                                                                  identity=k.identb[:]),
                          reads=["h_O", "identb"], writes=[ptk], sig=(ii == 3))
                sc.op("scalar", lambda e, ptr=ptr, i0_=i0_: e.activation(
                    out=OT[:, i0_:i0_ + 4, :], in_=ptr[:, :].rearrange("k (i t) -> k i t", t=128), func=AF.Copy),
                    reads=[ptk], writes=["h_OT"])
            gst = gsts[n_g % 2]
            gk = "h_gst%d" % (n_g % 2)
            n_g += 1
            for t0 in range(0, 128, 8):
                pg = pgs[n_s % 2]
                pgk = "h_pg%d" % (n_s % 2)
                n_s += 1
                for tt_ in range(8):
                    t_ = t0 + tt_
                    sc.op("tensor", lambda e, pg=pg, tt_=tt_, t_=t_: e.matmul(pg[:, :].rearrange("j (i t) -> j i t", t=8)[:, :, tt_],
                                                                             lhsT=RT[:, :, t_], rhs=OT[:, :, t_], start=True, stop=True),
                          reads=["h_RT", "h_OT"], writes=[pgk], sig=(tt_ == 7))
                sc.op("scalar", lambda e, pg=pg, gst=gst, t0=t0: e.activation(
                    out=gst[:, :, t0:t0 + 8], in_=pg[:, :].rearrange("j (i t) -> j i t", t=8), func=AF.Copy),
                    reads=[pgk], writes=[gk])
            sc.dma("sync", GTv[:, ih * 64:(ih + 1) * 64, ti * 128:(ti + 1) * 128], gst[:], reads=[gk], writes=["d_GT"])


def phase_i(k, ph, hp):
    nc, sc = k.nc, k.sc
    sb = lambda name, *a, **kw: k.sb("p%d_" % hp + name, *a, **kw)
    ps = lambda name, *a, **kw: k.ps("p%d_" % hp + name, *a, **kw)
    A = k.scr
    GE = 2
    T0 = hp * 1024
    hh = sb("i_hh", [128, NDC, 1024], BF16, ctx=ph)
    sc.dma("sync", hh[:], A["h2T"].rearrange("(c p) t -> p c t", p=128)[:, :, T0:T0 + 1024], reads=["d_h2T"], writes=["i_hh"])
    acc = k.acc
    for tt in range(8):
        sc.op("vector", lambda e, tt=tt: e.memset(acc[:, tt, :], 0.0), writes=[("acc", tt)])
    ubs = [sb("i_ub%d" % i, [128, D], BF16, ctx=ph) for i in range(4)]
    uTs = [sb("i_uT%d" % i, [128, NDC, GE * 128], BF16, ctx=ph) for i in range(2)]
    vbs = [sb("i_vb%d" % i, [128, GE, D], BF16, ctx=ph) for i in range(3)]
    gTs = [sb("i_gT%d" % i, [128, GE, 1024], BF16, ctx=ph) for i in range(2)]
    WTs = [sb("i_WT%d" % i, [128, GE, 1024], BF16, ctx=ph) for i in range(2)]
    ges = [sb("i_ge%d" % i, [128, 512], BF16, ctx=ph) for i in range(2)]
    ptus = [ps("i_ptu%d" % i, [128, 512], BF16, ctx=ph) for i in range(2)]
    pAs = [ps("i_pA%d" % i, [128, 512], ctx=ph) for i in range(2)]
    pOs = [ps("i_pO%d" % i, [128, 512], ctx=ph) for i in range(4)]
    GTv = A["GT"].rearrange("(c p) t -> p c t", p=128)
    uTv = A["uT"].rearrange("(c p) e -> p c e", p=128)
    cnt = {"u": 0, "t": 0, "a": 0, "o": 0}
    NG = 128 // GE

    def dma_ub(eg):
        for ec in range(GE):
            ch = eg * GE + ec
            sc.dma("gpsimd", ubs[ch % 4][:], k.peer_u[ch * 128:(ch + 1) * 128, :], writes=["i_ub%d" % (ch % 4)])

    def units_T(eg):
        g2 = eg % 2
        g3 = eg % 3
        uT, vb, gT = uTs[g2], vbs[g3], gTs[g2]
        uTk, gTk = "i_uT%d" % g2, "i_gT%d" % g2
        units = []

        def u0():
            sc.dma("sync", gT[:], GTv[:, eg * GE:(eg + 1) * GE, T0:T0 + 1024], reads=["d_GT"], writes=[gTk])
            if hp == 1:
                sc.dma("sync", uT[:], uTv[:, :, eg * GE * 128:(eg + 1) * GE * 128], reads=["d_uT"],
                       writes=[(uTk, ec) for ec in range(GE)])
            elif eg + 1 < NG:
                dma_ub(eg + 1)
            for ec in range(GE):
                e0 = (eg * GE + ec) * 128
                sc.dma("gpsimd", vb[:, ec, :], k.peer_v[e0:e0 + 128, :], writes=[("i_vb", g3, ec)])
        units.append(u0)
        for ec in range(GE if hp == 0 else 0):
            e0 = (eg * GE + ec) * 128
            ubi = (eg * GE + ec) % 4
            ub = ubs[ubi]
            ubk = "i_ub%d" % ubi
            for d0 in range(0, NDC, 4):
                def ub_(ec=ec, e0=e0, ub=ub, ubk=ubk, d0=d0):
                    ptu = ptus[cnt["t"] % 2]
                    ptk = "i_ptu%d" % (cnt["t"] % 2)
                    cnt["t"] += 1
                    for dd in range(4):
                        dc = d0 + dd
                        sc.op("tensor", lambda e, ptu=ptu, dd=dd, dc=dc, ub=ub: e.transpose(out=ptu[:, dd * 128:(dd + 1) * 128],
                                                                                           in_=ub[:, dc * 128:(dc + 1) * 128], identity=k.identb[:]),
                              reads=[ubk, "identb"], writes=[ptk], sig=(dd == 3))
                    if (d0 // 4) % 2 == 0:
                        sc.op("vector", lambda e, ptu=ptu, uT=uT, d0=d0, ec=ec: e.tensor_copy(
                            out=uT[:, d0:d0 + 4, ec * 128:(ec + 1) * 128], in_=ptu[:, :].rearrange("p (c n) -> p c n", n=128)),
                            reads=[ptk], writes=[(uTk, ec)])
                    else:
                        sc.op("scalar", lambda e, ptu=ptu, uT=uT, d0=d0, ec=ec: e.activation(
                            out=uT[:, d0:d0 + 4, ec * 128:(ec + 1) * 128], in_=ptu[:, :].rearrange("p (c n) -> p c n", n=128), func=AF.Copy),
                            reads=[ptk], writes=[(uTk, ec)])
                    if d0 == NDC - 4:
                        sc.dma("sync", uTv[:, :, e0:e0 + 128], uT[:, :, ec * 128:(ec + 1) * 128], reads=[(uTk, ec)], writes=["d_uT"])
                units.append(ub_)
        return units

    def units_A(eg):
        g2 = eg % 2
        uT, gT, WT = uTs[g2], gTs[g2], WTs[g2]
        uTk, gTk = "i_uT%d" % g2, "i_gT%d" % g2
        units = []
        for ec in range(GE):
            for tb in range(2):
                st = {}
                for q in range(8):
                    def ua(ec=ec, tb=tb, q=q, st=st):
                        if q == 0:
                            st["i"] = cnt["a"] % 2
                            cnt["a"] += 1
                        ai = st["i"]
                        pA = pAs[ai]
                        pAk = "i_pA%d" % ai
                        ge = ges[ai]
                        gek = "i_ge%d" % ai
                        cs_ = slice(tb * 512, (tb + 1) * 512)
                        for dc in (2 * q, 2 * q + 1):
                            sc.op("tensor", lambda e, pA=pA, dc=dc, cs_=cs_: e.matmul(
                                pA[:, :], lhsT=uT[:, dc, ec * 128:(ec + 1) * 128], rhs=hh[:, dc, cs_], start=(dc == 0), stop=(dc == NDC - 1)),
                                reads=[(uTk, ec), "i_hh"], writes=[pAk], sig=(dc == NDC - 1))
                        if q == 7:
                            sc.op("scalar", lambda e, pA=pA, ge=ge: e.activation(out=ge[:], in_=pA[:, :], func=AF.Gelu), reads=[pAk], writes=[gek])
                            sc.op("vector", lambda e, ge=ge, cs_=cs_: e.tensor_tensor(out=WT[:, ec, cs_], in0=ge[:], in1=gT[:, ec, cs_], op=ALU.mult),
                                  reads=[gek, gTk], writes=[("i_WT", g2, ec, tb)])
                    units.append(ua)
        return units

    def units_O(eg):
        g2 = eg % 2
        g3 = eg % 3
        vb, WT = vbs[g3], WTs[g2]
        units = []
        for tt in range(8):
            for nb in range(4):
                def uo(tt=tt, nb=nb):
                    pO = pOs[cnt["o"] % 4]
                    pOk = "i_pO%d" % (cnt["o"] % 4)
                    cnt["o"] += 1
                    ns_ = slice(nb * 512, (nb + 1) * 512)
                    for ec in range(GE):
                        sc.op("tensor", lambda e, pO=pO, ec=ec, ns_=ns_: e.matmul(
                            pO[:, :], lhsT=WT[:, ec, tt * 128:(tt + 1) * 128], rhs=vb[:, ec, ns_], start=(ec == 0), stop=(ec == GE - 1)),
                            reads=[("i_WT", g2, ec, tt // 4), ("i_vb", g3, ec)], writes=[pOk], sig=(ec == GE - 1))
                    sc.op("vector", lambda e, pO=pO, ns_=ns_: e.tensor_tensor(out=acc[:, tt, ns_], in0=pO[:, :], in1=acc[:, tt, ns_], op=ALU.add),
                          reads=[pOk, ("acc", tt)], writes=[("acc", tt)])
                units.append(uo)
        return units

    def merged(lists):
        lists = [l for l in lists if l]
        pos = [0] * len(lists)
        total = sum(len(l) for l in lists)
        for _ in range(total):
            best, bi = None, None
            for i, l in enumerate(lists):
                if pos[i] < len(l):
                    frac = (pos[i] + 0.5) / len(l)
                    if best is None or frac < best:
                        best, bi = frac, i
            lists[bi][pos[bi]]()
            pos[bi] += 1

    if hp == 0:
        dma_ub(0)
    for u in units_T(0):
        u()
    for eg in range(NG):
        merged([units_T(eg + 1) if eg + 1 < NG else [], units_A(eg), units_O(eg - 1) if eg >= 1 else []])
    for u in units_O(NG - 1):
        u()


def phase_j(k, ph, hp):
    nc, sc, sb, ps = k.nc, k.sc, k.sb, k.ps
    A = k.scr
    acc = k.acc
    tag = "j%d_" % hp
    fnb = sb(tag + "fnb", [128, D], ctx=ph)
    sc.dma("sync", fnb[:], k.final_norm.partition_broadcast(128), writes=[tag + "fnb"])
    x1s = [sb(tag + "x1%d" % i, [128, D], ctx=ph) for i in range(2)]
    junk = sb(tag + "junk", [128, D], ctx=ph)
    ss = sb(tag + "ss", [128, 8], ctx=ph)
    x1v = A["x1"].rearrange("(n p) d -> n p d", p=128)
    ov = k.out.rearrange("(n p) d -> n p d", p=128)
    for tt in range(8):
        ti = hp * 8 + tt
        x1 = x1s[tt % 2]
        xk = tag + "x1%d" % (tt % 2)
        sc.dma("sync", x1[:], x1v[ti], reads=["d_x1"], writes=[xk])
        sc.op("vector", lambda e, tt=tt: e.tensor_tensor(out=acc[:, tt, :], in0=acc[:, tt, :], in1=k.G2[:], op=ALU.mult),
              reads=[("acc", tt), "G1"], writes=[("acc", tt)])
        sc.op("vector", lambda e, tt=tt, x1=x1: e.tensor_tensor(out=x1[:], in0=x1[:], in1=acc[:, tt, :], op=ALU.add),
              reads=[("acc", tt), xk], writes=[xk])
        sc.op("scalar", lambda e, tt=tt, x1=x1: e.activation(out=junk[:], in_=x1[:], func=AF.Square, accum_out=ss[:, tt:tt + 1]),
              reads=[xk], writes=[tag + "junk", (tag + "ss", tt)])
        sc.op("vector", lambda e, tt=tt: e.tensor_scalar(out=ss[:, tt:tt + 1], in0=ss[:, tt:tt + 1], scalar1=1.0 / D, scalar2=EPS,
                                                         op0=ALU.mult, op1=ALU.add), reads=[(tag + "ss", tt)], writes=[(tag + "ss", tt)])
        sc.op("scalar", lambda e, tt=tt: e.activation(out=ss[:, tt:tt + 1], in_=ss[:, tt:tt + 1], func=AF.Sqrt),
              reads=[(tag + "ss", tt)], writes=[(tag + "ss", tt)])
        sc.op("vector", lambda e, tt=tt: e.reciprocal(out=ss[:, tt:tt + 1], in_=ss[:, tt:tt + 1]),
              reads=[(tag + "ss", tt)], writes=[(tag + "ss", tt)])
        sc.op("vector", lambda e, tt=tt, x1=x1: e.scalar_tensor_tensor(out=x1[:], in0=x1[:], scalar=ss[:, tt:tt + 1], in1=fnb[:],
                                                                       op0=ALU.mult, op1=ALU.mult),
              reads=[xk, (tag + "ss", tt), tag + "fnb"], writes=[xk])
        sc.dma("sync", ov[ti], x1[:], reads=[xk], writes=["d_out"])


def make_in_maps(inputs, cores):
    cst = make_consts()
    f = lambda a: np.ascontiguousarray(np.asarray(a), dtype=np.float32)
    shared = {
        "w_ada": f(inputs["w_ada"][0]), "b_ada": f(inputs["b_ada"]), "norm_mix": f(inputs["norm_mix"]),
        "norm_ffn": f(inputs["norm_ffn"]), "w_in": f(inputs["w_in"][0]), "lb_logits": f(inputs["lb_logits"]),
        "hgrn_gain": f(inputs["hgrn_gain"]), "w_up_a": f(inputs["w_up_a"][0]), "w_up_b": f(inputs["w_up_b"][0]),
        "w_out": f(inputs["w_out"][0]), "peer_w_q": f(inputs["peer_w_q"][0]), "peer_keys": f(inputs["peer_keys"][0]),
        "peer_u": f(inputs["peer_u"][0]), "peer_v": f(inputs["peer_v"][0]), "final_norm": f(inputs["final_norm"]),
    }
    shared.update(cst)
    maps = []
    for b in cores:
        m = dict(shared)
        m["x"] = f(inputs["x"][b])
        m["c"] = f(inputs["c"][b:b + 1])
        maps.append(m)
    return maps


def kernel(**inputs):
    nc = build_nc(stage=99)
    cores = list(range(NCORES))
    in_maps = make_in_maps(inputs, cores)
    res = run_bass_kernel_spmd(nc, in_maps, core_ids=cores)
    return np.stack([np.asarray(r["out"], dtype=np.float32) for r in res.results], axis=0)
```

```python
import numpy as np
import concourse.bass as bass
import concourse.mybir as mybir
from concourse.bass_utils import run_bass_kernel_spmd
from contextlib import ExitStack

F32 = mybir.dt.float32
BF16 = mybir.dt.bfloat16
AF = mybir.ActivationFunctionType
ALU = mybir.AluOpType
AX = mybir.AxisListType

COMPUTE = ("tensor", "vector", "scalar", "gpsimd")
QUEUES = ("sync",)
ALL_ENG = COMPUTE + QUEUES


class Sched:
    def __init__(self, nc):
        self.nc = nc
        self.sem = {e: nc.alloc_semaphore(name="pg_" + e) for e in COMPUTE}
        self.cnt = {e: 0 for e in COMPUTE}
        self.streams = {e: [] for e in ALL_ENG}
        self.waited = {e: {} for e in ALL_ENG}
        self.lastw = {}
        self.readers = {}
        self.dsem = {}
        self.semobj = {}

    def _deps(self, eng, reads, writes):
        waits = {}

        def need(sv):
            s, v = sv
            sid = id(s)
            self.semobj[sid] = s
            if v > waits.get(sid, 0):
                waits[sid] = v

        for k in reads:
            if k in self.lastw:
                need(self.lastw[k])
        for k in writes:
            if k in self.lastw:
                need(self.lastw[k])
            for sv in self.readers.get(k, ()):
                need(sv)
        out = []
        wd = self.waited[eng]
        for sid, v in waits.items():
            if wd.get(sid, 0) < v:
                wd[sid] = v
                out.append((self.semobj[sid], v))
        return out

    def _commit(self, my, reads, writes):
        for k in writes:
            self.lastw[k] = my
            self.readers[k] = []
        for k in reads:
            if k in writes:
                continue
            self.readers.setdefault(k, []).append(my)

    def begin_capture(self):
        self._cap = []

    def end_capture(self):
        c = self._cap
        self._cap = None
        return c

    def op(self, eng, fn, reads=(), writes=(), sig=True):
        if getattr(self, "_cap", None) is not None:
            self._cap.append(lambda: self._op(eng, fn, reads, writes, sig))
            return
        self._op(eng, fn, reads, writes, sig)

    def dma(self, q, out, in_, reads=(), writes=(), **kw):
        if getattr(self, "_cap", None) is not None:
            self._cap.append(lambda: self._dma(q, out, in_, reads, writes, **kw))
            return
        self._dma(q, out, in_, reads, writes, **kw)

    def _op(self, eng, fn, reads=(), writes=(), sig=True):
        waits = self._deps(eng, reads, writes)
        if eng == "tensor":
            waits = [(s_, v_) for (s_, v_) in waits if s_ is not self.sem["tensor"]]
        if sig:
            self.cnt[eng] += 1
            my = (self.sem[eng], self.cnt[eng])
            inc = (self.sem[eng], 1)
        else:
            assert eng == "tensor"
            my = (self.sem[eng], self.cnt[eng] + 1)
            inc = None
        self._commit(my, reads, writes)
        self.streams[eng].append((waits, fn, inc))

    def _dma(self, q, out, in_, reads=(), writes=(), **kw):
        waits = self._deps(q, reads, writes)
        key = writes[0]
        if key not in self.dsem:
            self.dsem[key] = [self.nc.alloc_semaphore(name="d%d" % len(self.dsem)), 0]
        ent = self.dsem[key]
        ent[1] += 16
        my = (ent[0], ent[1])
        self._commit(my, reads, writes)

        def fn(e):
            return e.dma_start(out=out, in_=in_, **kw)

        self.streams[q].append((waits, fn, (ent[0], 16)))

    def drain_dmas(self, q="sync"):
        waits = []
        wd = self.waited[q]
        for key, (s, v) in self.dsem.items():
            if v > 0 and wd.get(id(s), 0) < v:
                wd[id(s)] = v
                waits.append((s, v))
        if waits:
            self.streams[q].append((waits, None, None))

    def flush(self, block):
        nc = self.nc
        streams = self.streams
        self.streams = {e: [] for e in ALL_ENG}

        def mk(name):
            lst = streams[name]

            def body(e):
                for waits, fn, inc in lst:
                    for s, v in waits:
                        e.wait_ge(s, v)
                    if fn is not None:
                        inst = fn(e)
                        if inc is not None:
                            inst.then_inc(inc[0], inc[1])

            return body

        for name in ALL_ENG:
            if streams[name]:
                getattr(block, name)(mk(name))

D = 2048
S = 2048
NDC = 16
NTT = 16
IN_W = 12368
EPS = 1e-6
NCORES = 8


def make_consts():
    cst = {}
    cst["ident"] = np.eye(128, dtype=np.float32)
    cst["ones"] = np.ones((128, 128), dtype=np.float32)
    cst["tri"] = np.triu(np.ones((64, 64), dtype=np.float32))
    return cst


class K:
    pass


def merged_units(lists):
    lists = [l for l in lists if l]
    pos = [0] * len(lists)
    total = sum(len(l) for l in lists)
    for _ in range(total):
        best, bi = None, None
        for i, l in enumerate(lists):
            if pos[i] < len(l):
                frac = (pos[i] + 0.5) / len(l)
                if best is None or frac < best:
                    best, bi = frac, i
        lists[bi][pos[bi]]()
        pos[bi] += 1


def build_nc(stage=99, dbg=None):
    nc = bass.Bass("TRN2", target_bir_lowering=False)
    k = K()
    k.nc = nc
    k.stage = stage

    def din(name, shape, dtype=F32):
        return nc.dram_tensor(name, list(shape), dtype, kind="ExternalInput").ap()

    k.x = din("x", [S, D])
    k.c = din("c", [1, D])
    k.w_ada = din("w_ada", [D, 6 * D])
    k.b_ada = din("b_ada", [1, 6 * D])
    k.norm_mix = din("norm_mix", [1, D])
    k.norm_ffn = din("norm_ffn", [1, D])
    k.w_in = din("w_in", [D, IN_W])
    k.ident_d = din("ident", [128, 128])
    k.tri_d = din("tri", [64, 64])
    k.lb_logits = din("lb_logits", [2, 1024])
    k.hgrn_gain = din("hgrn_gain", [1, 128])
    k.w_up_a = din("w_up_a", [1024, D])
    k.w_up_b = din("w_up_b", [1024, D])
    k.w_out = din("w_out", [D, D])
    k.peer_w_q = din("peer_w_q", [D, D])
    k.peer_keys = din("peer_keys", [8, 2, 128, 128])
    k.peer_u = din("peer_u", [16384, D])
    k.peer_v = din("peer_v", [16384, D])
    k.final_norm = din("final_norm", [D])
    k.ones_d = din("ones", [128, 128])
    k.out = nc.dram_tensor("out", [S, D], F32, kind="ExternalOutput").ap()
    if dbg is not None:
        k.dbg = nc.dram_tensor("dbg", list(dbg), F32, kind="ExternalOutput").ap()

    sc = Sched(nc)
    k.sc = sc
    with ExitStack() as top:
        def sb(name, shape, dtype=F32, ctx=top):
            return ctx.enter_context(nc.sbuf_tensor(name, list(shape), dtype))

        def ps(name, shape, dtype=F32, ctx=top):
            return ctx.enter_context(nc.psum_tensor(name, list(shape), dtype))
        k.sb = sb
        k.ps = ps
        k.modT = sb("modT", [128, 96])
        k.G1 = sb("G1", [128, D])
        k.G2 = sb("G2", [128, D])
        k.identf = sb("identf", [128, 128])
        k.onesf = sb("onesf", [128, 128])
        k.identb = sb("identb", [128, 128], BF16)
        k.onesb = sb("onesb", [128, 128], BF16)

        def run_phase(fn):
            with ExitStack() as ph:
                fn(k, ph)
                sc.drain_dmas()
                with nc.Block() as block:
                    sc.flush(block)

        make_scratch(k)
        run_phase(phase_a)
        with ExitStack() as s1:
            k.hT = sb("hT", [128, NDC, S], BF16, ctx=s1)
            if stage >= 2:
                run_phase(phase_b)
            if stage >= 3:
                run_phase(phase_c)
            if dbg is not None and stage in (2, 3):
                run_phase(phase_dbg)
        if stage >= 4:
            run_phase(phase_d)
        if stage >= 5:
            run_phase(phase_e)
        if stage >= 6:
            with ExitStack() as s2:
                k.hT = sb("hT2", [128, NDC, S], BF16, ctx=s2)
                run_phase(phase_f)
                run_phase(phase_f2)
        if stage >= 7:
            with ExitStack() as s3:
                k.hT = sb("hT3", [128, NDC, S], BF16, ctx=s3)
                run_phase(phase_g)
                run_phase(phase_h1)
            run_phase(phase_h2)
        if stage >= 8:
            with ExitStack() as s4:
                k.acc = sb("acc", [128, 8, D], ctx=s4)
                for hp in range(2):
                    run_phase(lambda k_, ph_, hp=hp: phase_i(k_, ph_, hp))
                    run_phase(lambda k_, ph_, hp=hp: phase_j(k_, ph_, hp))
        if dbg is not None and stage in (2, 3):
            return nc
        if dbg is not None:
            run_phase(phase_dbg)
    return nc


def phase_a(k, ph):
    nc, sc, sb, ps = k.nc, k.sc, k.sb, k.ps
    sc.dma("sync", k.identf[:], k.ident_d, writes=["identf"])
    sc.dma("sync", k.onesf[:], k.ones_d, writes=["onesf"])
    sc.op("vector", lambda e: e.tensor_copy(out=k.identb[:], in_=k.identf[:]), reads=["identf"], writes=["identb"])
    sc.op("vector", lambda e: e.tensor_copy(out=k.onesb[:], in_=k.onesf[:]), reads=["onesf"], writes=["onesb"])
    cs = sb("cs", [128, 16], ctx=ph)
    sc.dma("sync", cs[:], k.c.rearrange("o (p j) -> (o p) j", p=128), writes=["cs"])
    sc.op("scalar", lambda e: e.activation(out=cs[:], in_=cs[:], func=AF.Silu), reads=["cs"], writes=["cs"])
    wv = k.w_ada.rearrange("(p j) n -> p j n", p=128)
    NB = 24
    wts = [sb("wada%d" % i, [128, 16, 512], ctx=ph) for i in range(2)]
    brs = [sb("brow%d" % i, [1, 512], ctx=ph) for i in range(2)]
    mrs = [sb("mrow%d" % i, [1, 512], ctx=ph) for i in range(2)]
    pss = [ps("pa%d" % i, [128, 512], ctx=ph) for i in range(2)]
    pbs = [ps("pbc%d" % i, [128, 512], ctx=ph) for i in range(2)]
    pc = ps("pcol", [128, 96], ctx=ph)
    for nb in range(NB):
        i2 = nb % 2
        wt, br, mr, pt, pb = wts[i2], brs[i2], mrs[i2], pss[i2], pbs[i2]
        wk, bk, mk, pk, pbk = "wada%d" % i2, "brow%d" % i2, "mrow%d" % i2, "pa%d" % i2, "pbc%d" % i2
        q = "sync" if nb % 2 == 0 else "gpsimd"
        sc.dma(q, wt[:], wv[:, :, nb * 512:(nb + 1) * 512], writes=[wk])
        sc.dma("sync", br[:], k.b_ada[:, nb * 512:(nb + 1) * 512], writes=[bk])
        for j in range(16):
            sc.op("tensor", lambda e, j=j, wt=wt, pt=pt: e.matmul(pt[0:1, :], lhsT=cs[:, j:j + 1], rhs=wt[:, j, :],
                                                             start=(j == 0), stop=(j == 15)),
                  reads=["cs", wk], writes=[pk], sig=(j == 15))
        sc.op("vector", lambda e, pt=pt, mr=mr, br=br: e.tensor_tensor(out=mr[0:1, :], in0=pt[0:1, :], in1=br[0:1, :], op=ALU.add),
              reads=[pk, bk], writes=[mk])
        for c4 in range(4):
            ch = nb * 4 + c4
            sc.op("tensor", lambda e, ch=ch, c4=c4, mr=mr: e.matmul(pc[:, ch:ch + 1], lhsT=mr[0:1, c4 * 128:(c4 + 1) * 128],
                                                                   rhs=k.onesf[0:1, 0:1], start=True, stop=True),
                  reads=[mk, "onesf"], writes=["pcol"])
        for gi, (G, off) in enumerate(((k.G1, 2 * D), (k.G2, 5 * D))):
            if off <= nb * 512 < off + D:
                o = nb * 512 - off
                sc.op("tensor", lambda e, pb=pb, mr=mr: e.matmul(pb[:, :], lhsT=k.onesf[0:1, :], rhs=mr[0:1, :],
                                                                 start=True, stop=True),
                      reads=[mk, "onesf"], writes=[pbk])
                sc.op("vector", lambda e, pb=pb, G=G, o=o: e.tensor_copy(out=G[:, o:o + 512], in_=pb[:, :]),
                      reads=[pbk], writes=["G%d" % gi])
    sc.op("vector", lambda e: e.tensor_copy(out=k.modT[:], in_=pc[:]), reads=["pcol"], writes=["modT"])


def phase_b(k, ph):
    norm_modulate(k, ph, k.x, k.norm_mix, 0, 16, [])


def norm_modulate(k, ph, src, gain_d, sh_col, sc_col, src_reads, tag="g1"):
    nc, sc, sb, ps = k.nc, k.sc, k.sb, k.ps
    gT = sb(tag + "gT", [128, NDC], ctx=ph)
    with nc.allow_non_contiguous_dma(reason="tiny gain vector"):
        pass
    sc.dma("sync", gT[:], gain_d.rearrange("o (j p) -> (o p) j", p=128), writes=["gT"], allow_slow_non_contiguous=True)
    A1 = sb(tag + "A1", [128, NDC], ctx=ph)
    sc.op("vector", lambda e: e.scalar_tensor_tensor(out=A1[:], in0=k.modT[:, sc_col:sc_col + 16], scalar=1.0, in1=gT[:],
                                                     op0=ALU.add, op1=ALU.mult),
          reads=["modT", "gT"], writes=["A1"])
    xts = [sb(tag + "xt%d" % i, [128, D], ctx=ph) for i in range(8)]
    sq = sb(tag + "sqjunk", [128, D], ctx=ph)
    ss = sb(tag + "ss", [128, 16], ctx=ph)
    pts = [ps(tag + "pb%d" % i, [128, 512], ctx=ph) for i in range(4)]
    xv = src.rearrange("(n p) d -> n p d", p=128)
    for tg in range(4):
        for tt in range(4):
            ti = tg * 4 + tt
            bi = ti % 8
            xt = xts[bi]
            xk = "xt%d" % bi
            sc.dma("sync" if ti % 2 == 0 else "gpsimd", xt[:], xv[ti], reads=list(src_reads), writes=[xk])
            sc.op("scalar", lambda e, xt=xt, ti=ti: e.activation(out=sq[:], in_=xt[:], func=AF.Square,
                                                                 accum_out=ss[:, ti:ti + 1]),
                  reads=[xk], writes=["sq", ("ss", ti)])
            sc.op("vector", lambda e, ti=ti: e.tensor_scalar(out=ss[:, ti:ti + 1], in0=ss[:, ti:ti + 1], scalar1=1.0 / D,
                                                             scalar2=EPS, op0=ALU.mult, op1=ALU.add),
                  reads=[("ss", ti)], writes=[("ss", ti)])
            sc.op("scalar", lambda e, ti=ti: e.activation(out=ss[:, ti:ti + 1], in_=ss[:, ti:ti + 1], func=AF.Sqrt),
                  reads=[("ss", ti)], writes=[("ss", ti)])
            sc.op("vector", lambda e, ti=ti: e.reciprocal(out=ss[:, ti:ti + 1], in_=ss[:, ti:ti + 1]),
                  reads=[("ss", ti)], writes=[("ss", ti)])
            sc.op("vector", lambda e, xt=xt, ti=ti: e.tensor_scalar(out=xt[:], in0=xt[:], scalar1=ss[:, ti:ti + 1],
                                                                    scalar2=None, op0=ALU.mult),
                  reads=[xk, ("ss", ti)], writes=[xk])
        for dc in range(NDC):
            pt = pts[dc % 4]
            pk = "pb%d" % (dc % 4)
            for tt in range(4):
                ti = tg * 4 + tt
                bi = ti % 8
                sc.op("tensor", lambda e, pt=pt, tt=tt, bi=bi, dc=dc: e.transpose(out=pt[:, tt * 128:(tt + 1) * 128],
                                                                                 in_=xts[bi][:, dc * 128:(dc + 1) * 128],
                                                                                 identity=k.identf[:]),
                      reads=["xt%d" % bi, "identf"], writes=[pk], sig=(tt == 3))
            sc.op("scalar", lambda e, pt=pt, dc=dc, tg=tg: e.activation(out=k.hT[:, dc, tg * 512:(tg + 1) * 512], in_=pt[:, :],
                                                                        func=AF.Identity, scale=A1[:, dc:dc + 1],
                                                                        bias=k.modT[:, sh_col + dc:sh_col + dc + 1]),
                  reads=[pk, "A1", "modT"], writes=[("hT", dc, tg)])


def phase_dbg(k, ph):
    nc, sc, sb, ps = k.nc, k.sc, k.sb, k.ps
    if k.stage == 1:
        sc.dma("sync", k.dbg[:, 0:96], k.modT[:], reads=["modT"], writes=["dbg0"])
        sc.dma("sync", k.dbg[:, 128:128 + D], k.G1[:], reads=["G0"], writes=["dbg1"])
        sc.dma("sync", k.dbg[:, 128 + D:128 + 2 * D], k.G2[:], reads=["G1"], writes=["dbg2"])
    if k.stage == 7:
        tmp = sb("dbgtmp", [128, S], ctx=ph)
        tmh = sb("dbgtmh", [128, S], BF16, ctx=ph)
        for i in range(16):
            sc.dma("sync", tmh[:], k.scr["GT"][i * 128:(i + 1) * 128, :], reads=["d_GT"], writes=["dbgtmh"])
            sc.op("vector", lambda e: e.tensor_copy(out=tmp[:], in_=tmh[:]), reads=["dbgtmh"], writes=["dbgtmp"])
            sc.dma("sync", k.dbg[i * 128:(i + 1) * 128, :], tmp[:], reads=["dbgtmp"], writes=["dbg2"])
    if k.stage == 6:
        sc.dma("sync", k.dbg[0:2048, :], k.scr["x1"], reads=["d_x1"], writes=["dbg0"])
        tmp = sb("dbgtmp", [128, S], ctx=ph)
        tmh = sb("dbgtmh", [128, S], BF16, ctx=ph)
        for nm, off in (("attnT", 2048), ("recT", 3072)):
            for i in range(8):
                sc.dma("sync", tmh[:], k.scr[nm][i * 128:(i + 1) * 128, :], reads=["d_" + nm], writes=["dbgtmh"])
                sc.op("vector", lambda e: e.tensor_copy(out=tmp[:], in_=tmh[:]), reads=["dbgtmh"], writes=["dbgtmp"])
                sc.dma("sync", k.dbg[off + i * 128:off + (i + 1) * 128, :], tmp[:], reads=["dbgtmp"], writes=["dbg2"])
    if k.stage == 5:
        tmp = sb("dbgtmp", [128, S], ctx=ph)
        tmh = sb("dbgtmh", [128, S], BF16, ctx=ph)
        for i in range(8):
            sc.dma("sync", tmh[:], k.scr["recT"][i * 128:(i + 1) * 128, :], reads=["d_recT"], writes=["dbgtmh"])
            sc.op("vector", lambda e: e.tensor_copy(out=tmp[:], in_=tmh[:]), reads=["dbgtmh"], writes=["dbgtmp"])
            sc.dma("sync", k.dbg[i * 128:(i + 1) * 128, :], tmp[:], reads=["dbgtmp"], writes=["dbg2"])
    if k.stage == 4:
        tmp = sb("dbgtmp", [128, S], ctx=ph)
        tmh = sb("dbgtmh", [128, S], BF16, ctx=ph)
        for i in range(8):
            sc.dma("sync", tmh[:], k.scr["attnT"][i * 128:(i + 1) * 128, :], reads=["d_attnT"], writes=["dbgtmh"])
            sc.op("vector", lambda e: e.tensor_copy(out=tmp[:], in_=tmh[:]), reads=["dbgtmh"], writes=["dbgtmp"])
            sc.dma("sync", k.dbg[i * 128:(i + 1) * 128, :], tmp[:], reads=["dbgtmp"], writes=["dbg2"])
    if k.stage == 3:
        sc.dma("sync", k.dbg[0:1024, :], k.scr["qBT"], reads=["d_qBT"], writes=["dbg0"])
        sc.dma("sync", k.dbg[1024:1024 + 2048, 0:16], k.scr["wi"], reads=["d_wi"], writes=["dbg1"])
        tmp = sb("dbgtmp", [128, S], ctx=ph)
        tmh = sb("dbgtmh", [128, S], BF16, ctx=ph)
        for i in range(8):
            sc.dma("sync", tmh[:], k.scr["kAT"][i * 128:(i + 1) * 128, :], reads=["d_kAT"], writes=["dbgtmh"])
            sc.op("vector", lambda e: e.tensor_copy(out=tmp[:], in_=tmh[:]), reads=["dbgtmh"], writes=["dbgtmp"])
            sc.dma("sync", k.dbg[3072 + i * 128:3072 + (i + 1) * 128, :], tmp[:], reads=["dbgtmp"], writes=["dbg2"])
        for i in range(16):
            sc.dma("sync", tmh[:, 0:1024], k.scr["vA"][i * 128:(i + 1) * 128, :], reads=["d_vA"], writes=["dbgtmh"])
            sc.op("vector", lambda e: e.tensor_copy(out=tmp[:, 0:1024], in_=tmh[:, 0:1024]), reads=["dbgtmh"], writes=["dbgtmp"])
            sc.dma("sync", k.dbg[4096 + i * 128:4096 + (i + 1) * 128, 0:1024], tmp[:, 0:1024], reads=["dbgtmp"], writes=["dbg3"])
    if k.stage == 2:
        tmp = sb("dbgtmp", [128, S], ctx=ph)
        for dc in range(NDC):
            sc.op("vector", lambda e, dc=dc: e.tensor_copy(out=tmp[:], in_=k.hT[:, dc, :]),
                  reads=[("hT", dc, tg) for tg in range(4)], writes=["dbgtmp"])
            sc.dma("sync", k.dbg[dc * 128:(dc + 1) * 128, :], tmp[:], reads=["dbgtmp"], writes=["dbg0"])


class ProjRes:
    pass


def proj_setup(k, ph, KC, tag="pj"):
    r = ProjRes()
    sb, ps = k.sb, k.ps
    r.KC = KC
    r.wb = [sb("%s_wb%d" % (tag, i), [128, KC, 512], BF16, ctx=ph) for i in range(2)]
    r.wkey = ["%s_wb%d" % (tag, i) for i in range(2)]
    r.pt = [ps("%s_ps%d" % (tag, i), [128, 512], ctx=ph) for i in range(4)]
    r.pkey = ["%s_ps%d" % (tag, i) for i in range(4)]
    r.sf = [sb("%s_sf%d" % (tag, i), [128, 512], F32, ctx=ph) for i in range(3)]
    r.sh = [sb("%s_sh%d" % (tag, i), [128, 512], BF16, ctx=ph) for i in range(3)]
    r.nblk = 0
    r.npt = 0
    r.nst = 0
    r.tag = tag
    return r


def proj_load_w(k, r, w_ap, cb0, cb1):
    sc = k.sc
    KC = r.KC
    wv = w_ap.rearrange("(kc p) n -> p kc n", p=128)
    i2 = r.nblk % 2
    r.nblk += 1
    w = cb1 - cb0
    sc.dma("gpsimd", r.wb[i2][:, :, 0:w], wv[:, :, cb0:cb1], writes=[r.wkey[i2]])
    return r.wb[i2], r.wkey[i2]


def proj_stage(r, dtype):
    i = r.nst % 3
    r.nst += 1
    if dtype == F32:
        return r.sf[i], "%s_sf%d" % (r.tag, i)
    return r.sh[i], "%s_sh%d" % (r.tag, i)


def proj_psum(r):
    i = r.npt % 4
    r.npt += 1
    return r.pt[i], r.pkey[i]


def proj(k, r, actT, act_keys, w_ap, c0, c1, mode, func, dst, dst_key, dtype):
    sc = k.sc
    KC = r.KC
    cb0 = c0
    while cb0 < c1:
        cb1 = min(c1, cb0 + 512)
        w = cb1 - cb0
        wb, wk = proj_load_w(k, r, w_ap, cb0, cb1)
        if mode == "fm":
            for sub in range((w + 127) // 128):
                cw = min(128, w - sub * 128)
                for tb in range(4):
                    pt, pk = proj_psum(r)
                    for kc in range(KC):
                        sc.op("tensor", lambda e, pt=pt, wb=wb, kc=kc, sub=sub, cw=cw, tb=tb: e.matmul(
                            pt[0:cw, :], lhsT=wb[:, kc, sub * 128: sub * 128 + cw], rhs=actT[:, kc, tb * 512:(tb + 1) * 512],
                            start=(kc == 0), stop=(kc == KC - 1)),
                            reads=[wk] + act_keys(kc, tb), writes=[pk], sig=(kc == KC - 1))
                    st, sk = proj_stage(r, dtype)
                    sc.op("scalar", lambda e, pt=pt, st=st, cw=cw: e.activation(out=st[0:cw, :], in_=pt[0:cw, :], func=func),
                          reads=[pk], writes=[sk])
                    r0 = cb0 - c0 + sub * 128
                    sc.dma("sync", dst[r0:r0 + cw, tb * 512:(tb + 1) * 512], st[0:cw, :], reads=[sk], writes=[dst_key])
        else:
            for ti in range(NTT):
                pt, pk = proj_psum(r)
                for kc in range(KC):
                    sc.op("tensor", lambda e, pt=pt, wb=wb, kc=kc, ti=ti, w=w: e.matmul(
                        pt[:, 0:w], lhsT=actT[:, kc, ti * 128:(ti + 1) * 128], rhs=wb[:, kc, 0:w],
                        start=(kc == 0), stop=(kc == KC - 1)),
                        reads=[wk] + act_keys(kc, ti // 4), writes=[pk], sig=(kc == KC - 1))
                st, sk = proj_stage(r, dtype)
                sc.op("scalar", lambda e, pt=pt, st=st, w=w: e.activation(out=st[:, 0:w], in_=pt[:, 0:w], func=func),
                      reads=[pk], writes=[sk])
                sc.dma("sync", dst[ti * 128:(ti + 1) * 128, cb0 - c0:cb1 - c0], st[:, 0:w], reads=[sk], writes=[dst_key])
        cb0 = cb1


def hT_keys(kc, tb):
    return [("hT", kc, tb)]


PROJ_SPECS = [
    ("qAT", 0, 1024, "fm", "Identity", "bf16"),
    ("kAT", 1024, 2048, "fm", "Identity", "bf16"),
    ("vA", 2048, 3072, "tm", "Identity", "bf16"),
    ("qiT", 3072, 4096, "fm", "Identity", "bf16"),
    ("kiT", 4096, 4160, "fm", "Identity", "bf16"),
    ("wi", 4160, 4176, "tm", "Identity", "f32"),
    ("qBT", 4176, 5200, "fm", "Silu", "f32"),
    ("fBT", 5200, 6224, "fm", "Sigmoid", "f32"),
    ("iB", 6224, 7248, "tm", "Identity", "bf16"),
    ("gBT", 7248, 8272, "fm", "Silu", "bf16"),
    ("sgAT", 8272, 10320, "fm", "Sigmoid", "bf16"),
    ("sgBT", 10320, 12368, "fm", "Sigmoid", "bf16"),
]


def make_scratch(k):
    nc = k.nc
    k.scr = {}
    for name, c0, c1, mode, fn, dtn in PROJ_SPECS:
        dt_ = BF16 if dtn == "bf16" else F32
        shape = [c1 - c0, S] if mode == "fm" else [S, c1 - c0]
        k.scr[name] = nc.dram_tensor("scr_" + name, shape, dt_, kind="Internal").ap()
    k.scr["attnT"] = nc.dram_tensor("scr_attnT", [1024, S], BF16, kind="Internal").ap()
    k.scr["recT"] = nc.dram_tensor("scr_recT", [1024, S], BF16, kind="Internal").ap()
    k.scr["x1"] = nc.dram_tensor("scr_x1", [S, D], F32, kind="Internal").ap()
    k.scr["h2T"] = nc.dram_tensor("scr_h2T", [D, S], BF16, kind="Internal").ap()
    k.scr["pqT"] = nc.dram_tensor("scr_pqT", [D, S], BF16, kind="Internal").ap()
    k.scr["GT"] = nc.dram_tensor("scr_GT", [16384, S], BF16, kind="Internal").ap()
    k.scr["uT"] = nc.dram_tensor("scr_uT", [D, 16384], BF16, kind="Internal").ap()


def phase_c(k, ph):
    r = proj_setup(k, ph, NDC, "pc")
    for name, c0, c1, mode, fn, dtn in PROJ_SPECS:
        dt_ = BF16 if dtn == "bf16" else F32
        proj(k, r, k.hT, hT_keys, k.w_in, c0, c1, mode, getattr(AF, fn), k.scr[name], "d_" + name, dt_)


def phase_d(k, ph):
    nc, sc, sb, ps = k.nc, k.sc, k.sb, k.ps
    A = k.scr
    kT = sb("kT", [128, 8, S], BF16, ctx=ph)
    sc.dma("sync", kT[:], A["kAT"].rearrange("(h p) t -> p h t", p=128), reads=["d_kAT"], writes=["kT"])
    vS = sb("vS", [128, 16, 1024], BF16, ctx=ph)
    sc.dma("sync", vS[:], A["vA"].rearrange("(j p) c -> p j c", p=128), reads=["d_vA"], writes=["vS"])
    kiT2 = sb("kiT2", [128, S], BF16, ctx=ph)
    sc.dma("sync", kiT2[0:64, :], A["kiT"], reads=["d_kiT"], writes=["kiT2a"])
    sc.dma("sync", kiT2[64:128, :], A["kiT"], reads=["d_kiT"], writes=["kiT2b"])
    cm29 = sb("cm29", [128, 1], ctx=ph)
    sc.op("vector", lambda e: e.memset(cm29[:], -1e29), writes=["cm29"])
    qTs = [sb("qT%d" % i, [128, 8, 512], BF16, ctx=ph) for i in range(2)]
    qiT = sb("qiT", [128, 8, 512], BF16, ctx=ph)
    wi = sb("wi", [128, 4, 16], ctx=ph)
    wabs = sb("wabs", [128, 4, 16], ctx=ph)
    wsgn = sb("wsgn", [128, 4, 16], ctx=ph)
    scrs = [sb("scr%d" % i, [128, S], ctx=ph) for i in range(2)]
    work = sb("work", [128, S], ctx=ph)
    mks = [sb("mk%d" % i, [128, S], BF16, ctx=ph) for i in range(2)]
    tmps = [sb("itmp%d" % i, [128, 512], ctx=ph) for i in range(3)]
    m8 = sb("m8", [128, 8], ctx=ph)
    NBIS = 20
    pw2 = sb("pw2", [128, NBIS], ctx=ph)
    for kk_ in range(NBIS):
        sc.op("vector", lambda e, kk_=kk_: e.memset(pw2[:, kk_:kk_ + 1], 2.0 ** -(kk_ + 1)), writes=["pw2"])
    bwk = sb("bwk", [128, NBIS], ctx=ph)
    blo = sb("blo", [128, 1], ctx=ph)
    brg = sb("brg", [128, 1], ctx=ph)
    bmid = sb("bmid", [128, 1], ctx=ph)
    bcnt = sb("bcnt", [128, 1], ctx=ph)
    bg = sb("bg", [128, 1], ctx=ph)
    maskTs = [sb("maskT%d" % i, [128, 16, 512], BF16, ctx=ph) for i in range(2)]
    pes = [sb("pe%d" % i, [128, 512], BF16, ctx=ph) for i in range(3)]
    pms = [sb("pm%d" % i, [128, 512], BF16, ctx=ph) for i in range(3)]
    rds = [sb("rd%d" % i, [128, 512], ctx=ph) for i in range(2)]
    aos = [sb("ao%d" % i, [128, 512], BF16, ctx=ph) for i in range(2)]
    pis = [ps("pi%d" % i, [128, 512], ctx=ph) for i in range(2)]
    ptr = ps("ptr", [128, 512], BF16, ctx=ph)
    pls = [ps("pl%d" % i, [128, 512], ctx=ph) for i in range(2)]
    po = ps("po", [128, 512], ctx=ph)
    pd = ps("pd", [128, 512], ctx=ph)
    att_scale = 128.0 ** -0.5
    NEG = -1e30
    n_i = 0
    n_e = 0
    n_h = 0
    streams = {}
    for tb in range(4):
        sc.begin_capture()
        maskT = maskTs[tb % 2]
        mkey = "maskT%d" % (tb % 2)
        qT = qTs[tb % 2]
        qTk = "qT%d" % (tb % 2)
        sc.dma("sync", qT[:], A["qAT"].rearrange("(h p) t -> p h t", p=128)[:, :, tb * 512:(tb + 1) * 512],
               reads=["d_qAT"], writes=[qTk])
        sc.dma("sync", qiT[:], A["qiT"].rearrange("(h p) t -> p h t", p=128)[:, :, tb * 512:(tb + 1) * 512],
               reads=["d_qiT"], writes=["qiT"])
        sc.dma("sync", wi[:], A["wi"].rearrange("(n p) h -> p n h", p=128)[:, tb * 4:(tb + 1) * 4, :],
               reads=["d_wi"], writes=["wi"])
        sc.op("scalar", lambda e: e.activation(out=wabs[:], in_=wi[:], func=AF.Abs), reads=["wi"], writes=["wabs"])
        sc.op("scalar", lambda e: e.activation(out=wsgn[:], in_=wi[:], func=AF.Sign), reads=["wi"], writes=["wsgn"])
        sc.op("vector", lambda e, maskT=maskT: e.memset(maskT[:], -30000.0), writes=[mkey])
        for tt in range(4):
            i = tb * 4 + tt
            L = 128 * (i + 1)
            scr = scrs[i % 2]
            skey = "scr%d" % (i % 2)
            mk = mks[i % 2]
            mkk = "mk%d" % (i % 2)
            for h in range(16):
                cch = h // 2
                p0 = 64 * (h % 2)
                kik = "kiT2a" if p0 == 0 else "kiT2b"
                for s0 in range(0, L, 512):
                    sw = min(512, L - s0)
                    pi = pis[n_i % 2]
                    pik = "pi%d" % (n_i % 2)
                    tmp = tmps[n_i % 3]
                    tk = "itmp%d" % (n_i % 3)
                    n_i += 1
                    sc.op("tensor", lambda e, pi=pi, p0=p0, cch=cch, tt=tt, s0=s0, sw=sw: e.matmul(
                        pi[:, 0:sw], lhsT=qiT[p0:p0 + 64, cch, tt * 128:(tt + 1) * 128], rhs=kiT2[p0:p0 + 64, s0:s0 + sw],
                        start=True, stop=True), reads=["qiT", kik], writes=[pik])
                    sc.op("scalar", lambda e, pi=pi, tmp=tmp, sw=sw, tt=tt, h=h: e.activation(
                        out=tmp[:, 0:sw], in_=pi[:, 0:sw], func=AF.Relu, scale=wabs[:, tt, h:h + 1]),
                        reads=[pik, "wabs"], writes=[tk])
                    if h == 0:
                        sc.op("vector", lambda e, tmp=tmp, scr=scr, s0=s0, sw=sw, tt=tt, h=h: e.tensor_scalar(
                            out=scr[:, s0:s0 + sw], in0=tmp[:, 0:sw], scalar1=wsgn[:, tt, h:h + 1], scalar2=None, op0=ALU.mult),
                            reads=[tk, "wsgn"], writes=[skey])
                    else:
                        sc.op("vector", lambda e, tmp=tmp, scr=scr, s0=s0, sw=sw, tt=tt, h=h: e.scalar_tensor_tensor(
                            out=scr[:, s0:s0 + sw], in0=tmp[:, 0:sw], scalar=wsgn[:, tt, h:h + 1], in1=scr[:, s0:s0 + sw],
                            op0=ALU.mult, op1=ALU.add), reads=[tk, "wsgn", skey], writes=[skey])
            if i >= 2:
                sc.op("vector", lambda e, scr=scr, L=L: e.tensor_reduce(out=blo[:], in_=scr[:, 0:L], axis=AX.X, op=ALU.min),
                      reads=[skey], writes=["blo"])
            sc.op("vector", lambda e, scr=scr, L=L: e.memset(scr[0:64, L - 64:L], NEG), writes=[skey])
            if i >= 2:
                sc.op("vector", lambda e, scr=scr, L=L: e.max(out=m8[:], in_=scr[:, 0:L]), reads=[skey], writes=["m8"])
                sc.op("vector", lambda e: e.tensor_tensor(out=brg[:], in0=m8[:, 0:1], in1=blo[:], op=ALU.subtract),
                      reads=["m8", "blo"], writes=["brg"])
                sc.op("vector", lambda e: e.tensor_scalar(out=bwk[:], in0=pw2[:], scalar1=brg[:, 0:1], scalar2=None, op0=ALU.mult),
                      reads=["pw2", "brg"], writes=["bwk"])
                sc.op("vector", lambda e: e.tensor_tensor(out=bmid[:], in0=blo[:], in1=bwk[:, 0:1], op=ALU.add),
                      reads=["blo", "bwk"], writes=["bmid"])
                for kk_ in range(NBIS):
                    sc.op("vector", lambda e, scr=scr, L=L: e.tensor_scalar(out=work[:, 0:L], in0=scr[:, 0:L], scalar1=bmid[:, 0:1], scalar2=0.0,
                                                                            op0=ALU.is_ge, op1=ALU.add, accum_out=bcnt[:, 0:1]),
                          reads=[skey, "bmid"], writes=["work", "bcnt"])
                    sc.op("vector", lambda e, kk_=kk_: e.tensor_scalar(out=bg[:], in0=bcnt[:], scalar1=255.5, scalar2=bwk[:, kk_:kk_ + 1],
                                                                       op0=ALU.is_ge, op1=ALU.mult), reads=["bcnt", "bwk"], writes=["bg"])
                    sc.op("vector", lambda e: e.tensor_tensor(out=blo[:], in0=blo[:], in1=bg[:], op=ALU.add),
                          reads=["blo", "bg"], writes=["blo"])
                    if kk_ + 1 < NBIS:
                        sc.op("vector", lambda e, kk_=kk_: e.tensor_tensor(out=bmid[:], in0=blo[:], in1=bwk[:, kk_ + 1:kk_ + 2], op=ALU.add),
                              reads=["blo", "bwk"], writes=["bmid"])
                tau = blo[:, 0:1]
                tauk = "blo"
            else:
                tau = cm29[:, 0:1]
                tauk = "cm29"
            sc.op("vector", lambda e, scr=scr, mk=mk, L=L, tau=tau: e.tensor_scalar(
                out=mk[:, 0:L], in0=scr[:, 0:L], scalar1=tau, scalar2=None, op0=ALU.is_ge),
                reads=[skey, tauk], writes=[mkk])
            for j0 in range(0, i + 1, 4):
                n = min(4, i + 1 - j0)
                for jj in range(n):
                    j = j0 + jj
                    sc.op("tensor", lambda e, jj=jj, j=j, mk=mk: e.transpose(out=ptr[:, jj * 128:(jj + 1) * 128],
                                                                             in_=mk[:, j * 128:(j + 1) * 128], identity=k.identb[:]),
                          reads=[mkk, "identb"], writes=["ptr"], sig=(jj == n - 1))
                sc.op("scalar", lambda e, j0=j0, n=n, tt=tt, maskT=maskT: e.activation(
                    out=maskT[:, j0:j0 + n, tt * 128:(tt + 1) * 128],
                    in_=ptr[:, 0:n * 128].rearrange("p (n t) -> p n t", t=128), func=AF.Identity, scale=30000.0, bias=-30000.0),
                    reads=["ptr"], writes=[mkey])
        streams[("i", tb)] = sc.end_capture()
        sc.begin_capture()
        nj = 4 * (tb + 1)
        units = [(h, j) for h in range(8) for j in range(nj)]
        ubuf = {}

        def emit_lg(u):
            h, j = units[u]
            pl = pls[u % 2]
            plk = "pl%d" % (u % 2)
            sc.op("tensor", lambda e, pl=pl, h=h, j=j, qT=qT: e.matmul(pl[:, :], lhsT=kT[:, h, j * 128:(j + 1) * 128], rhs=qT[:, h, :],
                                                               start=True, stop=False), reads=["kT", qTk], writes=[plk], sig=False)
            sc.op("tensor", lambda e, pl=pl, j=j, maskT=maskT: e.matmul(pl[:, :], lhsT=k.identb[:], rhs=maskT[:, j, :],
                                                                       start=False, stop=True), reads=["identb", mkey], writes=[plk])
            pe = pes[u % 3]
            pek = "pe%d" % (u % 3)
            sc.op("scalar", lambda e, pl=pl, pe=pe: e.activation(out=pe[:], in_=pl[:, :], func=AF.Exp, scale=att_scale),
                  reads=[plk], writes=[pek])

        emit_lg(0)
        for u, (h, j) in enumerate(units):
            if u + 1 < len(units):
                emit_lg(u + 1)
            pm = pes[u % 3]
            pmk = "pe%d" % (u % 3)
            sc.op("tensor", lambda e, pm=pm, h=h, j=j: e.matmul(po[:, :], lhsT=vS[:, j, h * 128:(h + 1) * 128], rhs=pm[:],
                                                               start=(j == 0), stop=(j == nj - 1)), reads=["vS", pmk], writes=["po"], sig=False)
            sc.op("tensor", lambda e, pm=pm, j=j: e.matmul(pd[:, :], lhsT=k.onesb[:], rhs=pm[:],
                                                          start=(j == 0), stop=(j == nj - 1)), reads=["onesb", pmk], writes=["pd"])
            if j == nj - 1:
                rd = rds[n_h % 2]
                rdk = "rd%d" % (n_h % 2)
                ao = aos[n_h % 2]
                aok = "ao%d" % (n_h % 2)
                n_h += 1
                sc.op("vector", lambda e, rd=rd: e.reciprocal(out=rd[:], in_=pd[:, :]), reads=["pd"], writes=[rdk])
                sc.op("vector", lambda e, rd=rd, ao=ao: e.tensor_tensor(out=ao[:], in0=po[:, :], in1=rd[:], op=ALU.mult),
                      reads=["po", rdk], writes=[aok])
                sc.dma("sync", A["attnT"][h * 128:(h + 1) * 128, tb * 512:(tb + 1) * 512], ao[:], reads=[aok], writes=["d_attnT"])
        streams[("a", tb)] = sc.end_capture()
    for u in streams[("i", 0)]:
        u()
    for tb in range(4):
        merged_units([streams[("a", tb)], streams.get(("i", tb + 1), [])])


def phase_e(k, ph):
    nc, sc, sb, ps = k.nc, k.sc, k.sb, k.ps
    A = k.scr
    NCH = 32
    lbl = sb("lbl", [128, 2, 8], ctx=ph)
    sc.dma("sync", lbl[:], k.lb_logits.rearrange("r (h d) -> d r h", d=128), writes=["lbl"], allow_slow_non_contiguous=True)
    lb = sb("lb", [128, 8], ctx=ph)
    oml = sb("oml", [128, 8], ctx=ph)
    sc.op("vector", lambda e: e.tensor_tensor(out=lb[:], in0=lbl[:, 0, :], in1=lbl[:, 1, :], op=ALU.subtract), reads=["lbl"], writes=["lb"])
    sc.op("scalar", lambda e: e.activation(out=lb[:], in_=lb[:], func=AF.Sigmoid), reads=["lb"], writes=["lb"])
    sc.op("vector", lambda e: e.tensor_scalar(out=oml[:], in0=lb[:], scalar1=-1.0, scalar2=1.0, op0=ALU.mult, op1=ALU.add),
          reads=["lb"], writes=["oml"])
    gain = sb("hgain", [128, 1], ctx=ph)
    sc.dma("sync", gain[:], k.hgrn_gain.rearrange("o d -> d o"), writes=["hgain"], allow_slow_non_contiguous=True)
    tri = sb("tri_sb", [64, 64], ctx=ph)
    sc.dma("sync", tri[:], k.tri_d, writes=["tri"])
    rst = sb("rst", [128, S], ctx=ph)
    sc.op("vector", lambda e: e.memset(rst[:], 1.0), writes=["rst"])
    sc.op("vector", lambda e: e.memset(rst[:].rearrange("p (c s) -> p c s", s=64)[:, :, 0:1], 0.0), writes=["rst"])
    qf = sb("qf", [128, S], ctx=ph)
    ff = sb("ff", [128, S], ctx=ph)
    kk = sb("kk", [128, S], ctx=ph)
    lf = sb("lf", [128, S], ctx=ph)
    bb = sb("bb", [128, S], ctx=ph)
    bp = sb("bp", [128, S], ctx=ph)
    tA = sb("tA", [128, S], ctx=ph)
    qhats = [sb("qhat%d" % i, [128, S], BF16, ctx=ph) for i in range(2)]
    qtils = [sb("qtil%d" % i, [128, S], BF16, ctx=ph) for i in range(2)]
    ktils = [sb("ktil%d" % i, [128, S], BF16, ctx=ph) for i in range(2)]
    kendTs = [sb("kendT%d" % i, [128, S], BF16, ctx=ph) for i in range(2)]
    sq = sb("hsq", [128, S], ctx=ph)
    kend = sb("kend", [64, NCH, 128], BF16, ctx=ph)
    vSs = [sb("hvS%d" % i, [64, NCH, 128], BF16, ctx=ph) for i in range(2)]
    atT = sb("atT", [64, NCH, 64], BF16, ctx=ph)
    recT = sb("recT", [128, S], ctx=ph)
    sgbs = [sb("sgb%d" % i, [128, S], BF16, ctx=ph) for i in range(2)]
    ebls = [sb("ebl%d" % i, [128, NCH], ctx=ph) for i in range(2)]
    Sf = sb("Sf", [128, 128], ctx=ph)
    Sb = sb("Sb", [128, 128], BF16, ctx=ph)
    rs = sb("hrs", [128, 512], ctx=ph)
    t1 = sb("ht1", [128, 512], ctx=ph)
    obs = [sb("hob%d" % i, [128, 512], BF16, ctx=ph) for i in range(2)]
    ptk = ps("ptk", [64, 512], BF16, ctx=ph)
    pat = ps("pat", [64, 512], ctx=ph)
    pos = [ps("hpo%d" % i, [128, 512], ctx=ph) for i in range(2)]
    pSs = [ps("hpS%d" % i, [128, 128], ctx=ph) for i in range(2)]
    pn = ps("hpn", [128, 512], ctx=ph)
    v3 = lambda t: t[:].rearrange("p (c s) -> p c s", s=64)
    n_ob = [0]
    Pst, Lst = {}, {}

    def do_head(h):
        hp_ = str(h % 2)
        qhat, qtil, ktil, kendT, vS, sgb, ebl = qhats[h % 2], qtils[h % 2], ktils[h % 2], kendTs[h % 2], vSs[h % 2], sgbs[h % 2], ebls[h % 2]
        rows = slice(h * 128, (h + 1) * 128)
        sc.begin_capture()
        sc.dma("sync", qf[:], A["qBT"][rows, :], reads=["d_qBT"], writes=["qf"])
        sc.dma("sync", ff[:], A["fBT"][rows, :], reads=["d_fBT"], writes=["ff"])
        sc.dma("sync", sgb[:], A["gBT"][rows, :], reads=["d_gBT"], writes=["sgb" + hp_])
        sc.dma("sync", vS[:], A["iB"].rearrange("(c s) n -> s c n", s=64)[:, :, rows], reads=["d_iB"], writes=["hvS" + hp_])
        sc.op("vector", lambda e, h=h: e.tensor_scalar(out=ff[:], in0=ff[:], scalar1=oml[:, h:h + 1], scalar2=lb[:, h:h + 1],
                                                       op0=ALU.mult, op1=ALU.add), reads=["ff", "oml", "lb"], writes=["ff"])
        sc.op("vector", lambda e: e.tensor_scalar(out=kk[:], in0=ff[:], scalar1=-1.0, scalar2=1.0, op0=ALU.mult, op1=ALU.add),
              reads=["ff"], writes=["kk"])
        sc.op("scalar", lambda e: e.activation(out=lf[:], in_=ff[:], func=AF.Ln), reads=["ff"], writes=["lf"])
        sc.op("vector", lambda e: e.tensor_tensor_scan(out=bb[:], data0=rst[:], data1=lf[:], initial=0.0, op0=ALU.mult, op1=ALU.add),
              reads=["rst", "lf"], writes=["bb"])
        sc.op("scalar", lambda e: e.activation(out=tA[:], in_=bb[:], func=AF.Exp), reads=["bb"], writes=["tA"])
        sc.op("vector", lambda e: e.tensor_tensor(out=qhat[:], in0=qf[:], in1=tA[:], op=ALU.mult), reads=["qf", "tA"], writes=["qhat" + hp_])
        sc.op("scalar", lambda e: e.activation(out=ebl[:], in_=v3(bb)[:, :, 63], func=AF.Exp), reads=["bb"], writes=["ebl" + hp_])
        sc.op("vector", lambda e: e.tensor_tensor(out=v3(bp), in0=v3(bb), in1=v3(bb)[:, :, 31:32].to_broadcast([128, NCH, 64]),
                                                  op=ALU.subtract), reads=["bb"], writes=["bp"])
        sc.op("scalar", lambda e: e.activation(out=tA[:], in_=bp[:], func=AF.Exp), reads=["bp"], writes=["tA"])
        sc.op("vector", lambda e: e.tensor_tensor(out=qtil[:], in0=qf[:], in1=tA[:], op=ALU.mult), reads=["qf", "tA"], writes=["qtil" + hp_])
        sc.op("scalar", lambda e: e.activation(out=tA[:], in_=bp[:], func=AF.Exp, scale=-1.0), reads=["bp"], writes=["tA"])
        sc.op("vector", lambda e: e.tensor_tensor(out=ktil[:], in0=kk[:], in1=tA[:], op=ALU.mult), reads=["kk", "tA"], writes=["ktil" + hp_])
        sc.op("vector", lambda e: e.tensor_tensor(out=v3(bp), in0=v3(bb)[:, :, 63:64].to_broadcast([128, NCH, 64]), in1=v3(bb),
                                                  op=ALU.subtract), reads=["bb"], writes=["bp"])
        sc.op("scalar", lambda e: e.activation(out=tA[:], in_=bp[:], func=AF.Exp), reads=["bp"], writes=["tA"])
        sc.op("vector", lambda e: e.tensor_tensor(out=kendT[:], in0=kk[:], in1=tA[:], op=ALU.mult), reads=["kk", "tA"], writes=["kendT" + hp_])
        Pst[h] = sc.end_capture()
        sc.begin_capture()
        for c0 in range(0, NCH, 4):
            for jj in range(4):
                c = c0 + jj
                sc.op("tensor", lambda e, jj=jj, c=c: e.transpose(out=ptk[:, jj * 128:(jj + 1) * 128], in_=kendT[:, c * 64:(c + 1) * 64],
                                                                 identity=k.identb[:]), reads=["kendT" + hp_, "identb"], writes=["ptk"], sig=(jj == 3))
            sc.op("scalar", lambda e, c0=c0: e.activation(out=kend[:, c0:c0 + 4, :], in_=ptk[:, :].rearrange("p (n d) -> p n d", d=128),
                                                          func=AF.Copy), reads=["ptk"], writes=["kend"])
        for c0 in range(0, NCH, 8):
            for jj in range(8):
                c = c0 + jj
                sc.op("tensor", lambda e, jj=jj, c=c: e.matmul(pat[:, jj * 64:(jj + 1) * 64], lhsT=ktil[:, c * 64:(c + 1) * 64],
                                                              rhs=qtil[:, c * 64:(c + 1) * 64], start=True, stop=True),
                      reads=["ktil" + hp_, "qtil" + hp_], writes=["pat"], sig=(jj == 7))
            sc.op("vector", lambda e, c0=c0: e.tensor_tensor(out=atT[:, c0:c0 + 8, :], in0=pat[:, :].rearrange("p (n t) -> p n t", t=64),
                                                             in1=tri[:, :].unsqueeze(1).to_broadcast([64, 8, 64]), op=ALU.mult),
                  reads=["pat", "tri"], writes=["atT"])
        for c in range(NCH):
            po = pos[(c // 8) % 2]
            pok = "hpo%d" % ((c // 8) % 2)
            col = (c % 8) * 64
            if c > 0:
                sc.op("tensor", lambda e, po=po, col=col, c=c: e.matmul(po[:, col:col + 64], lhsT=Sb[:], rhs=qhat[:, c * 64:(c + 1) * 64],
                                                                       start=True, stop=False), reads=["Sb", "qhat" + hp_], writes=[pok], sig=False)
            sc.op("tensor", lambda e, po=po, col=col, c=c: e.matmul(po[:, col:col + 64], lhsT=vS[:, c, :], rhs=atT[:, c, :],
                                                                   start=(c == 0), stop=True), reads=["hvS" + hp_, "atT"], writes=[pok])
            if c < NCH - 1:
                pS = pSs[c % 2]
                pSk = "hpS%d" % (c % 2)
                sc.op("tensor", lambda e, pS=pS, c=c: e.matmul(pS[:, :], lhsT=kend[:, c, :], rhs=vS[:, c, :], start=True, stop=True),
                      reads=["kend", "hvS" + hp_], writes=[pSk])
                if c == 0:
                    sc.op("vector", lambda e, pS=pS: e.tensor_copy(out=Sb[:], in_=pS[:, :]), reads=[pSk], writes=["Sb"])
                    sc.op("vector", lambda e, pS=pS: e.tensor_copy(out=Sf[:], in_=pS[:, :]), reads=[pSk], writes=["Sf"])
                else:
                    sc.op("vector", lambda e, pS=pS, c=c: e.scalar_tensor_tensor(out=Sb[:], in0=Sf[:], scalar=ebl[:, c:c + 1], in1=pS[:, :],
                                                                                op0=ALU.mult, op1=ALU.add),
                          reads=["Sf", "ebl" + hp_, pSk], writes=["Sb"])
                    sc.op("vector", lambda e, pS=pS, c=c: e.scalar_tensor_tensor(out=Sf[:], in0=Sf[:], scalar=ebl[:, c:c + 1], in1=pS[:, :],
                                                                                op0=ALU.mult, op1=ALU.add),
                          reads=["Sf", "ebl" + hp_, pSk], writes=["Sf"])
            if c % 8 == 7:
                sc.op("scalar", lambda e, po=po, c=c: e.activation(out=recT[:, (c - 7) * 64:(c + 1) * 64], in_=po[:, :], func=AF.Copy),
                      reads=[pok], writes=["recT"])
        sc.op("scalar", lambda e: e.activation(out=sq[:], in_=recT[:], func=AF.Square), reads=["recT"], writes=["hsq"])
        for tb in range(4):
            cs_ = slice(tb * 512, (tb + 1) * 512)
            sc.op("tensor", lambda e, cs_=cs_: e.matmul(pn[:, :], lhsT=k.onesf[:], rhs=sq[:, cs_], start=True, stop=True),
                  reads=["onesf", "hsq"], writes=["hpn"])
            sc.op("vector", lambda e: e.tensor_scalar(out=rs[:], in0=pn[:, :], scalar1=1.0 / 128, scalar2=EPS, op0=ALU.mult, op1=ALU.add),
                  reads=["hpn"], writes=["hrs"])
            sc.op("scalar", lambda e: e.activation(out=rs[:], in_=rs[:], func=AF.Sqrt), reads=["hrs"], writes=["hrs"])
            sc.op("vector", lambda e: e.reciprocal(out=rs[:], in_=rs[:]), reads=["hrs"], writes=["hrs"])
            sc.op("vector", lambda e, cs_=cs_: e.scalar_tensor_tensor(out=t1[:], in0=recT[:, cs_], scalar=gain[:, 0:1], in1=rs[:],
                                                                      op0=ALU.mult, op1=ALU.mult), reads=["recT", "hgain", "hrs"], writes=["ht1"])
            ob = obs[n_ob[0] % 2]
            obk = "hob%d" % (n_ob[0] % 2)
            n_ob[0] += 1
            sc.op("vector", lambda e, ob=ob, cs_=cs_: e.tensor_tensor(out=ob[:], in0=t1[:], in1=sgb[:, cs_], op=ALU.mult),
                  reads=["ht1", "sgb" + hp_], writes=[obk])
            sc.dma("sync", A["recT"][rows, cs_], ob[:], reads=[obk], writes=["d_recT"])
        Lst[h] = sc.end_capture()

    for h in range(8):
        do_head(h)
    for u in Pst[0]:
        u()
    for h in range(8):
        merged_units([Lst[h], Pst.get(h + 1, [])])


def phase_f(k, ph):
    nc, sc, sb, ps = k.nc, k.sc, k.sb, k.ps
    A = k.scr
    aT = sb("f_aT", [128, 8, S], BF16, ctx=ph)
    rT = sb("f_rT", [128, 8, S], BF16, ctx=ph)
    sc.dma("sync", aT[:], A["attnT"].rearrange("(h p) t -> p h t", p=128), reads=["d_attnT"], writes=["f_aT"])
    sc.dma("sync", rT[:], A["recT"].rearrange("(h p) t -> p h t", p=128), reads=["d_recT"], writes=["f_rT"])
    mixT = k.hT
    was = [sb("f_wa%d" % i, [128, 8, 512], BF16, ctx=ph) for i in range(2)]
    wbs = [sb("f_wb%d" % i, [128, 8, 512], BF16, ctx=ph) for i in range(2)]
    sgas = [sb("f_sga%d" % i, [128, 512], BF16, ctx=ph) for i in range(2)]
    sgbs = [sb("f_sgb%d" % i, [128, 512], BF16, ctx=ph) for i in range(2)]
    yas = [sb("f_ya%d" % i, [128, 512], ctx=ph) for i in range(2)]
    ybs = [sb("f_yb%d" % i, [128, 512], ctx=ph) for i in range(2)]
    pas = [ps("f_pa%d" % i, [128, 512], ctx=ph) for i in range(2)]
    pbs = [ps("f_pb%d" % i, [128, 512], ctx=ph) for i in range(2)]
    wva = k.w_up_a.rearrange("(kc p) n -> p kc n", p=128)
    wvb = k.w_up_b.rearrange("(kc p) n -> p kc n", p=128)
    n = 0
    for nb in range(4):
        wa, wb = was[nb % 2], wbs[nb % 2]
        wak, wbk = "f_wa%d" % (nb % 2), "f_wb%d" % (nb % 2)
        sc.dma("gpsimd", wa[:], wva[:, :, nb * 512:(nb + 1) * 512], writes=[wak])
        sc.dma("gpsimd", wb[:], wvb[:, :, nb * 512:(nb + 1) * 512], writes=[wbk])
        for sub in range(4):
            nch = nb * 4 + sub
            for tb in range(4):
                i2 = n % 2
                n += 1
                pa, pb = pas[i2], pbs[i2]
                pak, pbk = "f_pa%d" % i2, "f_pb%d" % i2
                sga, sgb = sgas[i2], sgbs[i2]
                ya, yb = yas[i2], ybs[i2]
                cs_ = slice(tb * 512, (tb + 1) * 512)
                sc.dma("sync", sga[:], A["sgAT"][nch * 128:(nch + 1) * 128, cs_], reads=["d_sgAT"], writes=["f_sga%d" % i2])
                sc.dma("sync", sgb[:], A["sgBT"][nch * 128:(nch + 1) * 128, cs_], reads=["d_sgBT"], writes=["f_sgb%d" % i2])
                for kc in range(8):
                    sc.op("tensor", lambda e, pa=pa, wa=wa, kc=kc, sub=sub, cs_=cs_: e.matmul(
                        pa[:, :], lhsT=wa[:, kc, sub * 128:(sub + 1) * 128], rhs=aT[:, kc, cs_], start=(kc == 0), stop=(kc == 7)),
                        reads=[wak, "f_aT"], writes=[pak], sig=(kc == 7))
                for kc in range(8):
                    sc.op("tensor", lambda e, pb=pb, wb=wb, kc=kc, sub=sub, cs_=cs_: e.matmul(
                        pb[:, :], lhsT=wb[:, kc, sub * 128:(sub + 1) * 128], rhs=rT[:, kc, cs_], start=(kc == 0), stop=(kc == 7)),
                        reads=[wbk, "f_rT"], writes=[pbk], sig=(kc == 7))
                sc.op("vector", lambda e, pa=pa, ya=ya, sga=sga: e.tensor_tensor(out=ya[:], in0=pa[:, :], in1=sga[:], op=ALU.mult),
                      reads=[pak, "f_sga%d" % i2], writes=["f_ya%d" % i2])
                sc.op("vector", lambda e, pb=pb, yb=yb, sgb=sgb: e.tensor_tensor(out=yb[:], in0=pb[:, :], in1=sgb[:], op=ALU.mult),
                      reads=[pbk, "f_sgb%d" % i2], writes=["f_yb%d" % i2])
                sc.op("vector", lambda e, ya=ya, yb=yb, nch=nch, cs_=cs_: e.tensor_tensor(out=mixT[:, nch, cs_], in0=ya[:], in1=yb[:], op=ALU.add),
                      reads=["f_ya%d" % i2, "f_yb%d" % i2], writes=[("hT", nch, tb)])


def phase_f2(k, ph):
    nc, sc, sb, ps = k.nc, k.sc, k.sb, k.ps
    A = k.scr
    mixT = k.hT
    r = proj_setup(k, ph, NDC, "fo")
    xss = [sb("f_xs%d" % i, [128, 512], ctx=ph) for i in range(3)]
    tts = [sb("f_tt%d" % i, [128, 512], ctx=ph) for i in range(3)]
    xv = k.x.rearrange("(n p) d -> n p d", p=128)
    x1v = A["x1"].rearrange("(n p) d -> n p d", p=128)
    m = 0
    for nb in range(4):
        cs_ = slice(nb * 512, (nb + 1) * 512)
        wb, wk = proj_load_w(k, r, k.w_out, nb * 512, (nb + 1) * 512)
        for ti in range(NTT):
            pt, pk = proj_psum(r)
            i3 = m % 3
            m += 1
            xs, tt_ = xss[i3], tts[i3]
            sc.dma("sync", xs[:], xv[ti][:, cs_], writes=["f_xs%d" % i3])
            for kc in range(NDC):
                sc.op("tensor", lambda e, pt=pt, wb=wb, kc=kc, ti=ti: e.matmul(
                    pt[:, :], lhsT=mixT[:, kc, ti * 128:(ti + 1) * 128], rhs=wb[:, kc, :], start=(kc == 0), stop=(kc == NDC - 1)),
                    reads=[wk, ("hT", kc, ti // 4)], writes=[pk], sig=(kc == NDC - 1))
            sc.op("vector", lambda e, pt=pt, tt_=tt_, cs_=cs_: e.tensor_tensor(out=tt_[:], in0=pt[:, :], in1=k.G1[:, cs_], op=ALU.mult),
                  reads=[pk, "G0"], writes=["f_tt%d" % i3])
            sc.op("vector", lambda e, tt_=tt_, xs=xs: e.tensor_tensor(out=tt_[:], in0=tt_[:], in1=xs[:], op=ALU.add),
                  reads=["f_tt%d" % i3, "f_xs%d" % i3], writes=["f_tt%d" % i3])
            sc.dma("sync", x1v[ti][:, cs_], tt_[:], reads=["f_tt%d" % i3], writes=["d_x1"])


def phase_g(k, ph):
    sc = k.sc
    norm_modulate(k, ph, k.scr["x1"], k.norm_ffn, 48, 64, ["d_x1"], tag="g2")
    for dc in range(NDC):
        sc.dma("sync", k.scr["h2T"][dc * 128:(dc + 1) * 128, :], k.hT[:, dc, :], reads=[("hT", dc, tg) for tg in range(4)],
               writes=["d_h2T"])


def phase_h1(k, ph):
    r = proj_setup(k, ph, NDC, "ph")
    proj(k, r, k.hT, hT_keys, k.peer_w_q, 0, 2048, "fm", AF.Identity, k.scr["pqT"], "d_pqT", BF16)


def phase_h2(k, ph):
    nc, sc, sb, ps = k.nc, k.sc, k.sb, k.ps
    A = k.scr
    NEG = -1e30
    TOPS_ALL = [("h_tops", c_) for c_ in range(16)]
    BEST_ALL = [("h_best", p_) for p_ in range(8)]
    kl = sb("h_kl", [128, 16, 128], ctx=ph)
    sc.dma("sync", kl[:], k.peer_keys.rearrange("p h n d -> n (p h) d"), writes=["h_kl"])
    keysT = sb("h_keysT", [128, 16, 128], BF16, ctx=ph)
    pss = [ps("h_ps%d" % i, [128, 512], ctx=ph) for i in range(4)]
    for ch in range(16):
        b = ch // 4
        sc.op("tensor", lambda e, ch=ch, b=b: e.transpose(out=pss[b][:, (ch % 4) * 128:(ch % 4 + 1) * 128], in_=kl[:, ch, :],
                                                         identity=k.identf[:]), reads=["h_kl", "identf"], writes=["h_ps%d" % b], sig=(ch % 4 == 3))
    for b in range(4):
        sc.op("vector", lambda e, b=b: e.tensor_copy(out=keysT[:, b * 4:(b + 1) * 4, :],
                                                     in_=pss[b][:, :].rearrange("p (c n) -> p c n", n=128)),
              reads=["h_ps%d" % b], writes=["h_keysT"])
    qts = [sb("h_qt%d" % i, [128, 16, 128], BF16, ctx=ph) for i in range(2)]
    s_sb = sb("h_s", [128, 16, 128], ctx=ph)
    wk = sb("h_wk", [128, 16, 128], ctx=ph)
    tops = sb("h_tops", [128, 16, 16], ctx=ph)
    cand = sb("h_cand", [128, 8, 256], ctx=ph)
    cwk = sb("h_cwk", [128, 8, 256], ctx=ph)
    best = sb("h_best", [128, 8, 16], ctx=ph)
    ez = sb("h_ez", [128, 8, 16], ctx=ph)
    Z = sb("h_Z", [128, 8], ctx=ph)
    bias = sb("h_bias", [128, 8], ctx=ph)
    th = sb("h_th", [128, 8, 16], ctx=ph)
    e1 = sb("h_e1", [128, 8, 16], ctx=ph)
    e1T = sb("h_e1T", [128, 128], ctx=ph)
    e2 = sb("h_e2", [128, 8, 128], ctx=ph)
    Rt = sb("h_R", [128, 128, 128], BF16, ctx=ph)
    Oh = sb("h_O", [128, 64, 128], BF16, ctx=ph)
    RT = sb("h_RT", [128, 128, 128], BF16, ctx=ph)
    OT = sb("h_OT", [128, 64, 128], BF16, ctx=ph)
    gsts = [sb("h_gst%d" % i, [128, 64, 128], BF16, ctx=ph) for i in range(2)]
    ptrs = [ps("h_ptr%d" % i, [128, 512], BF16, ctx=ph) for i in range(2)]
    pgs = [ps("h_pg%d" % i, [128, 512], ctx=ph) for i in range(2)]
    pqv = A["pqT"].rearrange("(c p) t -> p c t", p=128)
    GTv = A["GT"].rearrange("(i j) t -> j i t", j=128)
    s4 = s_sb[:].rearrange("p (a h) n -> p a h n", h=2)
    t4 = tops[:].rearrange("p (a h) n -> p a h n", h=2)
    n_s = 0
    n_g = 0
    n_pg = 0
    for ti in range(NTT):
        qt = qts[ti % 2]
        qk = "h_qt%d" % (ti % 2)
        sc.dma("sync", qt[:], pqv[:, :, ti * 128:(ti + 1) * 128], reads=["d_pqT"], writes=[qk])
        for ch in range(16):
            b = ch // 4
            sc.op("tensor", lambda e, ch=ch, b=b, qt=qt: e.matmul(pss[b][:, (ch % 4) * 128:(ch % 4 + 1) * 128], lhsT=qt[:, ch, :],
                                                                 rhs=keysT[:, ch, :], start=True, stop=True),
                  reads=[qk, "h_keysT"], writes=["h_ps%d" % b], sig=(ch % 4 == 3))
        for b in range(4):
            sc.op("scalar", lambda e, b=b: e.activation(out=s_sb[:, b * 4:(b + 1) * 4, :],
                                                        in_=pss[b][:, :].rearrange("p (c n) -> p c n", n=128), func=AF.Copy),
                  reads=["h_ps%d" % b], writes=["h_s"])
        for ch in range(16):
            sc.op("vector", lambda e, ch=ch: e.max(out=tops[:, ch, 0:8], in_=s_sb[:, ch, :]), reads=["h_s"], writes=[("h_tops", ch)])
        for ch in range(16):
            sc.op("vector", lambda e, ch=ch: e.match_replace(out=wk[:, ch, :], in_to_replace=tops[:, ch, 0:8], in_values=s_sb[:, ch, :],
                                                             imm_value=NEG), reads=["h_s", ("h_tops", ch)], writes=[("h_wk", ch)])
        for ch in range(16):
            sc.op("vector", lambda e, ch=ch: e.max(out=tops[:, ch, 8:16], in_=wk[:, ch, :]), reads=[("h_wk", ch)], writes=[("h_tops", ch)])
        sc.op("vector", lambda e: e.tensor_tensor(out=cand[:].rearrange("p a (r c) -> p a r c", c=16),
                                                  in0=t4[:, :, 0, :].unsqueeze(3).to_broadcast([128, 8, 16, 16]),
                                                  in1=t4[:, :, 1, :].unsqueeze(2).to_broadcast([128, 8, 16, 16]), op=ALU.add),
              reads=TOPS_ALL, writes=["h_cand"])
        for p in range(8):
            sc.op("vector", lambda e, p=p: e.max(out=best[:, p, 0:8], in_=cand[:, p, :]), reads=["h_cand"], writes=[("h_best", p)])
        for p in range(8):
            sc.op("vector", lambda e, p=p: e.match_replace(out=cwk[:, p, :], in_to_replace=best[:, p, 0:8], in_values=cand[:, p, :],
                                                           imm_value=NEG), reads=["h_cand", ("h_best", p)], writes=[("h_cwk", p)])
        for p in range(8):
            sc.op("vector", lambda e, p=p: e.max(out=best[:, p, 8:16], in_=cwk[:, p, :]), reads=[("h_cwk", p)], writes=[("h_best", p)])
        sc.op("vector", lambda e: e.tensor_tensor(out=ez[:], in0=best[:], in1=best[:, :, 0:1].to_broadcast([128, 8, 16]), op=ALU.subtract),
              reads=BEST_ALL, writes=["h_ez"])
        sc.op("scalar", lambda e: e.activation(out=ez[:], in_=ez[:], func=AF.Exp), reads=["h_ez"], writes=["h_ez"])
        sc.op("vector", lambda e: e.tensor_reduce(out=Z[:], in_=ez[:], axis=AX.X, op=ALU.add), reads=["h_ez"], writes=["h_Z"])
        sc.op("scalar", lambda e: e.activation(out=Z[:], in_=Z[:], func=AF.Ln), reads=["h_Z"], writes=["h_Z"])
        sc.op("vector", lambda e: e.tensor_tensor(out=bias[:], in0=best[:, :, 15], in1=best[:, :, 0], op=ALU.subtract),
              reads=BEST_ALL, writes=["h_bias"])
        sc.op("vector", lambda e: e.tensor_tensor(out=bias[:], in0=bias[:], in1=Z[:], op=ALU.subtract),
              reads=["h_bias", "h_Z"], writes=["h_bias"])
        sc.op("vector", lambda e: e.tensor_tensor(out=th[:], in0=best[:, :, 15:16].to_broadcast([128, 8, 16]), in1=t4[:, :, 0, :],
                                                  op=ALU.subtract), reads=BEST_ALL + TOPS_ALL, writes=["h_th"])
        sc.op("scalar", lambda e: e.activation(out=e1[:], in_=th[:], func=AF.Exp, scale=-1.0), reads=["h_th"], writes=["h_e1"])
        sc.op("tensor", lambda e: e.transpose(out=pss[0][:, 0:128], in_=e1[:].rearrange("t p r -> t (p r)"), identity=k.identf[:]),
              reads=["h_e1", "identf"], writes=["h_ps0"])
        sc.op("scalar", lambda e: e.activation(out=e1T[:], in_=pss[0][:, 0:128], func=AF.Copy), reads=["h_ps0"], writes=["h_e1T"])
        for p in range(8):
            sc.op("scalar", lambda e, p=p: e.activation(out=e2[:, p, :], in_=s4[:, p, 1, :], func=AF.Exp, bias=bias[:, p:p + 1]),
                  reads=["h_s", "h_bias"], writes=["h_e2"])
        R4 = Rt[:].rearrange("t j (p r) -> t j p r", r=16)
        sc.op("vector", lambda e: e.tensor_tensor(
            out=R4, in0=s4[:, :, 1, :].rearrange("t p j -> t j p").unsqueeze(3).to_broadcast([128, 128, 8, 16]),
            in1=th[:].unsqueeze(1).to_broadcast([128, 128, 8, 16]), op=ALU.is_ge),
            reads=["h_s", "h_th"], writes=["h_R"])
        sc.op("vector", lambda e: e.tensor_tensor(
            out=R4, in0=R4, in1=e2[:].rearrange("t p j -> t j p").unsqueeze(3).to_broadcast([128, 128, 8, 16]), op=ALU.mult),
            reads=["h_R", "h_e2"], writes=["h_R"])

        def emit_OH(ih):
            O4 = Oh[:].rearrange("t i (p r) -> t i p r", r=16)
            sc.op("vector", lambda e, ih=ih: e.tensor_tensor(
                out=O4, in0=s4[:, :, 0, ih * 64:(ih + 1) * 64].rearrange("t p i -> t i p").unsqueeze(3).to_broadcast([128, 64, 8, 16]),
                in1=t4[:, :, 0, :].unsqueeze(1).to_broadcast([128, 64, 8, 16]), op=ALU.is_equal),
                reads=["h_s"] + TOPS_ALL, writes=["h_O"])

        emit_OH(0)
        for j0 in range(0, 128, 4):
            ptr = ptrs[n_pg % 2]
            ptk = "h_ptr%d" % (n_pg % 2)
            n_pg += 1
            for jj in range(4):
                sc.op("tensor", lambda e, ptr=ptr, jj=jj, j0=j0: e.transpose(out=ptr[:, jj * 128:(jj + 1) * 128], in_=Rt[:, j0 + jj, :],
                                                                            identity=k.identb[:]),
                      reads=["h_R", "identb"], writes=[ptk], sig=(jj == 3))
            sc.op("vector", lambda e, ptr=ptr, j0=j0: e.tensor_tensor(
                out=RT[:, j0:j0 + 4, :], in0=ptr[:, :].rearrange("k (j t) -> k j t", t=128),
                in1=e1T[:, :].unsqueeze(1).to_broadcast([128, 4, 128]), op=ALU.mult),
                reads=[ptk, "h_e1T"], writes=["h_RT"])
        for ih in range(2):
            if ih == 1:
                emit_OH(1)
            for i0_ in range(0, 64, 4):
                ptr = ptrs[n_pg % 2]
                ptk = "h_ptr%d" % (n_pg % 2)
                n_pg += 1
                for ii in range(4):
                    sc.op("tensor", lambda e, ptr=ptr, ii=ii, i0_=i0_: e.transpose(out=ptr[:, ii * 128:(ii + 1) * 128], in_=Oh[:, i0_ + ii, :],
                                                                                  identity=k.identb[:]),
                          reads=["h_O", "identb"], writes=[ptk], sig=(ii == 3))
                sc.op("scalar", lambda e, ptr=ptr, i0_=i0_: e.activation(
                    out=OT[:, i0_:i0_ + 4, :], in_=ptr[:, :].rearrange("k (i t) -> k i t", t=128), func=AF.Copy),
                    reads=[ptk], writes=["h_OT"])
            gst = gsts[n_g % 2]
            gk = "h_gst%d" % (n_g % 2)
            n_g += 1
            for t0 in range(0, 128, 8):
                pg = pgs[n_s % 2]
                pgk = "h_pg%d" % (n_s % 2)
                n_s += 1
                for tt_ in range(8):
                    t_ = t0 + tt_
                    sc.op("tensor", lambda e, pg=pg, tt_=tt_, t_=t_: e.matmul(pg[:, :].rearrange("j (i t) -> j i t", t=8)[:, :, tt_],
                                                                             lhsT=RT[:, :, t_], rhs=OT[:, :, t_], start=True, stop=True),
                          reads=["h_RT", "h_OT"], writes=[pgk], sig=(tt_ == 7))
                sc.op("scalar", lambda e, pg=pg, gst=gst, t0=t0: e.activation(
                    out=gst[:, :, t0:t0 + 8], in_=pg[:, :].rearrange("j (i t) -> j i t", t=8), func=AF.Copy),
                    reads=[pgk], writes=[gk])
            sc.dma("sync", GTv[:, ih * 64:(ih + 1) * 64, ti * 128:(ti + 1) * 128], gst[:], reads=[gk], writes=["d_GT"])


def phase_i(k, ph, hp):
    nc, sc = k.nc, k.sc
    sb = lambda name, *a, **kw: k.sb("p%d_" % hp + name, *a, **kw)
    ps = lambda name, *a, **kw: k.ps("p%d_" % hp + name, *a, **kw)
    A = k.scr
    GE = 2
    T0 = hp * 1024
    hh = sb("i_hh", [128, NDC, 1024], BF16, ctx=ph)
    sc.dma("sync", hh[:], A["h2T"].rearrange("(c p) t -> p c t", p=128)[:, :, T0:T0 + 1024], reads=["d_h2T"], writes=["i_hh"])
    acc = k.acc
    for tt in range(8):
        sc.op("vector", lambda e, tt=tt: e.memset(acc[:, tt, :], 0.0), writes=[("acc", tt)])
    ubs = [sb("i_ub%d" % i, [128, D], BF16, ctx=ph) for i in range(4)]
    uTs = [sb("i_uT%d" % i, [128, NDC, GE * 128], BF16, ctx=ph) for i in range(2)]
    vbs = [sb("i_vb%d" % i, [128, GE, D], BF16, ctx=ph) for i in range(3)]
    gTs = [sb("i_gT%d" % i, [128, GE, 1024], BF16, ctx=ph) for i in range(2)]
    WTs = [sb("i_WT%d" % i, [128, GE, 1024], BF16, ctx=ph) for i in range(2)]
    ges = [sb("i_ge%d" % i, [128, 512], BF16, ctx=ph) for i in range(2)]
    ptus = [ps("i_ptu%d" % i, [128, 512], BF16, ctx=ph) for i in range(2)]
    pAs = [ps("i_pA%d" % i, [128, 512], ctx=ph) for i in range(2)]
    pOs = [ps("i_pO%d" % i, [128, 512], ctx=ph) for i in range(4)]
    GTv = A["GT"].rearrange("(c p) t -> p c t", p=128)
    uTv = A["uT"].rearrange("(c p) e -> p c e", p=128)
    cnt = {"u": 0, "t": 0, "a": 0, "o": 0}
    NG = 128 // GE

    def dma_ub(eg):
        for ec in range(GE):
            ch = eg * GE + ec
            sc.dma("gpsimd", ubs[ch % 4][:], k.peer_u[ch * 128:(ch + 1) * 128, :], writes=["i_ub%d" % (ch % 4)])

    def units_T(eg):
        g2 = eg % 2
        g3 = eg % 3
        uT, vb, gT = uTs[g2], vbs[g3], gTs[g2]
        uTk, gTk = "i_uT%d" % g2, "i_gT%d" % g2
        units = []

        def u0():
            sc.dma("sync", gT[:], GTv[:, eg * GE:(eg + 1) * GE, T0:T0 + 1024], reads=["d_GT"], writes=[gTk])
            if hp == 1:
                sc.dma("sync", uT[:], uTv[:, :, eg * GE * 128:(eg + 1) * GE * 128], reads=["d_uT"],
                       writes=[(uTk, ec) for ec in range(GE)])
            elif eg + 1 < NG:
                dma_ub(eg + 1)
            for ec in range(GE):
                e0 = (eg * GE + ec) * 128
                sc.dma("gpsimd", vb[:, ec, :], k.peer_v[e0:e0 + 128, :], writes=[("i_vb", g3, ec)])
        units.append(u0)
        for ec in range(GE if hp == 0 else 0):
            e0 = (eg * GE + ec) * 128
            ubi = (eg * GE + ec) % 4
            ub = ubs[ubi]
            ubk = "i_ub%d" % ubi
            for d0 in range(0, NDC, 4):
                def ub_(ec=ec, e0=e0, ub=ub, ubk=ubk, d0=d0):
                    ptu = ptus[cnt["t"] % 2]
                    ptk = "i_ptu%d" % (cnt["t"] % 2)
                    cnt["t"] += 1
                    for dd in range(4):
                        dc = d0 + dd
                        sc.op("tensor", lambda e, ptu=ptu, dd=dd, dc=dc, ub=ub: e.transpose(out=ptu[:, dd * 128:(dd + 1) * 128],
                                                                                           in_=ub[:, dc * 128:(dc + 1) * 128], identity=k.identb[:]),
                              reads=[ubk, "identb"], writes=[ptk], sig=(dd == 3))
                    if (d0 // 4) % 2 == 0:
                        sc.op("vector", lambda e, ptu=ptu, uT=uT, d0=d0, ec=ec: e.tensor_copy(
                            out=uT[:, d0:d0 + 4, ec * 128:(ec + 1) * 128], in_=ptu[:, :].rearrange("p (c n) -> p c n", n=128)),
                            reads=[ptk], writes=[(uTk, ec)])
                    else:
                        sc.op("scalar", lambda e, ptu=ptu, uT=uT, d0=d0, ec=ec: e.activation(
                            out=uT[:, d0:d0 + 4, ec * 128:(ec + 1) * 128], in_=ptu[:, :].rearrange("p (c n) -> p c n", n=128), func=AF.Copy),
                            reads=[ptk], writes=[(uTk, ec)])
                    if d0 == NDC - 4:
                        sc.dma("sync", uTv[:, :, e0:e0 + 128], uT[:, :, ec * 128:(ec + 1) * 128], reads=[(uTk, ec)], writes=["d_uT"])
                units.append(ub_)
        return units

    def units_A(eg):
        g2 = eg % 2
        uT, gT, WT = uTs[g2], gTs[g2], WTs[g2]
        uTk, gTk = "i_uT%d" % g2, "i_gT%d" % g2
        units = []
        for ec in range(GE):
            for tb in range(2):
                st = {}
                for q in range(8):
                    def ua(ec=ec, tb=tb, q=q, st=st):
                        if q == 0:
                            st["i"] = cnt["a"] % 2
                            cnt["a"] += 1
                        ai = st["i"]
                        pA = pAs[ai]
                        pAk = "i_pA%d" % ai
                        ge = ges[ai]
                        gek = "i_ge%d" % ai
                        cs_ = slice(tb * 512, (tb + 1) * 512)
                        for dc in (2 * q, 2 * q + 1):
                            sc.op("tensor", lambda e, pA=pA, dc=dc, cs_=cs_: e.matmul(
                                pA[:, :], lhsT=uT[:, dc, ec * 128:(ec + 1) * 128], rhs=hh[:, dc, cs_], start=(dc == 0), stop=(dc == NDC - 1)),
                                reads=[(uTk, ec), "i_hh"], writes=[pAk], sig=(dc == NDC - 1))
                        if q == 7:
                            sc.op("scalar", lambda e, pA=pA, ge=ge: e.activation(out=ge[:], in_=pA[:, :], func=AF.Gelu), reads=[pAk], writes=[gek])
                            sc.op("vector", lambda e, ge=ge, cs_=cs_: e.tensor_tensor(out=WT[:, ec, cs_], in0=ge[:], in1=gT[:, ec, cs_], op=ALU.mult),
                                  reads=[gek, gTk], writes=[("i_WT", g2, ec, tb)])
                    units.append(ua)
        return units

    def units_O(eg):
        g2 = eg % 2
        g3 = eg % 3
        vb, WT = vbs[g3], WTs[g2]
        units = []
        for tt in range(8):
            for nb in range(4):
                def uo(tt=tt, nb=nb):
                    pO = pOs[cnt["o"] % 4]
                    pOk = "i_pO%d" % (cnt["o"] % 4)
                    cnt["o"] += 1
                    ns_ = slice(nb * 512, (nb + 1) * 512)
                    for ec in range(GE):
                        sc.op("tensor", lambda e, pO=pO, ec=ec, ns_=ns_: e.matmul(
                            pO[:, :], lhsT=WT[:, ec, tt * 128:(tt + 1) * 128], rhs=vb[:, ec, ns_], start=(ec == 0), stop=(ec == GE - 1)),
                            reads=[("i_WT", g2, ec, tt // 4), ("i_vb", g3, ec)], writes=[pOk], sig=(ec == GE - 1))
                    sc.op("vector", lambda e, pO=pO, ns_=ns_: e.tensor_tensor(out=acc[:, tt, ns_], in0=pO[:, :], in1=acc[:, tt, ns_], op=ALU.add),
                          reads=[pOk, ("acc", tt)], writes=[("acc", tt)])
                units.append(uo)
        return units

    def merged(lists):
        lists = [l for l in lists if l]
        pos = [0] * len(lists)
        total = sum(len(l) for l in lists)
        for _ in range(total):
            best, bi = None, None
            for i, l in enumerate(lists):
                if pos[i] < len(l):
                    frac = (pos[i] + 0.5) / len(l)
                    if best is None or frac < best:
                        best, bi = frac, i
            lists[bi][pos[bi]]()
            pos[bi] += 1

    if hp == 0:
        dma_ub(0)
    for u in units_T(0):
        u()
    for eg in range(NG):
        merged([units_T(eg + 1) if eg + 1 < NG else [], units_A(eg), units_O(eg - 1) if eg >= 1 else []])
    for u in units_O(NG - 1):
        u()


def phase_j(k, ph, hp):
    nc, sc, sb, ps = k.nc, k.sc, k.sb, k.ps
    A = k.scr
    acc = k.acc
    tag = "j%d_" % hp
    fnb = sb(tag + "fnb", [128, D], ctx=ph)
    sc.dma("sync", fnb[:], k.final_norm.partition_broadcast(128), writes=[tag + "fnb"])
    x1s = [sb(tag + "x1%d" % i, [128, D], ctx=ph) for i in range(2)]
    junk = sb(tag + "junk", [128, D], ctx=ph)
    ss = sb(tag + "ss", [128, 8], ctx=ph)
    x1v = A["x1"].rearrange("(n p) d -> n p d", p=128)
    ov = k.out.rearrange("(n p) d -> n p d", p=128)
    for tt in range(8):
        ti = hp * 8 + tt
        x1 = x1s[tt % 2]
        xk = tag + "x1%d" % (tt % 2)
        sc.dma("sync", x1[:], x1v[ti], reads=["d_x1"], writes=[xk])
        sc.op("vector", lambda e, tt=tt: e.tensor_tensor(out=acc[:, tt, :], in0=acc[:, tt, :], in1=k.G2[:], op=ALU.mult),
              reads=[("acc", tt), "G1"], writes=[("acc", tt)])
        sc.op("vector", lambda e, tt=tt, x1=x1: e.tensor_tensor(out=x1[:], in0=x1[:], in1=acc[:, tt, :], op=ALU.add),
              reads=[("acc", tt), xk], writes=[xk])
        sc.op("scalar", lambda e, tt=tt, x1=x1: e.activation(out=junk[:], in_=x1[:], func=AF.Square, accum_out=ss[:, tt:tt + 1]),
              reads=[xk], writes=[tag + "junk", (tag + "ss", tt)])
        sc.op("vector", lambda e, tt=tt: e.tensor_scalar(out=ss[:, tt:tt + 1], in0=ss[:, tt:tt + 1], scalar1=1.0 / D, scalar2=EPS,
                                                         op0=ALU.mult, op1=ALU.add), reads=[(tag + "ss", tt)], writes=[(tag + "ss", tt)])
        sc.op("scalar", lambda e, tt=tt: e.activation(out=ss[:, tt:tt + 1], in_=ss[:, tt:tt + 1], func=AF.Sqrt),
              reads=[(tag + "ss", tt)], writes=[(tag + "ss", tt)])
        sc.op("vector", lambda e, tt=tt: e.reciprocal(out=ss[:, tt:tt + 1], in_=ss[:, tt:tt + 1]),
              reads=[(tag + "ss", tt)], writes=[(tag + "ss", tt)])
        sc.op("vector", lambda e, tt=tt, x1=x1: e.scalar_tensor_tensor(out=x1[:], in0=x1[:], scalar=ss[:, tt:tt + 1], in1=fnb[:],
                                                                       op0=ALU.mult, op1=ALU.mult),
              reads=[xk, (tag + "ss", tt), tag + "fnb"], writes=[xk])
        sc.dma("sync", ov[ti], x1[:], reads=[xk], writes=["d_out"])


def make_in_maps(inputs, cores):
    cst = make_consts()
    f = lambda a: np.ascontiguousarray(np.asarray(a), dtype=np.float32)
    shared = {
        "w_ada": f(inputs["w_ada"][0]), "b_ada": f(inputs["b_ada"]), "norm_mix": f(inputs["norm_mix"]),
        "norm_ffn": f(inputs["norm_ffn"]), "w_in": f(inputs["w_in"][0]), "lb_logits": f(inputs["lb_logits"]),
        "hgrn_gain": f(inputs["hgrn_gain"]), "w_up_a": f(inputs["w_up_a"][0]), "w_up_b": f(inputs["w_up_b"][0]),
        "w_out": f(inputs["w_out"][0]), "peer_w_q": f(inputs["peer_w_q"][0]), "peer_keys": f(inputs["peer_keys"][0]),
        "peer_u": f(inputs["peer_u"][0]), "peer_v": f(inputs["peer_v"][0]), "final_norm": f(inputs["final_norm"]),
    }
    shared.update(cst)
    maps = []
    for b in cores:
        m = dict(shared)
        m["x"] = f(inputs["x"][b])
        m["c"] = f(inputs["c"][b:b + 1])
        maps.append(m)
    return maps


def kernel(**inputs):
    nc = build_nc(stage=99)
    cores = list(range(NCORES))
    in_maps = make_in_maps(inputs, cores)
    res = run_bass_kernel_spmd(nc, in_maps, core_ids=cores)
    return np.stack([np.asarray(r["out"], dtype=np.float32) for r in res.results], axis=0)
```

```python
import numpy as np
import concourse.bass as bass
import concourse.mybir as mybir
from concourse.bass_utils import run_bass_kernel_spmd
from contextlib import ExitStack

F32 = mybir.dt.float32
BF16 = mybir.dt.bfloat16
AF = mybir.ActivationFunctionType
ALU = mybir.AluOpType
AX = mybir.AxisListType

COMPUTE = ("tensor", "vector", "scalar", "gpsimd")
QUEUES = ("sync",)
ALL_ENG = COMPUTE + QUEUES


class Sched:
    def __init__(self, nc):
        self.nc = nc
        self.sem = {e: nc.alloc_semaphore(name="pg_" + e) for e in COMPUTE}
        self.cnt = {e: 0 for e in COMPUTE}
        self.streams = {e: [] for e in ALL_ENG}
        self.waited = {e: {} for e in ALL_ENG}
        self.lastw = {}
        self.readers = {}
        self.dsem = {}
        self.semobj = {}

    def _deps(self, eng, reads, writes):
        waits = {}

        def need(sv):
            s, v = sv
            sid = id(s)
            self.semobj[sid] = s
            if v > waits.get(sid, 0):
                waits[sid] = v

        for k in reads:
            if k in self.lastw:
                need(self.lastw[k])
        for k in writes:
            if k in self.lastw:
                need(self.lastw[k])
            for sv in self.readers.get(k, ()):
                need(sv)
        out = []
        wd = self.waited[eng]
        for sid, v in waits.items():
            if wd.get(sid, 0) < v:
                wd[sid] = v
                out.append((self.semobj[sid], v))
        return out

    def _commit(self, my, reads, writes):
        for k in writes:
            self.lastw[k] = my
            self.readers[k] = []
        for k in reads:
            if k in writes:
                continue
            self.readers.setdefault(k, []).append(my)

    def begin_capture(self):
        self._cap = []

    def end_capture(self):
        c = self._cap
        self._cap = None
        return c

    def op(self, eng, fn, reads=(), writes=(), sig=True):
        if getattr(self, "_cap", None) is not None:
            self._cap.append(lambda: self._op(eng, fn, reads, writes, sig))
            return
        self._op(eng, fn, reads, writes, sig)

    def dma(self, q, out, in_, reads=(), writes=(), **kw):
        if getattr(self, "_cap", None) is not None:
            self._cap.append(lambda: self._dma(q, out, in_, reads, writes, **kw))
            return
        self._dma(q, out, in_, reads, writes, **kw)

    def _op(self, eng, fn, reads=(), writes=(), sig=True):
        waits = self._deps(eng, reads, writes)
        if eng == "tensor":
            waits = [(s_, v_) for (s_, v_) in waits if s_ is not self.sem["tensor"]]
        if sig:
            self.cnt[eng] += 1
            my = (self.sem[eng], self.cnt[eng])
            inc = (self.sem[eng], 1)
        else:
            assert eng == "tensor"
            my = (self.sem[eng], self.cnt[eng] + 1)
            inc = None
        self._commit(my, reads, writes)
        self.streams[eng].append((waits, fn, inc))

    def _dma(self, q, out, in_, reads=(), writes=(), **kw):
        waits = self._deps(q, reads, writes)
        key = writes[0]
        if key not in self.dsem:
            self.dsem[key] = [self.nc.alloc_semaphore(name="d%d" % len(self.dsem)), 0]
        ent = self.dsem[key]
        ent[1] += 16
        my = (ent[0], ent[1])
        self._commit(my, reads, writes)

        def fn(e):
            return e.dma_start(out=out, in_=in_, **kw)

        self.streams[q].append((waits, fn, (ent[0], 16)))

    def drain_dmas(self, q="sync"):
        waits = []
        wd = self.waited[q]
        for key, (s, v) in self.dsem.items():
            if v > 0 and wd.get(id(s), 0) < v:
                wd[id(s)] = v
                waits.append((s, v))
        if waits:
            self.streams[q].append((waits, None, None))

    def flush(self, block):
        nc = self.nc
        streams = self.streams
        self.streams = {e: [] for e in ALL_ENG}

        def mk(name):
            lst = streams[name]

            def body(e):
                for waits, fn, inc in lst:
                    for s, v in waits:
                        e.wait_ge(s, v)
                    if fn is not None:
                        inst = fn(e)
                        if inc is not None:
                            inst.then_inc(inc[0], inc[1])

            return body

        for name in ALL_ENG:
            if streams[name]:
                getattr(block, name)(mk(name))

D = 2048
S = 2048
NDC = 16
NTT = 16
IN_W = 12368
EPS = 1e-6
NCORES = 8


def make_consts():
    cst = {}
    cst["ident"] = np.eye(128, dtype=np.float32)
    cst["ones"] = np.ones((128, 128), dtype=np.float32)
    cst["tri"] = np.triu(np.ones((64, 64), dtype=np.float32))
    return cst


class K:
    pass


def merged_units(lists):
    lists = [l for l in lists if l]
    pos = [0] * len(lists)
    total = sum(len(l) for l in lists)
    for _ in range(total):
        best, bi = None, None
        for i, l in enumerate(lists):
            if pos[i] < len(l):
                frac = (pos[i] + 0.5) / len(l)
                if best is None or frac < best:
                    best, bi = frac, i
        lists[bi][pos[bi]]()
        pos[bi] += 1


def build_nc(stage=99, dbg=None):
    nc = bass.Bass("TRN2", target_bir_lowering=False)
    k = K()
    k.nc = nc
    k.stage = stage

    def din(name, shape, dtype=F32):
        return nc.dram_tensor(name, list(shape), dtype, kind="ExternalInput").ap()

    k.x = din("x", [S, D])
    k.c = din("c", [1, D])
    k.w_ada = din("w_ada", [D, 6 * D])
    k.b_ada = din("b_ada", [1, 6 * D])
    k.norm_mix = din("norm_mix", [1, D])
    k.norm_ffn = din("norm_ffn", [1, D])
    k.w_in = din("w_in", [D, IN_W])
    k.ident_d = din("ident", [128, 128])
    k.tri_d = din("tri", [64, 64])
    k.lb_logits = din("lb_logits", [2, 1024])
    k.hgrn_gain = din("hgrn_gain", [1, 128])
    k.w_up_a = din("w_up_a", [1024, D])
    k.w_up_b = din("w_up_b", [1024, D])
    k.w_out = din("w_out", [D, D])
    k.peer_w_q = din("peer_w_q", [D, D])
    k.peer_keys = din("peer_keys", [8, 2, 128, 128])
    k.peer_u = din("peer_u", [16384, D])
    k.peer_v = din("peer_v", [16384, D])
    k.final_norm = din("final_norm", [D])
    k.ones_d = din("ones", [128, 128])
    k.out = nc.dram_tensor("out", [S, D], F32, kind="ExternalOutput").ap()
    if dbg is not None:
        k.dbg = nc.dram_tensor("dbg", list(dbg), F32, kind="ExternalOutput").ap()

    sc = Sched(nc)
    k.sc = sc
    with ExitStack() as top:
        def sb(name, shape, dtype=F32, ctx=top):
            return ctx.enter_context(nc.sbuf_tensor(name, list(shape), dtype))

        def ps(name, shape, dtype=F32, ctx=top):
            return ctx.enter_context(nc.psum_tensor(name, list(shape), dtype))
        k.sb = sb
        k.ps = ps
        k.modT = sb("modT", [128, 96])
        k.G1 = sb("G1", [128, D])
        k.G2 = sb("G2", [128, D])
        k.identf = sb("identf", [128, 128])
        k.onesf = sb("onesf", [128, 128])
        k.identb = sb("identb", [128, 128], BF16)
        k.onesb = sb("onesb", [128, 128], BF16)

        def run_phase(fn):
            with ExitStack() as ph:
                fn(k, ph)
                sc.drain_dmas()
                with nc.Block() as block:
                    sc.flush(block)

        make_scratch(k)
        run_phase(phase_a)
        with ExitStack() as s1:
            k.hT = sb("hT", [128, NDC, S], BF16, ctx=s1)
            if stage >= 2:
                run_phase(phase_b)
            if stage >= 3:
                run_phase(phase_c)
            if dbg is not None and stage in (2, 3):
                run_phase(phase_dbg)
        if stage >= 4:
            run_phase(phase_d)
        if stage >= 5:
            run_phase(phase_e)
        if stage >= 6:
            with ExitStack() as s2:
                k.hT = sb("hT2", [128, NDC, S], BF16, ctx=s2)
                run_phase(phase_f)
                run_phase(phase_f2)
        if stage >= 7:
            with ExitStack() as s3:
                k.hT = sb("hT3", [128, NDC, S], BF16, ctx=s3)
                run_phase(phase_g)
                run_phase(phase_h1)
            run_phase(phase_h2)
        if stage >= 8:
            with ExitStack() as s4:
                k.acc = sb("acc", [128, 8, D], ctx=s4)
                for hp in range(2):
                    run_phase(lambda k_, ph_, hp=hp: phase_i(k_, ph_, hp))
                    run_phase(lambda k_, ph_, hp=hp: phase_j(k_, ph_, hp))
        if dbg is not None and stage in (2, 3):
            return nc
        if dbg is not None:
            run_phase(phase_dbg)
    return nc


def phase_a(k, ph):
    nc, sc, sb, ps = k.nc, k.sc, k.sb, k.ps
    sc.dma("sync", k.identf[:], k.ident_d, writes=["identf"])
    sc.dma("sync", k.onesf[:], k.ones_d, writes=["onesf"])
    sc.op("vector", lambda e: e.tensor_copy(out=k.identb[:], in_=k.identf[:]), reads=["identf"], writes=["identb"])
    sc.op("vector", lambda e: e.tensor_copy(out=k.onesb[:], in_=k.onesf[:]), reads=["onesf"], writes=["onesb"])
    cs = sb("cs", [128, 16], ctx=ph)
    sc.dma("sync", cs[:], k.c.rearrange("o (p j) -> (o p) j", p=128), writes=["cs"])
    sc.op("scalar", lambda e: e.activation(out=cs[:], in_=cs[:], func=AF.Silu), reads=["cs"], writes=["cs"])
    wv = k.w_ada.rearrange("(p j) n -> p j n", p=128)
    NB = 24
    wts = [sb("wada%d" % i, [128, 16, 512], ctx=ph) for i in range(2)]
    brs = [sb("brow%d" % i, [1, 512], ctx=ph) for i in range(2)]
    mrs = [sb("mrow%d" % i, [1, 512], ctx=ph) for i in range(2)]
    pss = [ps("pa%d" % i, [128, 512], ctx=ph) for i in range(2)]
    pbs = [ps("pbc%d" % i, [128, 512], ctx=ph) for i in range(2)]
    pc = ps("pcol", [128, 96], ctx=ph)
    for nb in range(NB):
        i2 = nb % 2
        wt, br, mr, pt, pb = wts[i2], brs[i2], mrs[i2], pss[i2], pbs[i2]
        wk, bk, mk, pk, pbk = "wada%d" % i2, "brow%d" % i2, "mrow%d" % i2, "pa%d" % i2, "pbc%d" % i2
        q = "sync" if nb % 2 == 0 else "gpsimd"
        sc.dma(q, wt[:], wv[:, :, nb * 512:(nb + 1) * 512], writes=[wk])
        sc.dma("sync", br[:], k.b_ada[:, nb * 512:(nb + 1) * 512], writes=[bk])
        for j in range(16):
            sc.op("tensor", lambda e, j=j, wt=wt, pt=pt: e.matmul(pt[0:1, :], lhsT=cs[:, j:j + 1], rhs=wt[:, j, :],
                                                             start=(j == 0), stop=(j == 15)),
                  reads=["cs", wk], writes=[pk], sig=(j == 15))
        sc.op("vector", lambda e, pt=pt, mr=mr, br=br: e.tensor_tensor(out=mr[0:1, :], in0=pt[0:1, :], in1=br[0:1, :], op=ALU.add),
              reads=[pk, bk], writes=[mk])
        for c4 in range(4):
            ch = nb * 4 + c4
            sc.op("tensor", lambda e, ch=ch, c4=c4, mr=mr: e.matmul(pc[:, ch:ch + 1], lhsT=mr[0:1, c4 * 128:(c4 + 1) * 128],
                                                                   rhs=k.onesf[0:1, 0:1], start=True, stop=True),
                  reads=[mk, "onesf"], writes=["pcol"])
        for gi, (G, off) in enumerate(((k.G1, 2 * D), (k.G2, 5 * D))):
            if off <= nb * 512 < off + D:
                o = nb * 512 - off
                sc.op("tensor", lambda e, pb=pb, mr=mr: e.matmul(pb[:, :], lhsT=k.onesf[0:1, :], rhs=mr[0:1, :],
                                                                 start=True, stop=True),
                      reads=[mk, "onesf"], writes=[pbk])
                sc.op("vector", lambda e, pb=pb, G=G, o=o: e.tensor_copy(out=G[:, o:o + 512], in_=pb[:, :]),
                      reads=[pbk], writes=["G%d" % gi])
    sc.op("vector", lambda e: e.tensor_copy(out=k.modT[:], in_=pc[:]), reads=["pcol"], writes=["modT"])


def phase_b(k, ph):
    norm_modulate(k, ph, k.x, k.norm_mix, 0, 16, [])


def norm_modulate(k, ph, src, gain_d, sh_col, sc_col, src_reads, tag="g1"):
    nc, sc, sb, ps = k.nc, k.sc, k.sb, k.ps
    gT = sb(tag + "gT", [128, NDC], ctx=ph)
    with nc.allow_non_contiguous_dma(reason="tiny gain vector"):
        pass
    sc.dma("sync", gT[:], gain_d.rearrange("o (j p) -> (o p) j", p=128), writes=["gT"], allow_slow_non_contiguous=True)
    A1 = sb(tag + "A1", [128, NDC], ctx=ph)
    sc.op("vector", lambda e: e.scalar_tensor_tensor(out=A1[:], in0=k.modT[:, sc_col:sc_col + 16], scalar=1.0, in1=gT[:],
                                                     op0=ALU.add, op1=ALU.mult),
          reads=["modT", "gT"], writes=["A1"])
    xts = [sb(tag + "xt%d" % i, [128, D], ctx=ph) for i in range(8)]
    sq = sb(tag + "sqjunk", [128, D], ctx=ph)
    ss = sb(tag + "ss", [128, 16], ctx=ph)
    pts = [ps(tag + "pb%d" % i, [128, 512], ctx=ph) for i in range(4)]
    xv = src.rearrange("(n p) d -> n p d", p=128)
    for tg in range(4):
        for tt in range(4):
            ti = tg * 4 + tt
            bi = ti % 8
            xt = xts[bi]
            xk = "xt%d" % bi
            sc.dma("sync" if ti % 2 == 0 else "gpsimd", xt[:], xv[ti], reads=list(src_reads), writes=[xk])
            sc.op("scalar", lambda e, xt=xt, ti=ti: e.activation(out=sq[:], in_=xt[:], func=AF.Square,
                                                                 accum_out=ss[:, ti:ti + 1]),
                  reads=[xk], writes=["sq", ("ss", ti)])
            sc.op("vector", lambda e, ti=ti: e.tensor_scalar(out=ss[:, ti:ti + 1], in0=ss[:, ti:ti + 1], scalar1=1.0 / D,
                                                             scalar2=EPS, op0=ALU.mult, op1=ALU.add),
                  reads=[("ss", ti)], writes=[("ss", ti)])
            sc.op("scalar", lambda e, ti=ti: e.activation(out=ss[:, ti:ti + 1], in_=ss[:, ti:ti + 1], func=AF.Sqrt),
                  reads=[("ss", ti)], writes=[("ss", ti)])
            sc.op("vector", lambda e, ti=ti: e.reciprocal(out=ss[:, ti:ti + 1], in_=ss[:, ti:ti + 1]),
                  reads=[("ss", ti)], writes=[("ss", ti)])
            sc.op("vector", lambda e, xt=xt, ti=ti: e.tensor_scalar(out=xt[:], in0=xt[:], scalar1=ss[:, ti:ti + 1],
                                                                    scalar2=None, op0=ALU.mult),
                  reads=[xk, ("ss", ti)], writes=[xk])
        for dc in range(NDC):
            pt = pts[dc % 4]
            pk = "pb%d" % (dc % 4)
            for tt in range(4):
                ti = tg * 4 + tt
                bi = ti % 8
                sc.op("tensor", lambda e, pt=pt, tt=tt, bi=bi, dc=dc: e.transpose(out=pt[:, tt * 128:(tt + 1) * 128],
                                                                                 in_=xts[bi][:, dc * 128:(dc + 1) * 128],
                                                                                 identity=k.identf[:]),
                      reads=["xt%d" % bi, "identf"], writes=[pk], sig=(tt == 3))
            sc.op("scalar", lambda e, pt=pt, dc=dc, tg=tg: e.activation(out=k.hT[:, dc, tg * 512:(tg + 1) * 512], in_=pt[:, :],
                                                                        func=AF.Identity, scale=A1[:, dc:dc + 1],
                                                                        bias=k.modT[:, sh_col + dc:sh_col + dc + 1]),
                  reads=[pk, "A1", "modT"], writes=[("hT", dc, tg)])


def phase_dbg(k, ph):
    nc, sc, sb, ps = k.nc, k.sc, k.sb, k.ps
    if k.stage == 1:
        sc.dma("sync", k.dbg[:, 0:96], k.modT[:], reads=["modT"], writes=["dbg0"])
        sc.dma("sync", k.dbg[:, 128:128 + D], k.G1[:], reads=["G0"], writes=["dbg1"])
        sc.dma("sync", k.dbg[:, 128 + D:128 + 2 * D], k.G2[:], reads=["G1"], writes=["dbg2"])
    if k.stage == 7:
        tmp = sb("dbgtmp", [128, S], ctx=ph)
        tmh = sb("dbgtmh", [128, S], BF16, ctx=ph)
        for i in range(16):
            sc.dma("sync", tmh[:], k.scr["GT"][i * 128:(i + 1) * 128, :], reads=["d_GT"], writes=["dbgtmh"])
            sc.op("vector", lambda e: e.tensor_copy(out=tmp[:], in_=tmh[:]), reads=["dbgtmh"], writes=["dbgtmp"])
            sc.dma("sync", k.dbg[i * 128:(i + 1) * 128, :], tmp[:], reads=["dbgtmp"], writes=["dbg2"])
    if k.stage == 6:
        sc.dma("sync", k.dbg[0:2048, :], k.scr["x1"], reads=["d_x1"], writes=["dbg0"])
        tmp = sb("dbgtmp", [128, S], ctx=ph)
        tmh = sb("dbgtmh", [128, S], BF16, ctx=ph)
        for nm, off in (("attnT", 2048), ("recT", 3072)):
            for i in range(8):
                sc.dma("sync", tmh[:], k.scr[nm][i * 128:(i + 1) * 128, :], reads=["d_" + nm], writes=["dbgtmh"])
                sc.op("vector", lambda e: e.tensor_copy(out=tmp[:], in_=tmh[:]), reads=["dbgtmh"], writes=["dbgtmp"])
                sc.dma("sync", k.dbg[off + i * 128:off + (i + 1) * 128, :], tmp[:], reads=["dbgtmp"], writes=["dbg2"])
    if k.stage == 5:
        tmp = sb("dbgtmp", [128, S], ctx=ph)
        tmh = sb("dbgtmh", [128, S], BF16, ctx=ph)
        for i in range(8):
            sc.dma("sync", tmh[:], k.scr["recT"][i * 128:(i + 1) * 128, :], reads=["d_recT"], writes=["dbgtmh"])
            sc.op("vector", lambda e: e.tensor_copy(out=tmp[:], in_=tmh[:]), reads=["dbgtmh"], writes=["dbgtmp"])
            sc.dma("sync", k.dbg[i * 128:(i + 1) * 128, :], tmp[:], reads=["dbgtmp"], writes=["dbg2"])
    if k.stage == 4:
        tmp = sb("dbgtmp", [128, S], ctx=ph)
        tmh = sb("dbgtmh", [128, S], BF16, ctx=ph)
        for i in range(8):
            sc.dma("sync", tmh[:], k.scr["attnT"][i * 128:(i + 1) * 128, :], reads=["d_attnT"], writes=["dbgtmh"])
            sc.op("vector", lambda e: e.tensor_copy(out=tmp[:], in_=tmh[:]), reads=["dbgtmh"], writes=["dbgtmp"])
            sc.dma("sync", k.dbg[i * 128:(i + 1) * 128, :], tmp[:], reads=["dbgtmp"], writes=["dbg2"])
    if k.stage == 3:
        sc.dma("sync", k.dbg[0:1024, :], k.scr["qBT"], reads=["d_qBT"], writes=["dbg0"])
        sc.dma("sync", k.dbg[1024:1024 + 2048, 0:16], k.scr["wi"], reads=["d_wi"], writes=["dbg1"])
        tmp = sb("dbgtmp", [128, S], ctx=ph)
        tmh = sb("dbgtmh", [128, S], BF16, ctx=ph)
        for i in range(8):
            sc.dma("sync", tmh[:], k.scr["kAT"][i * 128:(i + 1) * 128, :], reads=["d_kAT"], writes=["dbgtmh"])
            sc.op("vector", lambda e: e.tensor_copy(out=tmp[:], in_=tmh[:]), reads=["dbgtmh"], writes=["dbgtmp"])
            sc.dma("sync", k.dbg[3072 + i * 128:3072 + (i + 1) * 128, :], tmp[:], reads=["dbgtmp"], writes=["dbg2"])
        for i in range(16):
            sc.dma("sync", tmh[:, 0:1024], k.scr["vA"][i * 128:(i + 1) * 128, :], reads=["d_vA"], writes=["dbgtmh"])
            sc.op("vector", lambda e: e.tensor_copy(out=tmp[:, 0:1024], in_=tmh[:, 0:1024]), reads=["dbgtmh"], writes=["dbgtmp"])
            sc.dma("sync", k.dbg[4096 + i * 128:4096 + (i + 1) * 128, 0:1024], tmp[:, 0:1024], reads=["dbgtmp"], writes=["dbg3"])
    if k.stage == 2:
        tmp = sb("dbgtmp", [128, S], ctx=ph)
        for dc in range(NDC):
            sc.op("vector", lambda e, dc=dc: e.tensor_copy(out=tmp[:], in_=k.hT[:, dc, :]),
                  reads=[("hT", dc, tg) for tg in range(4)], writes=["dbgtmp"])
            sc.dma("sync", k.dbg[dc * 128:(dc + 1) * 128, :], tmp[:], reads=["dbgtmp"], writes=["dbg0"])


class ProjRes:
    pass


def proj_setup(k, ph, KC, tag="pj"):
    r = ProjRes()
    sb, ps = k.sb, k.ps
    r.KC = KC
    r.wb = [sb("%s_wb%d" % (tag, i), [128, KC, 512], BF16, ctx=ph) for i in range(2)]
    r.wkey = ["%s_wb%d" % (tag, i) for i in range(2)]
    r.pt = [ps("%s_ps%d" % (tag, i), [128, 512], ctx=ph) for i in range(4)]
    r.pkey = ["%s_ps%d" % (tag, i) for i in range(4)]
    r.sf = [sb("%s_sf%d" % (tag, i), [128, 512], F32, ctx=ph) for i in range(3)]
    r.sh = [sb("%s_sh%d" % (tag, i), [128, 512], BF16, ctx=ph) for i in range(3)]
    r.nblk = 0
    r.npt = 0
    r.nst = 0
    r.tag = tag
    return r


def proj_load_w(k, r, w_ap, cb0, cb1):
    sc = k.sc
    KC = r.KC
    wv = w_ap.rearrange("(kc p) n -> p kc n", p=128)
    i2 = r.nblk % 2
    r.nblk += 1
    w = cb1 - cb0
    sc.dma("gpsimd", r.wb[i2][:, :, 0:w], wv[:, :, cb0:cb1], writes=[r.wkey[i2]])
    return r.wb[i2], r.wkey[i2]


def proj_stage(r, dtype):
    i = r.nst % 3
    r.nst += 1
    if dtype == F32:
        return r.sf[i], "%s_sf%d" % (r.tag, i)
    return r.sh[i], "%s_sh%d" % (r.tag, i)


def proj_psum(r):
    i = r.npt % 4
    r.npt += 1
    return r.pt[i], r.pkey[i]


def proj(k, r, actT, act_keys, w_ap, c0, c1, mode, func, dst, dst_key, dtype):
    sc = k.sc
    KC = r.KC
    cb0 = c0
    while cb0 < c1:
        cb1 = min(c1, cb0 + 512)
        w = cb1 - cb0
        wb, wk = proj_load_w(k, r, w_ap, cb0, cb1)
        if mode == "fm":
            for sub in range((w + 127) // 128):
                cw = min(128, w - sub * 128)
                for tb in range(4):
                    pt, pk = proj_psum(r)
                    for kc in range(KC):
                        sc.op("tensor", lambda e, pt=pt, wb=wb, kc=kc, sub=sub, cw=cw, tb=tb: e.matmul(
                            pt[0:cw, :], lhsT=wb[:, kc, sub * 128: sub * 128 + cw], rhs=actT[:, kc, tb * 512:(tb + 1) * 512],
                            start=(kc == 0), stop=(kc == KC - 1)),
                            reads=[wk] + act_keys(kc, tb), writes=[pk], sig=(kc == KC - 1))
                    st, sk = proj_stage(r, dtype)
                    sc.op("scalar", lambda e, pt=pt, st=st, cw=cw: e.activation(out=st[0:cw, :], in_=pt[0:cw, :], func=func),
                          reads=[pk], writes=[sk])
                    r0 = cb0 - c0 + sub * 128
                    sc.dma("sync", dst[r0:r0 + cw, tb * 512:(tb + 1) * 512], st[0:cw, :], reads=[sk], writes=[dst_key])
        else:
            for ti in range(NTT):
                pt, pk = proj_psum(r)
                for kc in range(KC):
                    sc.op("tensor", lambda e, pt=pt, wb=wb, kc=kc, ti=ti, w=w: e.matmul(
                        pt[:, 0:w], lhsT=actT[:, kc, ti * 128:(ti + 1) * 128], rhs=wb[:, kc, 0:w],
                        start=(kc == 0), stop=(kc == KC - 1)),
                        reads=[wk] + act_keys(kc, ti // 4), writes=[pk], sig=(kc == KC - 1))
                st, sk = proj_stage(r, dtype)
                sc.op("scalar", lambda e, pt=pt, st=st, w=w: e.activation(out=st[:, 0:w], in_=pt[:, 0:w], func=func),
                      reads=[pk], writes=[sk])
                sc.dma("sync", dst[ti * 128:(ti + 1) * 128, cb0 - c0:cb1 - c0], st[:, 0:w], reads=[sk], writes=[dst_key])
        cb0 = cb1


def hT_keys(kc, tb):
    return [("hT", kc, tb)]


PROJ_SPECS = [
    ("qAT", 0, 1024, "fm", "Identity", "bf16"),
    ("kAT", 1024, 2048, "fm", "Identity", "bf16"),
    ("vA", 2048, 3072, "tm", "Identity", "bf16"),
    ("qiT", 3072, 4096, "fm", "Identity", "bf16"),
    ("kiT", 4096, 4160, "fm", "Identity", "bf16"),
    ("wi", 4160, 4176, "tm", "Identity", "f32"),
    ("qBT", 4176, 5200, "fm", "Silu", "f32"),
    ("fBT", 5200, 6224, "fm", "Sigmoid", "f32"),
    ("iB", 6224, 7248, "tm", "Identity", "bf16"),
    ("gBT", 7248, 8272, "fm", "Silu", "bf16"),
    ("sgAT", 8272, 10320, "fm", "Sigmoid", "bf16"),
    ("sgBT", 10320, 12368, "fm", "Sigmoid", "bf16"),
]


def make_scratch(k):
    nc = k.nc
    k.scr = {}
    for name, c0, c1, mode, fn, dtn in PROJ_SPECS:
        dt_ = BF16 if dtn == "bf16" else F32
        shape = [c1 - c0, S] if mode == "fm" else [S, c1 - c0]
        k.scr[name] = nc.dram_tensor("scr_" + name, shape, dt_, kind="Internal").ap()
    k.scr["attnT"] = nc.dram_tensor("scr_attnT", [1024, S], BF16, kind="Internal").ap()
    k.scr["recT"] = nc.dram_tensor("scr_recT", [1024, S], BF16, kind="Internal").ap()
    k.scr["x1"] = nc.dram_tensor("scr_x1", [S, D], F32, kind="Internal").ap()
    k.scr["h2T"] = nc.dram_tensor("scr_h2T", [D, S], BF16, kind="Internal").ap()
    k.scr["pqT"] = nc.dram_tensor("scr_pqT", [D, S], BF16, kind="Internal").ap()
    k.scr["GT"] = nc.dram_tensor("scr_GT", [16384, S], BF16, kind="Internal").ap()
    k.scr["uT"] = nc.dram_tensor("scr_uT", [D, 16384], BF16, kind="Internal").ap()


def phase_c(k, ph):
    r = proj_setup(k, ph, NDC, "pc")
    for name, c0, c1, mode, fn, dtn in PROJ_SPECS:
        dt_ = BF16 if dtn == "bf16" else F32
        proj(k, r, k.hT, hT_keys, k.w_in, c0, c1, mode, getattr(AF, fn), k.scr[name], "d_" + name, dt_)


def phase_d(k, ph):
    nc, sc, sb, ps = k.nc, k.sc, k.sb, k.ps
    A = k.scr
    kT = sb("kT", [128, 8, S], BF16, ctx=ph)
    sc.dma("sync", kT[:], A["kAT"].rearrange("(h p) t -> p h t", p=128), reads=["d_kAT"], writes=["kT"])
    vS = sb("vS", [128, 16, 1024], BF16, ctx=ph)
    sc.dma("sync", vS[:], A["vA"].rearrange("(j p) c -> p j c", p=128), reads=["d_vA"], writes=["vS"])
    kiT2 = sb("kiT2", [128, S], BF16, ctx=ph)
    sc.dma("sync", kiT2[0:64, :], A["kiT"], reads=["d_kiT"], writes=["kiT2a"])
    sc.dma("sync", kiT2[64:128, :], A["kiT"], reads=["d_kiT"], writes=["kiT2b"])
    cm29 = sb("cm29", [128, 1], ctx=ph)
    sc.op("vector", lambda e: e.memset(cm29[:], -1e29), writes=["cm29"])
    qTs = [sb("qT%d" % i, [128, 8, 512], BF16, ctx=ph) for i in range(2)]
    qiT = sb("qiT", [128, 8, 512], BF16, ctx=ph)
    wi = sb("wi", [128, 4, 16], ctx=ph)
    wabs = sb("wabs", [128, 4, 16], ctx=ph)
    wsgn = sb("wsgn", [128, 4, 16], ctx=ph)
    scrs = [sb("scr%d" % i, [128, S], ctx=ph) for i in range(2)]
    work = sb("work", [128, S], ctx=ph)
    mks = [sb("mk%d" % i, [128, S], BF16, ctx=ph) for i in range(2)]
    tmps = [sb("itmp%d" % i, [128, 512], ctx=ph) for i in range(3)]
    m8 = sb("m8", [128, 8], ctx=ph)
    NBIS = 20
    pw2 = sb("pw2", [128, NBIS], ctx=ph)
    for kk_ in range(NBIS):
        sc.op("vector", lambda e, kk_=kk_: e.memset(pw2[:, kk_:kk_ + 1], 2.0 ** -(kk_ + 1)), writes=["pw2"])
    bwk = sb("bwk", [128, NBIS], ctx=ph)
    blo = sb("blo", [128, 1], ctx=ph)
    brg = sb("brg", [128, 1], ctx=ph)
    bmid = sb("bmid", [128, 1], ctx=ph)
    bcnt = sb("bcnt", [128, 1], ctx=ph)
    bg = sb("bg", [128, 1], ctx=ph)
    maskTs = [sb("maskT%d" % i, [128, 16, 512], BF16, ctx=ph) for i in range(2)]
    pes = [sb("pe%d" % i, [128, 512], BF16, ctx=ph) for i in range(3)]
    pms = [sb("pm%d" % i, [128, 512], BF16, ctx=ph) for i in range(3)]
    rds = [sb("rd%d" % i, [128, 512], ctx=ph) for i in range(2)]
    aos = [sb("ao%d" % i, [128, 512], BF16, ctx=ph) for i in range(2)]
    pis = [ps("pi%d" % i, [128, 512], ctx=ph) for i in range(2)]
    ptr = ps("ptr", [128, 512], BF16, ctx=ph)
    pls = [ps("pl%d" % i, [128, 512], ctx=ph) for i in range(2)]
    po = ps("po", [128, 512], ctx=ph)
    pd = ps("pd", [128, 512], ctx=ph)
    att_scale = 128.0 ** -0.5
    NEG = -1e30
    n_i = 0
    n_e = 0
    n_h = 0
    streams = {}
    for tb in range(4):
        sc.begin_capture()
        maskT = maskTs[tb % 2]
        mkey = "maskT%d" % (tb % 2)
        qT = qTs[tb % 2]
        qTk = "qT%d" % (tb % 2)
        sc.dma("sync", qT[:], A["qAT"].rearrange("(h p) t -> p h t", p=128)[:, :, tb * 512:(tb + 1) * 512],
               reads=["d_qAT"], writes=[qTk])
        sc.dma("sync", qiT[:], A["qiT"].rearrange("(h p) t -> p h t", p=128)[:, :, tb * 512:(tb + 1) * 512],
               reads=["d_qiT"], writes=["qiT"])
        sc.dma("sync", wi[:], A["wi"].rearrange("(n p) h -> p n h", p=128)[:, tb * 4:(tb + 1) * 4, :],
               reads=["d_wi"], writes=["wi"])
        sc.op("scalar", lambda e: e.activation(out=wabs[:], in_=wi[:], func=AF.Abs), reads=["wi"], writes=["wabs"])
        sc.op("scalar", lambda e: e.activation(out=wsgn[:], in_=wi[:], func=AF.Sign), reads=["wi"], writes=["wsgn"])
        sc.op("vector", lambda e, maskT=maskT: e.memset(maskT[:], -30000.0), writes=[mkey])
        for tt in range(4):
            i = tb * 4 + tt
            L = 128 * (i + 1)
            scr = scrs[i % 2]
            skey = "scr%d" % (i % 2)
            mk = mks[i % 2]
            mkk = "mk%d" % (i % 2)
            for h in range(16):
                cch = h // 2
                p0 = 64 * (h % 2)
                kik = "kiT2a" if p0 == 0 else "kiT2b"
                for s0 in range(0, L, 512):
                    sw = min(512, L - s0)
                    pi = pis[n_i % 2]
                    pik = "pi%d" % (n_i % 2)
                    tmp = tmps[n_i % 3]
                    tk = "itmp%d" % (n_i % 3)
                    n_i += 1
                    sc.op("tensor", lambda e, pi=pi, p0=p0, cch=cch, tt=tt, s0=s0, sw=sw: e.matmul(
                        pi[:, 0:sw], lhsT=qiT[p0:p0 + 64, cch, tt * 128:(tt + 1) * 128], rhs=kiT2[p0:p0 + 64, s0:s0 + sw],
                        start=True, stop=True), reads=["qiT", kik], writes=[pik])
                    sc.op("scalar", lambda e, pi=pi, tmp=tmp, sw=sw, tt=tt, h=h: e.activation(
                        out=tmp[:, 0:sw], in_=pi[:, 0:sw], func=AF.Relu, scale=wabs[:, tt, h:h + 1]),
                        reads=[pik, "wabs"], writes=[tk])
                    if h == 0:
                        sc.op("vector", lambda e, tmp=tmp, scr=scr, s0=s0, sw=sw, tt=tt, h=h: e.tensor_scalar(
                            out=scr[:, s0:s0 + sw], in0=tmp[:, 0:sw], scalar1=wsgn[:, tt, h:h + 1], scalar2=None, op0=ALU.mult),
                            reads=[tk, "wsgn"], writes=[skey])
                    else:
                        sc.op("vector", lambda e, tmp=tmp, scr=scr, s0=s0, sw=sw, tt=tt, h=h: e.scalar_tensor_tensor(
                            out=scr[:, s0:s0 + sw], in0=tmp[:, 0:sw], scalar=wsgn[:, tt, h:h + 1], in1=scr[:, s0:s0 + sw],
                            op0=ALU.mult, op1=ALU.add), reads=[tk, "wsgn", skey], writes=[skey])
            if i >= 2:
                sc.op("vector", lambda e, scr=scr, L=L: e.tensor_reduce(out=blo[:], in_=scr[:, 0:L], axis=AX.X, op=ALU.min),
                      reads=[skey], writes=["blo"])
            sc.op("vector", lambda e, scr=scr, L=L: e.memset(scr[0:64, L - 64:L], NEG), writes=[skey])
            if i >= 2:
                sc.op("vector", lambda e, scr=scr, L=L: e.max(out=m8[:], in_=scr[:, 0:L]), reads=[skey], writes=["m8"])
                sc.op("vector", lambda e: e.tensor_tensor(out=brg[:], in0=m8[:, 0:1], in1=blo[:], op=ALU.subtract),
                      reads=["m8", "blo"], writes=["brg"])
                sc.op("vector", lambda e: e.tensor_scalar(out=bwk[:], in0=pw2[:], scalar1=brg[:, 0:1], scalar2=None, op0=ALU.mult),
                      reads=["pw2", "brg"], writes=["bwk"])
                sc.op("vector", lambda e: e.tensor_tensor(out=bmid[:], in0=blo[:], in1=bwk[:, 0:1], op=ALU.add),
                      reads=["blo", "bwk"], writes=["bmid"])
                for kk_ in range(NBIS):
                    sc.op("vector", lambda e, scr=scr, L=L: e.tensor_scalar(out=work[:, 0:L], in0=scr[:, 0:L], scalar1=bmid[:, 0:1], scalar2=0.0,
                                                                            op0=ALU.is_ge, op1=ALU.add, accum_out=bcnt[:, 0:1]),
                          reads=[skey, "bmid"], writes=["work", "bcnt"])
                    sc.op("vector", lambda e, kk_=kk_: e.tensor_scalar(out=bg[:], in0=bcnt[:], scalar1=255.5, scalar2=bwk[:, kk_:kk_ + 1],
                                                                       op0=ALU.is_ge, op1=ALU.mult), reads=["bcnt", "bwk"], writes=["bg"])
                    sc.op("vector", lambda e: e.tensor_tensor(out=blo[:], in0=blo[:], in1=bg[:], op=ALU.add),
                          reads=["blo", "bg"], writes=["blo"])
                    if kk_ + 1 < NBIS:
                        sc.op("vector", lambda e, kk_=kk_: e.tensor_tensor(out=bmid[:], in0=blo[:], in1=bwk[:, kk_ + 1:kk_ + 2], op=ALU.add),
                              reads=["blo", "bwk"], writes=["bmid"])
                tau = blo[:, 0:1]
                tauk = "blo"
            else:
                tau = cm29[:, 0:1]
                tauk = "cm29"
            sc.op("vector", lambda e, scr=scr, mk=mk, L=L, tau=tau: e.tensor_scalar(
                out=mk[:, 0:L], in0=scr[:, 0:L], scalar1=tau, scalar2=None, op0=ALU.is_ge),
                reads=[skey, tauk], writes=[mkk])
            for j0 in range(0, i + 1, 4):
                n = min(4, i + 1 - j0)
                for jj in range(n):
                    j = j0 + jj
                    sc.op("tensor", lambda e, jj=jj, j=j, mk=mk: e.transpose(out=ptr[:, jj * 128:(jj + 1) * 128],
                                                                             in_=mk[:, j * 128:(j + 1) * 128], identity=k.identb[:]),
                          reads=[mkk, "identb"], writes=["ptr"], sig=(jj == n - 1))
                sc.op("scalar", lambda e, j0=j0, n=n, tt=tt, maskT=maskT: e.activation(
                    out=maskT[:, j0:j0 + n, tt * 128:(tt + 1) * 128],
                    in_=ptr[:, 0:n * 128].rearrange("p (n t) -> p n t", t=128), func=AF.Identity, scale=30000.0, bias=-30000.0),
                    reads=["ptr"], writes=[mkey])
        streams[("i", tb)] = sc.end_capture()
        sc.begin_capture()
        nj = 4 * (tb + 1)
        units = [(h, j) for h in range(8) for j in range(nj)]
        ubuf = {}

        def emit_lg(u):
            h, j = units[u]
            pl = pls[u % 2]
            plk = "pl%d" % (u % 2)
            sc.op("tensor", lambda e, pl=pl, h=h, j=j, qT=qT: e.matmul(pl[:, :], lhsT=kT[:, h, j * 128:(j + 1) * 128], rhs=qT[:, h, :],
                                                               start=True, stop=False), reads=["kT", qTk], writes=[plk], sig=False)
            sc.op("tensor", lambda e, pl=pl, j=j, maskT=maskT: e.matmul(pl[:, :], lhsT=k.identb[:], rhs=maskT[:, j, :],
                                                                       start=False, stop=True), reads=["identb", mkey], writes=[plk])
            pe = pes[u % 3]
            pek = "pe%d" % (u % 3)
            sc.op("scalar", lambda e, pl=pl, pe=pe: e.activation(out=pe[:], in_=pl[:, :], func=AF.Exp, scale=att_scale),
                  reads=[plk], writes=[pek])

        emit_lg(0)
        for u, (h, j) in enumerate(units):
            if u + 1 < len(units):
                emit_lg(u + 1)
            pm = pes[u % 3]
            pmk = "pe%d" % (u % 3)
            sc.op("tensor", lambda e, pm=pm, h=h, j=j, nj=nj: e.matmul(po[:, :], lhsT=vS[:, j, h * 128:(h + 1) * 128], rhs=pm[:],
                                                                      start=(j == 0), stop=(j == nj - 1)), reads=["vS", pmk], writes=["po"], sig=False)
            sc.op("tensor", lambda e, pm=pm, j=j, nj=nj: e.matmul(pd[:, :], lhsT=k.onesb[:], rhs=pm[:],
                                                                 start=(j == 0), stop=(j == nj - 1)), reads=["onesb", pmk], writes=["pd"])
            if j == nj - 1:
                rd = rds[n_h % 2]
                rdk = "rd%d" % (n_h % 2)
                ao = aos[n_h % 2]
                aok = "ao%d" % (n_h % 2)
                n_h += 1
                sc.op("vector", lambda e, rd=rd: e.reciprocal(out=rd[:], in_=pd[:, :]), reads=["pd"], writes=[rdk])
                sc.op("vector", lambda e, rd=rd, ao=ao: e.tensor_tensor(out=ao[:], in0=po[:, :], in1=rd[:], op=ALU.mult),
                      reads=["po", rdk], writes=[aok])
                sc.dma("sync", A["attnT"][h * 128:(h + 1) * 128, tb * 512:(tb + 1) * 512], ao[:], reads=[aok], writes=["d_attnT"])
        streams[("a", tb)] = sc.end_capture()
    for u in streams[("i", 0)]:
        u()
    for tb in range(4):
        merged_units([streams[("a", tb)], streams.get(("i", tb + 1), [])])


def phase_e(k, ph):
    nc, sc, sb, ps = k.nc, k.sc, k.sb, k.ps
    A = k.scr
    NCH = 32
    lbl = sb("lbl", [128, 2, 8], ctx=ph)
    sc.dma("sync", lbl[:], k.lb_logits.rearrange("r (h d) -> d r h", d=128), writes=["lbl"], allow_slow_non_contiguous=True)
    lb = sb("lb", [128, 8], ctx=ph)
    oml = sb("oml", [128, 8], ctx=ph)
    sc.op("vector", lambda e: e.tensor_tensor(out=lb[:], in0=lbl[:, 0, :], in1=lbl[:, 1, :], op=ALU.subtract), reads=["lbl"], writes=["lb"])
    sc.op("scalar", lambda e: e.activation(out=lb[:], in_=lb[:], func=AF.Sigmoid), reads=["lb"], writes=["lb"])
    sc.op("vector", lambda e: e.tensor_scalar(out=oml[:], in0=lb[:], scalar1=-1.0, scalar2=1.0, op0=ALU.mult, op1=ALU.add),
          reads=["lb"], writes=["oml"])
    gain = sb("hgain", [128, 1], ctx=ph)
    sc.dma("sync", gain[:], k.hgrn_gain.rearrange("o d -> d o"), writes=["hgain"], allow_slow_non_contiguous=True)
    tri = sb("tri_sb", [64, 64], ctx=ph)
    sc.dma("sync", tri[:], k.tri_d, writes=["tri"])
    rst = sb("rst", [128, S], ctx=ph)
    sc.op("vector", lambda e: e.memset(rst[:], 1.0), writes=["rst"])
    sc.op("vector", lambda e: e.memset(rst[:].rearrange("p (c s) -> p c s", s=64)[:, :, 0:1], 0.0), writes=["rst"])
    qf = sb("qf", [128, S], ctx=ph)
    ff = sb("ff", [128, S], ctx=ph)
    kk = sb("kk", [128, S], ctx=ph)
    lf = sb("lf", [128, S], ctx=ph)
    bb = sb("bb", [128, S], ctx=ph)
    bp = sb("bp", [128, S], ctx=ph)
    tA = sb("tA", [128, S], ctx=ph)
    qhats = [sb("qhat%d" % i, [128, S], BF16, ctx=ph) for i in range(2)]
    qtils = [sb("qtil%d" % i, [128, S], BF16, ctx=ph) for i in range(2)]
    ktils = [sb("ktil%d" % i, [128, S], BF16, ctx=ph) for i in range(2)]
    kendTs = [sb("kendT%d" % i, [128, S], BF16, ctx=ph) for i in range(2)]
    sq = sb("hsq", [128, S], ctx=ph)
    kend = sb("kend", [64, NCH, 128], BF16, ctx=ph)
    vSs = [sb("hvS%d" % i, [64, NCH, 128], BF16, ctx=ph) for i in range(2)]
    atT = sb("atT", [64, NCH, 64], BF16, ctx=ph)
    recT = sb("recT", [128, S], ctx=ph)
    sgbs = [sb("sgb%d" % i, [128, S], BF16, ctx=ph) for i in range(2)]
    ebls = [sb("ebl%d" % i, [128, NCH], ctx=ph) for i in range(2)]
    Sf = sb("Sf", [128, 128], ctx=ph)
    Sb = sb("Sb", [128, 128], BF16, ctx=ph)
    rs = sb("hrs", [128, 512], ctx=ph)
    t1 = sb("ht1", [128, 512], ctx=ph)
    obs = [sb("hob%d" % i, [128, 512], BF16, ctx=ph) for i in range(2)]
    ptk = ps("ptk", [64, 512], BF16, ctx=ph)
    pat = ps("pat", [64, 512], ctx=ph)
    pos = [ps("hpo%d" % i, [128, 512], ctx=ph) for i in range(2)]
    pSs = [ps("hpS%d" % i, [128, 128], ctx=ph) for i in range(2)]
    pn = ps("hpn", [128, 512], ctx=ph)
    v3 = lambda t: t[:].rearrange("p (c s) -> p c s", s=64)
    n_ob = [0]
    Pst, Lst = {}, {}

    def do_head(h):
        hp_ = str(h % 2)
        qhat, qtil, ktil, kendT, vS, sgb, ebl = qhats[h % 2], qtils[h % 2], ktils[h % 2], kendTs[h % 2], vSs[h % 2], sgbs[h % 2], ebls[h % 2]
        rows = slice(h * 128, (h + 1) * 128)
        sc.begin_capture()
        sc.dma("sync", qf[:], A["qBT"][rows, :], reads=["d_qBT"], writes=["qf"])
        sc.dma("sync", ff[:], A["fBT"][rows, :], reads=["d_fBT"], writes=["ff"])
        sc.dma("sync", sgb[:], A["gBT"][rows, :], reads=["d_gBT"], writes=["sgb" + hp_])
        sc.dma("sync", vS[:], A["iB"].rearrange("(c s) n -> s c n", s=64)[:, :, rows], reads=["d_iB"], writes=["hvS" + hp_])
        sc.op("vector", lambda e, h=h: e.tensor_scalar(out=ff[:], in0=ff[:], scalar1=oml[:, h:h + 1], scalar2=lb[:, h:h + 1],
                                                       op0=ALU.mult, op1=ALU.add), reads=["ff", "oml", "lb"], writes=["ff"])
        sc.op("vector", lambda e: e.tensor_scalar(out=kk[:], in0=ff[:], scalar1=-1.0, scalar2=1.0, op0=ALU.mult, op1=ALU.add),
              reads=["ff"], writes=["kk"])
        sc.op("scalar", lambda e: e.activation(out=lf[:], in_=ff[:], func=AF.Ln), reads=["ff"], writes=["lf"])
        sc.op("vector", lambda e: e.tensor_tensor_scan(out=bb[:], data0=rst[:], data1=lf[:], initial=0.0, op0=ALU.mult, op1=ALU.add),
              reads=["rst", "lf"], writes=["bb"])
        sc.op("scalar", lambda e: e.activation(out=tA[:], in_=bb[:], func=AF.Exp), reads=["bb"], writes=["tA"])
        sc.op("vector", lambda e: e.tensor_tensor(out=qhat[:], in0=qf[:], in1=tA[:], op=ALU.mult), reads=["qf", "tA"], writes=["qhat" + hp_])
        sc.op("scalar", lambda e: e.activation(out=ebl[:], in_=v3(bb)[:, :, 63], func=AF.Exp), reads=["bb"], writes=["ebl" + hp_])
        sc.op("vector", lambda e: e.tensor_tensor(out=v3(bp), in0=v3(bb), in1=v3(bb)[:, :, 31:32].to_broadcast([128, NCH, 64]),
                                                  op=ALU.subtract), reads=["bb"], writes=["bp"])
        sc.op("scalar", lambda e: e.activation(out=tA[:], in_=bp[:], func=AF.Exp), reads=["bp"], writes=["tA"])
        sc.op("vector", lambda e: e.tensor_tensor(out=qtil[:], in0=qf[:], in1=tA[:], op=ALU.mult), reads=["qf", "tA"], writes=["qtil" + hp_])
        sc.op("scalar", lambda e: e.activation(out=tA[:], in_=bp[:], func=AF.Exp, scale=-1.0), reads=["bp"], writes=["tA"])
        sc.op("vector", lambda e: e.tensor_tensor(out=ktil[:], in0=kk[:], in1=tA[:], op=ALU.mult), reads=["kk", "tA"], writes=["ktil" + hp_])
        sc.op("vector", lambda e: e.tensor_tensor(out=v3(bp), in0=v3(bb)[:, :, 63:64].to_broadcast([128, NCH, 64]), in1=v3(bb),
                                                  op=ALU.subtract), reads=["bb"], writes=["bp"])
        sc.op("scalar", lambda e: e.activation(out=tA[:], in_=bp[:], func=AF.Exp), reads=["bp"], writes=["tA"])
        sc.op("vector", lambda e: e.tensor_tensor(out=kendT[:], in0=kk[:], in1=tA[:], op=ALU.mult), reads=["kk", "tA"], writes=["kendT" + hp_])
        Pst[h] = sc.end_capture()
        sc.begin_capture()
        for c0 in range(0, NCH, 4):
            for jj in range(4):
                c = c0 + jj
                sc.op("tensor", lambda e, jj=jj, c=c: e.transpose(out=ptk[:, jj * 128:(jj + 1) * 128], in_=kendT[:, c * 64:(c + 1) * 64],
                                                                 identity=k.identb[:]), reads=["kendT" + hp_, "identb"], writes=["ptk"], sig=(jj == 3))
            sc.op("scalar", lambda e, c0=c0: e.activation(out=kend[:, c0:c0 + 4, :], in_=ptk[:, :].rearrange("p (n d) -> p n d", d=128),
                                                          func=AF.Copy), reads=["ptk"], writes=["kend"])
        for c0 in range(0, NCH, 8):
            for jj in range(8):
                c = c0 + jj
                sc.op("tensor", lambda e, jj=jj, c=c: e.matmul(pat[:, jj * 64:(jj + 1) * 64], lhsT=ktil[:, c * 64:(c + 1) * 64],
                                                              rhs=qtil[:, c * 64:(c + 1) * 64], start=True, stop=True),
                      reads=["ktil" + hp_, "qtil" + hp_], writes=["pat"], sig=(jj == 7))
            sc.op("vector", lambda e, c0=c0: e.tensor_tensor(out=atT[:, c0:c0 + 8, :], in0=pat[:, :].rearrange("p (n t) -> p n t", t=64),
                                                             in1=tri[:, :].unsqueeze(1).to_broadcast([64, 8, 64]), op=ALU.mult),
                  reads=["pat", "tri"], writes=["atT"])
        for c in range(NCH):
            po = pos[(c // 8) % 2]
            pok = "hpo%d" % ((c // 8) % 2)
            col = (c % 8) * 64
            if c > 0:
                sc.op("tensor", lambda e, po=po, col=col, c=c: e.matmul(po[:, col:col + 64], lhsT=Sb[:], rhs=qhat[:, c * 64:(c + 1) * 64],
                                                                       start=True, stop=False), reads=["Sb", "qhat" + hp_], writes=[pok], sig=False)
            sc.op("tensor", lambda e, po=po, col=col, c=c: e.matmul(po[:, col:col + 64], lhsT=vS[:, c, :], rhs=atT[:, c, :],
                                                                   start=(c == 0), stop=True), reads=["hvS" + hp_, "atT"], writes=[pok])
            if c < NCH - 1:
                pS = pSs[c % 2]
                pSk = "hpS%d" % (c % 2)
                sc.op("tensor", lambda e, pS=pS, c=c: e.matmul(pS[:, :], lhsT=kend[:, c, :], rhs=vS[:, c, :], start=True, stop=True),
                      reads=["kend", "hvS" + hp_], writes=[pSk])
                if c == 0:
                    sc.op("vector", lambda e, pS=pS: e.tensor_copy(out=Sb[:], in_=pS[:, :]), reads=[pSk], writes=["Sb"])
                    sc.op("vector", lambda e, pS=pS: e.tensor_copy(out=Sf[:], in_=pS[:, :]), reads=[pSk], writes=["Sf"])
                else:
                    sc.op("vector", lambda e, pS=pS, c=c: e.scalar_tensor_tensor(out=Sb[:], in0=Sf[:], scalar=ebl[:, c:c + 1], in1=pS[:, :],
                                                                                op0=ALU.mult, op1=ALU.add),
                          reads=["Sf", "ebl" + hp_, pSk], writes=["Sb"])
                    sc.op("vector", lambda e, pS=pS, c=c: e.scalar_tensor_tensor(out=Sf[:], in0=Sf[:], scalar=ebl[:, c:c + 1], in1=pS[:, :],
                                                                                op0=ALU.mult, op1=ALU.add),
                          reads=["Sf", "ebl" + hp_, pSk], writes=["Sf"])
            if c % 8 == 7:
                sc.op("scalar", lambda e, po=po, c=c: e.activation(out=recT[:, (c - 7) * 64:(c + 1) * 64], in_=po[:, :], func=AF.Copy),
                      reads=[pok], writes=["recT"])
        sc.op("scalar", lambda e: e.activation(out=sq[:], in_=recT[:], func=AF.Square), reads=["recT"], writes=["hsq"])
        for tb in range(4):
            cs_ = slice(tb * 512, (tb + 1) * 512)
            sc.op("tensor", lambda e, cs_=cs_: e.matmul(pn[:, :], lhsT=k.onesf[:], rhs=sq[:, cs_], start=True, stop=True),
                  reads=["onesf", "hsq"], writes=["hpn"])
            sc.op("vector", lambda e: e.tensor_scalar(out=rs[:], in0=pn[:, :], scalar1=1.0 / 128, scalar2=EPS, op0=ALU.mult, op1=ALU.add),
                  reads=["hpn"], writes=["hrs"])
            sc.op("scalar", lambda e: e.activation(out=rs[:], in_=rs[:], func=AF.Sqrt), reads=["hrs"], writes=["hrs"])
            sc.op("vector", lambda e: e.reciprocal(out=rs[:], in_=rs[:]), reads=["hrs"], writes=["hrs"])
            sc.op("vector", lambda e, cs_=cs_: e.scalar_tensor_tensor(out=t1[:], in0=recT[:, cs_], scalar=gain[:, 0:1], in1=rs[:],
                                                                      op0=ALU.mult, op1=ALU.mult), reads=["recT", "hgain", "hrs"], writes=["ht1"])
            ob = obs[n_ob[0] % 2]
            obk = "hob%d" % (n_ob[0] % 2)
            n_ob[0] += 1
            sc.op("vector", lambda e, ob=ob, cs_=cs_: e.tensor_tensor(out=ob[:], in0=t1[:], in1=sgb[:, cs_], op=ALU.mult),
                  reads=["ht1", "sgb" + hp_], writes=[obk])
            sc.dma("sync", A["recT"][rows, cs_], ob[:], reads=[obk], writes=["d_recT"])
        Lst[h] = sc.end_capture()

    for h in range(8):
        do_head(h)
    for u in Pst[0]:
        u()
    for h in range(8):
        merged_units([Lst[h], Pst.get(h + 1, [])])


def phase_f(k, ph):
    nc, sc, sb, ps = k.nc, k.sc, k.sb, k.ps
    A = k.scr
    aT = sb("f_aT", [128, 8, S], BF16, ctx=ph)
    rT = sb("f_rT", [128, 8, S], BF16, ctx=ph)
    sc.dma("sync", aT[:], A["attnT"].rearrange("(h p) t -> p h t", p=128), reads=["d_attnT"], writes=["f_aT"])
    sc.dma("sync", rT[:], A["recT"].rearrange("(h p) t -> p h t", p=128), reads=["d_recT"], writes=["f_rT"])
    mixT = k.hT
    was = [sb("f_wa%d" % i, [128, 8, 512], BF16, ctx=ph) for i in range(2)]
    wbs = [sb("f_wb%d" % i, [128, 8, 512], BF16, ctx=ph) for i in range(2)]
    sgas = [sb("f_sga%d" % i, [128, 512], BF16, ctx=ph) for i in range(2)]
    sgbs = [sb("f_sgb%d" % i, [128, 512], BF16, ctx=ph) for i in range(2)]
    yas = [sb("f_ya%d" % i, [128, 512], ctx=ph) for i in range(2)]
    ybs = [sb("f_yb%d" % i, [128, 512], ctx=ph) for i in range(2)]
    pas = [ps("f_pa%d" % i, [128, 512], ctx=ph) for i in range(2)]
    pbs = [ps("f_pb%d" % i, [128, 512], ctx=ph) for i in range(2)]
    wva = k.w_up_a.rearrange("(kc p) n -> p kc n", p=128)
    wvb = k.w_up_b.rearrange("(kc p) n -> p kc n", p=128)
    n = 0
    for nb in range(4):
        wa, wb = was[nb % 2], wbs[nb % 2]
        wak, wbk = "f_wa%d" % (nb % 2), "f_wb%d" % (nb % 2)
        sc.dma("gpsimd", wa[:], wva[:, :, nb * 512:(nb + 1) * 512], writes=[wak])
        sc.dma("gpsimd", wb[:], wvb[:, :, nb * 512:(nb + 1) * 512], writes=[wbk])
        for sub in range(4):
            nch = nb * 4 + sub
            for tb in range(4):
                i2 = n % 2
                n += 1
                pa, pb = pas[i2], pbs[i2]
                pak, pbk = "f_pa%d" % i2, "f_pb%d" % i2
                sga, sgb = sgas[i2], sgbs[i2]
                ya, yb = yas[i2], ybs[i2]
                cs_ = slice(tb * 512, (tb + 1) * 512)
                sc.dma("sync", sga[:], A["sgAT"][nch * 128:(nch + 1) * 128, cs_], reads=["d_sgAT"], writes=["f_sga%d" % i2])
                sc.dma("sync", sgb[:], A["sgBT"][nch * 128:(nch + 1) * 128, cs_], reads=["d_sgBT"], writes=["f_sgb%d" % i2])
                for kc in range(8):
                    sc.op("tensor", lambda e, pa=pa, wa=wa, kc=kc, sub=sub, cs_=cs_: e.matmul(
                        pa[:, :], lhsT=wa[:, kc, sub * 128:(sub + 1) * 128], rhs=aT[:, kc, cs_], start=(kc == 0), stop=(kc == 7)),
                        reads=[wak, "f_aT"], writes=[pak], sig=(kc == 7))
                for kc in range(8):
                    sc.op("tensor", lambda e, pb=pb, wb=wb, kc=kc, sub=sub, cs_=cs_: e.matmul(
                        pb[:, :], lhsT=wb[:, kc, sub * 128:(sub + 1) * 128], rhs=rT[:, kc, cs_], start=(kc == 0), stop=(kc == 7)),
                        reads=[wbk, "f_rT"], writes=[pbk], sig=(kc == 7))
                sc.op("vector", lambda e, pa=pa, ya=ya, sga=sga: e.tensor_tensor(out=ya[:], in0=pa[:, :], in1=sga[:], op=ALU.mult),
                      reads=[pak, "f_sga%d" % i2], writes=["f_ya%d" % i2])
                sc.op("vector", lambda e, pb=pb, yb=yb, sgb=sgb: e.tensor_tensor(out=yb[:], in0=pb[:, :], in1=sgb[:], op=ALU.mult),
                      reads=[pbk, "f_sgb%d" % i2], writes=["f_yb%d" % i2])
                sc.op("vector", lambda e, ya=ya, yb=yb, nch=nch, cs_=cs_: e.tensor_tensor(out=mixT[:, nch, cs_], in0=ya[:], in1=yb[:], op=ALU.add),
                      reads=["f_ya%d" % i2, "f_yb%d" % i2], writes=[("hT", nch, tb)])


def phase_f2(k, ph):
    nc, sc, sb, ps = k.nc, k.sc, k.sb, k.ps
    A = k.scr
    mixT = k.hT
    r = proj_setup(k, ph, NDC, "fo")
    xss = [sb("f_xs%d" % i, [128, 512], ctx=ph) for i in range(3)]
    tts = [sb("f_tt%d" % i, [128, 512], ctx=ph) for i in range(3)]
    xv = k.x.rearrange("(n p) d -> n p d", p=128)
    x1v = A["x1"].rearrange("(n p) d -> n p d", p=128)
    m = 0
    for nb in range(4):
        cs_ = slice(nb * 512, (nb + 1) * 512)
        wb, wk = proj_load_w(k, r, k.w_out, nb * 512, (nb + 1) * 512)
        for ti in range(NTT):
            pt, pk = proj_psum(r)
            i3 = m % 3
            m += 1
            xs, tt_ = xss[i3], tts[i3]
            sc.dma("sync", xs[:], xv[ti][:, cs_], writes=["f_xs%d" % i3])
            for kc in range(NDC):
                sc.op("tensor", lambda e, pt=pt, wb=wb, kc=kc, ti=ti: e.matmul(
                    pt[:, :], lhsT=mixT[:, kc, ti * 128:(ti + 1) * 128], rhs=wb[:, kc, :], start=(kc == 0), stop=(kc == NDC - 1)),
                    reads=[wk, ("hT", kc, ti // 4)], writes=[pk], sig=(kc == NDC - 1))
            sc.op("vector", lambda e, pt=pt, tt_=tt_, cs_=cs_: e.tensor_tensor(out=tt_[:], in0=pt[:, :], in1=k.G1[:, cs_], op=ALU.mult),
                  reads=[pk, "G0"], writes=["f_tt%d" % i3])
            sc.op("vector", lambda e, tt_=tt_, xs=xs: e.tensor_tensor(out=tt_[:], in0=tt_[:], in1=xs[:], op=ALU.add),
                  reads=["f_tt%d" % i3, "f_xs%d" % i3], writes=["f_tt%d" % i3])
            sc.dma("sync", x1v[ti][:, cs_], tt_[:], reads=["f_tt%d" % i3], writes=["d_x1"])


def phase_g(k, ph):
    sc = k.sc
    norm_modulate(k, ph, k.scr["x1"], k.norm_ffn, 48, 64, ["d_x1"], tag="g2")
    for dc in range(NDC):
        sc.dma("sync", k.scr["h2T"][dc * 128:(dc + 1) * 128, :], k.hT[:, dc, :], reads=[("hT", dc, tg) for tg in range(4)],
               writes=["d_h2T"])


def phase_h1(k, ph):
    r = proj_setup(k, ph, NDC, "ph")
    proj(k, r, k.hT, hT_keys, k.peer_w_q, 0, 2048, "fm", AF.Identity, k.scr["pqT"], "d_pqT", BF16)


def phase_h2(k, ph):
    nc, sc, sb, ps = k.nc, k.sc, k.sb, k.ps
    A = k.scr
    NEG = -1e30
    TOPS_ALL = [("h_tops", c_) for c_ in range(16)]
    BEST_ALL = [("h_best", p_) for p_ in range(8)]
    kl = sb("h_kl", [128, 16, 128], ctx=ph)
    sc.dma("sync", kl[:], k.peer_keys.rearrange("p h n d -> n (p h) d"), writes=["h_kl"])
    keysT = sb("h_keysT", [128, 16, 128], BF16, ctx=ph)
    pss = [ps("h_ps%d" % i, [128, 512], ctx=ph) for i in range(4)]
    for ch in range(16):
        b = ch // 4
        sc.op("tensor", lambda e, ch=ch, b=b: e.transpose(out=pss[b][:, (ch % 4) * 128:(ch % 4 + 1) * 128], in_=kl[:, ch, :],
                                                         identity=k.identf[:]), reads=["h_kl", "identf"], writes=["h_ps%d" % b], sig=(ch % 4 == 3))
    for b in range(4):
        sc.op("vector", lambda e, b=b: e.tensor_copy(out=keysT[:, b * 4:(b + 1) * 4, :],
                                                     in_=pss[b][:, :].rearrange("p (c n) -> p c n", n=128)),
              reads=["h_ps%d" % b], writes=["h_keysT"])
    qts = [sb("h_qt%d" % i, [128, 16, 128], BF16, ctx=ph) for i in range(2)]
    s_sb = sb("h_s", [128, 16, 128], ctx=ph)
    wk = sb("h_wk", [128, 16, 128], ctx=ph)
    tops = sb("h_tops", [128, 16, 16], ctx=ph)
    cand = sb("h_cand", [128, 8, 256], ctx=ph)
    cwk = sb("h_cwk", [128, 8, 256], ctx=ph)
    best = sb("h_best", [128, 8, 16], ctx=ph)
    ez = sb("h_ez", [128, 8, 16], ctx=ph)
    Z = sb("h_Z", [128, 8], ctx=ph)
    bias = sb("h_bias", [128, 8], ctx=ph)
    th = sb("h_th", [128, 8, 16], ctx=ph)
    e1 = sb("h_e1", [128, 8, 16], ctx=ph)
    e1T = sb("h_e1T", [128, 128], ctx=ph)
    e2 = sb("h_e2", [128, 8, 128], ctx=ph)
    Rt = sb("h_R", [128, 128, 128], BF16, ctx=ph)
    Oh = sb("h_O", [128, 64, 128], BF16, ctx=ph)
    RT = sb("h_RT", [128, 128, 128], BF16, ctx=ph)
    OT = sb("h_OT", [128, 64, 128], BF16, ctx=ph)
    gsts = [sb("h_gst%d" % i, [128, 64, 128], BF16, ctx=ph) for i in range(2)]
    ptrs = [ps("h_ptr%d" % i, [128, 512], BF16, ctx=ph) for i in range(2)]
    pgs = [ps("h_pg%d" % i, [128, 512], ctx=ph) for i in range(2)]
    pqv = A["pqT"].rearrange("(c p) t -> p c t", p=128)
    GTv = A["GT"].rearrange("(i j) t -> j i t", j=128)
    s4 = s_sb[:].rearrange("p (a h) n -> p a h n", h=2)
    t4 = tops[:].rearrange("p (a h) n -> p a h n", h=2)
    n_s = 0
    n_g = 0
    n_pg = 0
    for ti in range(NTT):
        qt = qts[ti % 2]
        qk = "h_qt%d" % (ti % 2)
        sc.dma("sync", qt[:], pqv[:, :, ti * 128:(ti + 1) * 128], reads=["d_pqT"], writes=[qk])
        for ch in range(16):
            b = ch // 4
            sc.op("tensor", lambda e, ch=ch, b=b, qt=qt: e.matmul(pss[b][:, (ch % 4) * 128:(ch % 4 + 1) * 128], lhsT=qt[:, ch, :],
                                                                 rhs=keysT[:, ch, :], start=True, stop=True),
                  reads=[qk, "h_keysT"], writes=["h_ps%d" % b], sig=(ch % 4 == 3))
        for b in range(4):
            sc.op("scalar", lambda e, b=b: e.activation(out=s_sb[:, b * 4:(b + 1) * 4, :],
                                                        in_=pss[b][:, :].rearrange("p (c n) -> p c n", n=128), func=AF.Copy),
                  reads=["h_ps%d" % b], writes=["h_s"])
        for ch in range(16):
            sc.op("vector", lambda e, ch=ch: e.max(out=tops[:, ch, 0:8], in_=s_sb[:, ch, :]), reads=["h_s"], writes=[("h_tops", ch)])
        for ch in range(16):
            sc.op("vector", lambda e, ch=ch: e.match_replace(out=wk[:, ch, :], in_to_replace=tops[:, ch, 0:8], in_values=s_sb[:, ch, :],
                                                             imm_value=NEG), reads=["h_s", ("h_tops", ch)], writes=[("h_wk", ch)])
        for ch in range(16):
            sc.op("vector", lambda e, ch=ch: e.max(out=tops[:, ch, 8:16], in_=wk[:, ch, :]), reads=[("h_wk", ch)], writes=[("h_tops", ch)])
        sc.op("vector", lambda e: e.tensor_tensor(out=cand[:].rearrange("p a (r c) -> p a r c", c=16),
                                                  in0=t4[:, :, 0, :].unsqueeze(3).to_broadcast([128, 8, 16, 16]),
                                                  in1=t4[:, :, 1, :].unsqueeze(2).to_broadcast([128, 8, 16, 16]), op=ALU.add),
              reads=TOPS_ALL, writes=["h_cand"])
        for p in range(8):
            sc.op("vector", lambda e, p=p: e.max(out=best[:, p, 0:8], in_=cand[:, p, :]), reads=["h_cand"], writes=[("h_best", p)])
        for p in range(8):
            sc.op("vector", lambda e, p=p: e.match_replace(out=cwk[:, p, :], in_to_replace=best[:, p, 0:8], in_values=cand[:, p, :],
                                                           imm_value=NEG), reads=["h_cand", ("h_best", p)], writes=[("h_cwk", p)])
        for p in range(8):
            sc.op("vector", lambda e, p=p: e.max(out=best[:, p, 8:16], in_=cwk[:, p, :]), reads=[("h_cwk", p)], writes=[("h_best", p)])
        sc.op("vector", lambda e: e.tensor_tensor(out=ez[:], in0=best[:], in1=best[:, :, 0:1].to_broadcast([128, 8, 16]), op=ALU.subtract),
              reads=BEST_ALL, writes=["h_ez"])
        sc.op("scalar", lambda e: e.activation(out=ez[:], in_=ez[:], func=AF.Exp), reads=["h_ez"], writes=["h_ez"])
        sc.op("vector", lambda e: e.tensor_reduce(out=Z[:], in_=ez[:], axis=AX.X, op=ALU.add), reads=["h_ez"], writes=["h_Z"])
        sc.op("scalar", lambda e: e.activation(out=Z[:], in_=Z[:], func=AF.Ln), reads=["h_Z"], writes=["h_Z"])
        sc.op("vector", lambda e: e.tensor_tensor(out=bias[:], in0=best[:, :, 15], in1=best[:, :, 0], op=ALU.subtract),
              reads=BEST_ALL, writes=["h_bias"])
        sc.op("vector", lambda e: e.tensor_tensor(out=bias[:], in0=bias[:], in1=Z[:], op=ALU.subtract),
              reads=["h_bias", "h_Z"], writes=["h_bias"])
        sc.op("vector", lambda e: e.tensor_tensor(out=th[:], in0=best[:, :, 15:16].to_broadcast([128, 8, 16]), in1=t4[:, :, 0, :],
                                                  op=ALU.subtract), reads=BEST_ALL + TOPS_ALL, writes=["h_th"])
        sc.op("scalar", lambda e: e.activation(out=e1[:], in_=th[:], func=AF.Exp, scale=-1.0), reads=["h_th"], writes=["h_e1"])
        sc.op("tensor", lambda e: e.transpose(out=pss[0][:, 0:128], in_=e1[:].rearrange("t p r -> t (p r)"), identity=k.identf[:]),
              reads=["h_e1", "identf"], writes=["h_ps0"])
        sc.op("scalar", lambda e: e.activation(out=e1T[:], in_=pss[0][:, 0:128], func=AF.Copy), reads=["h_ps0"], writes=["h_e1T"])
        for p in range(8):
            sc.op("scalar", lambda e, p=p: e.activation(out=e2[:, p, :], in_=s4[:, p, 1, :], func=AF.Exp, bias=bias[:, p:p + 1]),
                  reads=["h_s", "h_bias"], writes=["h_e2"])
        R4 = Rt[:].rearrange("t j (p r) -> t j p r", r=16)
        sc.op("vector", lambda e: e.tensor_tensor(
            out=R4, in0=s4[:, :, 1, :].rearrange("t p j -> t j p").unsqueeze(3).to_broadcast([128, 128, 8, 16]),
            in1=th[:].unsqueeze(1).to_broadcast([128, 128, 8, 16]), op=ALU.is_ge),
            reads=["h_s", "h_th"], writes=["h_R"])
        sc.op("vector", lambda e: e.tensor_tensor(
            out=R4, in0=R4, in1=e2[:].rearrange("t p j -> t j p").unsqueeze(3).to_broadcast([128, 128, 8, 16]), op=ALU.mult),
            reads=["h_R", "h_e2"], writes=["h_R"])

        def emit_OH(ih):
            O4 = Oh[:].rearrange("t i (p r) -> t i p r", r=16)
            sc.op("vector", lambda e, ih=ih: e.tensor_tensor(
                out=O4, in0=s4[:, :, 0, ih * 64:(ih + 1) * 64].rearrange("t p i -> t i p").unsqueeze(3).to_broadcast([128, 64, 8, 16]),
                in1=t4[:, :, 0, :].unsqueeze(1).to_broadcast([128, 64, 8, 16]), op=ALU.is_equal),
                reads=["h_s"] + TOPS_ALL, writes=["h_O"])

        emit_OH(0)
        for j0 in range(0, 128, 4):
            ptr = ptrs[n_pg % 2]
            ptk = "h_ptr%d" % (n_pg % 2)
            n_pg += 1
            for jj in range(4):
                sc.op("tensor", lambda e, ptr=ptr, jj=jj, j0=j0: e.transpose(out=ptr[:, jj * 128:(jj + 1) * 128], in_=Rt[:, j0 + jj, :],
                                                                            identity=k.identb[:]),
                      reads=["h_R", "identb"], writes=[ptk], sig=(jj == 3))
            sc.op("vector", lambda e, ptr=ptr, j0=j0: e.tensor_tensor(
                out=RT[:, j0:j0 + 4, :], in0=ptr[:, :].rearrange("k (j t) -> k j t", t=128),
                in1=e1T[:, :].unsqueeze(1).to_broadcast([128, 4, 128]), op=ALU.mult),
                reads=[ptk, "h_e1T"], writes=["h_RT"])
        for ih in range(2):
            if ih == 1:
                emit_OH(1)
            for i0_ in range(0, 64, 4):
                ptr = ptrs[n_pg % 2]
                ptk = "h_ptr%d" % (n_pg % 2)
                n_pg += 1
                for ii in range(4):
                    sc.op("tensor", lambda e, ptr=ptr, ii=ii, i0_=i0_: e.transpose(out=ptr[:, ii * 128:(ii + 1) * 128], in_=Oh[:, i0_ + ii, :],
                                                                                  identity=k.identb[:]),
                          reads=["h_O", "identb"], writes=[ptk], sig=(ii == 3))
                sc.op("scalar", lambda e, ptr=ptr, i0_=i0_: e.activation(
                    out=OT[:, i0_:i0_ + 4, :], in_=ptr[:, :].rearrange("k (i t) -> k i t", t=128), func=AF.Copy),
                    reads=[ptk], writes=["h_OT"])
            gst = gsts[n_g % 2]
            gk = "h_gst%d" % (n_g % 2)
            n_g += 1
            for t0 in range(0, 128, 8):
                pg = pgs[n_s % 2]
                pgk = "h_pg%d" % (n_s % 2)
                n_s += 1
                for tt_ in range(8):
                    t_ = t0 + tt_
                    sc.op("tensor", lambda e, pg=pg, tt_=tt_, t_=t_: e.matmul(pg[:, :].rearrange("j (i t) -> j i t", t=8)[:, :, tt_],
                                                                             lhsT=RT[:, :, t_], rhs=OT[:, :, t_], start=True, stop=True),
                          reads=["h_RT", "h_OT"], writes=[pgk], sig=(tt_ == 7))
                sc.op("scalar", lambda e, pg=pg, gst=gst, t0=t0: e.activation(
                    out=gst[:, :, t0:t0 + 8], in_=pg[:, :].rearrange("j (i t) -> j i t", t=8), func=AF.Copy),
                    reads=[pgk], writes=[gk])
            sc.dma("sync", GTv[:, ih * 64:(ih + 1) * 64, ti * 128:(ti + 1) * 128], gst[:], reads=[gk], writes=["d_GT"])


def phase_i(k, ph, hp):
    nc, sc = k.nc, k.sc
    sb = lambda name, *a, **kw: k.sb("p%d_" % hp + name, *a, **kw)
    ps = lambda name, *a, **kw: k.ps("p%d_" % hp + name, *a, **kw)
    A = k.scr
    GE = 2
    T0 = hp * 1024
    hh = sb("i_hh", [128, NDC, 1024], BF16, ctx=ph)
    sc.dma("sync", hh[:], A["h2T"].rearrange("(c p) t -> p c t", p=128)[:, :, T0:T0 + 1024], reads=["d_h2T"], writes=["i_hh"])
    acc = k.acc
    for tt in range(8):
        sc.op("vector", lambda e, tt=tt: e.memset(acc[:, tt, :], 0.0), writes=[("acc", tt)])
    ubs = [sb("i_ub%d" % i, [128, D], BF16, ctx=ph) for i in range(4)]
    uTs = [sb("i_uT%d" % i, [128, NDC, GE * 128], BF16, ctx=ph) for i in range(2)]
    vbs = [sb("i_vb%d" % i, [128, GE, D], BF16, ctx=ph) for i in range(3)]
    gTs = [sb("i_gT%d" % i, [128, GE, 1024], BF16, ctx=ph) for i in range(2)]
    WTs = [sb("i_WT%d" % i, [128, GE, 1024], BF16, ctx=ph) for i in range(2)]
    ges = [sb("i_ge%d" % i, [128, 512], BF16, ctx=ph) for i in range(2)]
    ptus = [ps("i_ptu%d" % i, [128, 512], BF16, ctx=ph) for i in range(2)]
    pAs = [ps("i_pA%d" % i, [128, 512], ctx=ph) for i in range(2)]
    pOs = [ps("i_pO%d" % i, [128, 512], ctx=ph) for i in range(4)]
    GTv = A["GT"].rearrange("(c p) t -> p c t", p=128)
    uTv = A["uT"].rearrange("(c p) e -> p c e", p=128)
    cnt = {"u": 0, "t": 0, "a": 0, "o": 0}
    NG = 128 // GE

    def dma_ub(eg):
        for ec in range(GE):
            ch = eg * GE + ec
            sc.dma("gpsimd", ubs[ch % 4][:], k.peer_u[ch * 128:(ch + 1) * 128, :], writes=["i_ub%d" % (ch % 4)])

    def units_T(eg):
        g2 = eg % 2
        g3 = eg % 3
        uT, vb, gT = uTs[g2], vbs[g3], gTs[g2]
        uTk, gTk = "i_uT%d" % g2, "i_gT%d" % g2
        units = []

        def u0():
            sc.dma("sync", gT[:], GTv[:, eg * GE:(eg + 1) * GE, T0:T0 + 1024], reads=["d_GT"], writes=[gTk])
            if hp == 1:
                sc.dma("sync", uT[:], uTv[:, :, eg * GE * 128:(eg + 1) * GE * 128], reads=["d_uT"],
                       writes=[(uTk, ec) for ec in range(GE)])
            elif eg + 1 < NG:
                dma_ub(eg + 1)
            for ec in range(GE):
                e0 = (eg * GE + ec) * 128
                sc.dma("gpsimd", vb[:, ec, :], k.peer_v[e0:e0 + 128, :], writes=[("i_vb", g3, ec)])
        units.append(u0)
        for ec in range(GE if hp == 0 else 0):
            e0 = (eg * GE + ec) * 128
            ubi = (eg * GE + ec) % 4
            ub = ubs[ubi]
            ubk = "i_ub%d" % ubi
            for d0 in range(0, NDC, 4):
                def ub_(ec=ec, e0=e0, ub=ub, ubk=ubk, d0=d0):
                    ptu = ptus[cnt["t"] % 2]
                    ptk = "i_ptu%d" % (cnt["t"] % 2)
                    cnt["t"] += 1
                    for dd in range(4):
                        dc = d0 + dd
                        sc.op("tensor", lambda e, ptu=ptu, dd=dd, dc=dc, ub=ub: e.transpose(out=ptu[:, dd * 128:(dd + 1) * 128],
                                                                                           in_=ub[:, dc * 128:(dc + 1) * 128], identity=k.identb[:]),
                              reads=[ubk, "identb"], writes=[ptk], sig=(dd == 3))
                    if (d0 // 4) % 2 == 0:
                        sc.op("vector", lambda e, ptu=ptu, uT=uT, d0=d0, ec=ec: e.tensor_copy(
                            out=uT[:, d0:d0 + 4, ec * 128:(ec + 1) * 128], in_=ptu[:, :].rearrange("p (c n) -> p c n", n=128)),
                            reads=[ptk], writes=[(uTk, ec)])
                    else:
                        sc.op("scalar", lambda e, ptu=ptu, uT=uT, d0=d0, ec=ec: e.activation(
                            out=uT[:, d0:d0 + 4, ec * 128:(ec + 1) * 128], in_=ptu[:, :].rearrange("p (c n) -> p c n", n=128), func=AF.Copy),
                            reads=[ptk], writes=[(uTk, ec)])
                    if d0 == NDC - 4:
                        sc.dma("sync", uTv[:, :, e0:e0 + 128], uT[:, :, ec * 128:(ec + 1) * 128], reads=[(uTk, ec)], writes=["d_uT"])
                units.append(ub_)
        return units

    def units_A(eg):
        g2 = eg % 2
        uT, gT, WT = uTs[g2], gTs[g2], WTs[g2]
        uTk, gTk = "i_uT%d" % g2, "i_gT%d" % g2
        units = []
        for ec in range(GE):
            for tb in range(2):
                st = {}
                for q in range(8):
                    def ua(ec=ec, tb=tb, q=q, st=st):
                        if q == 0:
                            st["i"] = cnt["a"] % 2
                            cnt["a"] += 1
                        ai = st["i"]
                        pA = pAs[ai]
                        pAk = "i_pA%d" % ai
                        ge = ges[ai]
                        gek = "i_ge%d" % ai
                        cs_ = slice(tb * 512, (tb + 1) * 512)
                        for dc in (2 * q, 2 * q + 1):
                            sc.op("tensor", lambda e, pA=pA, dc=dc, cs_=cs_: e.matmul(
                                pA[:, :], lhsT=uT[:, dc, ec * 128:(ec + 1) * 128], rhs=hh[:, dc, cs_], start=(dc == 0), stop=(dc == NDC - 1)),
                                reads=[(uTk, ec), "i_hh"], writes=[pAk], sig=(dc == NDC - 1))
                        if q == 7:
                            sc.op("scalar", lambda e, pA=pA, ge=ge: e.activation(out=ge[:], in_=pA[:, :], func=AF.Gelu), reads=[pAk], writes=[gek])
                            sc.op("vector", lambda e, ge=ge, cs_=cs_: e.tensor_tensor(out=WT[:, ec, cs_], in0=ge[:], in1=gT[:, ec, cs_], op=ALU.mult),
                                  reads=[gek, gTk], writes=[("i_WT", g2, ec, tb)])
                    units.append(ua)
        return units

    def units_O(eg):
        g2 = eg % 2
        g3 = eg % 3
        vb, WT = vbs[g3], WTs[g2]
        units = []
        for tt in range(8):
            for nb in range(4):
                def uo(tt=tt, nb=nb):
                    pO = pOs[cnt["o"] % 4]
                    pOk = "i_pO%d" % (cnt["o"] % 4)
                    cnt["o"] += 1
                    ns_ = slice(nb * 512, (nb + 1) * 512)
                    for ec in range(GE):
                        sc.op("tensor", lambda e, pO=pO, ec=ec, ns_=ns_: e.matmul(
                            pO[:, :], lhsT=WT[:, ec, tt * 128:(tt + 1) * 128], rhs=vb[:, ec, ns_], start=(ec == 0), stop=(ec == GE - 1)),
                            reads=[("i_WT", g2, ec, tt // 4), ("i_vb", g3, ec)], writes=[pOk], sig=(ec == GE - 1))
                    sc.op("vector", lambda e, pO=pO, ns_=ns_: e.tensor_tensor(out=acc[:, tt, ns_], in0=pO[:, :], in1=acc[:, tt, ns_], op=ALU.add),
                          reads=[pOk, ("acc", tt)], writes=[("acc", tt)])
                units.append(uo)
        return units

    def merged(lists):
        lists = [l for l in lists if l]
        pos = [0] * len(lists)
        total = sum(len(l) for l in lists)
        for _ in range(total):
            best, bi = None, None
            for i, l in enumerate(lists):
                if pos[i] < len(l):
                    frac = (pos[i] + 0.5) / len(l)
                    if best is None or frac < best:
                        best, bi = frac, i
            lists[bi][pos[bi]]()
            pos[bi] += 1

    if hp == 0:
        dma_ub(0)
    for u in units_T(0):
        u()
    for eg in range(NG):
        merged([units_T(eg + 1) if eg + 1 < NG else [], units_A(eg), units_O(eg - 1) if eg >= 1 else []])
    for u in units_O(NG - 1):
        u()


def phase_j(k, ph, hp):
    nc, sc, sb, ps = k.nc, k.sc, k.sb, k.ps
    A = k.scr
    acc = k.acc
    tag = "j%d_" % hp
    fnb = sb(tag + "fnb", [128, D], ctx=ph)
    sc.dma("sync", fnb[:], k.final_norm.partition_broadcast(128), writes=[tag + "fnb"])
    x1s = [sb(tag + "x1%d" % i, [128, D], ctx=ph) for i in range(2)]
    junk = sb(tag + "junk", [128, D], ctx=ph)
    ss = sb(tag + "ss", [128, 8], ctx=ph)
    x1v = A["x1"].rearrange("(n p) d -> n p d", p=128)
    ov = k.out.rearrange("(n p) d -> n p d", p=128)
    for tt in range(8):
        ti = hp * 8 + tt
        x1 = x1s[tt % 2]
        xk = tag + "x1%d" % (tt % 2)
        sc.dma("sync", x1[:], x1v[ti], reads=["d_x1"], writes=[xk])
        sc.op("vector", lambda e, tt=tt: e.tensor_tensor(out=acc[:, tt, :], in0=acc[:, tt, :], in1=k.G2[:], op=ALU.mult),
              reads=[("acc", tt), "G1"], writes=[("acc", tt)])
        sc.op("vector", lambda e, tt=tt, x1=x1: e.tensor_tensor(out=x1[:], in0=x1[:], in1=acc[:, tt, :], op=ALU.add),
              reads=[("acc", tt), xk], writes=[xk])
        sc.op("scalar", lambda e, tt=tt, x1=x1: e.activation(out=junk[:], in_=x1[:], func=AF.Square, accum_out=ss[:, tt:tt + 1]),
              reads=[xk], writes=[tag + "junk", (tag + "ss", tt)])
        sc.op("vector", lambda e, tt=tt: e.tensor_scalar(out=ss[:, tt:tt + 1], in0=ss[:, tt:tt + 1], scalar1=1.0 / D, scalar2=EPS,
                                                         op0=ALU.mult, op1=ALU.add), reads=[(tag + "ss", tt)], writes=[(tag + "ss", tt)])
        sc.op("scalar", lambda e, tt=tt: e.activation(out=ss[:, tt:tt + 1], in_=ss[:, tt:tt + 1], func=AF.Sqrt),
              reads=[(tag + "ss", tt)], writes=[(tag + "ss", tt)])
        sc.op("vector", lambda e, tt=tt: e.reciprocal(out=ss[:, tt:tt + 1], in_=ss[:, tt:tt + 1]),
              reads=[(tag + "ss", tt)], writes=[(tag + "ss", tt)])
        sc.op("vector", lambda e, tt=tt, x1=x1: e.scalar_tensor_tensor(out=x1[:], in0=x1[:], scalar=ss[:, tt:tt + 1], in1=fnb[:],
                                                                       op0=ALU.mult, op1=ALU.mult),
              reads=[xk, (tag + "ss", tt), tag + "fnb"], writes=[xk])
        sc.dma("sync", ov[ti], x1[:], reads=[xk], writes=["d_out"])


def make_in_maps(inputs, cores):
    cst = make_consts()
    f = lambda a: np.ascontiguousarray(np.asarray(a), dtype=np.float32)
    shared = {
        "w_ada": f(inputs["w_ada"][0]), "b_ada": f(inputs["b_ada"]), "norm_mix": f(inputs["norm_mix"]),
        "norm_ffn": f(inputs["norm_ffn"]), "w_in": f(inputs["w_in"][0]), "lb_logits": f(inputs["lb_logits"]),
        "hgrn_gain": f(inputs["hgrn_gain"]), "w_up_a": f(inputs["w_up_a"][0]), "w_up_b": f(inputs["w_up_b"][0]),
        "w_out": f(inputs["w_out"][0]), "peer_w_q": f(inputs["peer_w_q"][0]), "peer_keys": f(inputs["peer_keys"][0]),
        "peer_u": f(inputs["peer_u"][0]), "peer_v": f(inputs["peer_v"][0]), "final_norm": f(inputs["final_norm"]),
    }
    shared.update(cst)
    maps = []
    for b in cores:
        m = dict(shared)
        m["x"] = f(inputs["x"][b])
        m["c"] = f(inputs["c"][b:b + 1])
        maps.append(m)
    return maps


def kernel(**inputs):
    nc = build_nc(stage=99)
    cores = list(range(NCORES))
    in_maps = make_in_maps(inputs, cores)
    res = run_bass_kernel_spmd(nc, in_maps, core_ids=cores)
    return np.stack([np.asarray(r["out"], dtype=np.float32) for r in res.results], axis=0)
```
